# Optimizing a Trainium2 kernel written in Bass

```python
import math
import jax, jax.numpy as jnp
from jax import lax
import numpy as np


D_MODEL = 2048
BATCH = 4
SEQ = 2048
DEPTH = 1
DEC_BATCH = 128
DEC_SEQ = 1
PAST_LEN = 2048
PAGE_SIZE = 128

EPS = 1e-6
CHUNK = 128
A_GROUPS = 8
A_GROUP_CH = 128
A_WIDTH = A_GROUPS * A_GROUP_CH
HEAD_DIM = 128
HEADS_PER_GROUP = 4
DIL_GROUPS = ((128, 1), (512, 4), (2048, 16))
N_DIL = 3
B_HEADS = N_DIL * HEADS_PER_GROUP
B_WIDTH = B_HEADS * HEAD_DIM
B_OUT_WIDTH = HEADS_PER_GROUP * HEAD_DIM
Q_BLOCK = 128
ATTN_SCALE = HEAD_DIM ** -0.5
ROPE_THETA = 500000.0
ROT_DIM = HEAD_DIM // 4
IN_COLS = 2 * A_WIDTH + 3 * B_WIDTH + 2 * D_MODEL
IN_SPLITS = (A_WIDTH, 2 * A_WIDTH, 2 * A_WIDTH + B_WIDTH, 2 * A_WIDTH + 2 * B_WIDTH,
             2 * A_WIDTH + 3 * B_WIDTH, 2 * A_WIDTH + 3 * B_WIDTH + D_MODEL)
N_KEYS = 128
N_EXPERTS = N_KEYS * N_KEYS
PEER_HEADS = 8
PEER_KEY_DIM = 256
PEER_HALF = PEER_KEY_DIM // 2
PEER_TOPK = 16
PEER_TOKEN_BLOCK = 128
PLE_DIM = 256

kernel_name = "hybrid_sgu_dilattn_peer_decoder_step"


def rmsnorm(x, g):
    xf = x.astype(jnp.float32)
    r = lax.rsqrt(jnp.mean(xf * xf, axis=-1, keepdims=True) + EPS)
    return (xf * r).astype(x.dtype) * g


def layernorm(x, g, b):
    xf = x.astype(jnp.float32)
    mu = jnp.mean(xf, axis=-1, keepdims=True)
    var = jnp.mean(jnp.square(xf - mu), axis=-1, keepdims=True)
    return ((xf - mu) * lax.rsqrt(var + EPS)).astype(x.dtype) * g + b


def partial_rope(x, pos):
    half = ROT_DIM // 2
    inv = ROPE_THETA ** (-jnp.arange(half, dtype=jnp.float32) / half)
    ang = pos.astype(jnp.float32)[:, None] * inv[None, :]
    cos = jnp.cos(ang)[:, None, :].astype(x.dtype)
    sin = jnp.sin(ang)[:, None, :].astype(x.dtype)
    x1, x2, rest = x[..., :half], x[..., half:ROT_DIM], x[..., ROT_DIM:]
    return jnp.concatenate([x1 * cos - x2 * sin, x2 * cos + x1 * sin, rest], axis=-1)


def softmax_lse(s):
    m = jnp.max(s, axis=-1, keepdims=True)
    e = jnp.exp(s - m)
    den = jnp.sum(e, axis=-1, keepdims=True)
    return e / den, (m + jnp.log(den))[..., 0]


def chunk_spatial_gating(u, v, w_s, b_s):
    bn, t, _ = v.shape
    n_chunk = -(-t // CHUNK)
    vp = jnp.pad(v, ((0, 0), (0, n_chunk * CHUNK - t), (0, 0)))
    vp = vp.reshape(bn, n_chunk, CHUNK, A_GROUPS, A_GROUP_CH)
    causal = jnp.tril(jnp.ones((CHUNK, CHUNK), dtype=bool))
    ws = jnp.where(causal[None], w_s, 0.0)
    mixed = jnp.einsum('gts,bcsgd->bctgd', ws, vp) + b_s.T[None, None, :, :, None]
    mixed = mixed.reshape(bn, n_chunk * CHUNK, A_WIDTH)[:, :t]
    return u * mixed


def dilated_group_prompt(q, k, v, window, dilation):
    bn, s_len, h, dh = q.shape
    nw = window // dilation
    ls = s_len // dilation
    qb = math.gcd(ls, Q_BLOCK)
    nblk = ls // qb
    strided = lambda t: t.reshape(bn, ls, dilation, h, dh).transpose(0, 2, 1, 3, 4)
    qs = strided(q).reshape(bn, dilation, nblk, qb, h, dh)
    pad = ((0, 0), (0, 0), (nw, 0), (0, 0), (0, 0))
    kp = jnp.pad(strided(k), pad)
    vp = jnp.pad(strided(v), pad)
    blk = jnp.arange(nblk)[:, None]
    col = jnp.arange(qb + nw)[None, :]
    idx = blk * qb + col
    kb = jnp.take(kp, idx, axis=2)
    vb = jnp.take(vp, idx, axis=2)
    s = jnp.einsum('brnqhd,brnkhd->brnhqk', qs, kb).astype(jnp.float32) * ATTN_SCALE
    dist = jnp.arange(qb)[:, None] - col + nw
    keypos = blk[:, :, None] * qb + col[None] - nw
    valid = (dist >= 0) & (dist <= nw) & (keypos >= 0)
    s = jnp.where(valid[None, None, :, None], s, -jnp.inf)
    prob, lse = softmax_lse(s)
    o = jnp.einsum('brnhqk,brnkhd->brnqhd', prob.astype(v.dtype), vb)
    o = o.reshape(bn, dilation, ls, h, dh).transpose(0, 2, 1, 3, 4).reshape(bn, s_len, h, dh)
    lse = lse.transpose(0, 1, 2, 4, 3).reshape(bn, dilation, ls, h)
    lse = lse.transpose(0, 2, 1, 3).reshape(bn, s_len, h)
    return o, lse


def dilated_group_sample(q, k_new, v_new, k_cache, v_cache, window, dilation):
    t = q.shape[1]
    l_buf = k_cache.shape[1]
    nw = window // dilation
    k_all = jnp.concatenate([k_cache, k_new], axis=1)
    v_all = jnp.concatenate([v_cache, v_new], axis=1)
    idx = l_buf + jnp.arange(t)[:, None] - dilation * jnp.arange(nw + 1)[None, :]
    valid = idx >= 0
    idx_c = jnp.maximum(idx, 0)
    kg = jnp.take(k_all, idx_c, axis=1)
    vg = jnp.take(v_all, idx_c, axis=1)
    s = jnp.einsum('nthd,ntkhd->nthk', q, kg).astype(jnp.float32) * ATTN_SCALE
    s = jnp.where(valid[None, :, None, :], s, -jnp.inf)
    prob, lse = softmax_lse(s)
    o = jnp.einsum('nthk,ntkhd->nthd', prob.astype(v_new.dtype), vg)
    return o, lse


def combine_dilations(outs, lses):
    o = jnp.stack(outs, axis=0)
    w = jax.nn.softmax(jnp.stack(lses, axis=0), axis=0)
    return jnp.sum(w[..., None].astype(o.dtype) * o, axis=0)


def peer_ffn(h, w_q, sub_k1, sub_k2, u_tab, v_tab):
    shp = h.shape
    xt = h.reshape(-1, D_MODEL)
    n_tok = xt.shape[0]
    nb = -(-n_tok // PEER_TOKEN_BLOCK)
    xt = jnp.pad(xt, ((0, nb * PEER_TOKEN_BLOCK - n_tok), (0, 0)))
    xt = xt.reshape(nb, PEER_TOKEN_BLOCK, D_MODEL)

    def block(xb):
        q = (xb @ w_q).reshape(PEER_TOKEN_BLOCK, PEER_HEADS, 2, PEER_HALF)
        s1 = jnp.einsum('thd,kd->thk', q[:, :, 0], sub_k1).astype(jnp.float32)
        s2 = jnp.einsum('thd,kd->thk', q[:, :, 1], sub_k2).astype(jnp.float32)
        t1, i1 = lax.top_k(s1, PEER_TOPK)
        t2, i2 = lax.top_k(s2, PEER_TOPK)
        cand = (t1[..., :, None] + t2[..., None, :]).reshape(PEER_TOKEN_BLOCK, PEER_HEADS, PEER_TOPK * PEER_TOPK)
        ts, ic = lax.top_k(cand, PEER_TOPK)
        e1 = jnp.take_along_axis(i1, ic // PEER_TOPK, axis=-1)
        e2 = jnp.take_along_axis(i2, ic % PEER_TOPK, axis=-1)
        expert = e1 * N_KEYS + e2
        g = jax.nn.softmax(ts, axis=-1)
        act = jax.nn.gelu(jnp.einsum('thkd,td->thk', u_tab[expert], xb).astype(jnp.float32))
        return jnp.einsum('thk,thkd->td', (g * act).astype(xb.dtype), v_tab[expert])

    y = lax.map(block, xt).reshape(nb * PEER_TOKEN_BLOCK, D_MODEL)[:n_tok]
    return y.reshape(shp)


def layer(x, p, pos, group_attention, g_mix, w_in, sgu_ln_g, sgu_ln_b, w_s, b_s,
          w_a_out, w_b_out, w_o, g_ffn, peer_w_q, peer_sub_k1, peer_sub_k2,
          peer_u, peer_v, w_ple, w_ple_gate):
    bn, t, _ = x.shape
    h = rmsnorm(x, g_mix)
    u_a, v_a, q, k, v, gate_a, gate_b = jnp.split(h @ w_in, IN_SPLITS, axis=-1)
    u = jax.nn.gelu(u_a)
    vn = layernorm(jax.nn.gelu(v_a), sgu_ln_g, sgu_ln_b)
    a_mix = chunk_spatial_gating(u, vn, w_s, b_s)
    q = partial_rope(q.reshape(bn, t, B_HEADS, HEAD_DIM), pos)
    k = partial_rope(k.reshape(bn, t, B_HEADS, HEAD_DIM), pos)
    v = v.reshape(bn, t, B_HEADS, HEAD_DIM)
    outs, lses = [], []
    for gi in range(N_DIL):
        sl = slice(gi * HEADS_PER_GROUP, (gi + 1) * HEADS_PER_GROUP)
        o, l = group_attention(gi, q[:, :, sl], k[:, :, sl], v[:, :, sl])
        outs.append(o)
        lses.append(l)
    b_mix = combine_dilations(outs, lses).reshape(bn, t, B_OUT_WIDTH)
    merged = jax.nn.sigmoid(gate_a) * (a_mix @ w_a_out) + jax.nn.sigmoid(gate_b) * (b_mix @ w_b_out)
    x = x + merged @ w_o
    x = x + peer_ffn(rmsnorm(x, g_ffn), peer_w_q, peer_sub_k1, peer_sub_k2, peer_u, peer_v)
    x = x + jax.nn.sigmoid(x @ w_ple_gate) * (p @ w_ple)
    return x, k, v, vn


def setup_inputs(seed: int = 0) -> dict:
    key = jax.random.key(seed)
    ks = jax.random.split(key, 32)
    nrm = lambda kk, shape, scale: jax.random.normal(kk, shape, jnp.float32) * scale
    l_buf = [min(w, PAST_LEN) for w, _ in DIL_GROUPS]
    cshape = lambda l: (DEPTH, DEC_BATCH, l, 2, HEADS_PER_GROUP, HEAD_DIM)
    return {
        "x_prompt": nrm(ks[0], (BATCH, SEQ, D_MODEL), 1.0),
        "x_sample": nrm(ks[1], (DEC_BATCH, DEC_SEQ, D_MODEL), 1.0),
        "cache_kv_w128": nrm(ks[2], cshape(l_buf[0]), 1.0),
        "cache_kv_w512": nrm(ks[3], cshape(l_buf[1]), 1.0),
        "cache_kv_w2048": nrm(ks[4], cshape(l_buf[2]), 1.0),
        "p_prompt": nrm(ks[5], (DEPTH, BATCH, SEQ, PLE_DIM), 1.0),
        "p_sample": nrm(ks[6], (DEPTH, DEC_BATCH, DEC_SEQ, PLE_DIM), 1.0),
        "g_mix": 1.0 + nrm(ks[7], (DEPTH, D_MODEL), 0.05),
        "w_in": nrm(ks[8], (DEPTH, D_MODEL, IN_COLS), D_MODEL ** -0.5),
        "sgu_ln_g": 1.0 + nrm(ks[9], (DEPTH, A_WIDTH), 0.05),
        "sgu_ln_b": nrm(ks[10], (DEPTH, A_WIDTH), 0.02),
        "w_s": nrm(ks[11], (DEPTH, A_GROUPS, CHUNK, CHUNK), CHUNK ** -0.5),
        "b_s": 1.0 + nrm(ks[12], (DEPTH, A_GROUPS, CHUNK), 0.05),
        "w_a_out": nrm(ks[13], (DEPTH, A_WIDTH, D_MODEL), A_WIDTH ** -0.5),
        "w_b_out": nrm(ks[14], (DEPTH, B_OUT_WIDTH, D_MODEL), B_OUT_WIDTH ** -0.5),
        "w_o": nrm(ks[15], (DEPTH, D_MODEL, D_MODEL), D_MODEL ** -0.5),
        "g_ffn": 1.0 + nrm(ks[16], (DEPTH, D_MODEL), 0.05),
        "peer_w_q": nrm(ks[17], (DEPTH, D_MODEL, PEER_HEADS * PEER_KEY_DIM), D_MODEL ** -0.5),
        "peer_sub_k1": nrm(ks[18], (DEPTH, N_KEYS, PEER_HALF), PEER_HALF ** -0.5),
        "peer_sub_k2": nrm(ks[19], (DEPTH, N_KEYS, PEER_HALF), PEER_HALF ** -0.5),
        "peer_u": nrm(ks[20], (DEPTH, N_EXPERTS, D_MODEL), D_MODEL ** -0.5),
        "peer_v": nrm(ks[21], (DEPTH, N_EXPERTS, D_MODEL), PEER_HEADS ** -0.5),
        "w_ple": nrm(ks[22], (DEPTH, PLE_DIM, D_MODEL), PLE_DIM ** -0.5),
        "w_ple_gate": nrm(ks[23], (DEPTH, D_MODEL, D_MODEL), D_MODEL ** -0.5),
        "g_final": 1.0 + nrm(ks[24], (D_MODEL,), 0.05),
    }


def reference(x_prompt, x_sample, cache_kv_w128, cache_kv_w512, cache_kv_w2048,
              p_prompt, p_sample, g_mix, w_in, sgu_ln_g, sgu_ln_b, w_s, b_s,
              w_a_out, w_b_out, w_o, g_ffn, peer_w_q, peer_sub_k1, peer_sub_k2,
              peer_u, peer_v, w_ple, w_ple_gate, g_final):
    caches = (cache_kv_w128, cache_kv_w512, cache_kv_w2048)
    s_len = x_prompt.shape[1]
    t_len = x_sample.shape[1]
    pos_p = jnp.arange(s_len, dtype=jnp.int32)
    pos_s = PAST_LEN + jnp.arange(t_len, dtype=jnp.int32)
    xp, xs = x_prompt, x_sample
    kv_p = ([], [], [])
    kv_s = ([], [], [])
    sgu_s = []
    for i in range(DEPTH):
        lw = (g_mix[i], w_in[i], sgu_ln_g[i], sgu_ln_b[i], w_s[i], b_s[i],
              w_a_out[i], w_b_out[i], w_o[i], g_ffn[i], peer_w_q[i], peer_sub_k1[i],
              peer_sub_k2[i], peer_u[i], peer_v[i], w_ple[i], w_ple_gate[i])

        def attend_prompt(gi, q, k, v):
            return dilated_group_prompt(q, k, v, DIL_GROUPS[gi][0], DIL_GROUPS[gi][1])

        def attend_sample(gi, q, k, v, layer_idx=i):
            c = caches[gi][layer_idx]
            return dilated_group_sample(q, k, v, c[:, :, 0], c[:, :, 1], DIL_GROUPS[gi][0], DIL_GROUPS[gi][1])

        xp, kp, vp, _ = layer(xp, p_prompt[i], pos_p, attend_prompt, *lw)
        xs, ksm, vsm, vn_s = layer(xs, p_sample[i], pos_s, attend_sample, *lw)
        for gi, (win, _) in enumerate(DIL_GROUPS):
            sl = slice(gi * HEADS_PER_GROUP, (gi + 1) * HEADS_PER_GROUP)
            rows = min(win, s_len)
            kv_p[gi].append(jnp.stack([kp[:, s_len - rows:, sl], vp[:, s_len - rows:, sl]], axis=2))
            kv_s[gi].append(jnp.stack([ksm[:, :, sl], vsm[:, :, sl]], axis=2))
        sgu_s.append(vn_s)
    y_prompt = rmsnorm(xp, g_final)
    y_sample = rmsnorm(xs, g_final)
    kv_w128_prompt = jnp.stack(kv_p[0], axis=0)
    kv_w512_prompt = jnp.stack(kv_p[1], axis=0)
    kv_w2048_prompt = jnp.stack(kv_p[2], axis=0)
    kv_w128_sample = jnp.stack(kv_s[0], axis=0)
    kv_w512_sample = jnp.stack(kv_s[1], axis=0)
    kv_w2048_sample = jnp.stack(kv_s[2], axis=0)
    sgu_v_sample = jnp.stack(sgu_s, axis=0)
    return (y_prompt, y_sample, kv_w128_prompt, kv_w512_prompt, kv_w2048_prompt,
            kv_w128_sample, kv_w512_sample, kv_w2048_sample, sgu_v_sample)
```

```python
import contextlib
import os
import math
import numpy as np
import concourse.bass as bass
import concourse.mybir as mybir
from concourse.bass_utils import run_bass_kernel_spmd

F32 = mybir.dt.float32
BF16 = mybir.dt.bfloat16
I32 = mybir.dt.int32
U32 = mybir.dt.uint32
AF = mybir.ActivationFunctionType
ALU = mybir.AluOpType
AX = mybir.AxisListType
DTSIZE = {F32: 4, BF16: 2, I32: 4, U32: 4}

D = 2048
NCORES = 8
TOK = 1024
NS = 16
NCOL = 2 * TOK + NS
EPS = 1e-6
A_W = 1024
IN_COLS = 10752
C_UA, C_VA, C_Q, C_K, C_V, C_GA, C_GB = 0, 1024, 2048, 3584, 5120, 6656, 8704
DIL = (1, 4, 16)
SCALE = 128 ** -0.5
NEG = -30000.0
NEXP = 16384
PASSES = ((TOK, 512), (TOK + 512, 512), (2 * TOK, NS))


class Buf:
    __slots__ = ("name", "w", "r", "sem", "cnt", "excl")

    def __init__(self, name, excl=False):
        self.name = name
        self.excl = excl
        self.w = None
        self.r = {}
        self.sem = None
        self.cnt = 0


class Sched:
    def __init__(self, nc, stack):
        self.nc = nc
        self.stack = stack
        self.eng = {"pe": nc.tensor, "act": nc.scalar, "dve": nc.vector,
                    "pool": nc.gpsimd, "sp": nc.sync}
        self.sem = {k: stack.enter_context(nc.semaphore("s_" + k)) for k in self.eng}
        self.count = {k: 0 for k in self.eng}
        self.seen = {k: {} for k in self.eng}
        self.dbufs = []
        self.ninstr = {k: 0 for k in self.eng}

    def _wait(self, e, tok):
        if tok is None:
            return
        sem, val, key = tok
        if key == "pe" and e == "pe":
            return
        if self.seen[e].get(key, 0) >= val:
            return
        self.eng[e].wait_ge(sem, val)
        self.seen[e][key] = val

    def _deps(self, e, reads, writes):
        for b in reads:
            self._wait(e, b.w)
        for b in writes:
            self._wait(e, b.w)
            for t in b.r.values():
                self._wait(e, t)

    def _commit(self, tok, reads, writes):
        for b in reads:
            b.r[tok[2]] = tok
        for b in writes:
            b.w = tok
            b.r = {}

    def op(self, e, fn, reads=(), writes=(), signal=True):
        if any(b.excl for b in reads):
            writes = list(writes) + [b for b in reads if b.excl]
            reads = [b for b in reads if not b.excl]
        self._deps(e, reads, writes)
        ins = fn(self.eng[e])
        self.ninstr[e] += 1
        if signal:
            self.count[e] += 1
            ins.then_inc(self.sem[e], 1)
            tok = (self.sem[e], self.count[e], e)
        else:
            tok = (self.sem[e], self.count[e] + 1, e)
        self._commit(tok, reads, writes)
        return tok

    def dma(self, q, fn, reads=(), writes=(), dbuf=None):
        self._deps(q, reads, writes)
        if dbuf is None:
            dbuf = writes[0] if writes else reads[0]
        if dbuf.sem is None:
            dbuf.sem = self.stack.enter_context(self.nc.semaphore("d_" + dbuf.name))
            self.dbufs.append(dbuf)
        ins = fn(self.eng[q])
        dbuf.cnt += 16
        ins.then_inc(dbuf.sem, 16)
        tok = (dbuf.sem, dbuf.cnt, "d_" + dbuf.name)
        self._commit(tok, reads, writes)
        self.ninstr[q] += 1
        return tok

    def wait_all(self, e, bufs):
        for b in bufs:
            self._wait(e, b.w)
            for t in b.r.values():
                self._wait(e, t)

    def barrier(self):
        for e in self.eng:
            for x in ("pe", "act", "dve", "pool"):
                if x != e and self.count[x] > 0:
                    self._wait(e, (self.sem[x], self.count[x], x))
            for b in self.dbufs:
                if b.cnt > 0:
                    self._wait(e, (b.sem, b.cnt, "d_" + b.name))


class Arena:
    def __init__(self, nc, lo, hi):
        self.nc = nc
        self.free = [(lo, hi)]
        self.n = 0
        self.peak = 0
        self.hi = hi

    def alloc(self, name, shape, dt, top=False):
        nb = int(np.prod(shape[1:])) * DTSIZE[dt]
        nb = (nb + 63) // 64 * 64
        order = range(len(self.free) - 1, -1, -1) if top else range(len(self.free))
        for i in order:
            a, b = self.free[i]
            if b - a >= nb:
                if top:
                    off = b - nb
                    self.free[i] = (a, off)
                else:
                    off = a
                    self.free[i] = (a + nb, b)
                if self.free[i][0] == self.free[i][1]:
                    del self.free[i]
                self.n += 1
                used = self.hi - sum(y - x for x, y in self.free)
                self.peak = max(self.peak, used)
                h = self.nc.alloc_sbuf_tensor_at("%s_%d" % (name, self.n), list(shape), dt, offset=off)
                return h, (off, off + nb)
        raise RuntimeError("SBUF arena exhausted allocating %s %s (free=%s)" % (name, shape, self.free))

    def release(self, region):
        self.free.append(region)
        self.free.sort()
        merged = []
        for a, b in self.free:
            if merged and merged[-1][1] == a:
                merged[-1] = (merged[-1][0], b)
            else:
                merged.append((a, b))
        self.free = merged


def build_nc():
    nc = bass.Bass("TRN2", target_bir_lowering=False)
    SKIP = set(os.environ.get('KSKIP', '').split(','))
    NEXP_ = NEXP if int(os.environ.get('KSTOP', '99')) >= 7 else 128
    di = lambda name, shape, dt=F32: nc.dram_tensor(name, list(shape), dt, kind="ExternalInput").ap()
    do = lambda name, shape, dt=F32: nc.dram_tensor(name, list(shape), dt, kind="ExternalOutput").ap()

    xloc = di("xloc", [2 * TOK, D]); xs = di("xs", [NS, D])
    ploc = di("ploc", [TOK, 256]); psm = di("psm", [NS, 256])
    cache = di("cache", [3, NS, 128, 1024])
    w_in = di("w_in", [D, IN_COLS]); w_a_out = di("w_a_out", [A_W, D]); w_b_out = di("w_b_out", [512, D])
    w_o = di("w_o", [D, D]); w_q = di("w_q", [D, D]); w_pg = di("w_pg", [D, D]); w_ple = di("w_ple", [256, D])
    peer_u = di("peer_u", [NEXP_, D]); peer_v = di("peer_v", [NEXP_, D])
    w_s = di("w_s", [8, 128, 128]); b_s = di("b_s", [8, 128]); subk = di("subk", [2, 128, 128])
    gvec = di("gvec", [3, D]); lnv = di("lnv", [2, A_W])
    rope = di("rope", [128, 56, 64]); ropes = di("ropes", [NS, 64]); masks = di("masks", [128, 5, 128])

    y = do("y", [TOK, D]); ys = do("ys", [NS, D])
    kv0 = do("kv0", [128, 1024]); kv1 = do("kv1", [512, 1024]); kv2 = do("kv2", [TOK, 1024])
    kvs = do("kvs", [3, NS, 1024]); sguv = do("sguv", [NS, A_W])
    kvout = (kv0, kv1, kv2)

    with contextlib.ExitStack() as st:
        S = Sched(nc, st)
        AR = Arena(nc, 16512, 229344)
        outb = Buf("outb")

        def sb(name, shape, dt, top=False):
            h, reg = AR.alloc(name, shape, dt, top)
            return h, Buf(name), reg

        PB = []
        for i in range(8):
            t = nc.alloc_psum_tensor("pb%d" % i, [128, 512], F32)
            PB.append((t, Buf("pb%d" % i, excl=True)))
        rr = {"A": 0, "B": 0, "C": 0}
        pools = {"A": (0, 1, 2, 3), "B": (4, 5), "C": (6, 7)}

        def bank(pool):
            ids = pools[pool]
            i = ids[rr[pool] % len(ids)]
            rr[pool] += 1
            return PB[i]

        def mm_group(out_ap, pbuf, pairs, reads):
            n = len(pairs)
            for i, (l, r) in enumerate(pairs):
                S.op("pe", lambda e, l=l, r=r, i=i: e.matmul(out_ap, lhsT=l, rhs=r, start=(i == 0), stop=(i == n - 1)),
                     reads=reads, writes=[pbuf], signal=(i == n - 1))

        identF, b_identF, _ = sb("identF", [128, 128], F32)
        identB, b_identB, _ = sb("identB", [128, 128], BF16)
        onesB, b_onesB, _ = sb("onesB", [128, 128], BF16)
        S.op("pool", lambda e: e.memset(identF[:], 1.0), writes=[b_identF])
        S.op("pool", lambda e: e.affine_select(out=identF[:], in_=identF[:], pattern=[[-1, 128]],
                                               compare_op=ALU.is_equal, fill=0.0, base=0, channel_multiplier=1),
             reads=[b_identF], writes=[b_identF])
        S.op("pool", lambda e: e.tensor_copy(out=identB[:], in_=identF[:]), reads=[b_identF], writes=[b_identB])
        S.op("pool", lambda e: e.memset(onesB[:], 1.0), writes=[b_onesB])

        gcol, b_gcol, _ = sb("gcol", [128, 2, 16], F32)
        grow, b_grow, r_grow = sb("grow", [16, 2, 128], F32)
        S.dma("sp", lambda e: e.dma_start(out=grow[:], in_=gvec[0:2, :].rearrange("g (k p) -> k g p", p=128)),
              writes=[b_grow])
        for gi in range(2):
            pt, pbuf = bank("C")
            S.op("pe", lambda e, gi=gi, pt=pt: e.transpose(out=pt[:, 0:16], in_=grow[:, gi, :], identity=identF[0:16, 0:16]),
                 reads=[b_grow, b_identF], writes=[pbuf])
            S.op("act", lambda e, gi=gi, pt=pt: e.copy(out=gcol[:, gi, :], in_=pt[:, 0:16]), reads=[pbuf], writes=[b_gcol])
        maskB, b_maskB, _ = sb("maskB", [128, 5, 128], BF16)
        S.dma("pool", lambda e: e.dma_start(out=maskB[:], in_=masks), writes=[b_maskB])
        c4, b_c4, _ = sb("c4", [128, 2], U32)
        S.op("pool", lambda e: e.memset(c4[:, 0:1], 4), writes=[b_c4])
        S.op("pool", lambda e: e.memset(c4[:, 1:2], 15), writes=[b_c4])
        ropeS, b_ropeS, _ = sb("ropeS", [NS, 64], F32)
        S.dma("sp", lambda e: e.dma_start(out=ropeS[:], in_=ropes), writes=[b_ropeS])

        NW = 2
        wslot = [sb("wslot%d" % i, [128, 16, 512], BF16) for i in range(NW)]
        wq = []
        wstate = {"issued": 0, "used": 0}

        def w_plan(blocks):
            wq.extend(blocks)

        def w_issue_upto(n):
            while wstate["issued"] < min(n, len(wq)):
                i = wstate["issued"]
                ap = wq[i]
                K, C = ap.shape
                kc = K // 128
                t, bf, _ = wslot[i % NW]
                S.dma("pool", lambda e, t=t, ap=ap, kc=kc, C=C: e.dma_start(
                    out=t[:, 0:kc, 0:C], in_=ap.rearrange("(k p) c -> p k c", p=128)), writes=[bf])
                wstate["issued"] += 1

        def w_next(issue=True):
            i = wstate["used"]
            if issue:
                w_issue_upto(i + NW)
            wstate["used"] += 1
            t, bf, _ = wslot[i % NW]
            return t, bf

        xst = [sb("xst%d" % i, [128, D], F32) for i in range(2)]
        xnb = [sb("xnb%d" % i, [128, D], BF16) for i in range(2)]
        SQ = {}
        SQ["t"], SQ["b"], SQ["r"] = sb("sqjunk", [128, D], BF16)
        stat, b_stat, _ = sb("stat", [128, 8], F32)

        def rms_stats(src_ap, rows, b_src, slot):
            ss = stat[0:rows, slot * 2:slot * 2 + 1]
            rs = stat[0:rows, slot * 2 + 1:slot * 2 + 2]
            sq_junk, b_sq_junk = SQ["t"], SQ["b"]
            S.op("act", lambda e: e.activation(out=sq_junk[0:rows, :], in_=src_ap, func=AF.Square, accum_out=ss),
                 reads=[b_src], writes=[b_sq_junk, b_stat])
            S.op("dve", lambda e: e.tensor_scalar(out=ss, in0=ss, scalar1=1.0 / D, scalar2=EPS, op0=ALU.mult, op1=ALU.add),
                 reads=[b_stat], writes=[b_stat])
            S.op("act", lambda e: e.sqrt(out=ss, in_=ss), reads=[b_stat], writes=[b_stat])
            S.op("dve", lambda e: e.reciprocal(out=rs, in_=ss), reads=[b_stat], writes=[b_stat])
            return rs

        def transpose_to(dstT, b_dstT, col0, src_bf, b_src, rows, gsel):
            for half in range(2):
                pt, pbuf = bank("B")
                ptb = pt[:].bitcast(BF16)
                for j in range(8):
                    kc = half * 8 + j
                    S.op("pe", lambda e, kc=kc, j=j, ptb=ptb: e.transpose(
                        out=ptb[:, j * 128:j * 128 + rows], in_=src_bf[0:rows, kc * 128:(kc + 1) * 128],
                        identity=identB[0:rows, 0:rows]),
                        reads=[b_src, b_identB], writes=[pbuf], signal=(j == 7))
                for j in range(8):
                    kc = half * 8 + j
                    eng = "dve" if half == 0 else "act"
                    if gsel is None:
                        if eng == "dve":
                            S.op("dve", lambda e, kc=kc, j=j, ptb=ptb: e.tensor_copy(
                                out=dstT[:, kc, col0:col0 + rows], in_=ptb[:, j * 128:j * 128 + rows]),
                                reads=[pbuf], writes=[b_dstT])
                        else:
                            S.op("act", lambda e, kc=kc, j=j, ptb=ptb: e.copy(
                                out=dstT[:, kc, col0:col0 + rows], in_=ptb[:, j * 128:j * 128 + rows]),
                                reads=[pbuf], writes=[b_dstT])
                    elif eng == "dve":
                        S.op("dve", lambda e, kc=kc, j=j, ptb=ptb: e.tensor_scalar(
                            out=dstT[:, kc, col0:col0 + rows], in0=ptb[:, j * 128:j * 128 + rows],
                            scalar1=gcol[:, gsel, kc:kc + 1], scalar2=None, op0=ALU.mult),
                            reads=[pbuf, b_gcol], writes=[b_dstT])
                    else:
                        S.op("act", lambda e, kc=kc, j=j, ptb=ptb: e.activation(
                            out=dstT[:, kc, col0:col0 + rows], in_=ptb[:, j * 128:j * 128 + rows],
                            func=AF.Copy, scale=gcol[:, gsel, kc:kc + 1]),
                            reads=[pbuf, b_gcol], writes=[b_dstT])

        STOP = int(os.environ.get('KSTOP', '99'))
        SUB = int(os.environ.get('KSUB', '99'))

        def phases():
            nonlocal xst, xnb
            if STOP < 0:
                return
            hT, b_hT, r_hT = sb("hT", [128, 16, NCOL], BF16)
            plan = []
            for g in range(3):
                plan += [w_in[:, C_K + g * 512:C_K + (g + 1) * 512], w_in[:, C_V + g * 512:C_V + (g + 1) * 512],
                         w_in[:, C_Q + g * 512:C_Q + (g + 1) * 512]]
            plan += [w_in[:, C_VA:C_VA + 512], w_in[:, C_VA + 512:C_VA + 1024]]
            plan += [w_in[:, C_UA:C_UA + 512], w_in[:, C_UA + 512:C_UA + 1024]]
            for cb in range(4):
                plan += [w_in[:, C_GA + cb * 512:C_GA + (cb + 1) * 512], w_a_out[:, cb * 512:(cb + 1) * 512],
                         w_in[:, C_GB + cb * 512:C_GB + (cb + 1) * 512], w_b_out[:, cb * 512:(cb + 1) * 512]]
            for cb in range(4):
                plan += [w_o[:, cb * 512:(cb + 1) * 512]]
            for cb in range(4):
                plan += [w_q[:, cb * 512:(cb + 1) * 512]]
            for cb in range(4):
                plan += [w_pg[:, cb * 512:(cb + 1) * 512]]
            w_plan(plan)
            w_issue_upto(NW)

            tiles = [(xloc[n * 128:(n + 1) * 128, :], 128, n * 128) for n in range(16)] + [(xs, NS, 2 * TOK)]

            def load_x(i):
                src, rows, _ = tiles[i]
                t, bf, _ = xst[i % 2]
                S.dma("sp", lambda e: e.dma_start(out=t[0:rows, :], in_=src), writes=[bf])

            load_x(0)
            for i, (src, rows, col0) in enumerate(tiles):
                if i + 1 < len(tiles):
                    load_x(i + 1)
                t, bf, _ = xst[i % 2]
                xn, bxn, _ = xnb[i % 2]
                rs = rms_stats(t[0:rows, :], rows, bf, i % 2)
                S.op("act", lambda e, t=t, xn=xn, rs=rs, rows=rows: e.activation(
                    out=xn[0:rows, :], in_=t[0:rows, :], func=AF.Copy, scale=rs), reads=[bf, b_stat], writes=[bxn])
                transpose_to(hT, b_hT, col0, xn, bxn, rows, 0)
            S.barrier()
            for r in (xst[0][2], xst[1][2], xnb[0][2], xnb[1][2], SQ["r"], r_grow):
                AR.release(r)

            if STOP < 1:
                return
            ACC, b_ACC, r_ACC = sb("ACC", [128, 2, 4, TOK], F32)
            KT, b_KT, r_KT = sb("KT", [128, 4, 2 * TOK], BF16)
            QT, b_QT, r_QT = sb("QT", [128, 4, TOK], BF16)
            VG, b_VG, r_VG = sb("VG", [128, 16, 512], BF16)
            kf = [sb("kf%d" % i, [128, 512], F32) for i in range(2)]
            kb = [sb("kb%d" % i, [128, 512], BF16) for i in range(2)]
            rtmp, b_rtmp, r_rtmp = sb("rtmp", [128, 2, 4, 32], F32)
            PT = [sb("PT%d" % i, [128, 2, 128], BF16) for i in range(2)]
            qkv_c, b_qkv_c, r_qkv_c = sb("qkv_c", [128, 2, 512], F32)
            stg = [sb("stg%d" % i, [NS, 512], F32) for i in range(2)]
            ropeG, b_ropeG, r_ropeG = sb("ropeG", [128, 16, 64], F32)
            ropeQ2, b_ropeQ2, r_ropeQ2 = sb("ropeQ2", [128, 8, 64], F32)
            S.dma("sp", lambda e: e.dma_start(out=ropeQ2[:], in_=rope[:, 48:56, :]), writes=[b_ropeQ2])
            cnt = {"kf": 0, "kb": 0, "pt": 0, "stg": 0}

            def stash_sample(pt, pbuf, which, g):
                blk = which * 3 + g
                if "stash" in SKIP:
                    return
                t, bt, _ = stg[cnt["stg"] % 2]; cnt["stg"] += 1
                S.op("act", lambda e: e.copy(out=t[:], in_=pt[0:NS, :]), reads=[pbuf], writes=[bt])
                j, slot = blk % 8, blk // 8
                S.dma("sp", lambda e: e.dma_start(out=qkv_c[16 * j:16 * j + 16, slot, :], in_=t[:]), reads=[bt], writes=[b_qkv_c])

            def rope_apply(t, bt, rows, tab_ap, b_tab):
                x4 = t[0:rows, :].rearrange("p (h d) -> p h d", h=4)
                cc = tab_ap[:, 0:32].unsqueeze(1).broadcast_to([rows, 4, 32])
                s1 = tab_ap[:, 32:48].unsqueeze(1).broadcast_to([rows, 4, 16])
                s2 = tab_ap[:, 48:64].unsqueeze(1).broadcast_to([rows, 4, 16])
                A = rtmp[0:rows, 0]
                B = rtmp[0:rows, 1]
                if "rope" in SKIP:
                    return
                S.op("dve", lambda e: e.tensor_tensor(out=A, in0=x4[:, :, 0:32], in1=cc, op=ALU.mult),
                     reads=[bt, b_tab], writes=[b_rtmp])
                S.op("dve", lambda e: e.tensor_tensor(out=B[:, :, 0:16], in0=x4[:, :, 16:32], in1=s1, op=ALU.mult),
                     reads=[bt, b_tab], writes=[b_rtmp])
                S.op("dve", lambda e: e.tensor_tensor(out=B[:, :, 16:32], in0=x4[:, :, 0:16], in1=s2, op=ALU.mult),
                     reads=[bt, b_tab], writes=[b_rtmp])
                S.op("dve", lambda e: e.tensor_tensor(out=x4[:, :, 0:32], in0=A, in1=B, op=ALU.add),
                     reads=[b_rtmp], writes=[bt])

            def gtile_cols(g, n):
                d = DIL[g]
                tpr = (2 * TOK // d) // 128
                r, i0 = n // tpr, (n % tpr) * 128
                start = i0 * d + r
                return slice(start, start + 127 * d + 1, d), r, i0

            first_group = True
            for g in range(3):
                d = DIL[g]
                L = 2 * TOK // d
                tpr = L // 128
                Lq = TOK // d
                if g == 0:
                    ktiles = list(range(7, 16))
                elif g == 1:
                    ktiles = [n for n in range(16) if n % 4 >= 1]
                else:
                    ktiles = list(range(16))
                S.dma("sp", lambda e, g=g: e.dma_start(out=ropeG[:], in_=rope[:, g * 16:(g + 1) * 16, :]), writes=[b_ropeG])
                wt, bw = w_next()
                for n in ktiles + ["s"]:
                    pt, pbuf = bank("A")
                    if n == "s":
                        rows = NS
                        lhs = lambda kc: hT[:, kc, 2 * TOK:2 * TOK + NS]
                    else:
                        rows = 128
                        sl, r, i0 = gtile_cols(g, n)
                        lhs = lambda kc, sl=sl: hT[:, kc, sl]
                    mm_group(pt[0:rows, :], pbuf, [(lhs(kc), wt[:, kc, :]) for kc in range(16)], [b_hT, bw])
                    if n == "s":
                        stash_sample(pt, pbuf, 1, g)
                        continue
                    t, bt, _ = kf[cnt["kf"] % 2]; cnt["kf"] += 1
                    S.op("act", lambda e, pt=pt, t=t: e.copy(out=t[:], in_=pt[:]), reads=[pbuf], writes=[bt])
                    rope_apply(t, bt, 128, ropeG[:, n, :], b_ropeG)
                    if "kvout" in SKIP:
                        pass
                    elif g == 0 and n == 15:
                        S.dma("sp", lambda e, t=t: e.dma_start(out=kv0[:, 0:512], in_=t[:]), reads=[bt], dbuf=outb)
                    elif g == 1 and n % 4 == 3:
                        S.dma("sp", lambda e, t=t, r=r: e.dma_start(out=kv1[r:512:4, 0:512], in_=t[:]), reads=[bt], dbuf=outb)
                    elif g == 2:
                        S.dma("sp", lambda e, t=t, r=r: e.dma_start(out=kv2[r:TOK:16, 0:512], in_=t[64:128, :]), reads=[bt], dbuf=outb)
                    tb, btb, _ = kb[cnt["kb"] % 2]; cnt["kb"] += 1
                    S.op("act", lambda e, t=t, tb=tb: e.copy(out=tb[:], in_=t[:]), reads=[bt], writes=[btb])
                    if "ktr" in SKIP:
                        continue
                    pt2, pbuf2 = bank("B")
                    ptb = pt2[:].bitcast(BF16)
                    for h in range(4):
                        S.op("pe", lambda e, h=h, ptb=ptb, tb=tb: e.transpose(out=ptb[:, h * 128:(h + 1) * 128],
                                                                        in_=tb[:, h * 128:(h + 1) * 128], identity=identB[:]),
                             reads=[btb, b_identB], writes=[pbuf2], signal=(h == 3))
                    S.op("dve", lambda e, ptb=ptb, n=n: e.tensor_copy(out=KT[:, :, n * 128:(n + 1) * 128],
                                                                 in_=ptb[:, 0:512].rearrange("p (h t) -> p h t", h=4)),
                         reads=[pbuf2], writes=[b_KT])
                if SUB < 0:
                    return
                wt, bw = w_next()
                for n in ktiles + ["s"]:
                    pt, pbuf = bank("A")
                    if n == "s":
                        mm_group(pt[0:NS, :], pbuf, [(hT[:, kc, 2 * TOK:2 * TOK + NS], wt[:, kc, :]) for kc in range(16)], [b_hT, bw])
                        stash_sample(pt, pbuf, 2, g)
                        continue
                    sl, r, i0 = gtile_cols(g, n)
                    mm_group(pt[:, :], pbuf, [(hT[:, kc, sl], wt[:, kc, :]) for kc in range(16)], [b_hT, bw])
                    own_out = ((g == 0 and n == 15) or (g == 1 and n % 4 == 3) or (g == 2)) and "kvout" not in SKIP
                    if own_out:
                        t, bt, _ = kf[cnt["kf"] % 2]; cnt["kf"] += 1
                        S.op("act", lambda e, pt=pt, t=t: e.copy(out=t[:], in_=pt[:]), reads=[pbuf], writes=[bt])
                        if g == 0:
                            S.dma("sp", lambda e, t=t: e.dma_start(out=kv0[:, 512:1024], in_=t[:]), reads=[bt], dbuf=outb)
                        elif g == 1:
                            S.dma("sp", lambda e, t=t, r=r: e.dma_start(out=kv1[r:512:4, 512:1024], in_=t[:]), reads=[bt], dbuf=outb)
                        else:
                            S.dma("sp", lambda e, t=t, r=r: e.dma_start(out=kv2[r:TOK:16, 512:1024], in_=t[64:128, :]),
                                  reads=[bt], dbuf=outb)
                    if own_out:
                        S.op("dve", lambda e, t=t, n=n: e.tensor_copy(out=VG[:, n, :], in_=t[:]), reads=[bt], writes=[b_VG])
                    else:
                        S.op("dve", lambda e, pt=pt, n=n: e.tensor_copy(out=VG[:, n, :], in_=pt[:]), reads=[pbuf], writes=[b_VG])
                wt, bw = w_next()
                if g == 0:
                    qtiles = [(n, gtile_cols(0, n)[0], ropeG[:, n, :], (n - 8) * 128) for n in range(8, 16)]
                elif g == 1:
                    qtiles = []
                    for n in range(16):
                        if n % 4 >= 2:
                            sl, r, i0 = gtile_cols(1, n)
                            qtiles.append((n, sl, ropeG[:, n, :], r * Lq + (i0 - Lq)))
                else:
                    qtiles = []
                    for r0 in range(0, 16, 2):
                        qtiles.append((r0, None, ropeQ2[:, r0 // 2, :], r0 * 64))
                for (n, sl, tab, qc0) in qtiles + [("s", None, None, None)]:
                    pt, pbuf = bank("A")
                    if n == "s":
                        mm_group(pt[0:NS, :], pbuf, [(hT[:, kc, 2 * TOK:2 * TOK + NS], wt[:, kc, :]) for kc in range(16)], [b_hT, bw])
                        stash_sample(pt, pbuf, 0, g)
                        continue
                    if g == 2:
                        for hf in range(2):
                            c0 = TOK + n + hf
                            mm_group(pt[hf * 64:(hf + 1) * 64, :], pbuf,
                                     [(hT[:, kc, c0:c0 + 63 * 16 + 1:16], wt[:, kc, :]) for kc in range(16)], [b_hT, bw])
                    else:
                        mm_group(pt[:, :], pbuf, [(hT[:, kc, sl], wt[:, kc, :]) for kc in range(16)], [b_hT, bw])
                    t, bt, _ = kf[cnt["kf"] % 2]; cnt["kf"] += 1
                    S.op("act", lambda e, pt=pt, t=t: e.copy(out=t[:], in_=pt[:]), reads=[pbuf], writes=[bt])
                    rope_apply(t, bt, 128, tab, b_ropeQ2 if g == 2 else b_ropeG)
                    tb, btb, _ = kb[cnt["kb"] % 2]; cnt["kb"] += 1
                    S.op("act", lambda e, t=t, tb=tb: e.copy(out=tb[:], in_=t[:]), reads=[bt], writes=[btb])
                    pt2, pbuf2 = bank("B")
                    ptb = pt2[:].bitcast(BF16)
                    for h in range(4):
                        S.op("pe", lambda e, h=h, ptb=ptb, tb=tb: e.transpose(out=ptb[:, h * 128:(h + 1) * 128],
                                                                        in_=tb[:, h * 128:(h + 1) * 128], identity=identB[:]),
                             reads=[btb, b_identB], writes=[pbuf2], signal=(h == 3))
                    S.op("dve", lambda e, ptb=ptb, qc0=qc0: e.tensor_copy(out=QT[:, :, qc0:qc0 + 128],
                                                                     in_=ptb[:, 0:512].rearrange("p (h t) -> p h t", h=4)),
                         reads=[pbuf2], writes=[b_QT])
                if SUB < 1 + 2 * g:
                    return
                TQ = 128 if g < 2 else 64
                for h in range(4):
                    for r in range(d):
                        for qt in range(Lq // TQ):
                            qc0 = r * Lq + qt * TQ
                            i0q = Lq + qt * TQ
                            if g < 2:
                                kprev = r * L + i0q - 128
                                blocks = [(kprev, 128, (kprev // 128), maskB[:, 2 if qt == 0 else 0, :]),
                                          (r * L + i0q, 128, (r * L + i0q) // 128, maskB[:, 1, :])]
                            else:
                                blocks = [(r * L, 128, r, maskB[:, 4, 0:64])]
                            nb = len(blocks)
                            ps_s, pb_s = bank("A")
                            for bi, (kc0, nk, vt, mk) in enumerate(blocks):
                                o = ps_s[0:nk, bi * 128:bi * 128 + TQ]
                                S.op("pe", lambda e, o=o, kc0=kc0, nk=nk, h=h, qc0=qc0: e.matmul(
                                    o, lhsT=KT[:, h, kc0:kc0 + nk], rhs=QT[:, h, qc0:qc0 + TQ], start=True, stop=False),
                                    reads=[b_KT, b_QT], writes=[pb_s], signal=False)
                                S.op("pe", lambda e, o=o, nk=nk, mk=mk: e.matmul(
                                    o, lhsT=identB[0:nk, 0:nk], rhs=mk, start=False, stop=True),
                                    reads=[b_identB, b_maskB], writes=[pb_s], signal=(bi == nb - 1))
                            p_t, b_p, _ = PT[cnt["pt"] % 2]; cnt["pt"] += 1
                            S.op("act", lambda e, ps_s=ps_s, p_t=p_t, nb=nb: e.activation(
                                out=p_t[:, 0:nb, 0:TQ], in_=ps_s[:].rearrange("p (b t) -> p b t", b=4)[:, 0:nb, 0:TQ],
                                func=AF.Exp, scale=SCALE), reads=[pb_s], writes=[b_p])
                            ps_o, pb_o = bank("B") if (cnt["pt"] % 2) else bank("C")
                            mm_group(ps_o[:, 0:TQ], pb_o, [(VG[:, vt, h * 128:(h + 1) * 128], p_t[:, bi, 0:TQ])
                                                          for bi, (kc0, nk, vt, mk) in enumerate(blocks)], [b_VG, b_p])
                            mm_group(ps_o[:, 128:128 + TQ], pb_o, [(onesB[:, :], p_t[:, bi, 0:TQ]) for bi in range(nb)],
                                     [b_onesB, b_p])
                            nat = slice(qt * TQ * d + r, qt * TQ * d + r + (TQ - 1) * d + 1, d)
                            src = ps_o[:].rearrange("p (b t) -> p b t", b=4)[:, 0:2, 0:TQ]
                            if first_group:
                                S.op("dve", lambda e, src=src, h=h, nat=nat: e.tensor_copy(out=ACC[:, :, h, nat], in_=src),
                                     reads=[pb_o], writes=[b_ACC])
                            else:
                                S.op("dve", lambda e, src=src, h=h, nat=nat: e.tensor_tensor(
                                    out=ACC[:, :, h, nat], in0=ACC[:, :, h, nat], in1=src, op=ALU.add),
                                    reads=[pb_o, b_ACC], writes=[b_ACC])
                first_group = False
                if SUB < 2 + 2 * g:
                    return
            S.barrier()
            for r in (r_KT, r_QT, r_VG, r_ropeG, r_ropeQ2, kf[0][2], kf[1][2], kb[0][2], kb[1][2], PT[0][2], PT[1][2]):
                AR.release(r)
            bmixT, b_bmixT, r_bmixT = sb("bmixT", [128, 4, TOK + NS], BF16)
            S.op("dve", lambda e: e.reciprocal(out=ACC[:, 1], in_=ACC[:, 1]), reads=[b_ACC], writes=[b_ACC])
            S.op("dve", lambda e: e.tensor_tensor(out=bmixT[:, :, 0:TOK], in0=ACC[:, 0], in1=ACC[:, 1], op=ALU.mult),
                 reads=[b_ACC], writes=[b_bmixT])
            qkv_s, b_qkv_s, r_qkv_s = sb("qkv_s", [NS, 3, 3, 512], F32)
            for which in range(3):
                for g in range(3):
                    blk = which * 3 + g
                    j, slot = blk % 8, blk // 8
                    S.dma("sp", lambda e, which=which, g=g, j=j, slot=slot: e.dma_start(
                        out=qkv_s[:, which, g, :], in_=qkv_c[16 * j:16 * j + 16, slot, :]), reads=[b_qkv_c], writes=[b_qkv_s])

            if SUB < 7:
                return
            for which in (0, 1):
                for g in range(3):
                    x4 = qkv_s[:, which, g, :].rearrange("p (h d) -> p h d", h=4)
                    cc = ropeS[:, 0:32].unsqueeze(1).broadcast_to([NS, 4, 32])
                    s1 = ropeS[:, 32:48].unsqueeze(1).broadcast_to([NS, 4, 16])
                    s2 = ropeS[:, 48:64].unsqueeze(1).broadcast_to([NS, 4, 16])
                    A = rtmp[0:NS, 0]
                    B = rtmp[0:NS, 1]
                    S.op("dve", lambda e, x4=x4, A=A, cc=cc: e.tensor_tensor(out=A, in0=x4[:, :, 0:32], in1=cc, op=ALU.mult),
                         reads=[b_qkv_s, b_ropeS], writes=[b_rtmp])
                    S.op("dve", lambda e, x4=x4, B=B, s1=s1: e.tensor_tensor(out=B[:, :, 0:16], in0=x4[:, :, 16:32], in1=s1, op=ALU.mult),
                         reads=[b_qkv_s, b_ropeS], writes=[b_rtmp])
                    S.op("dve", lambda e, x4=x4, B=B, s2=s2: e.tensor_tensor(out=B[:, :, 16:32], in0=x4[:, :, 0:16], in1=s2, op=ALU.mult),
                         reads=[b_qkv_s, b_ropeS], writes=[b_rtmp])
                    S.op("dve", lambda e, x4=x4, A=A, B=B: e.tensor_tensor(out=x4[:, :, 0:32], in0=A, in1=B, op=ALU.add),
                         reads=[b_rtmp], writes=[b_qkv_s])
            for g in range(3):
                S.dma("sp", lambda e, g=g: e.dma_start(out=kvs[g, :, 0:512], in_=qkv_s[:, 1, g, :]), reads=[b_qkv_s], dbuf=outb)
                S.dma("sp", lambda e, g=g: e.dma_start(out=kvs[g, :, 512:1024], in_=qkv_s[:, 2, g, :]), reads=[b_qkv_s], dbuf=outb)

            CK = [sb("CK%d" % i, [128, 1024], F32) for i in range(3)]
            SEL, b_SEL, r_SEL = sb("SEL", [NS, NS, 128], F32)
            SELT, b_SELT, r_SELT = sb("SELT", [128, NS, NS], F32)
            S.op("pool", lambda e: e.memset(SEL[:], 1.0), writes=[b_SEL])
            S.op("pool", lambda e: e.affine_select(out=SEL[:], in_=SEL[:], pattern=[[1, NS], [0, 128]], compare_op=ALU.is_equal,
                                                   fill=0.0, base=0, channel_multiplier=-1), reads=[b_SEL], writes=[b_SEL])
            S.op("pool", lambda e: e.memset(SELT[:], 1.0), writes=[b_SELT])
            S.op("pool", lambda e: e.affine_select(out=SELT[:], in_=SELT[:], pattern=[[1, NS], [-1, NS]], compare_op=ALU.is_equal,
                                                   fill=0.0, base=0, channel_multiplier=0), reads=[b_SELT], writes=[b_SELT])
            sprod, b_sprod, r_sprod = sb("sprod", [128, 512], F32)
            ssc, b_ssc, r_ssc = sb("ssc", [128, 8], F32)
            snew, b_snew, r_snew = sb("snew", [NS, 3, 512], F32)
            sn_s, b_sn_s, r_sn_s = sb("sn_s", [NS, 3, 8], F32)
            so, b_so, r_so = sb("so", [NS, 516], F32)
            sob, b_sob, r_sob = sb("sob", [NS, 512], BF16)
            ps_os, pb_os = PB[6]
            ps_ds, pb_ds = PB[7]
            S.op("dve", lambda e: e.tensor_tensor(out=snew[:], in0=qkv_s[:, 0], in1=qkv_s[:, 1], op=ALU.mult),
                 reads=[b_qkv_s], writes=[b_snew])
            S.op("dve", lambda e: e.tensor_reduce(out=sn_s[:, :, 0:4], in_=snew[:].rearrange("p g (h d) -> p g h d", h=4),
                                                  axis=AX.X, op=ALU.add), reads=[b_snew], writes=[b_sn_s])
            S.op("act", lambda e: e.activation(out=sn_s[:, :, 4:8], in_=sn_s[:, :, 0:4], func=AF.Exp, scale=SCALE),
                 reads=[b_sn_s], writes=[b_sn_s])
            S.op("dve", lambda e: e.tensor_tensor(
                out=snew[:].rearrange("p g (h d) -> p g h d", h=4), in0=qkv_s[:, 2].rearrange("p g (h d) -> p g h d", h=4),
                in1=sn_s[:, :, 4:8].unsqueeze(3).broadcast_to([NS, 3, 4, 128]), op=ALU.mult),
                reads=[b_qkv_s, b_sn_s], writes=[b_snew])
            k = 0
            for n in range(NS):
                for g in range(3):
                    ck, b_ck, _ = CK[k % 3]
                    S.dma("sp", lambda e, ck=ck, g=g, n=n: e.dma_start(out=ck[:], in_=cache[g, n]), writes=[b_ck])
                    pq, pbq = bank("A")
                    S.op("pe", lambda e, pq=pq, n=n, g=g: e.matmul(pq[:, :], lhsT=SEL[:, n, :], rhs=qkv_s[:, 0, g, :],
                                                               start=True, stop=True), reads=[b_SEL, b_qkv_s], writes=[pbq])
                    S.op("dve", lambda e, ck=ck, pq=pq: e.tensor_tensor(out=sprod[:], in0=ck[:, 0:512], in1=pq[:, :], op=ALU.mult),
                         reads=[b_ck, pbq], writes=[b_sprod])
                    S.op("dve", lambda e: e.tensor_reduce(out=ssc[:, 0:4], in_=sprod[:].rearrange("p (h d) -> p h d", h=4),
                                                          axis=AX.X, op=ALU.add), reads=[b_sprod], writes=[b_ssc])
                    S.op("act", lambda e: e.activation(out=ssc[:, 4:8], in_=ssc[:, 0:4], func=AF.Exp, scale=SCALE),
                         reads=[b_ssc], writes=[b_ssc])
                    S.op("dve", lambda e, ck=ck: e.tensor_tensor(
                        out=sprod[:].rearrange("p (h d) -> p h d", h=4), in0=ck[:, 512:1024].rearrange("p (h d) -> p h d", h=4),
                        in1=ssc[:, 4:8].unsqueeze(2).broadcast_to([128, 4, 128]), op=ALU.mult),
                        reads=[b_ck, b_ssc], writes=[b_sprod])
                    first, last = (k == 0), (k == NS * 3 - 1)
                    S.op("pe", lambda e, n=n, first=first, last=last: e.matmul(ps_os[0:NS, :], lhsT=SELT[:, n, :], rhs=sprod[:],
                                                                          start=first, stop=last),
                         reads=[b_SELT, b_sprod], writes=[pb_os], signal=True)
                    S.op("pe", lambda e, n=n, first=first, last=last: e.matmul(ps_ds[0:NS, 0:4], lhsT=SELT[:, n, :], rhs=ssc[:, 4:8],
                                                                          start=first, stop=last),
                         reads=[b_SELT, b_ssc], writes=[pb_ds], signal=True)
                    k += 1
            S.op("dve", lambda e: e.tensor_tensor(out=so[:, 0:512], in0=snew[:, 0, :], in1=snew[:, 1, :], op=ALU.add),
                 reads=[b_snew], writes=[b_so])
            S.op("dve", lambda e: e.tensor_tensor(out=so[:, 0:512], in0=so[:, 0:512], in1=snew[:, 2, :], op=ALU.add),
                 reads=[b_snew, b_so], writes=[b_so])
            S.op("dve", lambda e: e.tensor_tensor(out=so[:, 0:512], in0=so[:, 0:512], in1=ps_os[0:NS, :], op=ALU.add),
                 reads=[pb_os, b_so], writes=[b_so])
            S.op("dve", lambda e: e.tensor_tensor(out=so[:, 512:516], in0=sn_s[:, 0, 4:8], in1=sn_s[:, 1, 4:8], op=ALU.add),
                 reads=[b_sn_s], writes=[b_so])
            S.op("dve", lambda e: e.tensor_tensor(out=so[:, 512:516], in0=so[:, 512:516], in1=sn_s[:, 2, 4:8], op=ALU.add),
                 reads=[b_sn_s, b_so], writes=[b_so])
            S.op("dve", lambda e: e.tensor_tensor(out=so[:, 512:516], in0=so[:, 512:516], in1=ps_ds[0:NS, 0:4], op=ALU.add),
                 reads=[pb_ds, b_so], writes=[b_so])
            S.op("dve", lambda e: e.reciprocal(out=so[:, 512:516], in_=so[:, 512:516]), reads=[b_so], writes=[b_so])
            S.op("dve", lambda e: e.tensor_tensor(out=sob[:].rearrange("p (h d) -> p h d", h=4),
                                                  in0=so[:, 0:512].rearrange("p (h d) -> p h d", h=4),
                                                  in1=so[:, 512:516].unsqueeze(2).broadcast_to([NS, 4, 128]), op=ALU.mult),
                 reads=[b_so], writes=[b_sob])
            pt2, pbuf2 = bank("B")
            ptb = pt2[:].bitcast(BF16)
            for h in range(4):
                S.op("pe", lambda e, h=h, ptb=ptb: e.transpose(out=ptb[:, h * NS:(h + 1) * NS], in_=sob[:, h * 128:(h + 1) * 128],
                                                          identity=identB[0:NS, 0:NS]),
                     reads=[b_sob, b_identB], writes=[pbuf2], signal=(h == 3))
            S.op("dve", lambda e, ptb=ptb: e.tensor_copy(out=bmixT[:, :, TOK:TOK + NS],
                                                    in_=ptb[:, 0:4 * NS].rearrange("p (h t) -> p h t", h=4)),
                 reads=[pbuf2], writes=[b_bmixT])

            S.barrier()
            for r in (r_ACC, r_rtmp, r_qkv_s, r_qkv_c, stg[0][2], stg[1][2], r_SEL, r_SELT, r_sprod, r_ssc, r_snew, r_sn_s,
                      r_so, r_sob, CK[0][2], CK[1][2], CK[2][2]):
                AR.release(r)

            if STOP < 2:
                return
            amixT, b_amixT, r_amixT = sb("amixT", [128, 8, TOK + NS], BF16)
            vn, b_vn, r_vn = sb("vn", [128, 9, A_W], BF16)
            gv = [sb("gv%d" % i, [128, A_W], F32) for i in range(2)]
            lng, b_lng, r_lng = sb("lng", [128, A_W], F32)
            lnb, b_lnb, r_lnb = sb("lnb", [128, A_W], F32)
            S.dma("sp", lambda e: e.dma_start(out=lng[:], in_=lnv[0, :].partition_broadcast(128)), writes=[b_lng])
            S.dma("sp", lambda e: e.dma_start(out=lnb[:], in_=lnv[1, :].partition_broadcast(128)), writes=[b_lnb])
            bns, b_bns, r_bns = sb("bns", [128, 2, 8], F32)
            wsT, b_wsT, r_wsT = sb("wsT", [128, 8, 128], BF16)
            wsF, b_wsF, r_wsF = sb("wsF", [128, 8, 128], F32)
            bsb, b_bsb, r_bsb = sb("bsb", [128, 8, 128], F32)
            bs0, b_bs0, r_bs0 = sb("bs0", [128, 8], F32)
            ws00, b_ws00, r_ws00 = sb("ws00", [16, 8], F32)
            dg, b_dg, r_dg = sb("dg", [16, 8, 16], BF16)
            sgt, b_sgt, r_sgt = sb("sgt", [128, 512], F32)
            S.dma("sp", lambda e: e.dma_start(out=wsF[:], in_=w_s.rearrange("g t s -> t g s")), writes=[b_wsF])
            S.dma("sp", lambda e: e.dma_start(out=bsb[:], in_=b_s.rearrange("g t -> (g t)").partition_broadcast(128)
                                              .rearrange("p (g t) -> p g t", g=8)), writes=[b_bsb])
            S.dma("sp", lambda e: e.dma_start(out=ws00[:], in_=w_s[:, 0, 0].partition_broadcast(16),
                                              allow_slow_non_contiguous=True), writes=[b_ws00])
            wsM, b_wsM, r_wsM = sb("wsM", [128, 8, 128], F32)
            for half in range(2):
                pt, pbuf = bank("C")
                for j in range(4):
                    gi = half * 4 + j
                    S.op("pe", lambda e, gi=gi, j=j, pt=pt: e.transpose(out=pt[:, j * 128:(j + 1) * 128], in_=wsF[:, gi, :],
                                                                   identity=identF[:]),
                         reads=[b_wsF, b_identF], writes=[pbuf], signal=(j == 3))
                S.op("act", lambda e, half=half, pt=pt: e.copy(out=wsM[:, half * 4:half * 4 + 4, :],
                                                           in_=pt[:].rearrange("p (g t) -> p g t", g=4)),
                     reads=[pbuf], writes=[b_wsM])
            S.op("pool", lambda e: e.affine_select(out=wsT[:], in_=wsM[:], pattern=[[0, 8], [1, 128]],
                                                   compare_op=ALU.is_ge, fill=0.0, base=0, channel_multiplier=-1),
                 reads=[b_wsM], writes=[b_wsT])
            S.op("dve", lambda e: e.tensor_tensor(out=dg[:], in0=identF[0:16, 0:16].unsqueeze(1).broadcast_to([16, 8, 16]),
                                                  in1=ws00[:].unsqueeze(2).broadcast_to([16, 8, 16]), op=ALU.mult),
                 reads=[b_identF, b_ws00], writes=[b_dg])

            wva0, bwva0 = w_next()
            wva1, bwva1 = w_next(issue=False)
            own_tiles = [(TOK + n * 128, 128) for n in range(8)] + [(2 * TOK, NS)]
            for ti, (c0, rows) in enumerate(own_tiles):
                g_t, b_g, _ = gv[ti % 2]
                for blk, (wt, bw) in enumerate(((wva0, bwva0), (wva1, bwva1))):
                    pt, pbuf = bank("A")
                    mm_group(pt[0:rows, :], pbuf, [(hT[:, kc, c0:c0 + rows], wt[:, kc, :]) for kc in range(16)], [b_hT, bw])
                    S.op("act", lambda e, pt=pt, blk=blk, g_t=g_t, rows=rows: e.activation(
                        out=g_t[0:rows, blk * 512:(blk + 1) * 512], in_=pt[0:rows, :], func=AF.Gelu_apprx_tanh),
                        reads=[pbuf], writes=[b_g])
                for blk in range(2):
                    S.op("dve", lambda e, blk=blk, g_t=g_t, rows=rows: e.bn_stats(
                        out=bns[0:rows, blk, 0:6], in_=g_t[0:rows, blk * 512:(blk + 1) * 512]), reads=[b_g], writes=[b_bns])
                mv = bns[0:rows, 0, 6:8]
                S.op("dve", lambda e, rows=rows, mv=mv: e.bn_aggr(out=mv, in_=bns[0:rows, :, 0:6]), reads=[b_bns], writes=[b_bns])
                sd = bns[0:rows, 1, 6:7]
                rsd = bns[0:rows, 1, 7:8]
                S.op("dve", lambda e, rows=rows, sd=sd: e.tensor_scalar(out=sd, in0=bns[0:rows, 0, 7:8], scalar1=EPS, scalar2=None,
                                                                     op0=ALU.add), reads=[b_bns], writes=[b_bns])
                S.op("act", lambda e, sd=sd: e.sqrt(out=sd, in_=sd), reads=[b_bns], writes=[b_bns])
                S.op("dve", lambda e, sd=sd, rsd=rsd: e.reciprocal(out=rsd, in_=sd), reads=[b_bns], writes=[b_bns])
                S.op("dve", lambda e, g_t=g_t, rows=rows, rsd=rsd: e.tensor_scalar(
                    out=g_t[0:rows, :], in0=g_t[0:rows, :], scalar1=bns[0:rows, 0, 6:7], scalar2=rsd,
                    op0=ALU.subtract, op1=ALU.mult), reads=[b_g, b_bns], writes=[b_g])
                S.op("dve", lambda e, g_t=g_t, rows=rows: e.tensor_tensor(out=g_t[0:rows, :], in0=g_t[0:rows, :], in1=lng[0:rows, :],
                                                                      op=ALU.mult), reads=[b_g, b_lng], writes=[b_g])
                if rows == 128:
                    S.op("dve", lambda e, g_t=g_t, ti=ti: e.tensor_tensor(out=vn[:, ti, :], in0=g_t[:], in1=lnb[:], op=ALU.add),
                         reads=[b_g, b_lnb], writes=[b_vn])
                else:
                    S.op("dve", lambda e, g_t=g_t, rows=rows: e.tensor_tensor(out=g_t[0:rows, :], in0=g_t[0:rows, :],
                                                                          in1=lnb[0:rows, :], op=ALU.add),
                         reads=[b_g, b_lnb], writes=[b_g])
                    S.dma("sp", lambda e, g_t=g_t, rows=rows: e.dma_start(out=sguv, in_=g_t[0:rows, :]), reads=[b_g], dbuf=outb)
                    S.op("act", lambda e, g_t=g_t, rows=rows, ti=ti: e.copy(out=vn[0:rows, ti, :], in_=g_t[0:rows, :]),
                         reads=[b_g], writes=[b_vn])

            for blk in range(2):
                wt, bw = w_next()
                for jj in range(4):
                    j = blk * 4 + jj
                    for (c0, wd) in PASSES:
                        pt, pbuf = bank("A")
                        mm_group(pt[:, 0:wd], pbuf, [(wt[:, kc, jj * 128:(jj + 1) * 128], hT[:, kc, c0:c0 + wd]) for kc in range(16)],
                                 [b_hT, bw])
                        S.op("act", lambda e, pt=pt, j=j, c0=c0, wd=wd: e.activation(
                            out=amixT[:, j, c0 - TOK:c0 - TOK + wd], in_=pt[:, 0:wd], func=AF.Gelu_apprx_tanh),
                            reads=[pbuf], writes=[b_amixT])

            for tt in range(8):
                for half in range(2):
                    pt, pbuf = bank("A")
                    for jj in range(4):
                        gi = half * 4 + jj
                        S.op("pe", lambda e, pt=pt, jj=jj, gi=gi, tt=tt: e.matmul(
                            pt[:, jj * 128:(jj + 1) * 128], lhsT=vn[:, tt, gi * 128:(gi + 1) * 128], rhs=wsT[:, gi, :],
                            start=True, stop=True), reads=[b_vn, b_wsT], writes=[pbuf], signal=(jj == 3))
                    S.op("dve", lambda e, pt=pt, half=half: e.tensor_tensor(
                        out=sgt[:].rearrange("p (g t) -> p g t", g=4), in0=pt[:].rearrange("p (g t) -> p g t", g=4),
                        in1=bsb[:, half * 4:half * 4 + 4, :], op=ALU.add), reads=[pbuf, b_bsb], writes=[b_sgt])
                    S.op("dve", lambda e, half=half, tt=tt: e.tensor_tensor(
                        out=amixT[:, half * 4:half * 4 + 4, tt * 128:(tt + 1) * 128],
                        in0=amixT[:, half * 4:half * 4 + 4, tt * 128:(tt + 1) * 128],
                        in1=sgt[:].rearrange("p (g t) -> p g t", g=4), op=ALU.mult), reads=[b_sgt, b_amixT], writes=[b_amixT])
            pt, pbuf = bank("A")
            for gi in range(8):
                S.op("pe", lambda e, pt=pt, gi=gi: e.matmul(pt[:, gi * NS:(gi + 1) * NS], lhsT=vn[0:NS, 8, gi * 128:(gi + 1) * 128],
                                                       rhs=dg[:, gi, :], start=True, stop=True),
                     reads=[b_vn, b_dg], writes=[pbuf], signal=(gi == 7))
            S.op("dve", lambda e, pt=pt: e.tensor_tensor(
                out=sgt[:, 0:128].rearrange("p (g t) -> p g t", g=8), in0=pt[:, 0:128].rearrange("p (g t) -> p g t", g=8),
                in1=bsb[:, :, 0:1].broadcast_to([128, 8, NS]), op=ALU.add), reads=[pbuf, b_bsb], writes=[b_sgt])
            S.op("dve", lambda e: e.tensor_tensor(out=amixT[:, :, TOK:TOK + NS], in0=amixT[:, :, TOK:TOK + NS],
                                                  in1=sgt[:, 0:128].rearrange("p (g t) -> p g t", g=8), op=ALU.mult),
                 reads=[b_sgt, b_amixT], writes=[b_amixT])

            S.barrier()
            for r in (r_vn, gv[0][2], gv[1][2], r_lng, r_lnb, r_bns, r_wsT, r_wsF, r_bsb, r_bs0, r_ws00, r_dg, r_sgt, r_wsM):
                AR.release(r)

            if STOP < 3:
                return
            mergedT, b_mergedT, r_mergedT = sb("mergedT", [128, 16, TOK + NS], BF16, top=True)
            sgA, b_sgA, r_sgA = sb("sgA", [128, 4, TOK + NS], BF16)
            sgB, b_sgB = sgA, b_sgA
            M1, b_M1, r_M1 = sb("M1", [128, 4, TOK + NS], F32)
            mtmp, b_mtmp, r_mtmp = sb("mtmp", [128, 512], F32)
            def gate_block():
                wt, bw = w_next()
                for jj in range(4):
                    for (c0, wd) in PASSES:
                        pt, pbuf = bank("A")
                        mm_group(pt[:, 0:wd], pbuf, [(wt[:, kc, jj * 128:(jj + 1) * 128], hT[:, kc, c0:c0 + wd]) for kc in range(16)],
                                 [b_hT, bw])
                        S.op("act", lambda e, pt=pt, jj=jj, c0=c0, wd=wd: e.activation(
                            out=sgA[:, jj, c0 - TOK:c0 - TOK + wd], in_=pt[:, 0:wd], func=AF.Sigmoid),
                            reads=[pbuf], writes=[b_sgA])

            for cb in range(4):
                gate_block()
                wt, bw = w_next()
                for jj in range(4):
                    for (c0, wd) in PASSES:
                        pt, pbuf = bank("A")
                        mm_group(pt[:, 0:wd], pbuf, [(wt[:, kc, jj * 128:(jj + 1) * 128], amixT[:, kc, c0 - TOK:c0 - TOK + wd])
                                                     for kc in range(8)], [b_amixT, bw])
                        S.op("dve", lambda e, pt=pt, jj=jj, c0=c0, wd=wd: e.tensor_tensor(
                            out=M1[:, jj, c0 - TOK:c0 - TOK + wd], in0=pt[:, 0:wd], in1=sgA[:, jj, c0 - TOK:c0 - TOK + wd], op=ALU.mult),
                            reads=[pbuf, b_sgA], writes=[b_M1])
                gate_block()
                wt, bw = w_next()
                for jj in range(4):
                    for (c0, wd) in PASSES:
                        pt, pbuf = bank("A")
                        mm_group(pt[:, 0:wd], pbuf, [(wt[:, kc, jj * 128:(jj + 1) * 128], bmixT[:, kc, c0 - TOK:c0 - TOK + wd])
                                                     for kc in range(4)], [b_bmixT, bw])
                        S.op("dve", lambda e, pt=pt, jj=jj, c0=c0, wd=wd: e.tensor_tensor(
                            out=mtmp[:, 0:wd], in0=pt[:, 0:wd], in1=sgB[:, jj, c0 - TOK:c0 - TOK + wd], op=ALU.mult),
                            reads=[pbuf, b_sgB], writes=[b_mtmp])
                        S.op("dve", lambda e, cb=cb, jj=jj, c0=c0, wd=wd: e.tensor_tensor(
                            out=mergedT[:, cb * 4 + jj, c0 - TOK:c0 - TOK + wd], in0=mtmp[:, 0:wd],
                            in1=M1[:, jj, c0 - TOK:c0 - TOK + wd], op=ALU.add),
                            reads=[b_mtmp, b_M1], writes=[b_mergedT])

            S.barrier()
            for r in (r_hT, r_amixT, r_bmixT, r_sgA, r_M1, r_mtmp):
                AR.release(r)

            if STOP < 4:
                return
            X2, b_X2, r_X2 = sb("X2", [128, 9, D], F32)
            bX2 = [Buf("X2_%d" % i) for i in range(9)]
            xpc = [sb("xpc%d" % i, [128, 512], F32) for i in range(3)]
            rows_of = [128] * 8 + [NS]
            k = 0

            def load_xpiece(k):
                cb, tt = divmod(k, 9)
                t, bf, _ = xpc[k % 3]
                rows = rows_of[tt]
                src = xloc[TOK + tt * 128:TOK + (tt + 1) * 128, cb * 512:(cb + 1) * 512] if tt < 8 else xs[:, cb * 512:(cb + 1) * 512]
                S.dma("sp", lambda e: e.dma_start(out=t[0:rows, :], in_=src), writes=[bf])

            load_xpiece(0); load_xpiece(1)
            for cb in range(4):
                wt, bw = w_next()
                for tt in range(9):
                    if k + 2 < 36:
                        load_xpiece(k + 2)
                    rows = rows_of[tt]
                    t, bf, _ = xpc[k % 3]
                    pt, pbuf = bank("A")
                    mm_group(pt[0:rows, :], pbuf, [(mergedT[:, kc, tt * 128:tt * 128 + rows], wt[:, kc, :]) for kc in range(16)],
                             [b_mergedT, bw])
                    S.op("dve", lambda e, pt=pt, t=t, tt=tt, cb=cb, rows=rows: e.tensor_tensor(
                        out=X2[0:rows, tt, cb * 512:(cb + 1) * 512], in0=t[0:rows, :], in1=pt[0:rows, :], op=ALU.add),
                        reads=[pbuf, bf], writes=[bX2[tt]])
                    k += 1
            S.barrier()
            AR.release(r_mergedT)
            for i in range(3):
                AR.release(xpc[i][2])

            if STOP < 5:
                return
            h2T, b_h2T, r_h2T = sb("h2T", [128, 16, TOK + NS], BF16)
            rstd2, b_rstd2, _ = sb("rstd2", [128, 9], F32)
            xnb = [sb("xnb%d" % i, [128, D], BF16) for i in range(2)]
            SQ["t"], SQ["b"], SQ["r"] = sb("sqjunk", [128, D], BF16)
            for tt in range(9):
                rows = rows_of[tt]
                rs = rms_stats(X2[0:rows, tt, :], rows, bX2[tt], tt % 2)
                S.op("dve", lambda e, rs=rs, tt=tt, rows=rows: e.tensor_copy(out=rstd2[0:rows, tt:tt + 1], in_=rs),
                     reads=[b_stat], writes=[b_rstd2])
                xn, bxn, _ = xnb[tt % 2]
                S.op("act", lambda e, xn=xn, rs=rs, tt=tt, rows=rows: e.activation(
                    out=xn[0:rows, :], in_=X2[0:rows, tt, :], func=AF.Copy, scale=rs), reads=[bX2[tt], b_stat], writes=[bxn])
                transpose_to(h2T, b_h2T, tt * 128, xn, bxn, rows, 1)

            if STOP < 6:
                return
            skF, b_skF, r_skF = sb("skF", [128, 2, 128], F32)
            skT, b_skT, _ = sb("skT", [128, 2, 128], BF16)
            S.dma("sp", lambda e: e.dma_start(out=skF[:], in_=subk.rearrange("j k d -> k j d")), writes=[b_skF])
            pt, pbuf = bank("C")
            for j in range(2):
                S.op("pe", lambda e, j=j, pt=pt: e.transpose(out=pt[:, j * 128:(j + 1) * 128], in_=skF[:, j, :], identity=identF[:]),
                     reads=[b_skF, b_identF], writes=[pbuf], signal=(j == 1))
            S.op("act", lambda e, pt=pt: e.copy(out=skT[:], in_=pt[:, 0:256].rearrange("p (j k) -> p j k", j=2)),
                 reads=[pbuf], writes=[b_skT])
            T1, b_T1, _ = sb("T1", [128, 9, 16, 16], F32)
            I1, b_I1, _ = sb("I1", [128, 9, 16, 16], U32)
            qc, b_qc, r_qc = sb("qc", [128, TOK + NS], BF16)
            scs = [sb("scs%d" % i, [128, 128], F32) for i in range(2)]
            sc2, b_sc2, r_sc2 = sb("sc2", [128, 128], F32)
            k = 0
            for cb in range(4):
                wt, bw = w_next()
                for jj in range(4):
                    c = cb * 4 + jj
                    for (c0, wd) in PASSES:
                        pt, pbuf = bank("A")
                        mm_group(pt[:, 0:wd], pbuf, [(wt[:, kc, jj * 128:(jj + 1) * 128], h2T[:, kc, c0 - TOK:c0 - TOK + wd])
                                                     for kc in range(16)], [b_h2T, bw])
                        S.op("act", lambda e, pt=pt, c0=c0, wd=wd: e.copy(out=qc[:, c0 - TOK:c0 - TOK + wd], in_=pt[:, 0:wd]),
                             reads=[pbuf], writes=[b_qc])
                    for tt in range(9):
                        rows = rows_of[tt]
                        pt, pbuf = bank("B") if tt % 2 else bank("C")
                        S.op("pe", lambda e, pt=pt, tt=tt, rows=rows, c=c: e.matmul(
                            pt[0:rows, 0:128], lhsT=qc[:, tt * 128:tt * 128 + rows], rhs=skT[:, c % 2, :], start=True, stop=True),
                            reads=[b_qc, b_skT], writes=[pbuf])
                        sc, b_sc, _ = scs[k % 2]; k += 1
                        S.op("act", lambda e, pt=pt, sc=sc, rows=rows: e.copy(out=sc[0:rows, :], in_=pt[0:rows, 0:128]),
                             reads=[pbuf], writes=[b_sc])
                        S.op("dve", lambda e, sc=sc, rows=rows, tt=tt, c=c: e.max(out=T1[0:rows, tt, c, 0:8], in_=sc[0:rows, :]),
                             reads=[b_sc], writes=[b_T1])
                        S.op("dve", lambda e, sc=sc, rows=rows, tt=tt, c=c: e.max_index(
                            out=I1[0:rows, tt, c, 0:8], in_max=T1[0:rows, tt, c, 0:8], in_values=sc[0:rows, :]),
                            reads=[b_sc, b_T1], writes=[b_I1])
                        S.op("dve", lambda e, sc=sc, rows=rows, tt=tt, c=c: e.match_replace(
                            out=sc2[0:rows, :], in_to_replace=T1[0:rows, tt, c, 0:8], in_values=sc[0:rows, :], imm_value=-3.0e38),
                            reads=[b_sc, b_T1], writes=[b_sc2])
                        S.op("dve", lambda e, rows=rows, tt=tt, c=c: e.max(out=T1[0:rows, tt, c, 8:16], in_=sc2[0:rows, :]),
                             reads=[b_sc2], writes=[b_T1])
                        S.op("dve", lambda e, rows=rows, tt=tt, c=c: e.max_index(
                            out=I1[0:rows, tt, c, 8:16], in_max=T1[0:rows, tt, c, 8:16], in_values=sc2[0:rows, :]),
                            reads=[b_sc2, b_T1], writes=[b_I1])
            S.barrier()
            for r in (r_skF, r_qc, scs[0][2], scs[1][2], r_sc2, r_h2T, xnb[0][2], xnb[1][2], SQ["r"]):
                AR.release(r)

            if STOP < 7:
                return
            gffn_b, b_gffn_b, r_gffn_b = sb("gffn_b", [128, D], F32)
            S.dma("sp", lambda e: e.dma_start(out=gffn_b[:], in_=gvec[1, :].partition_broadcast(128)), writes=[b_gffn_b])
            cand, b_cand, r_cand = sb("cand", [128, 8, 256], F32)
            cand2, b_cand2, _ = sb("cand2", [128, 256], F32)
            ts, b_ts, _ = sb("ts", [128, 8, 16], F32)
            ic, b_ic, _ = sb("ic", [128, 8, 16], U32)
            icw, b_icw, _ = sb("icw", [128, 2, 8, 16], U32)
            icf, b_icf, _ = sb("icf", [128, 2, 8, 16], F32)
            i1f, b_i1f, _ = sb("i1f", [128, 16, 16], F32)
            iota16, b_iota16, _ = sb("iota16", [128, 16], F32)
            oh, b_oh = cand[:].rearrange("p h (a b) -> p h a b", a=16), b_cand
            ef, b_ef, _ = sb("ef", [128, 2, 8, 16], F32)
            eidx, b_eidx, _ = sb("eidx", [128, 128], I32)
            gsm, b_gsm, _ = sb("gsm", [128, 8, 16], F32)
            gs2, b_gs2, _ = sb("gs2", [128, 16], F32)
            actp, b_actp, _ = sb("actp", [128, 128], F32)
            coef, b_coef, _ = sb("coef", [128, 128], F32)
            h2t, b_h2t, r_h2t = sb("h2t", [128, D], F32)
            yacc, b_yacc, r_yacc = sb("yacc", [128, D], F32)
            NG = 3
            ug = [sb("ug%d" % i, [128, D], F32) for i in range(NG)]
            S.op("pool", lambda e: e.iota(iota16[:], pattern=[[1, 16]], base=0, channel_multiplier=0,
                                          allow_small_or_imprecise_dtypes=True), writes=[b_iota16])
            S.op("pool", lambda e: e.memset(eidx[:], 0), writes=[b_eidx])
            for tt in range(9):
                rows = rows_of[tt]
                T = T1[0:rows, tt].rearrange("p (h j) k -> p h j k", j=2)
                S.op("dve", lambda e, T=T, rows=rows: e.tensor_tensor(
                    out=cand[0:rows].rearrange("p h (a b) -> p h a b", a=16),
                    in0=T[:, :, 0, :].unsqueeze(3).broadcast_to([rows, 8, 16, 16]),
                    in1=T[:, :, 1, :].unsqueeze(2).broadcast_to([rows, 8, 16, 16]), op=ALU.add),
                    reads=[b_T1], writes=[b_cand])
                for h in range(8):
                    S.op("dve", lambda e, h=h, rows=rows: e.max(out=ts[0:rows, h, 0:8], in_=cand[0:rows, h, :]),
                         reads=[b_cand], writes=[b_ts])
                    S.op("dve", lambda e, h=h, rows=rows: e.max_index(out=ic[0:rows, h, 0:8], in_max=ts[0:rows, h, 0:8],
                                                                   in_values=cand[0:rows, h, :]),
                         reads=[b_cand, b_ts], writes=[b_ic])
                    S.op("dve", lambda e, h=h, rows=rows: e.match_replace(out=cand2[0:rows, :], in_to_replace=ts[0:rows, h, 0:8],
                                                                       in_values=cand[0:rows, h, :], imm_value=-3.0e38),
                         reads=[b_cand, b_ts], writes=[b_cand2])
                    S.op("dve", lambda e, h=h, rows=rows: e.max(out=ts[0:rows, h, 8:16], in_=cand2[0:rows, :]),
                         reads=[b_cand2], writes=[b_ts])
                    S.op("dve", lambda e, h=h, rows=rows: e.max_index(out=ic[0:rows, h, 8:16], in_max=ts[0:rows, h, 8:16],
                                                                   in_values=cand2[0:rows, :]),
                         reads=[b_cand2, b_ts], writes=[b_ic])
                S.op("dve", lambda e, rows=rows: e.tensor_single_scalar(out=icw[0:rows, 0], in_=ic[0:rows], scalar=c4[0:rows, 0:1],
                                                                       op=ALU.logical_shift_right), reads=[b_ic, b_c4], writes=[b_icw])
                S.op("dve", lambda e, rows=rows: e.tensor_single_scalar(out=icw[0:rows, 1], in_=ic[0:rows], scalar=c4[0:rows, 1:2],
                                                                       op=ALU.bitwise_and), reads=[b_ic, b_c4], writes=[b_icw])
                S.op("dve", lambda e, rows=rows: e.tensor_copy(out=icf[0:rows], in_=icw[0:rows]), reads=[b_icw], writes=[b_icf])
                S.op("dve", lambda e, rows=rows, tt=tt: e.tensor_copy(out=i1f[0:rows], in_=I1[0:rows, tt]), reads=[b_I1], writes=[b_i1f])
                I = i1f[0:rows].rearrange("p (h j) k -> p h j k", j=2)
                for side in range(2):
                    S.op("dve", lambda e, side=side, rows=rows: e.tensor_tensor(
                        out=oh[0:rows], in0=icf[0:rows, side].unsqueeze(3).broadcast_to([rows, 8, 16, 16]),
                        in1=iota16[0:rows].unsqueeze(1).unsqueeze(1).broadcast_to([rows, 8, 16, 16]), op=ALU.is_equal),
                        reads=[b_icf, b_iota16], writes=[b_oh])
                    S.op("dve", lambda e, side=side, rows=rows, I=I: e.tensor_tensor(
                        out=oh[0:rows], in0=oh[0:rows], in1=I[:, :, side, :].unsqueeze(2).broadcast_to([rows, 8, 16, 16]),
                        op=ALU.mult), reads=[b_oh, b_i1f], writes=[b_oh])
                    S.op("dve", lambda e, side=side, rows=rows: e.tensor_reduce(out=ef[0:rows, side], in_=oh[0:rows], axis=AX.X,
                                                                             op=ALU.add), reads=[b_oh], writes=[b_ef])
                S.op("dve", lambda e, rows=rows: e.scalar_tensor_tensor(
                    out=ef[0:rows, 0], in0=ef[0:rows, 0], scalar=128.0, in1=ef[0:rows, 1], op0=ALU.mult, op1=ALU.add),
                    reads=[b_ef], writes=[b_ef])
                S.op("dve", lambda e, rows=rows: e.tensor_copy(out=eidx[0:rows, :].rearrange("p (h k) -> p h k", h=8), in_=ef[0:rows, 0]),
                     reads=[b_ef], writes=[b_eidx])
                S.op("dve", lambda e, rows=rows: e.tensor_tensor(
                    out=gsm[0:rows], in0=ts[0:rows], in1=ts[0:rows, :, 0:1].broadcast_to([rows, 8, 16]), op=ALU.subtract),
                    reads=[b_ts], writes=[b_gsm])
                S.op("act", lambda e, rows=rows: e.activation(out=gsm[0:rows], in_=gsm[0:rows], func=AF.Exp), reads=[b_gsm], writes=[b_gsm])
                S.op("dve", lambda e, rows=rows: e.tensor_reduce(out=gs2[0:rows, 0:8], in_=gsm[0:rows], axis=AX.X, op=ALU.add),
                     reads=[b_gsm], writes=[b_gs2])
                S.op("dve", lambda e, rows=rows: e.reciprocal(out=gs2[0:rows, 8:16], in_=gs2[0:rows, 0:8]), reads=[b_gs2], writes=[b_gs2])
                S.op("dve", lambda e, rows=rows: e.tensor_tensor(
                    out=gsm[0:rows], in0=gsm[0:rows], in1=gs2[0:rows, 8:16].unsqueeze(2).broadcast_to([rows, 8, 16]), op=ALU.mult),
                    reads=[b_gsm, b_gs2], writes=[b_gsm])
                S.op("dve", lambda e, rows=rows, tt=tt: e.scalar_tensor_tensor(
                    out=h2t[0:rows, :], in0=X2[0:rows, tt, :], scalar=rstd2[0:rows, tt:tt + 1], in1=gffn_b[0:rows, :],
                    op0=ALU.mult, op1=ALU.mult), reads=[bX2[tt], b_rstd2, b_gffn_b], writes=[b_h2t])
                def gather(table, s, slot):
                    t, bf, _ = ug[slot % NG]
                    S.dma("pool", lambda e, t=t, s=s: e.indirect_dma_start(
                        out=t[:], out_offset=None, in_=table,
                        in_offset=bass.IndirectOffsetOnAxis(ap=eidx[:, s:s + 1], axis=0)), reads=[b_eidx], writes=[bf])
                    return t, bf
                LOOK = NG - 1
                pend = {}
                base = tt * 256
                for s in range(min(LOOK, 128)):
                    pend[s] = gather(peer_u, s, base + s)
                for s in range(128):
                    if s + LOOK < 128:
                        pend[s + LOOK] = gather(peer_u, s + LOOK, base + s + LOOK)
                    t, bf = pend.pop(s)
                    S.op("dve", lambda e, t=t, s=s, rows=rows: e.scalar_tensor_tensor(
                        out=t[0:rows, :], in0=t[0:rows, :], scalar=1.0, in1=h2t[0:rows, :], op0=ALU.mult, op1=ALU.mult,
                        accum_out=actp[0:rows, s:s + 1]), reads=[b_h2t], writes=[bf, b_actp])
                S.op("act", lambda e, rows=rows: e.activation(out=coef[0:rows, :], in_=actp[0:rows, :], func=AF.Gelu_apprx_tanh),
                     reads=[b_actp], writes=[b_coef])
                S.op("dve", lambda e, rows=rows: e.tensor_tensor(out=coef[0:rows, :], in0=coef[0:rows, :],
                                                                in1=gsm[0:rows].rearrange("p h k -> p (h k)"), op=ALU.mult),
                     reads=[b_coef, b_gsm], writes=[b_coef])
                base = tt * 256 + 128
                for s in range(min(LOOK, 128)):
                    pend[s] = gather(peer_v, s, base + s)
                for s in range(128):
                    if s + LOOK < 128:
                        pend[s + LOOK] = gather(peer_v, s + LOOK, base + s + LOOK)
                    t, bf = pend.pop(s)
                    if s == 0:
                        S.op("dve", lambda e, t=t, s=s, rows=rows: e.tensor_scalar(
                            out=yacc[0:rows, :], in0=t[0:rows, :], scalar1=coef[0:rows, s:s + 1], scalar2=None, op0=ALU.mult),
                            reads=[bf, b_coef], writes=[b_yacc])
                    else:
                        S.op("dve", lambda e, t=t, s=s, rows=rows: e.scalar_tensor_tensor(
                            out=yacc[0:rows, :], in0=t[0:rows, :], scalar=coef[0:rows, s:s + 1], in1=yacc[0:rows, :],
                            op0=ALU.mult, op1=ALU.add), reads=[bf, b_coef, b_yacc], writes=[b_yacc])
                S.op("dve", lambda e, rows=rows, tt=tt: e.tensor_tensor(out=X2[0:rows, tt, :], in0=X2[0:rows, tt, :],
                                                                      in1=yacc[0:rows, :], op=ALU.add),
                     reads=[b_yacc, bX2[tt]], writes=[bX2[tt]])

            if STOP < 8:
                return
            S.barrier()
            for r in (r_gffn_b, r_cand, r_h2t, r_yacc, ug[0][2], ug[1][2], ug[2][2]):
                AR.release(r)
            x3T, b_x3T, r_x3T = sb("x3T", [128, 16, TOK + NS], BF16)
            xnb = [sb("xnb%d" % i, [128, D], BF16) for i in range(2)]
            pT_, b_pT, _ = sb("pT", [128, 2, TOK + NS], BF16)
            pst, b_pst, _ = sb("pst", [128, 256], F32)
            psb, b_psb, _ = sb("psb", [128, 256], BF16)
            wpl, b_wpl, _ = sb("wpl", [128, 2, D], BF16)
            S.dma("pool", lambda e: e.dma_start(out=wpl[:], in_=w_ple.rearrange("(k p) c -> p k c", p=128)), writes=[b_wpl])
            for tt in range(9):
                rows = rows_of[tt]
                xn, bxn, _ = xnb[tt % 2]
                S.op("act", lambda e, xn=xn, tt=tt, rows=rows: e.copy(out=xn[0:rows, :], in_=X2[0:rows, tt, :]),
                     reads=[bX2[tt]], writes=[bxn])
                transpose_to(x3T, b_x3T, tt * 128, xn, bxn, rows, None)
                src = ploc[tt * 128:(tt + 1) * 128, :] if tt < 8 else psm
                S.dma("sp", lambda e, src=src, rows=rows: e.dma_start(out=pst[0:rows, 0:256], in_=src), writes=[b_pst])
                psbv = psb
                S.op("act", lambda e, rows=rows, psbv=psbv: e.copy(out=psbv[0:rows, 0:256], in_=pst[0:rows, 0:256]),
                     reads=[b_pst], writes=[b_psb])
                pt2, pbuf2 = bank("C")
                ptb = pt2[:].bitcast(BF16)
                for j in range(2):
                    S.op("pe", lambda e, j=j, ptb=ptb, rows=rows, psbv=psbv: e.transpose(
                        out=ptb[:, j * 128:j * 128 + rows], in_=psbv[0:rows, j * 128:(j + 1) * 128], identity=identB[0:rows, 0:rows]),
                        reads=[b_psb, b_identB], writes=[pbuf2], signal=(j == 1))
                S.op("dve", lambda e, ptb=ptb, tt=tt, rows=rows: e.tensor_copy(
                    out=pT_[:, :, tt * 128:tt * 128 + rows], in_=ptb[:, 0:256].rearrange("p (j t) -> p j t", j=2)[:, :, 0:rows]),
                    reads=[pbuf2], writes=[b_pT])
            sig, b_sig, _ = sb("sig", [128, 512], F32)
            for cb in range(4):
                wg, bwg = w_next()
                for tt in range(9):
                    rows = rows_of[tt]
                    pg, pbg = bank("A")
                    mm_group(pg[0:rows, :], pbg, [(x3T[:, kc, tt * 128:tt * 128 + rows], wg[:, kc, :]) for kc in range(16)],
                             [b_x3T, bwg])
                    pe_, pbe = bank("A")
                    mm_group(pe_[0:rows, :], pbe, [(pT_[:, kc, tt * 128:tt * 128 + rows], wpl[:, kc, cb * 512:(cb + 1) * 512])
                                                   for kc in range(2)], [b_pT, b_wpl])
                    S.op("act", lambda e, pg=pg, rows=rows: e.activation(out=sig[0:rows, 0:512], in_=pg[0:rows, :], func=AF.Sigmoid),
                         reads=[pbg], writes=[b_sig])
                    S.op("dve", lambda e, pe_=pe_, rows=rows: e.tensor_tensor(out=sig[0:rows, 0:512], in0=sig[0:rows, 0:512],
                                                                             in1=pe_[0:rows, :], op=ALU.mult),
                         reads=[pbe, b_sig], writes=[b_sig])
                    S.op("dve", lambda e, tt=tt, cb=cb, rows=rows: e.tensor_tensor(
                        out=X2[0:rows, tt, cb * 512:(cb + 1) * 512], in0=X2[0:rows, tt, cb * 512:(cb + 1) * 512],
                        in1=sig[0:rows, 0:512], op=ALU.add), reads=[b_sig, bX2[tt]], writes=[bX2[tt]])

            if STOP < 9:
                return
            S.barrier()
            AR.release(r_x3T)
            gfin_b, b_gfin_b, _ = sb("gfin_b", [128, D], F32)
            S.dma("sp", lambda e: e.dma_start(out=gfin_b[:], in_=gvec[2, :].partition_broadcast(128)), writes=[b_gfin_b])
            SQ["t"], SQ["b"], SQ["r"] = sb("sqjunk", [128, D], BF16)
            xst = [sb("yo%d" % i, [128, D], F32) for i in range(2)]
            for tt in range(9):
                rows = rows_of[tt]
                rs = rms_stats(X2[0:rows, tt, :], rows, bX2[tt], tt % 2)
                yo, b_yo, _ = xst[tt % 2]
                S.op("dve", lambda e, yo=yo, rs=rs, tt=tt, rows=rows: e.scalar_tensor_tensor(
                    out=yo[0:rows, :], in0=X2[0:rows, tt, :], scalar=rs, in1=gfin_b[0:rows, :], op0=ALU.mult, op1=ALU.mult),
                    reads=[bX2[tt], b_stat, b_gfin_b], writes=[b_yo])
                dst = y[tt * 128:(tt + 1) * 128, :] if tt < 8 else ys
                S.dma("sp", lambda e, yo=yo, dst=dst, rows=rows: e.dma_start(out=dst, in_=yo[0:rows, :]), reads=[b_yo], dbuf=outb)

        phases()
        S.barrier()
        build_nc.sbuf_base = (nc.sbuf_base, nc.sbuf_top)
        S.wait_all("sp", [outb])
        S._wait("sp", (outb.sem, outb.cnt, "d_outb"))
        build_nc.stats = dict(ninstr=dict(S.ninstr), nsem=5 + len(S.dbufs))
    return nc


def _rope_rows(pos):
    half = 16
    inv = (np.float32(500000.0) ** (-(np.arange(half, dtype=np.float32)) / np.float32(half))).astype(np.float32)
    ang = pos.astype(np.float32)[:, None] * inv[None, :]
    c, s = np.cos(ang).astype(np.float32), np.sin(ang).astype(np.float32)
    return np.concatenate([c, c, -s, s], axis=1).astype(np.float32)


def _consts(half):
    pos = np.maximum(np.arange(2 * TOK) - TOK + TOK * half, 0)
    tab = _rope_rows(pos)
    rope = np.zeros((128, 56, 64), np.float32)
    p = np.arange(128)
    for g, d in enumerate(DIL):
        tpr = (2 * TOK // d) // 128
        for n in range(16):
            r, i0 = n // tpr, (n % tpr) * 128
            rope[:, g * 16 + n, :] = tab[(i0 + p) * d + r]
    for j in range(8):
        r = 2 * j + (p >= 64)
        i = 64 + (p % 64)
        rope[:, 48 + j, :] = tab[i * 16 + r]
    ropes = np.repeat(_rope_rows(np.array([2048])), NS, axis=0)
    cb = NEG if half == 0 else 0.0
    pp, ff = np.meshgrid(np.arange(128), np.arange(128), indexing="ij")
    m = np.zeros((128, 5, 128), np.float32)
    m[:, 0, :] = np.where(ff <= pp, 0.0, NEG)
    m[:, 1, :] = np.where(pp <= ff, 0.0, NEG)
    m[:, 2, :] = m[:, 0, :] + cb
    m[:, 3, :] = cb
    m[:, 4, :] = np.where(pp < 64, cb, np.where(pp - 64 <= ff, 0.0, NEG))
    return rope, ropes, m


def _in_maps(x_prompt, x_sample, cache_kv_w128, cache_kv_w512, cache_kv_w2048, p_prompt, p_sample, g_mix, w_in,
             sgu_ln_g, sgu_ln_b, w_s, b_s, w_a_out, w_b_out, w_o, g_ffn, peer_w_q, peer_sub_k1, peer_sub_k2,
             peer_u, peer_v, w_ple, w_ple_gate, g_final, cores=None):
    f = lambda a: np.ascontiguousarray(np.asarray(a, dtype=np.float32))
    shared = {
        "w_in": f(w_in[0]), "w_a_out": f(w_a_out[0]), "w_b_out": f(w_b_out[0]), "w_o": f(w_o[0]), "w_q": f(peer_w_q[0]),
        "w_pg": f(w_ple_gate[0]), "w_ple": f(w_ple[0]), "peer_u": f(peer_u[0]), "peer_v": f(peer_v[0]),
        "w_s": f(w_s[0]), "b_s": f(b_s[0]), "subk": f(np.stack([peer_sub_k1[0], peer_sub_k2[0]])),
        "gvec": f(np.stack([g_mix[0], g_ffn[0], g_final])), "lnv": f(np.stack([sgu_ln_g[0], sgu_ln_b[0]])),
    }
    maps = []
    for c in (range(NCORES) if cores is None else cores):
        b, half = c // 2, c % 2
        own = x_prompt[b, half * TOK:(half + 1) * TOK]
        ctx = x_prompt[b, 0:TOK]
        sl = slice(c * NS, (c + 1) * NS)
        caches = np.stack([
            np.asarray(cache_kv_w128[0, sl]).reshape(NS, 128, 1024),
            np.asarray(cache_kv_w512[0, sl, 0::4]).reshape(NS, 128, 1024),
            np.asarray(cache_kv_w2048[0, sl, 0::16]).reshape(NS, 128, 1024)])
        rope, ropes, m = _consts(half)
        d = dict(shared)
        d.update({"xloc": f(np.concatenate([ctx, own], axis=0)), "xs": f(x_sample[sl, 0]),
                  "ploc": f(p_prompt[0, b, half * TOK:(half + 1) * TOK]), "psm": f(p_sample[0, sl, 0]),
                  "cache": f(caches), "rope": rope, "ropes": ropes, "masks": m})
        maps.append(d)
    return maps


def _assemble(res):
    y = np.zeros((4, 2 * TOK, D), np.float32)
    ysm = np.zeros((128, 1, D), np.float32)
    k0 = np.zeros((1, 4, 128, 2, 4, 128), np.float32)
    k1 = np.zeros((1, 4, 512, 2, 4, 128), np.float32)
    k2 = np.zeros((1, 4, 2 * TOK, 2, 4, 128), np.float32)
    ks = [np.zeros((1, 128, 1, 2, 4, 128), np.float32) for _ in range(3)]
    sg = np.zeros((1, 128, 1, A_W), np.float32)
    for c, r in enumerate(res):
        b, half = c // 2, c % 2
        y[b, half * TOK:(half + 1) * TOK] = r["y"]
        ysm[c * NS:(c + 1) * NS, 0] = r["ys"]
        k2[0, b, half * TOK:(half + 1) * TOK] = r["kv2"].reshape(TOK, 2, 4, 128)
        if half == 1:
            k0[0, b] = r["kv0"].reshape(128, 2, 4, 128)
            k1[0, b] = r["kv1"].reshape(512, 2, 4, 128)
        for g in range(3):
            ks[g][0, c * NS:(c + 1) * NS, 0] = r["kvs"][g].reshape(NS, 2, 4, 128)
        sg[0, c * NS:(c + 1) * NS, 0] = r["sguv"]
    return (y, ysm, k0, k1, k2, ks[0], ks[1], ks[2], sg)


def kernel(**inputs):
    maps = _in_maps(**inputs)
    nc = build_nc()
    res = run_bass_kernel_spmd(nc, maps, core_ids=list(range(NCORES)))
    return _assemble(res.results)
```

```python
import contextlib
import os
import math
import numpy as np
import concourse.bass as bass
import concourse.mybir as mybir
from concourse.bass_utils import run_bass_kernel_spmd

F32 = mybir.dt.float32
BF16 = mybir.dt.bfloat16
I32 = mybir.dt.int32
U32 = mybir.dt.uint32
AF = mybir.ActivationFunctionType
ALU = mybir.AluOpType
AX = mybir.AxisListType
DTSIZE = {F32: 4, BF16: 2, I32: 4, U32: 4}

D = 2048
NCORES = 8
TOK = 1024
NS = 16
NCOL = 2 * TOK + NS
EPS = 1e-6
A_W = 1024
IN_COLS = 10752
C_UA, C_VA, C_Q, C_K, C_V, C_GA, C_GB = 0, 1024, 2048, 3584, 5120, 6656, 8704
DIL = (1, 4, 16)
SCALE = 128 ** -0.5
NEG = -30000.0
NEXP = 16384
PASSES = ((TOK, 512), (TOK + 512, 512), (2 * TOK, NS))


class Buf:
    __slots__ = ("name", "w", "r", "sem", "cnt", "excl")

    def __init__(self, name, excl=False):
        self.name = name
        self.excl = excl
        self.w = None
        self.r = {}
        self.sem = None
        self.cnt = 0


class Sched:
    def __init__(self, nc, stack):
        self.nc = nc
        self.stack = stack
        self.eng = {"pe": nc.tensor, "act": nc.scalar, "dve": nc.vector,
                    "pool": nc.gpsimd, "sp": nc.sync}
        self.sem = {k: stack.enter_context(nc.semaphore("s_" + k)) for k in self.eng}
        self.count = {k: 0 for k in self.eng}
        self.seen = {k: {} for k in self.eng}
        self.dbufs = []
        self.ninstr = {k: 0 for k in self.eng}

    def _wait(self, e, tok):
        if tok is None:
            return
        sem, val, key = tok
        if key == "pe" and e == "pe":
            return
        if self.seen[e].get(key, 0) >= val:
            return
        self.eng[e].wait_ge(sem, val)
        self.seen[e][key] = val

    def _deps(self, e, reads, writes):
        for b in reads:
            self._wait(e, b.w)
        for b in writes:
            self._wait(e, b.w)
            for t in b.r.values():
                self._wait(e, t)

    def _commit(self, tok, reads, writes):
        for b in reads:
            b.r[tok[2]] = tok
        for b in writes:
            b.w = tok
            b.r = {}

    def op(self, e, fn, reads=(), writes=(), signal=True):
        if any(b.excl for b in reads):
            writes = list(writes) + [b for b in reads if b.excl]
            reads = [b for b in reads if not b.excl]
        self._deps(e, reads, writes)
        ins = fn(self.eng[e])
        self.ninstr[e] += 1
        if signal:
            self.count[e] += 1
            ins.then_inc(self.sem[e], 1)
            tok = (self.sem[e], self.count[e], e)
        else:
            tok = (self.sem[e], self.count[e] + 1, e)
        self._commit(tok, reads, writes)
        return tok

    def dma(self, q, fn, reads=(), writes=(), dbuf=None):
        self._deps(q, reads, writes)
        if dbuf is None:
            dbuf = writes[0] if writes else reads[0]
        if dbuf.sem is None:
            dbuf.sem = self.stack.enter_context(self.nc.semaphore("d_" + dbuf.name))
            self.dbufs.append(dbuf)
        ins = fn(self.eng[q])
        dbuf.cnt += 16
        ins.then_inc(dbuf.sem, 16)
        tok = (dbuf.sem, dbuf.cnt, "d_" + dbuf.name)
        self._commit(tok, reads, writes)
        self.ninstr[q] += 1
        return tok

    def wait_all(self, e, bufs):
        for b in bufs:
            self._wait(e, b.w)
            for t in b.r.values():
                self._wait(e, t)

    def barrier(self):
        for e in self.eng:
            for x in ("pe", "act", "dve", "pool"):
                if x != e and self.count[x] > 0:
                    self._wait(e, (self.sem[x], self.count[x], x))
            for b in self.dbufs:
                if b.cnt > 0:
                    self._wait(e, (b.sem, b.cnt, "d_" + b.name))


class Arena:
    def __init__(self, nc, lo, hi):
        self.nc = nc
        self.free = [(lo, hi)]
        self.n = 0
        self.peak = 0
        self.hi = hi

    def alloc(self, name, shape, dt, top=False):
        nb = int(np.prod(shape[1:])) * DTSIZE[dt]
        nb = (nb + 63) // 64 * 64
        order = range(len(self.free) - 1, -1, -1) if top else range(len(self.free))
        for i in order:
            a, b = self.free[i]
            if b - a >= nb:
                if top:
                    off = b - nb
                    self.free[i] = (a, off)
                else:
                    off = a
                    self.free[i] = (a + nb, b)
                if self.free[i][0] == self.free[i][1]:
                    del self.free[i]
                self.n += 1
                used = self.hi - sum(y - x for x, y in self.free)
                self.peak = max(self.peak, used)
                h = self.nc.alloc_sbuf_tensor_at("%s_%d" % (name, self.n), list(shape), dt, offset=off)
                return h, (off, off + nb)
        raise RuntimeError("SBUF arena exhausted allocating %s %s (free=%s)" % (name, shape, self.free))

    def release(self, region):
        self.free.append(region)
        self.free.sort()
        merged = []
        for a, b in self.free:
            if merged and merged[-1][1] == a:
                merged[-1] = (merged[-1][0], b)
            else:
                merged.append((a, b))
        self.free = merged


def build_nc():
    nc = bass.Bass("TRN2", target_bir_lowering=False)
    SKIP = set(os.environ.get('KSKIP', '').split(','))
    NEXP_ = NEXP if int(os.environ.get('KSTOP', '99')) >= 7 else 128
    di = lambda name, shape, dt=F32: nc.dram_tensor(name, list(shape), dt, kind="ExternalInput").ap()
    do = lambda name, shape, dt=F32: nc.dram_tensor(name, list(shape), dt, kind="ExternalOutput").ap()

    xloc = di("xloc", [2 * TOK, D]); xs = di("xs", [NS, D])
    ploc = di("ploc", [TOK, 256]); psm = di("psm", [NS, 256])
    cache = di("cache", [3, NS, 128, 1024])
    w_in = di("w_in", [D, IN_COLS]); w_a_out = di("w_a_out", [A_W, D]); w_b_out = di("w_b_out", [512, D])
    w_o = di("w_o", [D, D]); w_q = di("w_q", [D, D]); w_pg = di("w_pg", [D, D]); w_ple = di("w_ple", [256, D])
    peer_u = di("peer_u", [NEXP_, D]); peer_v = di("peer_v", [NEXP_, D])
    w_s = di("w_s", [8, 128, 128]); b_s = di("b_s", [8, 128]); subk = di("subk", [2, 128, 128])
    gvec = di("gvec", [3, D]); lnv = di("lnv", [2, A_W])
    rope = di("rope", [128, 56, 64]); ropes = di("ropes", [NS, 64]); masks = di("masks", [128, 5, 128])

    y = do("y", [TOK, D]); ys = do("ys", [NS, D])
    kv0 = do("kv0", [128, 1024]); kv1 = do("kv1", [512, 1024]); kv2 = do("kv2", [TOK, 1024])
    kvs = do("kvs", [3, NS, 1024]); sguv = do("sguv", [NS, A_W])
    kvout = (kv0, kv1, kv2)
    pu16 = nc.dram_tensor("pu16", [NEXP_, D], BF16, kind="Internal").ap()
    pv16 = nc.dram_tensor("pv16", [NEXP_, D], BF16, kind="Internal").ap()

    with contextlib.ExitStack() as st:
        S = Sched(nc, st)
        AR = Arena(nc, 16512, 229344)
        outb = Buf("outb")

        def sb(name, shape, dt, top=False):
            h, reg = AR.alloc(name, shape, dt, top)
            return h, Buf(name), reg

        PB = []
        for i in range(8):
            t = nc.alloc_psum_tensor("pb%d" % i, [128, 512], F32)
            PB.append((t, Buf("pb%d" % i, excl=True)))
        rr = {"A": 0, "B": 0, "C": 0}
        pools = {"A": (0, 1, 2, 3), "B": (4, 5), "C": (6, 7)}

        def bank(pool):
            ids = pools[pool]
            i = ids[rr[pool] % len(ids)]
            rr[pool] += 1
            return PB[i]

        def mm_group(out_ap, pbuf, pairs, reads):
            n = len(pairs)
            for i, (l, r) in enumerate(pairs):
                S.op("pe", lambda e, l=l, r=r, i=i: e.matmul(out_ap, lhsT=l, rhs=r, start=(i == 0), stop=(i == n - 1)),
                     reads=reads, writes=[pbuf], signal=(i == n - 1))

        identF, b_identF, _ = sb("identF", [128, 128], F32)
        identB, b_identB, _ = sb("identB", [128, 128], BF16)
        onesB, b_onesB, _ = sb("onesB", [128, 128], BF16)
        S.op("pool", lambda e: e.memset(identF[:], 1.0), writes=[b_identF])
        S.op("pool", lambda e: e.affine_select(out=identF[:], in_=identF[:], pattern=[[-1, 128]],
                                               compare_op=ALU.is_equal, fill=0.0, base=0, channel_multiplier=1),
             reads=[b_identF], writes=[b_identF])
        S.op("pool", lambda e: e.tensor_copy(out=identB[:], in_=identF[:]), reads=[b_identF], writes=[b_identB])
        S.op("pool", lambda e: e.memset(onesB[:], 1.0), writes=[b_onesB])

        gcol, b_gcol, _ = sb("gcol", [128, 2, 16], F32)
        grow, b_grow, r_grow = sb("grow", [16, 2, 128], F32)
        S.dma("sp", lambda e: e.dma_start(out=grow[:], in_=gvec[0:2, :].rearrange("g (k p) -> k g p", p=128)),
              writes=[b_grow])
        for gi in range(2):
            pt, pbuf = bank("C")
            S.op("pe", lambda e, gi=gi, pt=pt: e.transpose(out=pt[:, 0:16], in_=grow[:, gi, :], identity=identF[0:16, 0:16]),
                 reads=[b_grow, b_identF], writes=[pbuf])
            S.op("act", lambda e, gi=gi, pt=pt: e.copy(out=gcol[:, gi, :], in_=pt[:, 0:16]), reads=[pbuf], writes=[b_gcol])
        maskB, b_maskB, _ = sb("maskB", [128, 5, 128], BF16)
        S.dma("pool", lambda e: e.dma_start(out=maskB[:], in_=masks), writes=[b_maskB])
        c4, b_c4, _ = sb("c4", [128, 2], U32)
        S.op("pool", lambda e: e.memset(c4[:, 0:1], 4), writes=[b_c4])
        S.op("pool", lambda e: e.memset(c4[:, 1:2], 15), writes=[b_c4])
        ropeS, b_ropeS, _ = sb("ropeS", [NS, 64], F32)
        S.dma("sp", lambda e: e.dma_start(out=ropeS[:], in_=ropes), writes=[b_ropeS])

        NW = 2
        wslot = [sb("wslot%d" % i, [128, 16, 512], BF16) for i in range(NW)]
        wq = []
        wstate = {"issued": 0, "used": 0}

        def w_plan(blocks):
            wq.extend(blocks)

        b_tab = Buf("ptab")
        TCH = 1024 if NEXP_ >= 1024 else NEXP_
        tab_jobs = [(src, dst, r0) for r0 in range(0, NEXP_, TCH) for (src, dst) in ((peer_u, pu16), (peer_v, pv16))]

        def tab_issue(k=1):
            for _ in range(k):
                if not tab_jobs:
                    return
                src, dst, r0 = tab_jobs.pop(0)
                tok = S.dma("pool", lambda e: e.dma_start(out=dst[r0:r0 + TCH, :], in_=src[r0:r0 + TCH, :]), dbuf=b_tab)
                b_tab.w = tok

        def w_issue_upto(n):
            while wstate["issued"] < min(n, len(wq)):
                i = wstate["issued"]
                ap = wq[i]
                K, C = ap.shape
                kc = K // 128
                t, bf, _ = wslot[i % NW]
                S.dma("pool", lambda e, t=t, ap=ap, kc=kc, C=C: e.dma_start(
                    out=t[:, 0:kc, 0:C], in_=ap.rearrange("(k p) c -> p k c", p=128)), writes=[bf])
                wstate["issued"] += 1
                tab_issue(1)

        def w_next(issue=True):
            i = wstate["used"]
            if issue:
                w_issue_upto(i + NW)
            wstate["used"] += 1
            t, bf, _ = wslot[i % NW]
            return t, bf

        xst = [sb("xst%d" % i, [128, D], F32) for i in range(2)]
        xnb = [sb("xnb%d" % i, [128, D], BF16) for i in range(2)]
        SQ = {}
        SQ["t"], SQ["b"], SQ["r"] = sb("sqjunk", [128, D], BF16)
        stat, b_stat, _ = sb("stat", [128, 8], F32)

        def rms_stats(src_ap, rows, b_src, slot):
            ss = stat[0:rows, slot * 2:slot * 2 + 1]
            rs = stat[0:rows, slot * 2 + 1:slot * 2 + 2]
            sq_junk, b_sq_junk = SQ["t"], SQ["b"]
            S.op("act", lambda e: e.activation(out=sq_junk[0:rows, :], in_=src_ap, func=AF.Square, accum_out=ss),
                 reads=[b_src], writes=[b_sq_junk, b_stat])
            S.op("dve", lambda e: e.tensor_scalar(out=ss, in0=ss, scalar1=1.0 / D, scalar2=EPS, op0=ALU.mult, op1=ALU.add),
                 reads=[b_stat], writes=[b_stat])
            S.op("act", lambda e: e.sqrt(out=ss, in_=ss), reads=[b_stat], writes=[b_stat])
            S.op("dve", lambda e: e.reciprocal(out=rs, in_=ss), reads=[b_stat], writes=[b_stat])
            return rs

        def transpose_to(dstT, b_dstT, col0, src_bf, b_src, rows, gsel):
            for half in range(2):
                pt, pbuf = bank("B")
                ptb = pt[:].bitcast(BF16)
                for j in range(8):
                    kc = half * 8 + j
                    S.op("pe", lambda e, kc=kc, j=j, ptb=ptb: e.transpose(
                        out=ptb[:, j * 128:j * 128 + rows], in_=src_bf[0:rows, kc * 128:(kc + 1) * 128],
                        identity=identB[0:rows, 0:rows]),
                        reads=[b_src, b_identB], writes=[pbuf], signal=(j == 7))
                for j in range(8):
                    kc = half * 8 + j
                    eng = "dve" if half == 0 else "act"
                    if gsel is None:
                        if eng == "dve":
                            S.op("dve", lambda e, kc=kc, j=j, ptb=ptb: e.tensor_copy(
                                out=dstT[:, kc, col0:col0 + rows], in_=ptb[:, j * 128:j * 128 + rows]),
                                reads=[pbuf], writes=[b_dstT])
                        else:
                            S.op("act", lambda e, kc=kc, j=j, ptb=ptb: e.copy(
                                out=dstT[:, kc, col0:col0 + rows], in_=ptb[:, j * 128:j * 128 + rows]),
                                reads=[pbuf], writes=[b_dstT])
                    elif eng == "dve":
                        S.op("dve", lambda e, kc=kc, j=j, ptb=ptb: e.tensor_scalar(
                            out=dstT[:, kc, col0:col0 + rows], in0=ptb[:, j * 128:j * 128 + rows],
                            scalar1=gcol[:, gsel, kc:kc + 1], scalar2=None, op0=ALU.mult),
                            reads=[pbuf, b_gcol], writes=[b_dstT])
                    else:
                        S.op("act", lambda e, kc=kc, j=j, ptb=ptb: e.activation(
                            out=dstT[:, kc, col0:col0 + rows], in_=ptb[:, j * 128:j * 128 + rows],
                            func=AF.Copy, scale=gcol[:, gsel, kc:kc + 1]),
                            reads=[pbuf, b_gcol], writes=[b_dstT])

        STOP = int(os.environ.get('KSTOP', '99'))
        SUB = int(os.environ.get('KSUB', '99'))

        def phases():
            nonlocal xst, xnb
            if STOP < 0:
                return
            hT, b_hT, r_hT = sb("hT", [128, 16, NCOL], BF16)
            plan = []
            for g in range(3):
                plan += [w_in[:, C_K + g * 512:C_K + (g + 1) * 512], w_in[:, C_V + g * 512:C_V + (g + 1) * 512],
                         w_in[:, C_Q + g * 512:C_Q + (g + 1) * 512]]
            plan += [w_in[:, C_VA:C_VA + 512], w_in[:, C_VA + 512:C_VA + 1024]]
            plan += [w_in[:, C_UA:C_UA + 512], w_in[:, C_UA + 512:C_UA + 1024]]
            for cb in range(4):
                plan += [w_in[:, C_GA + cb * 512:C_GA + (cb + 1) * 512], w_a_out[:, cb * 512:(cb + 1) * 512],
                         w_in[:, C_GB + cb * 512:C_GB + (cb + 1) * 512], w_b_out[:, cb * 512:(cb + 1) * 512]]
            for cb in range(4):
                plan += [w_o[:, cb * 512:(cb + 1) * 512]]
            for cb in range(4):
                plan += [w_q[:, cb * 512:(cb + 1) * 512]]
            for cb in range(4):
                plan += [w_pg[:, cb * 512:(cb + 1) * 512]]
            w_plan(plan)
            w_issue_upto(NW)

            tiles = [(xloc[n * 128:(n + 1) * 128, :], 128, n * 128) for n in range(16)] + [(xs, NS, 2 * TOK)]

            def load_x(i):
                src, rows, _ = tiles[i]
                t, bf, _ = xst[i % 2]
                S.dma("sp", lambda e: e.dma_start(out=t[0:rows, :], in_=src), writes=[bf])

            load_x(0)
            for i, (src, rows, col0) in enumerate(tiles):
                if i + 1 < len(tiles):
                    load_x(i + 1)
                t, bf, _ = xst[i % 2]
                xn, bxn, _ = xnb[i % 2]
                rs = rms_stats(t[0:rows, :], rows, bf, i % 2)
                S.op("act", lambda e, t=t, xn=xn, rs=rs, rows=rows: e.activation(
                    out=xn[0:rows, :], in_=t[0:rows, :], func=AF.Copy, scale=rs), reads=[bf, b_stat], writes=[bxn])
                transpose_to(hT, b_hT, col0, xn, bxn, rows, 0)
            S.barrier()
            for r in (xst[0][2], xst[1][2], xnb[0][2], xnb[1][2], SQ["r"], r_grow):
                AR.release(r)

            if STOP < 1:
                return
            ACC, b_ACC, r_ACC = sb("ACC", [128, 2, 4, TOK], F32)
            KT, b_KT, r_KT = sb("KT", [128, 4, 2 * TOK], BF16)
            QT, b_QT, r_QT = sb("QT", [128, 4, TOK], BF16)
            VG, b_VG, r_VG = sb("VG", [128, 16, 512], BF16)
            kf = [sb("kf%d" % i, [128, 512], F32) for i in range(2)]
            kb = [sb("kb%d" % i, [128, 512], BF16) for i in range(2)]
            rtmp, b_rtmp, r_rtmp = sb("rtmp", [128, 2, 4, 32], F32)
            PT = [sb("PT%d" % i, [128, 2, 128], BF16) for i in range(2)]
            qkv_c, b_qkv_c, r_qkv_c = sb("qkv_c", [128, 2, 512], F32)
            stg = [sb("stg%d" % i, [NS, 512], F32) for i in range(2)]
            ropeG, b_ropeG, r_ropeG = sb("ropeG", [128, 16, 64], F32)
            ropeQ2, b_ropeQ2, r_ropeQ2 = sb("ropeQ2", [128, 8, 64], F32)
            S.dma("sp", lambda e: e.dma_start(out=ropeQ2[:], in_=rope[:, 48:56, :]), writes=[b_ropeQ2])
            cnt = {"kf": 0, "kb": 0, "pt": 0, "stg": 0}

            def stash_sample(pt, pbuf, which, g):
                blk = which * 3 + g
                if "stash" in SKIP:
                    return
                t, bt, _ = stg[cnt["stg"] % 2]; cnt["stg"] += 1
                S.op("act", lambda e: e.copy(out=t[:], in_=pt[0:NS, :]), reads=[pbuf], writes=[bt])
                j, slot = blk % 8, blk // 8
                S.dma("sp", lambda e: e.dma_start(out=qkv_c[16 * j:16 * j + 16, slot, :], in_=t[:]), reads=[bt], writes=[b_qkv_c])

            def rope_apply(t, bt, rows, tab_ap, b_tab):
                x4 = t[0:rows, :].rearrange("p (h d) -> p h d", h=4)
                cc = tab_ap[:, 0:32].unsqueeze(1).broadcast_to([rows, 4, 32])
                s1 = tab_ap[:, 32:48].unsqueeze(1).broadcast_to([rows, 4, 16])
                s2 = tab_ap[:, 48:64].unsqueeze(1).broadcast_to([rows, 4, 16])
                A = rtmp[0:rows, 0]
                B = rtmp[0:rows, 1]
                if "rope" in SKIP:
                    return
                S.op("dve", lambda e: e.tensor_tensor(out=A, in0=x4[:, :, 0:32], in1=cc, op=ALU.mult),
                     reads=[bt, b_tab], writes=[b_rtmp])
                S.op("dve", lambda e: e.tensor_tensor(out=B[:, :, 0:16], in0=x4[:, :, 16:32], in1=s1, op=ALU.mult),
                     reads=[bt, b_tab], writes=[b_rtmp])
                S.op("dve", lambda e: e.tensor_tensor(out=B[:, :, 16:32], in0=x4[:, :, 0:16], in1=s2, op=ALU.mult),
                     reads=[bt, b_tab], writes=[b_rtmp])
                S.op("dve", lambda e: e.tensor_tensor(out=x4[:, :, 0:32], in0=A, in1=B, op=ALU.add),
                     reads=[b_rtmp], writes=[bt])

            def gtile_cols(g, n):
                d = DIL[g]
                tpr = (2 * TOK // d) // 128
                r, i0 = n // tpr, (n % tpr) * 128
                start = i0 * d + r
                return slice(start, start + 127 * d + 1, d), r, i0

            first_group = True
            for g in range(3):
                d = DIL[g]
                L = 2 * TOK // d
                tpr = L // 128
                Lq = TOK // d
                if g == 0:
                    ktiles = list(range(7, 16))
                elif g == 1:
                    ktiles = [n for n in range(16) if n % 4 >= 1]
                else:
                    ktiles = list(range(16))
                S.dma("sp", lambda e, g=g: e.dma_start(out=ropeG[:], in_=rope[:, g * 16:(g + 1) * 16, :]), writes=[b_ropeG])
                wt, bw = w_next()
                for n in ktiles + ["s"]:
                    pt, pbuf = bank("A")
                    if n == "s":
                        rows = NS
                        lhs = lambda kc: hT[:, kc, 2 * TOK:2 * TOK + NS]
                    else:
                        rows = 128
                        sl, r, i0 = gtile_cols(g, n)
                        lhs = lambda kc, sl=sl: hT[:, kc, sl]
                    mm_group(pt[0:rows, :], pbuf, [(lhs(kc), wt[:, kc, :]) for kc in range(16)], [b_hT, bw])
                    if n == "s":
                        stash_sample(pt, pbuf, 1, g)
                        continue
                    t, bt, _ = kf[cnt["kf"] % 2]; cnt["kf"] += 1
                    S.op("act", lambda e, pt=pt, t=t: e.copy(out=t[:], in_=pt[:]), reads=[pbuf], writes=[bt])
                    rope_apply(t, bt, 128, ropeG[:, n, :], b_ropeG)
                    if "kvout" in SKIP:
                        pass
                    elif g == 0 and n == 15:
                        S.dma("sp", lambda e, t=t: e.dma_start(out=kv0[:, 0:512], in_=t[:]), reads=[bt], dbuf=outb)
                    elif g == 1 and n % 4 == 3:
                        S.dma("sp", lambda e, t=t, r=r: e.dma_start(out=kv1[r:512:4, 0:512], in_=t[:]), reads=[bt], dbuf=outb)
                    elif g == 2:
                        S.dma("sp", lambda e, t=t, r=r: e.dma_start(out=kv2[r:TOK:16, 0:512], in_=t[64:128, :]), reads=[bt], dbuf=outb)
                    tb, btb, _ = kb[cnt["kb"] % 2]; cnt["kb"] += 1
                    S.op("act", lambda e, t=t, tb=tb: e.copy(out=tb[:], in_=t[:]), reads=[bt], writes=[btb])
                    if "ktr" in SKIP:
                        continue
                    pt2, pbuf2 = bank("B")
                    ptb = pt2[:].bitcast(BF16)
                    for h in range(4):
                        S.op("pe", lambda e, h=h, ptb=ptb, tb=tb: e.transpose(out=ptb[:, h * 128:(h + 1) * 128],
                                                                        in_=tb[:, h * 128:(h + 1) * 128], identity=identB[:]),
                             reads=[btb, b_identB], writes=[pbuf2], signal=(h == 3))
                    S.op("dve", lambda e, ptb=ptb, n=n: e.tensor_copy(out=KT[:, :, n * 128:(n + 1) * 128],
                                                                 in_=ptb[:, 0:512].rearrange("p (h t) -> p h t", h=4)),
                         reads=[pbuf2], writes=[b_KT])
                if SUB < 0:
                    return
                wt, bw = w_next()
                for n in ktiles + ["s"]:
                    pt, pbuf = bank("A")
                    if n == "s":
                        mm_group(pt[0:NS, :], pbuf, [(hT[:, kc, 2 * TOK:2 * TOK + NS], wt[:, kc, :]) for kc in range(16)], [b_hT, bw])
                        stash_sample(pt, pbuf, 2, g)
                        continue
                    sl, r, i0 = gtile_cols(g, n)
                    mm_group(pt[:, :], pbuf, [(hT[:, kc, sl], wt[:, kc, :]) for kc in range(16)], [b_hT, bw])
                    own_out = ((g == 0 and n == 15) or (g == 1 and n % 4 == 3) or (g == 2)) and "kvout" not in SKIP
                    if own_out:
                        t, bt, _ = kf[cnt["kf"] % 2]; cnt["kf"] += 1
                        S.op("act", lambda e, pt=pt, t=t: e.copy(out=t[:], in_=pt[:]), reads=[pbuf], writes=[bt])
                        if g == 0:
                            S.dma("sp", lambda e, t=t: e.dma_start(out=kv0[:, 512:1024], in_=t[:]), reads=[bt], dbuf=outb)
                        elif g == 1:
                            S.dma("sp", lambda e, t=t, r=r: e.dma_start(out=kv1[r:512:4, 512:1024], in_=t[:]), reads=[bt], dbuf=outb)
                        else:
                            S.dma("sp", lambda e, t=t, r=r: e.dma_start(out=kv2[r:TOK:16, 512:1024], in_=t[64:128, :]),
                                  reads=[bt], dbuf=outb)
                    if own_out:
                        S.op("dve", lambda e, t=t, n=n: e.tensor_copy(out=VG[:, n, :], in_=t[:]), reads=[bt], writes=[b_VG])
                    else:
                        S.op("dve", lambda e, pt=pt, n=n: e.tensor_copy(out=VG[:, n, :], in_=pt[:]), reads=[pbuf], writes=[b_VG])
                wt, bw = w_next()
                if g == 0:
                    qtiles = [(n, gtile_cols(0, n)[0], ropeG[:, n, :], (n - 8) * 128) for n in range(8, 16)]
                elif g == 1:
                    qtiles = []
                    for n in range(16):
                        if n % 4 >= 2:
                            sl, r, i0 = gtile_cols(1, n)
                            qtiles.append((n, sl, ropeG[:, n, :], r * Lq + (i0 - Lq)))
                else:
                    qtiles = []
                    for r0 in range(0, 16, 2):
                        qtiles.append((r0, None, ropeQ2[:, r0 // 2, :], r0 * 64))
                for (n, sl, tab, qc0) in qtiles + [("s", None, None, None)]:
                    pt, pbuf = bank("A")
                    if n == "s":
                        mm_group(pt[0:NS, :], pbuf, [(hT[:, kc, 2 * TOK:2 * TOK + NS], wt[:, kc, :]) for kc in range(16)], [b_hT, bw])
                        stash_sample(pt, pbuf, 0, g)
                        continue
                    if g == 2:
                        for hf in range(2):
                            c0 = TOK + n + hf
                            mm_group(pt[hf * 64:(hf + 1) * 64, :], pbuf,
                                     [(hT[:, kc, c0:c0 + 63 * 16 + 1:16], wt[:, kc, :]) for kc in range(16)], [b_hT, bw])
                    else:
                        mm_group(pt[:, :], pbuf, [(hT[:, kc, sl], wt[:, kc, :]) for kc in range(16)], [b_hT, bw])
                    t, bt, _ = kf[cnt["kf"] % 2]; cnt["kf"] += 1
                    S.op("act", lambda e, pt=pt, t=t: e.copy(out=t[:], in_=pt[:]), reads=[pbuf], writes=[bt])
                    rope_apply(t, bt, 128, tab, b_ropeQ2 if g == 2 else b_ropeG)
                    tb, btb, _ = kb[cnt["kb"] % 2]; cnt["kb"] += 1
                    S.op("act", lambda e, t=t, tb=tb: e.copy(out=tb[:], in_=t[:]), reads=[bt], writes=[btb])
                    pt2, pbuf2 = bank("B")
                    ptb = pt2[:].bitcast(BF16)
                    for h in range(4):
                        S.op("pe", lambda e, h=h, ptb=ptb, tb=tb: e.transpose(out=ptb[:, h * 128:(h + 1) * 128],
                                                                        in_=tb[:, h * 128:(h + 1) * 128], identity=identB[:]),
                             reads=[btb, b_identB], writes=[pbuf2], signal=(h == 3))
                    S.op("dve", lambda e, ptb=ptb, qc0=qc0: e.tensor_copy(out=QT[:, :, qc0:qc0 + 128],
                                                                     in_=ptb[:, 0:512].rearrange("p (h t) -> p h t", h=4)),
                         reads=[pbuf2], writes=[b_QT])
                if SUB < 1 + 2 * g:
                    return
                TQ = 128 if g < 2 else 64
                for h in range(4):
                    for r in range(d):
                        for qt in range(Lq // TQ):
                            qc0 = r * Lq + qt * TQ
                            i0q = Lq + qt * TQ
                            if g < 2:
                                kprev = r * L + i0q - 128
                                blocks = [(kprev, 128, (kprev // 128), maskB[:, 2 if qt == 0 else 0, :]),
                                          (r * L + i0q, 128, (r * L + i0q) // 128, maskB[:, 1, :])]
                            else:
                                blocks = [(r * L, 128, r, maskB[:, 4, 0:64])]
                            nb = len(blocks)
                            ps_s, pb_s = bank("A")
                            for bi, (kc0, nk, vt, mk) in enumerate(blocks):
                                o = ps_s[0:nk, bi * 128:bi * 128 + TQ]
                                S.op("pe", lambda e, o=o, kc0=kc0, nk=nk, h=h, qc0=qc0: e.matmul(
                                    o, lhsT=KT[:, h, kc0:kc0 + nk], rhs=QT[:, h, qc0:qc0 + TQ], start=True, stop=False),
                                    reads=[b_KT, b_QT], writes=[pb_s], signal=False)
                                S.op("pe", lambda e, o=o, nk=nk, mk=mk: e.matmul(
                                    o, lhsT=identB[0:nk, 0:nk], rhs=mk, start=False, stop=True),
                                    reads=[b_identB, b_maskB], writes=[pb_s], signal=(bi == nb - 1))
                            p_t, b_p, _ = PT[cnt["pt"] % 2]; cnt["pt"] += 1
                            S.op("act", lambda e, ps_s=ps_s, p_t=p_t, nb=nb: e.activation(
                                out=p_t[:, 0:nb, 0:TQ], in_=ps_s[:].rearrange("p (b t) -> p b t", b=4)[:, 0:nb, 0:TQ],
                                func=AF.Exp, scale=SCALE), reads=[pb_s], writes=[b_p])
                            ps_o, pb_o = bank("B") if (cnt["pt"] % 2) else bank("C")
                            mm_group(ps_o[:, 0:TQ], pb_o, [(VG[:, vt, h * 128:(h + 1) * 128], p_t[:, bi, 0:TQ])
                                                          for bi, (kc0, nk, vt, mk) in enumerate(blocks)], [b_VG, b_p])
                            mm_group(ps_o[:, 128:128 + TQ], pb_o, [(onesB[:, :], p_t[:, bi, 0:TQ]) for bi in range(nb)],
                                     [b_onesB, b_p])
                            nat = slice(qt * TQ * d + r, qt * TQ * d + r + (TQ - 1) * d + 1, d)
                            src = ps_o[:].rearrange("p (b t) -> p b t", b=4)[:, 0:2, 0:TQ]
                            if first_group:
                                S.op("dve", lambda e, src=src, h=h, nat=nat: e.tensor_copy(out=ACC[:, :, h, nat], in_=src),
                                     reads=[pb_o], writes=[b_ACC])
                            else:
                                S.op("dve", lambda e, src=src, h=h, nat=nat: e.tensor_tensor(
                                    out=ACC[:, :, h, nat], in0=ACC[:, :, h, nat], in1=src, op=ALU.add),
                                    reads=[pb_o, b_ACC], writes=[b_ACC])
                first_group = False
                if SUB < 2 + 2 * g:
                    return
            S.barrier()
            for r in (r_KT, r_QT, r_VG, r_ropeG, r_ropeQ2, kf[0][2], kf[1][2], kb[0][2], kb[1][2], PT[0][2], PT[1][2]):
                AR.release(r)
            bmixT, b_bmixT, r_bmixT = sb("bmixT", [128, 4, TOK + NS], BF16)
            S.op("dve", lambda e: e.reciprocal(out=ACC[:, 1], in_=ACC[:, 1]), reads=[b_ACC], writes=[b_ACC])
            S.op("dve", lambda e: e.tensor_tensor(out=bmixT[:, :, 0:TOK], in0=ACC[:, 0], in1=ACC[:, 1], op=ALU.mult),
                 reads=[b_ACC], writes=[b_bmixT])
            qkv_s, b_qkv_s, r_qkv_s = sb("qkv_s", [NS, 3, 3, 512], F32)
            for which in range(3):
                for g in range(3):
                    blk = which * 3 + g
                    j, slot = blk % 8, blk // 8
                    S.dma("sp", lambda e, which=which, g=g, j=j, slot=slot: e.dma_start(
                        out=qkv_s[:, which, g, :], in_=qkv_c[16 * j:16 * j + 16, slot, :]), reads=[b_qkv_c], writes=[b_qkv_s])

            if SUB < 7:
                return
            for which in (0, 1):
                for g in range(3):
                    x4 = qkv_s[:, which, g, :].rearrange("p (h d) -> p h d", h=4)
                    cc = ropeS[:, 0:32].unsqueeze(1).broadcast_to([NS, 4, 32])
                    s1 = ropeS[:, 32:48].unsqueeze(1).broadcast_to([NS, 4, 16])
                    s2 = ropeS[:, 48:64].unsqueeze(1).broadcast_to([NS, 4, 16])
                    A = rtmp[0:NS, 0]
                    B = rtmp[0:NS, 1]
                    S.op("dve", lambda e, x4=x4, A=A, cc=cc: e.tensor_tensor(out=A, in0=x4[:, :, 0:32], in1=cc, op=ALU.mult),
                         reads=[b_qkv_s, b_ropeS], writes=[b_rtmp])
                    S.op("dve", lambda e, x4=x4, B=B, s1=s1: e.tensor_tensor(out=B[:, :, 0:16], in0=x4[:, :, 16:32], in1=s1, op=ALU.mult),
                         reads=[b_qkv_s, b_ropeS], writes=[b_rtmp])
                    S.op("dve", lambda e, x4=x4, B=B, s2=s2: e.tensor_tensor(out=B[:, :, 16:32], in0=x4[:, :, 0:16], in1=s2, op=ALU.mult),
                         reads=[b_qkv_s, b_ropeS], writes=[b_rtmp])
                    S.op("dve", lambda e, x4=x4, A=A, B=B: e.tensor_tensor(out=x4[:, :, 0:32], in0=A, in1=B, op=ALU.add),
                         reads=[b_rtmp], writes=[b_qkv_s])
            for g in range(3):
                S.dma("sp", lambda e, g=g: e.dma_start(out=kvs[g, :, 0:512], in_=qkv_s[:, 1, g, :]), reads=[b_qkv_s], dbuf=outb)
                S.dma("sp", lambda e, g=g: e.dma_start(out=kvs[g, :, 512:1024], in_=qkv_s[:, 2, g, :]), reads=[b_qkv_s], dbuf=outb)

            CK = [sb("CK%d" % i, [128, 1024], F32) for i in range(3)]
            SEL, b_SEL, r_SEL = sb("SEL", [NS, NS, 128], F32)
            SELT, b_SELT, r_SELT = sb("SELT", [128, NS, NS], F32)
            S.op("pool", lambda e: e.memset(SEL[:], 1.0), writes=[b_SEL])
            S.op("pool", lambda e: e.affine_select(out=SEL[:], in_=SEL[:], pattern=[[1, NS], [0, 128]], compare_op=ALU.is_equal,
                                                   fill=0.0, base=0, channel_multiplier=-1), reads=[b_SEL], writes=[b_SEL])
            S.op("pool", lambda e: e.memset(SELT[:], 1.0), writes=[b_SELT])
            S.op("pool", lambda e: e.affine_select(out=SELT[:], in_=SELT[:], pattern=[[1, NS], [-1, NS]], compare_op=ALU.is_equal,
                                                   fill=0.0, base=0, channel_multiplier=0), reads=[b_SELT], writes=[b_SELT])
            sprod, b_sprod, r_sprod = sb("sprod", [128, 512], F32)
            ssc, b_ssc, r_ssc = sb("ssc", [128, 8], F32)
            snew, b_snew, r_snew = sb("snew", [NS, 3, 512], F32)
            sn_s, b_sn_s, r_sn_s = sb("sn_s", [NS, 3, 8], F32)
            so, b_so, r_so = sb("so", [NS, 516], F32)
            sob, b_sob, r_sob = sb("sob", [NS, 512], BF16)
            ps_os, pb_os = PB[6]
            ps_ds, pb_ds = PB[7]
            S.op("dve", lambda e: e.tensor_tensor(out=snew[:], in0=qkv_s[:, 0], in1=qkv_s[:, 1], op=ALU.mult),
                 reads=[b_qkv_s], writes=[b_snew])
            S.op("dve", lambda e: e.tensor_reduce(out=sn_s[:, :, 0:4], in_=snew[:].rearrange("p g (h d) -> p g h d", h=4),
                                                  axis=AX.X, op=ALU.add), reads=[b_snew], writes=[b_sn_s])
            S.op("act", lambda e: e.activation(out=sn_s[:, :, 4:8], in_=sn_s[:, :, 0:4], func=AF.Exp, scale=SCALE),
                 reads=[b_sn_s], writes=[b_sn_s])
            S.op("dve", lambda e: e.tensor_tensor(
                out=snew[:].rearrange("p g (h d) -> p g h d", h=4), in0=qkv_s[:, 2].rearrange("p g (h d) -> p g h d", h=4),
                in1=sn_s[:, :, 4:8].unsqueeze(3).broadcast_to([NS, 3, 4, 128]), op=ALU.mult),
                reads=[b_qkv_s, b_sn_s], writes=[b_snew])
            k = 0
            for n in range(NS):
                for g in range(3):
                    ck, b_ck, _ = CK[k % 3]
                    S.dma("sp", lambda e, ck=ck, g=g, n=n: e.dma_start(out=ck[:], in_=cache[g, n]), writes=[b_ck])
                    pq, pbq = bank("A")
                    S.op("pe", lambda e, pq=pq, n=n, g=g: e.matmul(pq[:, :], lhsT=SEL[:, n, :], rhs=qkv_s[:, 0, g, :],
                                                               start=True, stop=True), reads=[b_SEL, b_qkv_s], writes=[pbq])
                    S.op("dve", lambda e, ck=ck, pq=pq: e.tensor_tensor(out=sprod[:], in0=ck[:, 0:512], in1=pq[:, :], op=ALU.mult),
                         reads=[b_ck, pbq], writes=[b_sprod])
                    S.op("dve", lambda e: e.tensor_reduce(out=ssc[:, 0:4], in_=sprod[:].rearrange("p (h d) -> p h d", h=4),
                                                          axis=AX.X, op=ALU.add), reads=[b_sprod], writes=[b_ssc])
                    S.op("act", lambda e: e.activation(out=ssc[:, 4:8], in_=ssc[:, 0:4], func=AF.Exp, scale=SCALE),
                         reads=[b_ssc], writes=[b_ssc])
                    S.op("dve", lambda e, ck=ck: e.tensor_tensor(
                        out=sprod[:].rearrange("p (h d) -> p h d", h=4), in0=ck[:, 512:1024].rearrange("p (h d) -> p h d", h=4),
                        in1=ssc[:, 4:8].unsqueeze(2).broadcast_to([128, 4, 128]), op=ALU.mult),
                        reads=[b_ck, b_ssc], writes=[b_sprod])
                    first, last = (k == 0), (k == NS * 3 - 1)
                    S.op("pe", lambda e, n=n, first=first, last=last: e.matmul(ps_os[0:NS, :], lhsT=SELT[:, n, :], rhs=sprod[:],
                                                                          start=first, stop=last),
                         reads=[b_SELT, b_sprod], writes=[pb_os], signal=True)
                    S.op("pe", lambda e, n=n, first=first, last=last: e.matmul(ps_ds[0:NS, 0:4], lhsT=SELT[:, n, :], rhs=ssc[:, 4:8],
                                                                          start=first, stop=last),
                         reads=[b_SELT, b_ssc], writes=[pb_ds], signal=True)
                    k += 1
            S.op("dve", lambda e: e.tensor_tensor(out=so[:, 0:512], in0=snew[:, 0, :], in1=snew[:, 1, :], op=ALU.add),
                 reads=[b_snew], writes=[b_so])
            S.op("dve", lambda e: e.tensor_tensor(out=so[:, 0:512], in0=so[:, 0:512], in1=snew[:, 2, :], op=ALU.add),
                 reads=[b_snew, b_so], writes=[b_so])
            S.op("dve", lambda e: e.tensor_tensor(out=so[:, 0:512], in0=so[:, 0:512], in1=ps_os[0:NS, :], op=ALU.add),
                 reads=[pb_os, b_so], writes=[b_so])
            S.op("dve", lambda e: e.tensor_tensor(out=so[:, 512:516], in0=sn_s[:, 0, 4:8], in1=sn_s[:, 1, 4:8], op=ALU.add),
                 reads=[b_sn_s], writes=[b_so])
            S.op("dve", lambda e: e.tensor_tensor(out=so[:, 512:516], in0=so[:, 512:516], in1=sn_s[:, 2, 4:8], op=ALU.add),
                 reads=[b_sn_s, b_so], writes=[b_so])
            S.op("dve", lambda e: e.tensor_tensor(out=so[:, 512:516], in0=so[:, 512:516], in1=ps_ds[0:NS, 0:4], op=ALU.add),
                 reads=[pb_ds, b_so], writes=[b_so])
            S.op("dve", lambda e: e.reciprocal(out=so[:, 512:516], in_=so[:, 512:516]), reads=[b_so], writes=[b_so])
            S.op("dve", lambda e: e.tensor_tensor(out=sob[:].rearrange("p (h d) -> p h d", h=4),
                                                  in0=so[:, 0:512].rearrange("p (h d) -> p h d", h=4),
                                                  in1=so[:, 512:516].unsqueeze(2).broadcast_to([NS, 4, 128]), op=ALU.mult),
                 reads=[b_so], writes=[b_sob])
            pt2, pbuf2 = bank("B")
            ptb = pt2[:].bitcast(BF16)
            for h in range(4):
                S.op("pe", lambda e, h=h, ptb=ptb: e.transpose(out=ptb[:, h * NS:(h + 1) * NS], in_=sob[:, h * 128:(h + 1) * 128],
                                                          identity=identB[0:NS, 0:NS]),
                     reads=[b_sob, b_identB], writes=[pbuf2], signal=(h == 3))
            S.op("dve", lambda e, ptb=ptb: e.tensor_copy(out=bmixT[:, :, TOK:TOK + NS],
                                                    in_=ptb[:, 0:4 * NS].rearrange("p (h t) -> p h t", h=4)),
                 reads=[pbuf2], writes=[b_bmixT])

            S.barrier()
            for r in (r_ACC, r_rtmp, r_qkv_s, r_qkv_c, stg[0][2], stg[1][2], r_SEL, r_SELT, r_sprod, r_ssc, r_snew, r_sn_s,
                      r_so, r_sob, CK[0][2], CK[1][2], CK[2][2]):
                AR.release(r)

            if STOP < 2:
                return
            amixT, b_amixT, r_amixT = sb("amixT", [128, 8, TOK + NS], BF16)
            vn, b_vn, r_vn = sb("vn", [128, 9, A_W], BF16)
            gv = [sb("gv%d" % i, [128, A_W], F32) for i in range(2)]
            lng, b_lng, r_lng = sb("lng", [128, A_W], F32)
            lnb, b_lnb, r_lnb = sb("lnb", [128, A_W], F32)
            S.dma("sp", lambda e: e.dma_start(out=lng[:], in_=lnv[0, :].partition_broadcast(128)), writes=[b_lng])
            S.dma("sp", lambda e: e.dma_start(out=lnb[:], in_=lnv[1, :].partition_broadcast(128)), writes=[b_lnb])
            bns, b_bns, r_bns = sb("bns", [128, 2, 8], F32)
            wsT, b_wsT, r_wsT = sb("wsT", [128, 8, 128], BF16)
            wsF, b_wsF, r_wsF = sb("wsF", [128, 8, 128], F32)
            bsb, b_bsb, r_bsb = sb("bsb", [128, 8, 128], F32)
            bs0, b_bs0, r_bs0 = sb("bs0", [128, 8], F32)
            ws00, b_ws00, r_ws00 = sb("ws00", [16, 8], F32)
            dg, b_dg, r_dg = sb("dg", [16, 8, 16], BF16)
            sgt, b_sgt, r_sgt = sb("sgt", [128, 512], F32)
            S.dma("sp", lambda e: e.dma_start(out=wsF[:], in_=w_s.rearrange("g t s -> t g s")), writes=[b_wsF])
            S.dma("sp", lambda e: e.dma_start(out=bsb[:], in_=b_s.rearrange("g t -> (g t)").partition_broadcast(128)
                                              .rearrange("p (g t) -> p g t", g=8)), writes=[b_bsb])
            S.dma("sp", lambda e: e.dma_start(out=ws00[:], in_=w_s[:, 0, 0].partition_broadcast(16),
                                              allow_slow_non_contiguous=True), writes=[b_ws00])
            wsM, b_wsM, r_wsM = sb("wsM", [128, 8, 128], F32)
            for half in range(2):
                pt, pbuf = bank("C")
                for j in range(4):
                    gi = half * 4 + j
                    S.op("pe", lambda e, gi=gi, j=j, pt=pt: e.transpose(out=pt[:, j * 128:(j + 1) * 128], in_=wsF[:, gi, :],
                                                                   identity=identF[:]),
                         reads=[b_wsF, b_identF], writes=[pbuf], signal=(j == 3))
                S.op("act", lambda e, half=half, pt=pt: e.copy(out=wsM[:, half * 4:half * 4 + 4, :],
                                                           in_=pt[:].rearrange("p (g t) -> p g t", g=4)),
                     reads=[pbuf], writes=[b_wsM])
            S.op("pool", lambda e: e.affine_select(out=wsT[:], in_=wsM[:], pattern=[[0, 8], [1, 128]],
                                                   compare_op=ALU.is_ge, fill=0.0, base=0, channel_multiplier=-1),
                 reads=[b_wsM], writes=[b_wsT])
            S.op("dve", lambda e: e.tensor_tensor(out=dg[:], in0=identF[0:16, 0:16].unsqueeze(1).broadcast_to([16, 8, 16]),
                                                  in1=ws00[:].unsqueeze(2).broadcast_to([16, 8, 16]), op=ALU.mult),
                 reads=[b_identF, b_ws00], writes=[b_dg])

            wva0, bwva0 = w_next()
            wva1, bwva1 = w_next(issue=False)
            own_tiles = [(TOK + n * 128, 128) for n in range(8)] + [(2 * TOK, NS)]
            for ti, (c0, rows) in enumerate(own_tiles):
                g_t, b_g, _ = gv[ti % 2]
                for blk, (wt, bw) in enumerate(((wva0, bwva0), (wva1, bwva1))):
                    pt, pbuf = bank("A")
                    mm_group(pt[0:rows, :], pbuf, [(hT[:, kc, c0:c0 + rows], wt[:, kc, :]) for kc in range(16)], [b_hT, bw])
                    S.op("act", lambda e, pt=pt, blk=blk, g_t=g_t, rows=rows: e.activation(
                        out=g_t[0:rows, blk * 512:(blk + 1) * 512], in_=pt[0:rows, :], func=AF.Gelu_apprx_tanh),
                        reads=[pbuf], writes=[b_g])
                for blk in range(2):
                    S.op("dve", lambda e, blk=blk, g_t=g_t, rows=rows: e.bn_stats(
                        out=bns[0:rows, blk, 0:6], in_=g_t[0:rows, blk * 512:(blk + 1) * 512]), reads=[b_g], writes=[b_bns])
                mv = bns[0:rows, 0, 6:8]
                S.op("dve", lambda e, rows=rows, mv=mv: e.bn_aggr(out=mv, in_=bns[0:rows, :, 0:6]), reads=[b_bns], writes=[b_bns])
                sd = bns[0:rows, 1, 6:7]
                rsd = bns[0:rows, 1, 7:8]
                S.op("dve", lambda e, rows=rows, sd=sd: e.tensor_scalar(out=sd, in0=bns[0:rows, 0, 7:8], scalar1=EPS, scalar2=None,
                                                                     op0=ALU.add), reads=[b_bns], writes=[b_bns])
                S.op("act", lambda e, sd=sd: e.sqrt(out=sd, in_=sd), reads=[b_bns], writes=[b_bns])
                S.op("dve", lambda e, sd=sd, rsd=rsd: e.reciprocal(out=rsd, in_=sd), reads=[b_bns], writes=[b_bns])
                S.op("dve", lambda e, g_t=g_t, rows=rows, rsd=rsd: e.tensor_scalar(
                    out=g_t[0:rows, :], in0=g_t[0:rows, :], scalar1=bns[0:rows, 0, 6:7], scalar2=rsd,
                    op0=ALU.subtract, op1=ALU.mult), reads=[b_g, b_bns], writes=[b_g])
                S.op("dve", lambda e, g_t=g_t, rows=rows: e.tensor_tensor(out=g_t[0:rows, :], in0=g_t[0:rows, :], in1=lng[0:rows, :],
                                                                      op=ALU.mult), reads=[b_g, b_lng], writes=[b_g])
                if rows == 128:
                    S.op("dve", lambda e, g_t=g_t, ti=ti: e.tensor_tensor(out=vn[:, ti, :], in0=g_t[:], in1=lnb[:], op=ALU.add),
                         reads=[b_g, b_lnb], writes=[b_vn])
                else:
                    S.op("dve", lambda e, g_t=g_t, rows=rows: e.tensor_tensor(out=g_t[0:rows, :], in0=g_t[0:rows, :],
                                                                          in1=lnb[0:rows, :], op=ALU.add),
                         reads=[b_g, b_lnb], writes=[b_g])
                    S.dma("sp", lambda e, g_t=g_t, rows=rows: e.dma_start(out=sguv, in_=g_t[0:rows, :]), reads=[b_g], dbuf=outb)
                    S.op("act", lambda e, g_t=g_t, rows=rows, ti=ti: e.copy(out=vn[0:rows, ti, :], in_=g_t[0:rows, :]),
                         reads=[b_g], writes=[b_vn])

            for blk in range(2):
                wt, bw = w_next()
                for jj in range(4):
                    j = blk * 4 + jj
                    for (c0, wd) in PASSES:
                        pt, pbuf = bank("A")
                        mm_group(pt[:, 0:wd], pbuf, [(wt[:, kc, jj * 128:(jj + 1) * 128], hT[:, kc, c0:c0 + wd]) for kc in range(16)],
                                 [b_hT, bw])
                        S.op("act", lambda e, pt=pt, j=j, c0=c0, wd=wd: e.activation(
                            out=amixT[:, j, c0 - TOK:c0 - TOK + wd], in_=pt[:, 0:wd], func=AF.Gelu_apprx_tanh),
                            reads=[pbuf], writes=[b_amixT])

            for tt in range(8):
                for half in range(2):
                    pt, pbuf = bank("A")
                    for jj in range(4):
                        gi = half * 4 + jj
                        S.op("pe", lambda e, pt=pt, jj=jj, gi=gi, tt=tt: e.matmul(
                            pt[:, jj * 128:(jj + 1) * 128], lhsT=vn[:, tt, gi * 128:(gi + 1) * 128], rhs=wsT[:, gi, :],
                            start=True, stop=True), reads=[b_vn, b_wsT], writes=[pbuf], signal=(jj == 3))
                    S.op("dve", lambda e, pt=pt, half=half: e.tensor_tensor(
                        out=sgt[:].rearrange("p (g t) -> p g t", g=4), in0=pt[:].rearrange("p (g t) -> p g t", g=4),
                        in1=bsb[:, half * 4:half * 4 + 4, :], op=ALU.add), reads=[pbuf, b_bsb], writes=[b_sgt])
                    S.op("dve", lambda e, half=half, tt=tt: e.tensor_tensor(
                        out=amixT[:, half * 4:half * 4 + 4, tt * 128:(tt + 1) * 128],
                        in0=amixT[:, half * 4:half * 4 + 4, tt * 128:(tt + 1) * 128],
                        in1=sgt[:].rearrange("p (g t) -> p g t", g=4), op=ALU.mult), reads=[b_sgt, b_amixT], writes=[b_amixT])
            pt, pbuf = bank("A")
            for gi in range(8):
                S.op("pe", lambda e, pt=pt, gi=gi: e.matmul(pt[:, gi * NS:(gi + 1) * NS], lhsT=vn[0:NS, 8, gi * 128:(gi + 1) * 128],
                                                       rhs=dg[:, gi, :], start=True, stop=True),
                     reads=[b_vn, b_dg], writes=[pbuf], signal=(gi == 7))
            S.op("dve", lambda e, pt=pt: e.tensor_tensor(
                out=sgt[:, 0:128].rearrange("p (g t) -> p g t", g=8), in0=pt[:, 0:128].rearrange("p (g t) -> p g t", g=8),
                in1=bsb[:, :, 0:1].broadcast_to([128, 8, NS]), op=ALU.add), reads=[pbuf, b_bsb], writes=[b_sgt])
            S.op("dve", lambda e: e.tensor_tensor(out=amixT[:, :, TOK:TOK + NS], in0=amixT[:, :, TOK:TOK + NS],
                                                  in1=sgt[:, 0:128].rearrange("p (g t) -> p g t", g=8), op=ALU.mult),
                 reads=[b_sgt, b_amixT], writes=[b_amixT])

            S.barrier()
            for r in (r_vn, gv[0][2], gv[1][2], r_lng, r_lnb, r_bns, r_wsT, r_wsF, r_bsb, r_bs0, r_ws00, r_dg, r_sgt, r_wsM):
                AR.release(r)

            if STOP < 3:
                return
            mergedT, b_mergedT, r_mergedT = sb("mergedT", [128, 16, TOK + NS], BF16, top=True)
            sgA, b_sgA, r_sgA = sb("sgA", [128, 4, TOK + NS], BF16)
            sgB, b_sgB = sgA, b_sgA
            M1, b_M1, r_M1 = sb("M1", [128, 4, TOK + NS], F32)
            mtmp, b_mtmp, r_mtmp = sb("mtmp", [128, 512], F32)
            def gate_block():
                wt, bw = w_next()
                for jj in range(4):
                    for (c0, wd) in PASSES:
                        pt, pbuf = bank("A")
                        mm_group(pt[:, 0:wd], pbuf, [(wt[:, kc, jj * 128:(jj + 1) * 128], hT[:, kc, c0:c0 + wd]) for kc in range(16)],
                                 [b_hT, bw])
                        S.op("act", lambda e, pt=pt, jj=jj, c0=c0, wd=wd: e.activation(
                            out=sgA[:, jj, c0 - TOK:c0 - TOK + wd], in_=pt[:, 0:wd], func=AF.Sigmoid),
                            reads=[pbuf], writes=[b_sgA])

            for cb in range(4):
                gate_block()
                wt, bw = w_next()
                for jj in range(4):
                    for (c0, wd) in PASSES:
                        pt, pbuf = bank("A")
                        mm_group(pt[:, 0:wd], pbuf, [(wt[:, kc, jj * 128:(jj + 1) * 128], amixT[:, kc, c0 - TOK:c0 - TOK + wd])
                                                     for kc in range(8)], [b_amixT, bw])
                        S.op("dve", lambda e, pt=pt, jj=jj, c0=c0, wd=wd: e.tensor_tensor(
                            out=M1[:, jj, c0 - TOK:c0 - TOK + wd], in0=pt[:, 0:wd], in1=sgA[:, jj, c0 - TOK:c0 - TOK + wd], op=ALU.mult),
                            reads=[pbuf, b_sgA], writes=[b_M1])
                gate_block()
                wt, bw = w_next()
                for jj in range(4):
                    for (c0, wd) in PASSES:
                        pt, pbuf = bank("A")
                        mm_group(pt[:, 0:wd], pbuf, [(wt[:, kc, jj * 128:(jj + 1) * 128], bmixT[:, kc, c0 - TOK:c0 - TOK + wd])
                                                     for kc in range(4)], [b_bmixT, bw])
                        S.op("dve", lambda e, pt=pt, jj=jj, c0=c0, wd=wd: e.tensor_tensor(
                            out=mtmp[:, 0:wd], in0=pt[:, 0:wd], in1=sgB[:, jj, c0 - TOK:c0 - TOK + wd], op=ALU.mult),
                            reads=[pbuf, b_sgB], writes=[b_mtmp])
                        S.op("dve", lambda e, cb=cb, jj=jj, c0=c0, wd=wd: e.tensor_tensor(
                            out=mergedT[:, cb * 4 + jj, c0 - TOK:c0 - TOK + wd], in0=mtmp[:, 0:wd],
                            in1=M1[:, jj, c0 - TOK:c0 - TOK + wd], op=ALU.add),
                            reads=[b_mtmp, b_M1], writes=[b_mergedT])

            S.barrier()
            for r in (r_hT, r_amixT, r_bmixT, r_sgA, r_M1, r_mtmp):
                AR.release(r)

            if STOP < 4:
                return
            X2, b_X2, r_X2 = sb("X2", [128, 9, D], F32)
            bX2 = [Buf("X2_%d" % i) for i in range(9)]
            xpc = [sb("xpc%d" % i, [128, 512], F32) for i in range(3)]
            rows_of = [128] * 8 + [NS]
            k = 0

            def load_xpiece(k):
                cb, tt = divmod(k, 9)
                t, bf, _ = xpc[k % 3]
                rows = rows_of[tt]
                src = xloc[TOK + tt * 128:TOK + (tt + 1) * 128, cb * 512:(cb + 1) * 512] if tt < 8 else xs[:, cb * 512:(cb + 1) * 512]
                S.dma("sp", lambda e: e.dma_start(out=t[0:rows, :], in_=src), writes=[bf])

            load_xpiece(0); load_xpiece(1)
            for cb in range(4):
                wt, bw = w_next()
                for tt in range(9):
                    if k + 2 < 36:
                        load_xpiece(k + 2)
                    rows = rows_of[tt]
                    t, bf, _ = xpc[k % 3]
                    pt, pbuf = bank("A")
                    mm_group(pt[0:rows, :], pbuf, [(mergedT[:, kc, tt * 128:tt * 128 + rows], wt[:, kc, :]) for kc in range(16)],
                             [b_mergedT, bw])
                    S.op("dve", lambda e, pt=pt, t=t, tt=tt, cb=cb, rows=rows: e.tensor_tensor(
                        out=X2[0:rows, tt, cb * 512:(cb + 1) * 512], in0=t[0:rows, :], in1=pt[0:rows, :], op=ALU.add),
                        reads=[pbuf, bf], writes=[bX2[tt]])
                    k += 1
            S.barrier()
            AR.release(r_mergedT)
            for i in range(3):
                AR.release(xpc[i][2])

            if STOP < 5:
                return
            h2T, b_h2T, r_h2T = sb("h2T", [128, 16, TOK + NS], BF16)
            rstd2, b_rstd2, _ = sb("rstd2", [128, 9], F32)
            xnb = [sb("xnb%d" % i, [128, D], BF16) for i in range(2)]
            SQ["t"], SQ["b"], SQ["r"] = sb("sqjunk", [128, D], BF16)
            for tt in range(9):
                rows = rows_of[tt]
                rs = rms_stats(X2[0:rows, tt, :], rows, bX2[tt], tt % 2)
                S.op("dve", lambda e, rs=rs, tt=tt, rows=rows: e.tensor_copy(out=rstd2[0:rows, tt:tt + 1], in_=rs),
                     reads=[b_stat], writes=[b_rstd2])
                xn, bxn, _ = xnb[tt % 2]
                S.op("act", lambda e, xn=xn, rs=rs, tt=tt, rows=rows: e.activation(
                    out=xn[0:rows, :], in_=X2[0:rows, tt, :], func=AF.Copy, scale=rs), reads=[bX2[tt], b_stat], writes=[bxn])
                transpose_to(h2T, b_h2T, tt * 128, xn, bxn, rows, 1)

            if STOP < 6:
                return
            skF, b_skF, r_skF = sb("skF", [128, 2, 128], F32)
            skT, b_skT, _ = sb("skT", [128, 2, 128], BF16)
            S.dma("sp", lambda e: e.dma_start(out=skF[:], in_=subk.rearrange("j k d -> k j d")), writes=[b_skF])
            pt, pbuf = bank("C")
            for j in range(2):
                S.op("pe", lambda e, j=j, pt=pt: e.transpose(out=pt[:, j * 128:(j + 1) * 128], in_=skF[:, j, :], identity=identF[:]),
                     reads=[b_skF, b_identF], writes=[pbuf], signal=(j == 1))
            S.op("act", lambda e, pt=pt: e.copy(out=skT[:], in_=pt[:, 0:256].rearrange("p (j k) -> p j k", j=2)),
                 reads=[pbuf], writes=[b_skT])
            T1, b_T1, r_T1 = sb("T1", [128, 9, 16, 16], F32)
            I1, b_I1, r_I1 = sb("I1", [128, 9, 16, 16], U32)
            qc, b_qc, r_qc = sb("qc", [128, TOK + NS], BF16)
            scs = [sb("scs%d" % i, [128, 128], F32) for i in range(2)]
            sc2, b_sc2, r_sc2 = sb("sc2", [128, 128], F32)
            k = 0
            for cb in range(4):
                wt, bw = w_next()
                for jj in range(4):
                    c = cb * 4 + jj
                    for (c0, wd) in PASSES:
                        pt, pbuf = bank("A")
                        mm_group(pt[:, 0:wd], pbuf, [(wt[:, kc, jj * 128:(jj + 1) * 128], h2T[:, kc, c0 - TOK:c0 - TOK + wd])
                                                     for kc in range(16)], [b_h2T, bw])
                        S.op("act", lambda e, pt=pt, c0=c0, wd=wd: e.copy(out=qc[:, c0 - TOK:c0 - TOK + wd], in_=pt[:, 0:wd]),
                             reads=[pbuf], writes=[b_qc])
                    for tt in range(9):
                        rows = rows_of[tt]
                        pt, pbuf = bank("B") if tt % 2 else bank("C")
                        S.op("pe", lambda e, pt=pt, tt=tt, rows=rows, c=c: e.matmul(
                            pt[0:rows, 0:128], lhsT=qc[:, tt * 128:tt * 128 + rows], rhs=skT[:, c % 2, :], start=True, stop=True),
                            reads=[b_qc, b_skT], writes=[pbuf])
                        sc, b_sc, _ = scs[k % 2]; k += 1
                        S.op("act", lambda e, pt=pt, sc=sc, rows=rows: e.copy(out=sc[0:rows, :], in_=pt[0:rows, 0:128]),
                             reads=[pbuf], writes=[b_sc])
                        S.op("dve", lambda e, sc=sc, rows=rows, tt=tt, c=c: e.max(out=T1[0:rows, tt, c, 0:8], in_=sc[0:rows, :]),
                             reads=[b_sc], writes=[b_T1])
                        S.op("dve", lambda e, sc=sc, rows=rows, tt=tt, c=c: e.max_index(
                            out=I1[0:rows, tt, c, 0:8], in_max=T1[0:rows, tt, c, 0:8], in_values=sc[0:rows, :]),
                            reads=[b_sc, b_T1], writes=[b_I1])
                        S.op("dve", lambda e, sc=sc, rows=rows, tt=tt, c=c: e.match_replace(
                            out=sc2[0:rows, :], in_to_replace=T1[0:rows, tt, c, 0:8], in_values=sc[0:rows, :], imm_value=-3.0e38),
                            reads=[b_sc, b_T1], writes=[b_sc2])
                        S.op("dve", lambda e, rows=rows, tt=tt, c=c: e.max(out=T1[0:rows, tt, c, 8:16], in_=sc2[0:rows, :]),
                             reads=[b_sc2], writes=[b_T1])
                        S.op("dve", lambda e, rows=rows, tt=tt, c=c: e.max_index(
                            out=I1[0:rows, tt, c, 8:16], in_max=T1[0:rows, tt, c, 8:16], in_values=sc2[0:rows, :]),
                            reads=[b_sc2, b_T1], writes=[b_I1])
            S.barrier()
            for r in (r_skF, r_qc, scs[0][2], scs[1][2], r_sc2, r_h2T, xnb[0][2], xnb[1][2], SQ["r"]):
                AR.release(r)

            if STOP < 7:
                return
            tab_issue(len(tab_jobs))
            cand, b_cand, r_cand = sb("cand", [128, 8, 256], F32)
            cand2, b_cand2, r_cand2 = sb("cand2", [128, 256], F32)
            ts, b_ts, r_ts = sb("ts", [128, 8, 16], F32)
            ic, b_ic, r_ic = sb("ic", [128, 8, 16], U32)
            icw, b_icw, r_icw = sb("icw", [128, 2, 8, 16], U32)
            icf, b_icf, r_icf = sb("icf", [128, 2, 8, 16], F32)
            i1f, b_i1f, r_i1f = sb("i1f", [128, 16, 16], F32)
            iota16, b_iota16, r_iota16 = sb("iota16", [128, 16], F32)
            oh, b_oh = cand[:].rearrange("p h (a b) -> p h a b", a=16), b_cand
            ef, b_ef, r_ef = sb("ef", [128, 2, 8, 16], F32)
            gs2, b_gs2, r_gs2 = sb("gs2", [128, 16], F32)
            eidx, b_eidx, _ = sb("eidx", [128, 9, 128], I32, top=True)
            gsm, b_gsm, _ = sb("gsm", [128, 9, 128], F32, top=True)
            S.op("pool", lambda e: e.iota(iota16[:], pattern=[[1, 16]], base=0, channel_multiplier=0,
                                          allow_small_or_imprecise_dtypes=True), writes=[b_iota16])
            S.op("pool", lambda e: e.memset(eidx[:], 0), writes=[b_eidx])
            for tt in range(9):
                rows = rows_of[tt]
                T = T1[0:rows, tt].rearrange("p (h j) k -> p h j k", j=2)
                G = gsm[0:rows, tt, :].rearrange("p (h k) -> p h k", h=8)
                S.op("dve", lambda e, T=T, rows=rows: e.tensor_tensor(
                    out=cand[0:rows].rearrange("p h (a b) -> p h a b", a=16),
                    in0=T[:, :, 0, :].unsqueeze(3).broadcast_to([rows, 8, 16, 16]),
                    in1=T[:, :, 1, :].unsqueeze(2).broadcast_to([rows, 8, 16, 16]), op=ALU.add),
                    reads=[b_T1], writes=[b_cand])
                for h in range(8):
                    S.op("dve", lambda e, h=h, rows=rows: e.max(out=ts[0:rows, h, 0:8], in_=cand[0:rows, h, :]),
                         reads=[b_cand], writes=[b_ts])
                    S.op("dve", lambda e, h=h, rows=rows: e.max_index(out=ic[0:rows, h, 0:8], in_max=ts[0:rows, h, 0:8],
                                                                   in_values=cand[0:rows, h, :]),
                         reads=[b_cand, b_ts], writes=[b_ic])
                    S.op("dve", lambda e, h=h, rows=rows: e.match_replace(out=cand2[0:rows, :], in_to_replace=ts[0:rows, h, 0:8],
                                                                       in_values=cand[0:rows, h, :], imm_value=-3.0e38),
                         reads=[b_cand, b_ts], writes=[b_cand2])
                    S.op("dve", lambda e, h=h, rows=rows: e.max(out=ts[0:rows, h, 8:16], in_=cand2[0:rows, :]),
                         reads=[b_cand2], writes=[b_ts])
                    S.op("dve", lambda e, h=h, rows=rows: e.max_index(out=ic[0:rows, h, 8:16], in_max=ts[0:rows, h, 8:16],
                                                                   in_values=cand2[0:rows, :]),
                         reads=[b_cand2, b_ts], writes=[b_ic])
                S.op("dve", lambda e, rows=rows: e.tensor_single_scalar(out=icw[0:rows, 0], in_=ic[0:rows], scalar=c4[0:rows, 0:1],
                                                                       op=ALU.logical_shift_right), reads=[b_ic, b_c4], writes=[b_icw])
                S.op("dve", lambda e, rows=rows: e.tensor_single_scalar(out=icw[0:rows, 1], in_=ic[0:rows], scalar=c4[0:rows, 1:2],
                                                                       op=ALU.bitwise_and), reads=[b_ic, b_c4], writes=[b_icw])
                S.op("dve", lambda e, rows=rows: e.tensor_copy(out=icf[0:rows], in_=icw[0:rows]), reads=[b_icw], writes=[b_icf])
                S.op("dve", lambda e, rows=rows, tt=tt: e.tensor_copy(out=i1f[0:rows], in_=I1[0:rows, tt]), reads=[b_I1], writes=[b_i1f])
                I = i1f[0:rows].rearrange("p (h j) k -> p h j k", j=2)
                for side in range(2):
                    S.op("dve", lambda e, side=side, rows=rows: e.tensor_tensor(
                        out=oh[0:rows], in0=icf[0:rows, side].unsqueeze(3).broadcast_to([rows, 8, 16, 16]),
                        in1=iota16[0:rows].unsqueeze(1).unsqueeze(1).broadcast_to([rows, 8, 16, 16]), op=ALU.is_equal),
                        reads=[b_icf, b_iota16], writes=[b_oh])
                    S.op("dve", lambda e, side=side, rows=rows, I=I: e.tensor_tensor(
                        out=oh[0:rows], in0=oh[0:rows], in1=I[:, :, side, :].unsqueeze(2).broadcast_to([rows, 8, 16, 16]),
                        op=ALU.mult), reads=[b_oh, b_i1f], writes=[b_oh])
                    S.op("dve", lambda e, side=side, rows=rows: e.tensor_reduce(out=ef[0:rows, side], in_=oh[0:rows], axis=AX.X,
                                                                             op=ALU.add), reads=[b_oh], writes=[b_ef])
                S.op("dve", lambda e, rows=rows: e.scalar_tensor_tensor(
                    out=ef[0:rows, 0], in0=ef[0:rows, 0], scalar=128.0, in1=ef[0:rows, 1], op0=ALU.mult, op1=ALU.add),
                    reads=[b_ef], writes=[b_ef])
                S.op("dve", lambda e, rows=rows, tt=tt: e.tensor_copy(out=eidx[0:rows, tt, :].rearrange("p (h k) -> p h k", h=8),
                                                                   in_=ef[0:rows, 0]), reads=[b_ef], writes=[b_eidx])
                S.op("dve", lambda e, rows=rows, G=G: e.tensor_tensor(
                    out=G, in0=ts[0:rows], in1=ts[0:rows, :, 0:1].broadcast_to([rows, 8, 16]), op=ALU.subtract),
                    reads=[b_ts], writes=[b_gsm])
                S.op("act", lambda e, G=G: e.activation(out=G, in_=G, func=AF.Exp), reads=[b_gsm], writes=[b_gsm])
                S.op("dve", lambda e, rows=rows, G=G: e.tensor_reduce(out=gs2[0:rows, 0:8], in_=G, axis=AX.X, op=ALU.add),
                     reads=[b_gsm], writes=[b_gs2])
                S.op("dve", lambda e, rows=rows: e.reciprocal(out=gs2[0:rows, 8:16], in_=gs2[0:rows, 0:8]), reads=[b_gs2], writes=[b_gs2])
                S.op("dve", lambda e, rows=rows, G=G: e.tensor_tensor(
                    out=G, in0=G, in1=gs2[0:rows, 8:16].unsqueeze(2).broadcast_to([rows, 8, 16]), op=ALU.mult),
                    reads=[b_gsm, b_gs2], writes=[b_gsm])
            S.barrier()
            for r in (r_cand, r_cand2, r_ts, r_ic, r_icw, r_icf, r_i1f, r_iota16, r_ef, r_gs2, r_T1, r_I1):
                AR.release(r)

            gffn_b, b_gffn_b, r_gffn_b = sb("gffn_b", [128, D], F32)
            S.dma("sp", lambda e: e.dma_start(out=gffn_b[:], in_=gvec[1, :].partition_broadcast(128)), writes=[b_gffn_b])
            actp, b_actp, r_actp = sb("actp", [128, 128], F32)
            coef, b_coef, r_coef = sb("coef", [128, 128], F32)
            h2t, b_h2t, r_h2t = sb("h2t", [128, D], F32)
            ujunk, b_ujunk, r_ujunk = sb("ujunk", [128, D], BF16)
            NG = 8
            LOOK = NG - 2
            ug = [sb("ug%d" % i, [128, D], BF16) for i in range(NG)]
            dgt = [sb("dgt%d" % i, [128, 128], BF16) for i in range(4)]
            jobs = []
            for tt in range(9):
                jobs += [("U", tt, 0, k) for k in range(16)]
                for h in range(8):
                    for k in range(16):
                        jobs.append(("V", tt, h, k))
                        if h < 7:
                            jobs.append(("U", tt, h + 1, k))
            gstate = {"issued": 0, "dg": 0}
            gbuf = {}

            def g_issue_upto(n):
                while gstate["issued"] < min(n, len(jobs)):
                    ji = gstate["issued"]
                    kind, tt, h, k = jobs[ji]
                    t, bf, _ = ug[ji % NG]
                    table = pu16 if kind == "U" else pv16
                    sidx = h * 16 + k
                    S.dma("pool", lambda e, t=t, table=table, tt=tt, sidx=sidx: e.indirect_dma_start(
                        out=t[:], out_offset=None, in_=table,
                        in_offset=bass.IndirectOffsetOnAxis(ap=eidx[:, tt, sidx:sidx + 1], axis=0)),
                        reads=[b_eidx, b_tab], writes=[bf])
                    gbuf[ji] = (t, bf)
                    gstate["issued"] += 1

            for ji, (kind, tt, h, k) in enumerate(jobs):
                rows = rows_of[tt]
                sidx = h * 16 + k
                if kind == "U" and h == 0 and k == 0:
                    S.op("dve", lambda e, rows=rows, tt=tt: e.scalar_tensor_tensor(
                        out=h2t[0:rows, :], in0=X2[0:rows, tt, :], scalar=rstd2[0:rows, tt:tt + 1], in1=gffn_b[0:rows, :],
                        op0=ALU.mult, op1=ALU.mult), reads=[bX2[tt], b_rstd2, b_gffn_b], writes=[b_h2t])
                g_issue_upto(ji + 1 + LOOK)
                t, bf = gbuf.pop(ji)
                if kind == "U":
                    S.op("dve", lambda e, t=t, sidx=sidx, rows=rows: e.scalar_tensor_tensor(
                        out=ujunk[0:rows, :], in0=t[0:rows, :], scalar=1.0, in1=h2t[0:rows, :], op0=ALU.mult, op1=ALU.mult,
                        accum_out=actp[0:rows, sidx:sidx + 1]), reads=[b_h2t, bf], writes=[b_actp])
                    if k == 15:
                        hs = slice(h * 16, (h + 1) * 16)
                        S.op("act", lambda e, rows=rows, hs=hs: e.activation(out=coef[0:rows, hs], in_=actp[0:rows, hs],
                                                                            func=AF.Gelu_apprx_tanh),
                             reads=[b_actp], writes=[b_coef])
                        S.op("dve", lambda e, rows=rows, hs=hs, tt=tt: e.tensor_tensor(
                            out=coef[0:rows, hs], in0=coef[0:rows, hs], in1=gsm[0:rows, tt, hs], op=ALU.mult),
                            reads=[b_coef, b_gsm], writes=[b_coef])
                else:
                    dg_t, b_dg, _ = dgt[gstate["dg"] % 4]; gstate["dg"] += 1
                    S.op("act", lambda e, dg_t=dg_t, rows=rows, sidx=sidx: e.activation(
                        out=dg_t[0:rows, 0:rows], in_=identB[0:rows, 0:rows], func=AF.Copy, scale=coef[0:rows, sidx:sidx + 1]),
                        reads=[b_identB, b_coef], writes=[b_dg])
                    first, last = (sidx == 0), (sidx == 127)
                    for c in range(4):
                        pt, pbuf = PB[(tt % 2) * 4 + c]
                        S.op("pe", lambda e, pt=pt, c=c, t=t, dg_t=dg_t, rows=rows, first=first, last=last: e.matmul(
                            pt[0:rows, :], lhsT=dg_t[0:rows, 0:rows], rhs=t[0:rows, c * 512:(c + 1) * 512], start=first, stop=last),
                            reads=[b_dg, bf], writes=[pbuf], signal=(c == 3))
                    if last:
                        for c in range(4):
                            pt, pbuf = PB[(tt % 2) * 4 + c]
                            S.op("dve", lambda e, pt=pt, c=c, rows=rows, tt=tt: e.tensor_tensor(
                                out=X2[0:rows, tt, c * 512:(c + 1) * 512], in0=X2[0:rows, tt, c * 512:(c + 1) * 512],
                                in1=pt[0:rows, :], op=ALU.add), reads=[pbuf, bX2[tt]], writes=[bX2[tt]])

            if STOP < 8:
                return
            S.barrier()
            for r in [r_gffn_b, r_h2t, r_actp, r_coef, r_ujunk] + [u[2] for u in ug] + [d_[2] for d_ in dgt]:
                AR.release(r)
            x3T, b_x3T, r_x3T = sb("x3T", [128, 16, TOK + NS], BF16)
            xnb = [sb("xnb%d" % i, [128, D], BF16) for i in range(2)]
            pT_, b_pT, _ = sb("pT", [128, 2, TOK + NS], BF16)
            pst, b_pst, _ = sb("pst", [128, 256], F32)
            psb, b_psb, _ = sb("psb", [128, 256], BF16)
            wpl, b_wpl, _ = sb("wpl", [128, 2, D], BF16)
            S.dma("pool", lambda e: e.dma_start(out=wpl[:], in_=w_ple.rearrange("(k p) c -> p k c", p=128)), writes=[b_wpl])
            for tt in range(9):
                rows = rows_of[tt]
                xn, bxn, _ = xnb[tt % 2]
                S.op("act", lambda e, xn=xn, tt=tt, rows=rows: e.copy(out=xn[0:rows, :], in_=X2[0:rows, tt, :]),
                     reads=[bX2[tt]], writes=[bxn])
                transpose_to(x3T, b_x3T, tt * 128, xn, bxn, rows, None)
                src = ploc[tt * 128:(tt + 1) * 128, :] if tt < 8 else psm
                S.dma("sp", lambda e, src=src, rows=rows: e.dma_start(out=pst[0:rows, 0:256], in_=src), writes=[b_pst])
                psbv = psb
                S.op("act", lambda e, rows=rows, psbv=psbv: e.copy(out=psbv[0:rows, 0:256], in_=pst[0:rows, 0:256]),
                     reads=[b_pst], writes=[b_psb])
                pt2, pbuf2 = bank("C")
                ptb = pt2[:].bitcast(BF16)
                for j in range(2):
                    S.op("pe", lambda e, j=j, ptb=ptb, rows=rows, psbv=psbv: e.transpose(
                        out=ptb[:, j * 128:j * 128 + rows], in_=psbv[0:rows, j * 128:(j + 1) * 128], identity=identB[0:rows, 0:rows]),
                        reads=[b_psb, b_identB], writes=[pbuf2], signal=(j == 1))
                S.op("dve", lambda e, ptb=ptb, tt=tt, rows=rows: e.tensor_copy(
                    out=pT_[:, :, tt * 128:tt * 128 + rows], in_=ptb[:, 0:256].rearrange("p (j t) -> p j t", j=2)[:, :, 0:rows]),
                    reads=[pbuf2], writes=[b_pT])
            sig, b_sig, _ = sb("sig", [128, 512], F32)
            for cb in range(4):
                wg, bwg = w_next()
                for tt in range(9):
                    rows = rows_of[tt]
                    pg, pbg = bank("A")
                    mm_group(pg[0:rows, :], pbg, [(x3T[:, kc, tt * 128:tt * 128 + rows], wg[:, kc, :]) for kc in range(16)],
                             [b_x3T, bwg])
                    pe_, pbe = bank("A")
                    mm_group(pe_[0:rows, :], pbe, [(pT_[:, kc, tt * 128:tt * 128 + rows], wpl[:, kc, cb * 512:(cb + 1) * 512])
                                                   for kc in range(2)], [b_pT, b_wpl])
                    S.op("act", lambda e, pg=pg, rows=rows: e.activation(out=sig[0:rows, 0:512], in_=pg[0:rows, :], func=AF.Sigmoid),
                         reads=[pbg], writes=[b_sig])
                    S.op("dve", lambda e, pe_=pe_, rows=rows: e.tensor_tensor(out=sig[0:rows, 0:512], in0=sig[0:rows, 0:512],
                                                                             in1=pe_[0:rows, :], op=ALU.mult),
                         reads=[pbe, b_sig], writes=[b_sig])
                    S.op("dve", lambda e, tt=tt, cb=cb, rows=rows: e.tensor_tensor(
                        out=X2[0:rows, tt, cb * 512:(cb + 1) * 512], in0=X2[0:rows, tt, cb * 512:(cb + 1) * 512],
                        in1=sig[0:rows, 0:512], op=ALU.add), reads=[b_sig, bX2[tt]], writes=[bX2[tt]])

            if STOP < 9:
                return
            S.barrier()
            AR.release(r_x3T)
            gfin_b, b_gfin_b, _ = sb("gfin_b", [128, D], F32)
            S.dma("sp", lambda e: e.dma_start(out=gfin_b[:], in_=gvec[2, :].partition_broadcast(128)), writes=[b_gfin_b])
            SQ["t"], SQ["b"], SQ["r"] = sb("sqjunk", [128, D], BF16)
            xst = [sb("yo%d" % i, [128, D], F32) for i in range(2)]
            for tt in range(9):
                rows = rows_of[tt]
                rs = rms_stats(X2[0:rows, tt, :], rows, bX2[tt], tt % 2)
                yo, b_yo, _ = xst[tt % 2]
                S.op("dve", lambda e, yo=yo, rs=rs, tt=tt, rows=rows: e.scalar_tensor_tensor(
                    out=yo[0:rows, :], in0=X2[0:rows, tt, :], scalar=rs, in1=gfin_b[0:rows, :], op0=ALU.mult, op1=ALU.mult),
                    reads=[bX2[tt], b_stat, b_gfin_b], writes=[b_yo])
                dst = y[tt * 128:(tt + 1) * 128, :] if tt < 8 else ys
                S.dma("sp", lambda e, yo=yo, dst=dst, rows=rows: e.dma_start(out=dst, in_=yo[0:rows, :]), reads=[b_yo], dbuf=outb)

        phases()
        S.barrier()
        build_nc.sbuf_base = (nc.sbuf_base, nc.sbuf_top)
        S.wait_all("sp", [outb])
        S._wait("sp", (outb.sem, outb.cnt, "d_outb"))
        build_nc.stats = dict(ninstr=dict(S.ninstr), nsem=5 + len(S.dbufs))
    return nc


def _rope_rows(pos):
    half = 16
    inv = (np.float32(500000.0) ** (-(np.arange(half, dtype=np.float32)) / np.float32(half))).astype(np.float32)
    ang = pos.astype(np.float32)[:, None] * inv[None, :]
    c, s = np.cos(ang).astype(np.float32), np.sin(ang).astype(np.float32)
    return np.concatenate([c, c, -s, s], axis=1).astype(np.float32)


def _consts(half):
    pos = np.maximum(np.arange(2 * TOK) - TOK + TOK * half, 0)
    tab = _rope_rows(pos)
    rope = np.zeros((128, 56, 64), np.float32)
    p = np.arange(128)
    for g, d in enumerate(DIL):
        tpr = (2 * TOK // d) // 128
        for n in range(16):
            r, i0 = n // tpr, (n % tpr) * 128
            rope[:, g * 16 + n, :] = tab[(i0 + p) * d + r]
    for j in range(8):
        r = 2 * j + (p >= 64)
        i = 64 + (p % 64)
        rope[:, 48 + j, :] = tab[i * 16 + r]
    ropes = np.repeat(_rope_rows(np.array([2048])), NS, axis=0)
    cb = NEG if half == 0 else 0.0
    pp, ff = np.meshgrid(np.arange(128), np.arange(128), indexing="ij")
    m = np.zeros((128, 5, 128), np.float32)
    m[:, 0, :] = np.where(ff <= pp, 0.0, NEG)
    m[:, 1, :] = np.where(pp <= ff, 0.0, NEG)
    m[:, 2, :] = m[:, 0, :] + cb
    m[:, 3, :] = cb
    m[:, 4, :] = np.where(pp < 64, cb, np.where(pp - 64 <= ff, 0.0, NEG))
    return rope, ropes, m


def _in_maps(x_prompt, x_sample, cache_kv_w128, cache_kv_w512, cache_kv_w2048, p_prompt, p_sample, g_mix, w_in,
             sgu_ln_g, sgu_ln_b, w_s, b_s, w_a_out, w_b_out, w_o, g_ffn, peer_w_q, peer_sub_k1, peer_sub_k2,
             peer_u, peer_v, w_ple, w_ple_gate, g_final, cores=None):
    f = lambda a: np.ascontiguousarray(np.asarray(a, dtype=np.float32))
    shared = {
        "w_in": f(w_in[0]), "w_a_out": f(w_a_out[0]), "w_b_out": f(w_b_out[0]), "w_o": f(w_o[0]), "w_q": f(peer_w_q[0]),
        "w_pg": f(w_ple_gate[0]), "w_ple": f(w_ple[0]), "peer_u": f(peer_u[0]), "peer_v": f(peer_v[0]),
        "w_s": f(w_s[0]), "b_s": f(b_s[0]), "subk": f(np.stack([peer_sub_k1[0], peer_sub_k2[0]])),
        "gvec": f(np.stack([g_mix[0], g_ffn[0], g_final])), "lnv": f(np.stack([sgu_ln_g[0], sgu_ln_b[0]])),
    }
    maps = []
    for c in (range(NCORES) if cores is None else cores):
        b, half = c // 2, c % 2
        own = x_prompt[b, half * TOK:(half + 1) * TOK]
        ctx = x_prompt[b, 0:TOK]
        sl = slice(c * NS, (c + 1) * NS)
        caches = np.stack([
            np.asarray(cache_kv_w128[0, sl]).reshape(NS, 128, 1024),
            np.asarray(cache_kv_w512[0, sl, 0::4]).reshape(NS, 128, 1024),
            np.asarray(cache_kv_w2048[0, sl, 0::16]).reshape(NS, 128, 1024)])
        rope, ropes, m = _consts(half)
        d = dict(shared)
        d.update({"xloc": f(np.concatenate([ctx, own], axis=0)), "xs": f(x_sample[sl, 0]),
                  "ploc": f(p_prompt[0, b, half * TOK:(half + 1) * TOK]), "psm": f(p_sample[0, sl, 0]),
                  "cache": f(caches), "rope": rope, "ropes": ropes, "masks": m})
        maps.append(d)
    return maps


def _assemble(res):
    y = np.zeros((4, 2 * TOK, D), np.float32)
    ysm = np.zeros((128, 1, D), np.float32)
    k0 = np.zeros((1, 4, 128, 2, 4, 128), np.float32)
    k1 = np.zeros((1, 4, 512, 2, 4, 128), np.float32)
    k2 = np.zeros((1, 4, 2 * TOK, 2, 4, 128), np.float32)
    ks = [np.zeros((1, 128, 1, 2, 4, 128), np.float32) for _ in range(3)]
    sg = np.zeros((1, 128, 1, A_W), np.float32)
    for c, r in enumerate(res):
        b, half = c // 2, c % 2
        y[b, half * TOK:(half + 1) * TOK] = r["y"]
        ysm[c * NS:(c + 1) * NS, 0] = r["ys"]
        k2[0, b, half * TOK:(half + 1) * TOK] = r["kv2"].reshape(TOK, 2, 4, 128)
        if half == 1:
            k0[0, b] = r["kv0"].reshape(128, 2, 4, 128)
            k1[0, b] = r["kv1"].reshape(512, 2, 4, 128)
        for g in range(3):
            ks[g][0, c * NS:(c + 1) * NS, 0] = r["kvs"][g].reshape(NS, 2, 4, 128)
        sg[0, c * NS:(c + 1) * NS, 0] = r["sguv"]
    return (y, ysm, k0, k1, k2, ks[0], ks[1], ks[2], sg)


def kernel(**inputs):
    maps = _in_maps(**inputs)
    nc = build_nc()
    res = run_bass_kernel_spmd(nc, maps, core_ids=list(range(NCORES)))
    return _assemble(res.results)
```

```python
import contextlib
import os
import math
import numpy as np
import concourse.bass as bass
import concourse.mybir as mybir
from concourse.bass_utils import run_bass_kernel_spmd

F32 = mybir.dt.float32
BF16 = mybir.dt.bfloat16
I32 = mybir.dt.int32
U32 = mybir.dt.uint32
AF = mybir.ActivationFunctionType
ALU = mybir.AluOpType
AX = mybir.AxisListType
DTSIZE = {F32: 4, BF16: 2, I32: 4, U32: 4}

D = 2048
NCORES = 8
TOK = 1024
NS = 16
NCOL = 2 * TOK + NS
EPS = 1e-6
A_W = 1024
IN_COLS = 10752
C_UA, C_VA, C_Q, C_K, C_V, C_GA, C_GB = 0, 1024, 2048, 3584, 5120, 6656, 8704
DIL = (1, 4, 16)
SCALE = 128 ** -0.5
NEG = -30000.0
NEXP = 16384
PASSES = ((TOK, 512), (TOK + 512, 512), (2 * TOK, NS))


class Buf:
    __slots__ = ("name", "w", "r", "sem", "cnt", "excl")

    def __init__(self, name, excl=False):
        self.name = name
        self.excl = excl
        self.w = None
        self.r = {}
        self.sem = None
        self.cnt = 0


class Sched:
    def __init__(self, nc, stack):
        self.nc = nc
        self.stack = stack
        self.eng = {"pe": nc.tensor, "act": nc.scalar, "dve": nc.vector,
                    "pool": nc.gpsimd, "sp": nc.sync}
        self.sem = {k: stack.enter_context(nc.semaphore("s_" + k)) for k in self.eng}
        self.count = {k: 0 for k in self.eng}
        self.seen = {k: {} for k in self.eng}
        self.dbufs = []
        self.ninstr = {k: 0 for k in self.eng}

    def _wait(self, e, tok):
        if tok is None:
            return
        sem, val, key = tok
        if key == "pe" and e == "pe":
            return
        if self.seen[e].get(key, 0) >= val:
            return
        self.eng[e].wait_ge(sem, val)
        self.seen[e][key] = val

    def _deps(self, e, reads, writes):
        for b in reads:
            self._wait(e, b.w)
        for b in writes:
            self._wait(e, b.w)
            for t in b.r.values():
                self._wait(e, t)

    def _commit(self, tok, reads, writes):
        for b in reads:
            b.r[tok[2]] = tok
        for b in writes:
            b.w = tok
            b.r = {}

    def op(self, e, fn, reads=(), writes=(), signal=True):
        if any(b.excl for b in reads):
            writes = list(writes) + [b for b in reads if b.excl]
            reads = [b for b in reads if not b.excl]
        self._deps(e, reads, writes)
        ins = fn(self.eng[e])
        self.ninstr[e] += 1
        if signal:
            self.count[e] += 1
            ins.then_inc(self.sem[e], 1)
            tok = (self.sem[e], self.count[e], e)
        else:
            tok = (self.sem[e], self.count[e] + 1, e)
        self._commit(tok, reads, writes)
        return tok

    def dma(self, q, fn, reads=(), writes=(), dbuf=None):
        self._deps(q, reads, writes)
        if dbuf is None:
            dbuf = writes[0] if writes else reads[0]
        if dbuf.sem is None:
            dbuf.sem = self.stack.enter_context(self.nc.semaphore("d_" + dbuf.name))
            self.dbufs.append(dbuf)
        ins = fn(self.eng[q])
        dbuf.cnt += 16
        ins.then_inc(dbuf.sem, 16)
        tok = (dbuf.sem, dbuf.cnt, "d_" + dbuf.name)
        self._commit(tok, reads, writes)
        self.ninstr[q] += 1
        return tok

    def wait_all(self, e, bufs):
        for b in bufs:
            self._wait(e, b.w)
            for t in b.r.values():
                self._wait(e, t)

    def barrier(self):
        for e in self.eng:
            for x in ("pe", "act", "dve", "pool"):
                if x != e and self.count[x] > 0:
                    self._wait(e, (self.sem[x], self.count[x], x))
            for b in self.dbufs:
                if b.cnt > 0:
                    self._wait(e, (b.sem, b.cnt, "d_" + b.name))


class Arena:
    def __init__(self, nc, lo, hi):
        self.nc = nc
        self.free = [(lo, hi)]
        self.n = 0
        self.peak = 0
        self.hi = hi

    def alloc(self, name, shape, dt, top=False):
        nb = int(np.prod(shape[1:])) * DTSIZE[dt]
        nb = (nb + 63) // 64 * 64
        order = range(len(self.free) - 1, -1, -1) if top else range(len(self.free))
        for i in order:
            a, b = self.free[i]
            if b - a >= nb:
                if top:
                    off = b - nb
                    self.free[i] = (a, off)
                else:
                    off = a
                    self.free[i] = (a + nb, b)
                if self.free[i][0] == self.free[i][1]:
                    del self.free[i]
                self.n += 1
                used = self.hi - sum(y - x for x, y in self.free)
                self.peak = max(self.peak, used)
                h = self.nc.alloc_sbuf_tensor_at("%s_%d" % (name, self.n), list(shape), dt, offset=off)
                return h, (off, off + nb)
        raise RuntimeError("SBUF arena exhausted allocating %s %s (free=%s)" % (name, shape, self.free))

    def release(self, region):
        self.free.append(region)
        self.free.sort()
        merged = []
        for a, b in self.free:
            if merged and merged[-1][1] == a:
                merged[-1] = (merged[-1][0], b)
            else:
                merged.append((a, b))
        self.free = merged


def build_nc():
    nc = bass.Bass("TRN2", target_bir_lowering=False)
    SKIP = set(os.environ.get('KSKIP', '').split(','))
    NEXP_ = NEXP if int(os.environ.get('KSTOP', '99')) >= 7 else 128
    di = lambda name, shape, dt=F32: nc.dram_tensor(name, list(shape), dt, kind="ExternalInput").ap()
    do = lambda name, shape, dt=F32: nc.dram_tensor(name, list(shape), dt, kind="ExternalOutput").ap()

    xloc = di("xloc", [2 * TOK, D]); xs = di("xs", [NS, D])
    ploc = di("ploc", [TOK, 256]); psm = di("psm", [NS, 256])
    cache = di("cache", [3, NS, 128, 1024])
    w_in = di("w_in", [D, IN_COLS]); w_a_out = di("w_a_out", [A_W, D]); w_b_out = di("w_b_out", [512, D])
    w_o = di("w_o", [D, D]); w_q = di("w_q", [D, D]); w_pg = di("w_pg", [D, D]); w_ple = di("w_ple", [256, D])
    peer_u = di("peer_u", [NEXP_, D]); peer_v = di("peer_v", [NEXP_, D])
    w_s = di("w_s", [8, 128, 128]); b_s = di("b_s", [8, 128]); subk = di("subk", [2, 128, 128])
    gvec = di("gvec", [3, D]); lnv = di("lnv", [2, A_W])
    rope = di("rope", [128, 56, 64]); ropes = di("ropes", [NS, 64]); masks = di("masks", [128, 5, 128])

    y = do("y", [TOK, D]); ys = do("ys", [NS, D])
    kv0 = do("kv0", [128, 1024]); kv1 = do("kv1", [512, 1024]); kv2 = do("kv2", [TOK, 1024])
    kvs = do("kvs", [3, NS, 1024]); sguv = do("sguv", [NS, A_W])
    kvout = (kv0, kv1, kv2)
    pc16 = nc.dram_tensor("pc16", [NEXP_, 2, D], BF16, kind="Internal").ap()

    with contextlib.ExitStack() as st:
        S = Sched(nc, st)
        AR = Arena(nc, 16512, 229344)
        outb = Buf("outb")

        def sb(name, shape, dt, top=False):
            h, reg = AR.alloc(name, shape, dt, top)
            return h, Buf(name), reg

        PB = []
        for i in range(8):
            t = nc.alloc_psum_tensor("pb%d" % i, [128, 512], F32)
            PB.append((t, Buf("pb%d" % i, excl=True)))
        rr = {"A": 0, "B": 0, "C": 0}
        pools = {"A": (0, 1, 2, 3), "B": (4, 5), "C": (6, 7)}

        def bank(pool):
            ids = pools[pool]
            i = ids[rr[pool] % len(ids)]
            rr[pool] += 1
            return PB[i]

        def mm_group(out_ap, pbuf, pairs, reads):
            n = len(pairs)
            for i, (l, r) in enumerate(pairs):
                S.op("pe", lambda e, l=l, r=r, i=i: e.matmul(out_ap, lhsT=l, rhs=r, start=(i == 0), stop=(i == n - 1)),
                     reads=reads, writes=[pbuf], signal=(i == n - 1))

        identF, b_identF, _ = sb("identF", [128, 128], F32)
        identB, b_identB, _ = sb("identB", [128, 128], BF16)
        onesB, b_onesB, _ = sb("onesB", [128, 128], BF16)
        S.op("pool", lambda e: e.memset(identF[:], 1.0), writes=[b_identF])
        S.op("pool", lambda e: e.affine_select(out=identF[:], in_=identF[:], pattern=[[-1, 128]],
                                               compare_op=ALU.is_equal, fill=0.0, base=0, channel_multiplier=1),
             reads=[b_identF], writes=[b_identF])
        S.op("pool", lambda e: e.tensor_copy(out=identB[:], in_=identF[:]), reads=[b_identF], writes=[b_identB])
        S.op("pool", lambda e: e.memset(onesB[:], 1.0), writes=[b_onesB])

        gcol, b_gcol, _ = sb("gcol", [128, 2, 16], F32)
        grow, b_grow, r_grow = sb("grow", [16, 2, 128], F32)
        S.dma("sp", lambda e: e.dma_start(out=grow[:], in_=gvec[0:2, :].rearrange("g (k p) -> k g p", p=128)),
              writes=[b_grow])
        for gi in range(2):
            pt, pbuf = bank("C")
            S.op("pe", lambda e, gi=gi, pt=pt: e.transpose(out=pt[:, 0:16], in_=grow[:, gi, :], identity=identF[0:16, 0:16]),
                 reads=[b_grow, b_identF], writes=[pbuf])
            S.op("act", lambda e, gi=gi, pt=pt: e.copy(out=gcol[:, gi, :], in_=pt[:, 0:16]), reads=[pbuf], writes=[b_gcol])
        maskB, b_maskB, _ = sb("maskB", [128, 5, 128], BF16)
        S.dma("pool", lambda e: e.dma_start(out=maskB[:], in_=masks), writes=[b_maskB])
        c4, b_c4, _ = sb("c4", [128, 2], U32)
        S.op("pool", lambda e: e.memset(c4[:, 0:1], 4), writes=[b_c4])
        S.op("pool", lambda e: e.memset(c4[:, 1:2], 15), writes=[b_c4])
        ropeS, b_ropeS, _ = sb("ropeS", [NS, 64], F32)
        S.dma("sp", lambda e: e.dma_start(out=ropeS[:], in_=ropes), writes=[b_ropeS])

        NW = 2
        wslot = [sb("wslot%d" % i, [128, 16, 512], BF16) for i in range(NW)]
        wq = []
        wstate = {"issued": 0, "used": 0}

        def w_plan(blocks):
            wq.extend(blocks)

        b_tab = Buf("ptab")
        TCH = 1024 if NEXP_ >= 1024 else NEXP_
        tab_jobs = [(src, j, r0) for r0 in range(0, NEXP_, TCH) for (src, j) in ((peer_u, 0), (peer_v, 1))]

        def tab_issue(k=1):
            for _ in range(k):
                if not tab_jobs:
                    return
                src, j, r0 = tab_jobs.pop(0)
                tok = S.dma("pool", lambda e: e.dma_start(out=pc16[r0:r0 + TCH, j, :], in_=src[r0:r0 + TCH, :]), dbuf=b_tab)
                b_tab.w = tok

        def w_issue_upto(n):
            while wstate["issued"] < min(n, len(wq)):
                i = wstate["issued"]
                ap = wq[i]
                K, C = ap.shape
                kc = K // 128
                t, bf, _ = wslot[i % NW]
                S.dma("pool", lambda e, t=t, ap=ap, kc=kc, C=C: e.dma_start(
                    out=t[:, 0:kc, 0:C], in_=ap.rearrange("(k p) c -> p k c", p=128)), writes=[bf])
                wstate["issued"] += 1
                tab_issue(1)

        def w_next(issue=True):
            i = wstate["used"]
            if issue:
                w_issue_upto(i + NW)
            wstate["used"] += 1
            t, bf, _ = wslot[i % NW]
            return t, bf

        xst = [sb("xst%d" % i, [128, D], F32) for i in range(2)]
        xnb = [sb("xnb%d" % i, [128, D], BF16) for i in range(2)]
        SQ = {}
        SQ["t"], SQ["b"], SQ["r"] = sb("sqjunk", [128, D], BF16)
        stat, b_stat, _ = sb("stat", [128, 8], F32)

        def rms_stats(src_ap, rows, b_src, slot):
            ss = stat[0:rows, slot * 2:slot * 2 + 1]
            rs = stat[0:rows, slot * 2 + 1:slot * 2 + 2]
            sq_junk, b_sq_junk = SQ["t"], SQ["b"]
            S.op("act", lambda e: e.activation(out=sq_junk[0:rows, :], in_=src_ap, func=AF.Square, accum_out=ss),
                 reads=[b_src], writes=[b_sq_junk, b_stat])
            S.op("dve", lambda e: e.tensor_scalar(out=ss, in0=ss, scalar1=1.0 / D, scalar2=EPS, op0=ALU.mult, op1=ALU.add),
                 reads=[b_stat], writes=[b_stat])
            S.op("act", lambda e: e.sqrt(out=ss, in_=ss), reads=[b_stat], writes=[b_stat])
            S.op("dve", lambda e: e.reciprocal(out=rs, in_=ss), reads=[b_stat], writes=[b_stat])
            return rs

        def transpose_to(dstT, b_dstT, col0, src_bf, b_src, rows, gsel):
            for half in range(2):
                pt, pbuf = bank("B")
                ptb = pt[:].bitcast(BF16)
                for j in range(8):
                    kc = half * 8 + j
                    S.op("pe", lambda e, kc=kc, j=j, ptb=ptb: e.transpose(
                        out=ptb[:, j * 128:j * 128 + rows], in_=src_bf[0:rows, kc * 128:(kc + 1) * 128],
                        identity=identB[0:rows, 0:rows]),
                        reads=[b_src, b_identB], writes=[pbuf], signal=(j == 7))
                for j in range(8):
                    kc = half * 8 + j
                    eng = "dve" if half == 0 else "act"
                    if gsel is None:
                        if eng == "dve":
                            S.op("dve", lambda e, kc=kc, j=j, ptb=ptb: e.tensor_copy(
                                out=dstT[:, kc, col0:col0 + rows], in_=ptb[:, j * 128:j * 128 + rows]),
                                reads=[pbuf], writes=[b_dstT])
                        else:
                            S.op("act", lambda e, kc=kc, j=j, ptb=ptb: e.copy(
                                out=dstT[:, kc, col0:col0 + rows], in_=ptb[:, j * 128:j * 128 + rows]),
                                reads=[pbuf], writes=[b_dstT])
                    elif eng == "dve":
                        S.op("dve", lambda e, kc=kc, j=j, ptb=ptb: e.tensor_scalar(
                            out=dstT[:, kc, col0:col0 + rows], in0=ptb[:, j * 128:j * 128 + rows],
                            scalar1=gcol[:, gsel, kc:kc + 1], scalar2=None, op0=ALU.mult),
                            reads=[pbuf, b_gcol], writes=[b_dstT])
                    else:
                        S.op("act", lambda e, kc=kc, j=j, ptb=ptb: e.activation(
                            out=dstT[:, kc, col0:col0 + rows], in_=ptb[:, j * 128:j * 128 + rows],
                            func=AF.Copy, scale=gcol[:, gsel, kc:kc + 1]),
                            reads=[pbuf, b_gcol], writes=[b_dstT])

        STOP = int(os.environ.get('KSTOP', '99'))
        SUB = int(os.environ.get('KSUB', '99'))

        def phases():
            nonlocal xst, xnb
            if STOP < 0:
                return
            hT, b_hT, r_hT = sb("hT", [128, 16, NCOL], BF16)
            plan = []
            for g in range(3):
                plan += [w_in[:, C_K + g * 512:C_K + (g + 1) * 512], w_in[:, C_V + g * 512:C_V + (g + 1) * 512],
                         w_in[:, C_Q + g * 512:C_Q + (g + 1) * 512]]
            plan += [w_in[:, C_VA:C_VA + 512], w_in[:, C_VA + 512:C_VA + 1024]]
            plan += [w_in[:, C_UA:C_UA + 512], w_in[:, C_UA + 512:C_UA + 1024]]
            for cb in range(4):
                plan += [w_in[:, C_GA + cb * 512:C_GA + (cb + 1) * 512], w_a_out[:, cb * 512:(cb + 1) * 512],
                         w_in[:, C_GB + cb * 512:C_GB + (cb + 1) * 512], w_b_out[:, cb * 512:(cb + 1) * 512]]
            for cb in range(4):
                plan += [w_o[:, cb * 512:(cb + 1) * 512]]
            for cb in range(4):
                plan += [w_q[:, cb * 512:(cb + 1) * 512]]
            for cb in range(4):
                plan += [w_pg[:, cb * 512:(cb + 1) * 512]]
            w_plan(plan)
            w_issue_upto(NW)

            tiles = [(xloc[n * 128:(n + 1) * 128, :], 128, n * 128) for n in range(16)] + [(xs, NS, 2 * TOK)]

            def load_x(i):
                src, rows, _ = tiles[i]
                t, bf, _ = xst[i % 2]
                S.dma("sp", lambda e: e.dma_start(out=t[0:rows, :], in_=src), writes=[bf])

            load_x(0)
            for i, (src, rows, col0) in enumerate(tiles):
                if i + 1 < len(tiles):
                    load_x(i + 1)
                t, bf, _ = xst[i % 2]
                xn, bxn, _ = xnb[i % 2]
                rs = rms_stats(t[0:rows, :], rows, bf, i % 2)
                S.op("act", lambda e, t=t, xn=xn, rs=rs, rows=rows: e.activation(
                    out=xn[0:rows, :], in_=t[0:rows, :], func=AF.Copy, scale=rs), reads=[bf, b_stat], writes=[bxn])
                transpose_to(hT, b_hT, col0, xn, bxn, rows, 0)
            S.barrier()
            for r in (xst[0][2], xst[1][2], xnb[0][2], xnb[1][2], SQ["r"], r_grow):
                AR.release(r)

            if STOP < 1:
                return
            ACC, b_ACC, r_ACC = sb("ACC", [128, 2, 4, TOK], F32)
            KT, b_KT, r_KT = sb("KT", [128, 4, 2 * TOK], BF16)
            QT, b_QT, r_QT = sb("QT", [128, 4, TOK], BF16)
            VG, b_VG, r_VG = sb("VG", [128, 16, 512], BF16)
            kf = [sb("kf%d" % i, [128, 512], F32) for i in range(2)]
            kb = [sb("kb%d" % i, [128, 512], BF16) for i in range(2)]
            rtmp, b_rtmp, r_rtmp = sb("rtmp", [128, 2, 4, 32], F32)
            PT = [sb("PT%d" % i, [128, 2, 128], BF16) for i in range(2)]
            qkv_c, b_qkv_c, r_qkv_c = sb("qkv_c", [128, 2, 512], F32)
            stg = [sb("stg%d" % i, [NS, 512], F32) for i in range(2)]
            ropeG, b_ropeG, r_ropeG = sb("ropeG", [128, 16, 64], F32)
            ropeQ2, b_ropeQ2, r_ropeQ2 = sb("ropeQ2", [128, 8, 64], F32)
            S.dma("sp", lambda e: e.dma_start(out=ropeQ2[:], in_=rope[:, 48:56, :]), writes=[b_ropeQ2])
            cnt = {"kf": 0, "kb": 0, "pt": 0, "stg": 0}

            def stash_sample(pt, pbuf, which, g):
                blk = which * 3 + g
                if "stash" in SKIP:
                    return
                t, bt, _ = stg[cnt["stg"] % 2]; cnt["stg"] += 1
                S.op("act", lambda e: e.copy(out=t[:], in_=pt[0:NS, :]), reads=[pbuf], writes=[bt])
                j, slot = blk % 8, blk // 8
                S.dma("sp", lambda e: e.dma_start(out=qkv_c[16 * j:16 * j + 16, slot, :], in_=t[:]), reads=[bt], writes=[b_qkv_c])

            def rope_apply(t, bt, rows, tab_ap, b_tab):
                x4 = t[0:rows, :].rearrange("p (h d) -> p h d", h=4)
                cc = tab_ap[:, 0:32].unsqueeze(1).broadcast_to([rows, 4, 32])
                s1 = tab_ap[:, 32:48].unsqueeze(1).broadcast_to([rows, 4, 16])
                s2 = tab_ap[:, 48:64].unsqueeze(1).broadcast_to([rows, 4, 16])
                A = rtmp[0:rows, 0]
                B = rtmp[0:rows, 1]
                if "rope" in SKIP:
                    return
                S.op("dve", lambda e: e.tensor_tensor(out=A, in0=x4[:, :, 0:32], in1=cc, op=ALU.mult),
                     reads=[bt, b_tab], writes=[b_rtmp])
                S.op("dve", lambda e: e.tensor_tensor(out=B[:, :, 0:16], in0=x4[:, :, 16:32], in1=s1, op=ALU.mult),
                     reads=[bt, b_tab], writes=[b_rtmp])
                S.op("dve", lambda e: e.tensor_tensor(out=B[:, :, 16:32], in0=x4[:, :, 0:16], in1=s2, op=ALU.mult),
                     reads=[bt, b_tab], writes=[b_rtmp])
                S.op("dve", lambda e: e.tensor_tensor(out=x4[:, :, 0:32], in0=A, in1=B, op=ALU.add),
                     reads=[b_rtmp], writes=[bt])

            def gtile_cols(g, n):
                d = DIL[g]
                tpr = (2 * TOK // d) // 128
                r, i0 = n // tpr, (n % tpr) * 128
                start = i0 * d + r
                return slice(start, start + 127 * d + 1, d), r, i0

            first_group = True
            for g in range(3):
                d = DIL[g]
                L = 2 * TOK // d
                tpr = L // 128
                Lq = TOK // d
                if g == 0:
                    ktiles = list(range(7, 16))
                elif g == 1:
                    ktiles = [n for n in range(16) if n % 4 >= 1]
                else:
                    ktiles = list(range(16))
                S.dma("sp", lambda e, g=g: e.dma_start(out=ropeG[:], in_=rope[:, g * 16:(g + 1) * 16, :]), writes=[b_ropeG])
                wt, bw = w_next()
                for n in ktiles + ["s"]:
                    pt, pbuf = bank("A")
                    if n == "s":
                        rows = NS
                        lhs = lambda kc: hT[:, kc, 2 * TOK:2 * TOK + NS]
                    else:
                        rows = 128
                        sl, r, i0 = gtile_cols(g, n)
                        lhs = lambda kc, sl=sl: hT[:, kc, sl]
                    mm_group(pt[0:rows, :], pbuf, [(lhs(kc), wt[:, kc, :]) for kc in range(16)], [b_hT, bw])
                    if n == "s":
                        stash_sample(pt, pbuf, 1, g)
                        continue
                    t, bt, _ = kf[cnt["kf"] % 2]; cnt["kf"] += 1
                    S.op("act", lambda e, pt=pt, t=t: e.copy(out=t[:], in_=pt[:]), reads=[pbuf], writes=[bt])
                    rope_apply(t, bt, 128, ropeG[:, n, :], b_ropeG)
                    if "kvout" in SKIP:
                        pass
                    elif g == 0 and n == 15:
                        S.dma("sp", lambda e, t=t: e.dma_start(out=kv0[:, 0:512], in_=t[:]), reads=[bt], dbuf=outb)
                    elif g == 1 and n % 4 == 3:
                        S.dma("sp", lambda e, t=t, r=r: e.dma_start(out=kv1[r:512:4, 0:512], in_=t[:]), reads=[bt], dbuf=outb)
                    elif g == 2:
                        S.dma("sp", lambda e, t=t, r=r: e.dma_start(out=kv2[r:TOK:16, 0:512], in_=t[64:128, :]), reads=[bt], dbuf=outb)
                    tb, btb, _ = kb[cnt["kb"] % 2]; cnt["kb"] += 1
                    S.op("act", lambda e, t=t, tb=tb: e.copy(out=tb[:], in_=t[:]), reads=[bt], writes=[btb])
                    if "ktr" in SKIP:
                        continue
                    pt2, pbuf2 = bank("B")
                    ptb = pt2[:].bitcast(BF16)
                    for h in range(4):
                        S.op("pe", lambda e, h=h, ptb=ptb, tb=tb: e.transpose(out=ptb[:, h * 128:(h + 1) * 128],
                                                                        in_=tb[:, h * 128:(h + 1) * 128], identity=identB[:]),
                             reads=[btb, b_identB], writes=[pbuf2], signal=(h == 3))
                    S.op("dve", lambda e, ptb=ptb, n=n: e.tensor_copy(out=KT[:, :, n * 128:(n + 1) * 128],
                                                                 in_=ptb[:, 0:512].rearrange("p (h t) -> p h t", h=4)),
                         reads=[pbuf2], writes=[b_KT])
                if SUB < 0:
                    return
                wt, bw = w_next()
                for n in ktiles + ["s"]:
                    pt, pbuf = bank("A")
                    if n == "s":
                        mm_group(pt[0:NS, :], pbuf, [(hT[:, kc, 2 * TOK:2 * TOK + NS], wt[:, kc, :]) for kc in range(16)], [b_hT, bw])
                        stash_sample(pt, pbuf, 2, g)
                        continue
                    sl, r, i0 = gtile_cols(g, n)
                    mm_group(pt[:, :], pbuf, [(hT[:, kc, sl], wt[:, kc, :]) for kc in range(16)], [b_hT, bw])
                    own_out = ((g == 0 and n == 15) or (g == 1 and n % 4 == 3) or (g == 2)) and "kvout" not in SKIP
                    if own_out:
                        t, bt, _ = kf[cnt["kf"] % 2]; cnt["kf"] += 1
                        S.op("act", lambda e, pt=pt, t=t: e.copy(out=t[:], in_=pt[:]), reads=[pbuf], writes=[bt])
                        if g == 0:
                            S.dma("sp", lambda e, t=t: e.dma_start(out=kv0[:, 512:1024], in_=t[:]), reads=[bt], dbuf=outb)
                        elif g == 1:
                            S.dma("sp", lambda e, t=t, r=r: e.dma_start(out=kv1[r:512:4, 512:1024], in_=t[:]), reads=[bt], dbuf=outb)
                        else:
                            S.dma("sp", lambda e, t=t, r=r: e.dma_start(out=kv2[r:TOK:16, 512:1024], in_=t[64:128, :]),
                                  reads=[bt], dbuf=outb)
                    if own_out:
                        S.op("dve", lambda e, t=t, n=n: e.tensor_copy(out=VG[:, n, :], in_=t[:]), reads=[bt], writes=[b_VG])
                    else:
                        S.op("dve", lambda e, pt=pt, n=n: e.tensor_copy(out=VG[:, n, :], in_=pt[:]), reads=[pbuf], writes=[b_VG])
                wt, bw = w_next()
                if g == 0:
                    qtiles = [(n, gtile_cols(0, n)[0], ropeG[:, n, :], (n - 8) * 128) for n in range(8, 16)]
                elif g == 1:
                    qtiles = []
                    for n in range(16):
                        if n % 4 >= 2:
                            sl, r, i0 = gtile_cols(1, n)
                            qtiles.append((n, sl, ropeG[:, n, :], r * Lq + (i0 - Lq)))
                else:
                    qtiles = []
                    for r0 in range(0, 16, 2):
                        qtiles.append((r0, None, ropeQ2[:, r0 // 2, :], r0 * 64))
                for (n, sl, tab, qc0) in qtiles + [("s", None, None, None)]:
                    pt, pbuf = bank("A")
                    if n == "s":
                        mm_group(pt[0:NS, :], pbuf, [(hT[:, kc, 2 * TOK:2 * TOK + NS], wt[:, kc, :]) for kc in range(16)], [b_hT, bw])
                        stash_sample(pt, pbuf, 0, g)
                        continue
                    if g == 2:
                        for hf in range(2):
                            c0 = TOK + n + hf
                            mm_group(pt[hf * 64:(hf + 1) * 64, :], pbuf,
                                     [(hT[:, kc, c0:c0 + 63 * 16 + 1:16], wt[:, kc, :]) for kc in range(16)], [b_hT, bw])
                    else:
                        mm_group(pt[:, :], pbuf, [(hT[:, kc, sl], wt[:, kc, :]) for kc in range(16)], [b_hT, bw])
                    t, bt, _ = kf[cnt["kf"] % 2]; cnt["kf"] += 1
                    S.op("act", lambda e, pt=pt, t=t: e.copy(out=t[:], in_=pt[:]), reads=[pbuf], writes=[bt])
                    rope_apply(t, bt, 128, tab, b_ropeQ2 if g == 2 else b_ropeG)
                    tb, btb, _ = kb[cnt["kb"] % 2]; cnt["kb"] += 1
                    S.op("act", lambda e, t=t, tb=tb: e.copy(out=tb[:], in_=t[:]), reads=[bt], writes=[btb])
                    pt2, pbuf2 = bank("B")
                    ptb = pt2[:].bitcast(BF16)
                    for h in range(4):
                        S.op("pe", lambda e, h=h, ptb=ptb, tb=tb: e.transpose(out=ptb[:, h * 128:(h + 1) * 128],
                                                                        in_=tb[:, h * 128:(h + 1) * 128], identity=identB[:]),
                             reads=[btb, b_identB], writes=[pbuf2], signal=(h == 3))
                    S.op("dve", lambda e, ptb=ptb, qc0=qc0: e.tensor_copy(out=QT[:, :, qc0:qc0 + 128],
                                                                     in_=ptb[:, 0:512].rearrange("p (h t) -> p h t", h=4)),
                         reads=[pbuf2], writes=[b_QT])
                if SUB < 1 + 2 * g:
                    return
                TQ = 128 if g < 2 else 64
                for h in range(4):
                    for r in range(d):
                        for qt in range(Lq // TQ):
                            qc0 = r * Lq + qt * TQ
                            i0q = Lq + qt * TQ
                            if g < 2:
                                kprev = r * L + i0q - 128
                                blocks = [(kprev, 128, (kprev // 128), maskB[:, 2 if qt == 0 else 0, :]),
                                          (r * L + i0q, 128, (r * L + i0q) // 128, maskB[:, 1, :])]
                            else:
                                blocks = [(r * L, 128, r, maskB[:, 4, 0:64])]
                            nb = len(blocks)
                            ps_s, pb_s = bank("A")
                            for bi, (kc0, nk, vt, mk) in enumerate(blocks):
                                o = ps_s[0:nk, bi * 128:bi * 128 + TQ]
                                S.op("pe", lambda e, o=o, kc0=kc0, nk=nk, h=h, qc0=qc0: e.matmul(
                                    o, lhsT=KT[:, h, kc0:kc0 + nk], rhs=QT[:, h, qc0:qc0 + TQ], start=True, stop=False),
                                    reads=[b_KT, b_QT], writes=[pb_s], signal=False)
                                S.op("pe", lambda e, o=o, nk=nk, mk=mk: e.matmul(
                                    o, lhsT=identB[0:nk, 0:nk], rhs=mk, start=False, stop=True),
                                    reads=[b_identB, b_maskB], writes=[pb_s], signal=(bi == nb - 1))
                            p_t, b_p, _ = PT[cnt["pt"] % 2]; cnt["pt"] += 1
                            S.op("act", lambda e, ps_s=ps_s, p_t=p_t, nb=nb: e.activation(
                                out=p_t[:, 0:nb, 0:TQ], in_=ps_s[:].rearrange("p (b t) -> p b t", b=4)[:, 0:nb, 0:TQ],
                                func=AF.Exp, scale=SCALE), reads=[pb_s], writes=[b_p])
                            ps_o, pb_o = bank("B") if (cnt["pt"] % 2) else bank("C")
                            mm_group(ps_o[:, 0:TQ], pb_o, [(VG[:, vt, h * 128:(h + 1) * 128], p_t[:, bi, 0:TQ])
                                                          for bi, (kc0, nk, vt, mk) in enumerate(blocks)], [b_VG, b_p])
                            mm_group(ps_o[:, 128:128 + TQ], pb_o, [(onesB[:, :], p_t[:, bi, 0:TQ]) for bi in range(nb)],
                                     [b_onesB, b_p])
                            nat = slice(qt * TQ * d + r, qt * TQ * d + r + (TQ - 1) * d + 1, d)
                            src = ps_o[:].rearrange("p (b t) -> p b t", b=4)[:, 0:2, 0:TQ]
                            if first_group:
                                S.op("dve", lambda e, src=src, h=h, nat=nat: e.tensor_copy(out=ACC[:, :, h, nat], in_=src),
                                     reads=[pb_o], writes=[b_ACC])
                            else:
                                S.op("dve", lambda e, src=src, h=h, nat=nat: e.tensor_tensor(
                                    out=ACC[:, :, h, nat], in0=ACC[:, :, h, nat], in1=src, op=ALU.add),
                                    reads=[pb_o, b_ACC], writes=[b_ACC])
                first_group = False
                if SUB < 2 + 2 * g:
                    return
            S.barrier()
            for r in (r_KT, r_QT, r_VG, r_ropeG, r_ropeQ2, kf[0][2], kf[1][2], kb[0][2], kb[1][2], PT[0][2], PT[1][2]):
                AR.release(r)
            bmixT, b_bmixT, r_bmixT = sb("bmixT", [128, 4, TOK + NS], BF16)
            S.op("dve", lambda e: e.reciprocal(out=ACC[:, 1], in_=ACC[:, 1]), reads=[b_ACC], writes=[b_ACC])
            S.op("dve", lambda e: e.tensor_tensor(out=bmixT[:, :, 0:TOK], in0=ACC[:, 0], in1=ACC[:, 1], op=ALU.mult),
                 reads=[b_ACC], writes=[b_bmixT])
            qkv_s, b_qkv_s, r_qkv_s = sb("qkv_s", [NS, 3, 3, 512], F32)
            for which in range(3):
                for g in range(3):
                    blk = which * 3 + g
                    j, slot = blk % 8, blk // 8
                    S.dma("sp", lambda e, which=which, g=g, j=j, slot=slot: e.dma_start(
                        out=qkv_s[:, which, g, :], in_=qkv_c[16 * j:16 * j + 16, slot, :]), reads=[b_qkv_c], writes=[b_qkv_s])

            if SUB < 7:
                return
            for which in (0, 1):
                for g in range(3):
                    x4 = qkv_s[:, which, g, :].rearrange("p (h d) -> p h d", h=4)
                    cc = ropeS[:, 0:32].unsqueeze(1).broadcast_to([NS, 4, 32])
                    s1 = ropeS[:, 32:48].unsqueeze(1).broadcast_to([NS, 4, 16])
                    s2 = ropeS[:, 48:64].unsqueeze(1).broadcast_to([NS, 4, 16])
                    A = rtmp[0:NS, 0]
                    B = rtmp[0:NS, 1]
                    S.op("dve", lambda e, x4=x4, A=A, cc=cc: e.tensor_tensor(out=A, in0=x4[:, :, 0:32], in1=cc, op=ALU.mult),
                         reads=[b_qkv_s, b_ropeS], writes=[b_rtmp])
                    S.op("dve", lambda e, x4=x4, B=B, s1=s1: e.tensor_tensor(out=B[:, :, 0:16], in0=x4[:, :, 16:32], in1=s1, op=ALU.mult),
                         reads=[b_qkv_s, b_ropeS], writes=[b_rtmp])
                    S.op("dve", lambda e, x4=x4, B=B, s2=s2: e.tensor_tensor(out=B[:, :, 16:32], in0=x4[:, :, 0:16], in1=s2, op=ALU.mult),
                         reads=[b_qkv_s, b_ropeS], writes=[b_rtmp])
                    S.op("dve", lambda e, x4=x4, A=A, B=B: e.tensor_tensor(out=x4[:, :, 0:32], in0=A, in1=B, op=ALU.add),
                         reads=[b_rtmp], writes=[b_qkv_s])
            for g in range(3):
                S.dma("sp", lambda e, g=g: e.dma_start(out=kvs[g, :, 0:512], in_=qkv_s[:, 1, g, :]), reads=[b_qkv_s], dbuf=outb)
                S.dma("sp", lambda e, g=g: e.dma_start(out=kvs[g, :, 512:1024], in_=qkv_s[:, 2, g, :]), reads=[b_qkv_s], dbuf=outb)

            CK = [sb("CK%d" % i, [128, 1024], F32) for i in range(3)]
            SEL, b_SEL, r_SEL = sb("SEL", [NS, NS, 128], F32)
            SELT, b_SELT, r_SELT = sb("SELT", [128, NS, NS], F32)
            S.op("pool", lambda e: e.memset(SEL[:], 1.0), writes=[b_SEL])
            S.op("pool", lambda e: e.affine_select(out=SEL[:], in_=SEL[:], pattern=[[1, NS], [0, 128]], compare_op=ALU.is_equal,
                                                   fill=0.0, base=0, channel_multiplier=-1), reads=[b_SEL], writes=[b_SEL])
            S.op("pool", lambda e: e.memset(SELT[:], 1.0), writes=[b_SELT])
            S.op("pool", lambda e: e.affine_select(out=SELT[:], in_=SELT[:], pattern=[[1, NS], [-1, NS]], compare_op=ALU.is_equal,
                                                   fill=0.0, base=0, channel_multiplier=0), reads=[b_SELT], writes=[b_SELT])
            sprod, b_sprod, r_sprod = sb("sprod", [128, 512], F32)
            ssc, b_ssc, r_ssc = sb("ssc", [128, 8], F32)
            snew, b_snew, r_snew = sb("snew", [NS, 3, 512], F32)
            sn_s, b_sn_s, r_sn_s = sb("sn_s", [NS, 3, 8], F32)
            so, b_so, r_so = sb("so", [NS, 516], F32)
            sob, b_sob, r_sob = sb("sob", [NS, 512], BF16)
            ps_os, pb_os = PB[6]
            ps_ds, pb_ds = PB[7]
            S.op("dve", lambda e: e.tensor_tensor(out=snew[:], in0=qkv_s[:, 0], in1=qkv_s[:, 1], op=ALU.mult),
                 reads=[b_qkv_s], writes=[b_snew])
            S.op("dve", lambda e: e.tensor_reduce(out=sn_s[:, :, 0:4], in_=snew[:].rearrange("p g (h d) -> p g h d", h=4),
                                                  axis=AX.X, op=ALU.add), reads=[b_snew], writes=[b_sn_s])
            S.op("act", lambda e: e.activation(out=sn_s[:, :, 4:8], in_=sn_s[:, :, 0:4], func=AF.Exp, scale=SCALE),
                 reads=[b_sn_s], writes=[b_sn_s])
            S.op("dve", lambda e: e.tensor_tensor(
                out=snew[:].rearrange("p g (h d) -> p g h d", h=4), in0=qkv_s[:, 2].rearrange("p g (h d) -> p g h d", h=4),
                in1=sn_s[:, :, 4:8].unsqueeze(3).broadcast_to([NS, 3, 4, 128]), op=ALU.mult),
                reads=[b_qkv_s, b_sn_s], writes=[b_snew])
            k = 0
            for n in range(NS):
                for g in range(3):
                    ck, b_ck, _ = CK[k % 3]
                    S.dma("sp", lambda e, ck=ck, g=g, n=n: e.dma_start(out=ck[:], in_=cache[g, n]), writes=[b_ck])
                    pq, pbq = bank("A")
                    S.op("pe", lambda e, pq=pq, n=n, g=g: e.matmul(pq[:, :], lhsT=SEL[:, n, :], rhs=qkv_s[:, 0, g, :],
                                                               start=True, stop=True), reads=[b_SEL, b_qkv_s], writes=[pbq])
                    S.op("dve", lambda e, ck=ck, pq=pq: e.tensor_tensor(out=sprod[:], in0=ck[:, 0:512], in1=pq[:, :], op=ALU.mult),
                         reads=[b_ck, pbq], writes=[b_sprod])
                    S.op("dve", lambda e: e.tensor_reduce(out=ssc[:, 0:4], in_=sprod[:].rearrange("p (h d) -> p h d", h=4),
                                                          axis=AX.X, op=ALU.add), reads=[b_sprod], writes=[b_ssc])
                    S.op("act", lambda e: e.activation(out=ssc[:, 4:8], in_=ssc[:, 0:4], func=AF.Exp, scale=SCALE),
                         reads=[b_ssc], writes=[b_ssc])
                    S.op("dve", lambda e, ck=ck: e.tensor_tensor(
                        out=sprod[:].rearrange("p (h d) -> p h d", h=4), in0=ck[:, 512:1024].rearrange("p (h d) -> p h d", h=4),
                        in1=ssc[:, 4:8].unsqueeze(2).broadcast_to([128, 4, 128]), op=ALU.mult),
                        reads=[b_ck, b_ssc], writes=[b_sprod])
                    first, last = (k == 0), (k == NS * 3 - 1)
                    S.op("pe", lambda e, n=n, first=first, last=last: e.matmul(ps_os[0:NS, :], lhsT=SELT[:, n, :], rhs=sprod[:],
                                                                          start=first, stop=last),
                         reads=[b_SELT, b_sprod], writes=[pb_os], signal=True)
                    S.op("pe", lambda e, n=n, first=first, last=last: e.matmul(ps_ds[0:NS, 0:4], lhsT=SELT[:, n, :], rhs=ssc[:, 4:8],
                                                                          start=first, stop=last),
                         reads=[b_SELT, b_ssc], writes=[pb_ds], signal=True)
                    k += 1
            S.op("dve", lambda e: e.tensor_tensor(out=so[:, 0:512], in0=snew[:, 0, :], in1=snew[:, 1, :], op=ALU.add),
                 reads=[b_snew], writes=[b_so])
            S.op("dve", lambda e: e.tensor_tensor(out=so[:, 0:512], in0=so[:, 0:512], in1=snew[:, 2, :], op=ALU.add),
                 reads=[b_snew, b_so], writes=[b_so])
            S.op("dve", lambda e: e.tensor_tensor(out=so[:, 0:512], in0=so[:, 0:512], in1=ps_os[0:NS, :], op=ALU.add),
                 reads=[pb_os, b_so], writes=[b_so])
            S.op("dve", lambda e: e.tensor_tensor(out=so[:, 512:516], in0=sn_s[:, 0, 4:8], in1=sn_s[:, 1, 4:8], op=ALU.add),
                 reads=[b_sn_s], writes=[b_so])
            S.op("dve", lambda e: e.tensor_tensor(out=so[:, 512:516], in0=so[:, 512:516], in1=sn_s[:, 2, 4:8], op=ALU.add),
                 reads=[b_sn_s, b_so], writes=[b_so])
            S.op("dve", lambda e: e.tensor_tensor(out=so[:, 512:516], in0=so[:, 512:516], in1=ps_ds[0:NS, 0:4], op=ALU.add),
                 reads=[pb_ds, b_so], writes=[b_so])
            S.op("dve", lambda e: e.reciprocal(out=so[:, 512:516], in_=so[:, 512:516]), reads=[b_so], writes=[b_so])
            S.op("dve", lambda e: e.tensor_tensor(out=sob[:].rearrange("p (h d) -> p h d", h=4),
                                                  in0=so[:, 0:512].rearrange("p (h d) -> p h d", h=4),
                                                  in1=so[:, 512:516].unsqueeze(2).broadcast_to([NS, 4, 128]), op=ALU.mult),
                 reads=[b_so], writes=[b_sob])
            pt2, pbuf2 = bank("B")
            ptb = pt2[:].bitcast(BF16)
            for h in range(4):
                S.op("pe", lambda e, h=h, ptb=ptb: e.transpose(out=ptb[:, h * NS:(h + 1) * NS], in_=sob[:, h * 128:(h + 1) * 128],
                                                          identity=identB[0:NS, 0:NS]),
                     reads=[b_sob, b_identB], writes=[pbuf2], signal=(h == 3))
            S.op("dve", lambda e, ptb=ptb: e.tensor_copy(out=bmixT[:, :, TOK:TOK + NS],
                                                    in_=ptb[:, 0:4 * NS].rearrange("p (h t) -> p h t", h=4)),
                 reads=[pbuf2], writes=[b_bmixT])

            S.barrier()
            for r in (r_ACC, r_rtmp, r_qkv_s, r_qkv_c, stg[0][2], stg[1][2], r_SEL, r_SELT, r_sprod, r_ssc, r_snew, r_sn_s,
                      r_so, r_sob, CK[0][2], CK[1][2], CK[2][2]):
                AR.release(r)

            if STOP < 2:
                return
            amixT, b_amixT, r_amixT = sb("amixT", [128, 8, TOK + NS], BF16)
            vn, b_vn, r_vn = sb("vn", [128, 9, A_W], BF16)
            gv = [sb("gv%d" % i, [128, A_W], F32) for i in range(2)]
            lng, b_lng, r_lng = sb("lng", [128, A_W], F32)
            lnb, b_lnb, r_lnb = sb("lnb", [128, A_W], F32)
            S.dma("sp", lambda e: e.dma_start(out=lng[:], in_=lnv[0, :].partition_broadcast(128)), writes=[b_lng])
            S.dma("sp", lambda e: e.dma_start(out=lnb[:], in_=lnv[1, :].partition_broadcast(128)), writes=[b_lnb])
            bns, b_bns, r_bns = sb("bns", [128, 2, 8], F32)
            wsT, b_wsT, r_wsT = sb("wsT", [128, 8, 128], BF16)
            wsF, b_wsF, r_wsF = sb("wsF", [128, 8, 128], F32)
            bsb, b_bsb, r_bsb = sb("bsb", [128, 8, 128], F32)
            bs0, b_bs0, r_bs0 = sb("bs0", [128, 8], F32)
            ws00, b_ws00, r_ws00 = sb("ws00", [16, 8], F32)
            dg, b_dg, r_dg = sb("dg", [16, 8, 16], BF16)
            sgt, b_sgt, r_sgt = sb("sgt", [128, 512], F32)
            S.dma("sp", lambda e: e.dma_start(out=wsF[:], in_=w_s.rearrange("g t s -> t g s")), writes=[b_wsF])
            S.dma("sp", lambda e: e.dma_start(out=bsb[:], in_=b_s.rearrange("g t -> (g t)").partition_broadcast(128)
                                              .rearrange("p (g t) -> p g t", g=8)), writes=[b_bsb])
            S.dma("sp", lambda e: e.dma_start(out=ws00[:], in_=w_s[:, 0, 0].partition_broadcast(16),
                                              allow_slow_non_contiguous=True), writes=[b_ws00])
            wsM, b_wsM, r_wsM = sb("wsM", [128, 8, 128], F32)
            for half in range(2):
                pt, pbuf = bank("C")
                for j in range(4):
                    gi = half * 4 + j
                    S.op("pe", lambda e, gi=gi, j=j, pt=pt: e.transpose(out=pt[:, j * 128:(j + 1) * 128], in_=wsF[:, gi, :],
                                                                   identity=identF[:]),
                         reads=[b_wsF, b_identF], writes=[pbuf], signal=(j == 3))
                S.op("act", lambda e, half=half, pt=pt: e.copy(out=wsM[:, half * 4:half * 4 + 4, :],
                                                           in_=pt[:].rearrange("p (g t) -> p g t", g=4)),
                     reads=[pbuf], writes=[b_wsM])
            S.op("pool", lambda e: e.affine_select(out=wsT[:], in_=wsM[:], pattern=[[0, 8], [1, 128]],
                                                   compare_op=ALU.is_ge, fill=0.0, base=0, channel_multiplier=-1),
                 reads=[b_wsM], writes=[b_wsT])
            S.op("dve", lambda e: e.tensor_tensor(out=dg[:], in0=identF[0:16, 0:16].unsqueeze(1).broadcast_to([16, 8, 16]),
                                                  in1=ws00[:].unsqueeze(2).broadcast_to([16, 8, 16]), op=ALU.mult),
                 reads=[b_identF, b_ws00], writes=[b_dg])

            wva0, bwva0 = w_next()
            wva1, bwva1 = w_next(issue=False)
            own_tiles = [(TOK + n * 128, 128) for n in range(8)] + [(2 * TOK, NS)]
            for ti, (c0, rows) in enumerate(own_tiles):
                g_t, b_g, _ = gv[ti % 2]
                for blk, (wt, bw) in enumerate(((wva0, bwva0), (wva1, bwva1))):
                    pt, pbuf = bank("A")
                    mm_group(pt[0:rows, :], pbuf, [(hT[:, kc, c0:c0 + rows], wt[:, kc, :]) for kc in range(16)], [b_hT, bw])
                    S.op("act", lambda e, pt=pt, blk=blk, g_t=g_t, rows=rows: e.activation(
                        out=g_t[0:rows, blk * 512:(blk + 1) * 512], in_=pt[0:rows, :], func=AF.Gelu_apprx_tanh),
                        reads=[pbuf], writes=[b_g])
                for blk in range(2):
                    S.op("dve", lambda e, blk=blk, g_t=g_t, rows=rows: e.bn_stats(
                        out=bns[0:rows, blk, 0:6], in_=g_t[0:rows, blk * 512:(blk + 1) * 512]), reads=[b_g], writes=[b_bns])
                mv = bns[0:rows, 0, 6:8]
                S.op("dve", lambda e, rows=rows, mv=mv: e.bn_aggr(out=mv, in_=bns[0:rows, :, 0:6]), reads=[b_bns], writes=[b_bns])
                sd = bns[0:rows, 1, 6:7]
                rsd = bns[0:rows, 1, 7:8]
                S.op("dve", lambda e, rows=rows, sd=sd: e.tensor_scalar(out=sd, in0=bns[0:rows, 0, 7:8], scalar1=EPS, scalar2=None,
                                                                     op0=ALU.add), reads=[b_bns], writes=[b_bns])
                S.op("act", lambda e, sd=sd: e.sqrt(out=sd, in_=sd), reads=[b_bns], writes=[b_bns])
                S.op("dve", lambda e, sd=sd, rsd=rsd: e.reciprocal(out=rsd, in_=sd), reads=[b_bns], writes=[b_bns])
                S.op("dve", lambda e, g_t=g_t, rows=rows, rsd=rsd: e.tensor_scalar(
                    out=g_t[0:rows, :], in0=g_t[0:rows, :], scalar1=bns[0:rows, 0, 6:7], scalar2=rsd,
                    op0=ALU.subtract, op1=ALU.mult), reads=[b_g, b_bns], writes=[b_g])
                S.op("dve", lambda e, g_t=g_t, rows=rows: e.tensor_tensor(out=g_t[0:rows, :], in0=g_t[0:rows, :], in1=lng[0:rows, :],
                                                                      op=ALU.mult), reads=[b_g, b_lng], writes=[b_g])
                if rows == 128:
                    S.op("dve", lambda e, g_t=g_t, ti=ti: e.tensor_tensor(out=vn[:, ti, :], in0=g_t[:], in1=lnb[:], op=ALU.add),
                         reads=[b_g, b_lnb], writes=[b_vn])
                else:
                    S.op("dve", lambda e, g_t=g_t, rows=rows: e.tensor_tensor(out=g_t[0:rows, :], in0=g_t[0:rows, :],
                                                                          in1=lnb[0:rows, :], op=ALU.add),
                         reads=[b_g, b_lnb], writes=[b_g])
                    S.dma("sp", lambda e, g_t=g_t, rows=rows: e.dma_start(out=sguv, in_=g_t[0:rows, :]), reads=[b_g], dbuf=outb)
                    S.op("act", lambda e, g_t=g_t, rows=rows, ti=ti: e.copy(out=vn[0:rows, ti, :], in_=g_t[0:rows, :]),
                         reads=[b_g], writes=[b_vn])

            for blk in range(2):
                wt, bw = w_next()
                for jj in range(4):
                    j = blk * 4 + jj
                    for (c0, wd) in PASSES:
                        pt, pbuf = bank("A")
                        mm_group(pt[:, 0:wd], pbuf, [(wt[:, kc, jj * 128:(jj + 1) * 128], hT[:, kc, c0:c0 + wd]) for kc in range(16)],
                                 [b_hT, bw])
                        S.op("act", lambda e, pt=pt, j=j, c0=c0, wd=wd: e.activation(
                            out=amixT[:, j, c0 - TOK:c0 - TOK + wd], in_=pt[:, 0:wd], func=AF.Gelu_apprx_tanh),
                            reads=[pbuf], writes=[b_amixT])

            for tt in range(8):
                for half in range(2):
                    pt, pbuf = bank("A")
                    for jj in range(4):
                        gi = half * 4 + jj
                        S.op("pe", lambda e, pt=pt, jj=jj, gi=gi, tt=tt: e.matmul(
                            pt[:, jj * 128:(jj + 1) * 128], lhsT=vn[:, tt, gi * 128:(gi + 1) * 128], rhs=wsT[:, gi, :],
                            start=True, stop=True), reads=[b_vn, b_wsT], writes=[pbuf], signal=(jj == 3))
                    S.op("dve", lambda e, pt=pt, half=half: e.tensor_tensor(
                        out=sgt[:].rearrange("p (g t) -> p g t", g=4), in0=pt[:].rearrange("p (g t) -> p g t", g=4),
                        in1=bsb[:, half * 4:half * 4 + 4, :], op=ALU.add), reads=[pbuf, b_bsb], writes=[b_sgt])
                    S.op("dve", lambda e, half=half, tt=tt: e.tensor_tensor(
                        out=amixT[:, half * 4:half * 4 + 4, tt * 128:(tt + 1) * 128],
                        in0=amixT[:, half * 4:half * 4 + 4, tt * 128:(tt + 1) * 128],
                        in1=sgt[:].rearrange("p (g t) -> p g t", g=4), op=ALU.mult), reads=[b_sgt, b_amixT], writes=[b_amixT])
            pt, pbuf = bank("A")
            for gi in range(8):
                S.op("pe", lambda e, pt=pt, gi=gi: e.matmul(pt[:, gi * NS:(gi + 1) * NS], lhsT=vn[0:NS, 8, gi * 128:(gi + 1) * 128],
                                                       rhs=dg[:, gi, :], start=True, stop=True),
                     reads=[b_vn, b_dg], writes=[pbuf], signal=(gi == 7))
            S.op("dve", lambda e, pt=pt: e.tensor_tensor(
                out=sgt[:, 0:128].rearrange("p (g t) -> p g t", g=8), in0=pt[:, 0:128].rearrange("p (g t) -> p g t", g=8),
                in1=bsb[:, :, 0:1].broadcast_to([128, 8, NS]), op=ALU.add), reads=[pbuf, b_bsb], writes=[b_sgt])
            S.op("dve", lambda e: e.tensor_tensor(out=amixT[:, :, TOK:TOK + NS], in0=amixT[:, :, TOK:TOK + NS],
                                                  in1=sgt[:, 0:128].rearrange("p (g t) -> p g t", g=8), op=ALU.mult),
                 reads=[b_sgt, b_amixT], writes=[b_amixT])

            S.barrier()
            for r in (r_vn, gv[0][2], gv[1][2], r_lng, r_lnb, r_bns, r_wsT, r_wsF, r_bsb, r_bs0, r_ws00, r_dg, r_sgt, r_wsM):
                AR.release(r)

            if STOP < 3:
                return
            mergedT, b_mergedT, r_mergedT = sb("mergedT", [128, 16, TOK + NS], BF16, top=True)
            sgA, b_sgA, r_sgA = sb("sgA", [128, 4, TOK + NS], BF16)
            sgB, b_sgB = sgA, b_sgA
            M1, b_M1, r_M1 = sb("M1", [128, 4, TOK + NS], F32)
            mtmp, b_mtmp, r_mtmp = sb("mtmp", [128, 512], F32)
            def gate_block():
                wt, bw = w_next()
                for jj in range(4):
                    for (c0, wd) in PASSES:
                        pt, pbuf = bank("A")
                        mm_group(pt[:, 0:wd], pbuf, [(wt[:, kc, jj * 128:(jj + 1) * 128], hT[:, kc, c0:c0 + wd]) for kc in range(16)],
                                 [b_hT, bw])
                        S.op("act", lambda e, pt=pt, jj=jj, c0=c0, wd=wd: e.activation(
                            out=sgA[:, jj, c0 - TOK:c0 - TOK + wd], in_=pt[:, 0:wd], func=AF.Sigmoid),
                            reads=[pbuf], writes=[b_sgA])

            for cb in range(4):
                gate_block()
                wt, bw = w_next()
                for jj in range(4):
                    for (c0, wd) in PASSES:
                        pt, pbuf = bank("A")
                        mm_group(pt[:, 0:wd], pbuf, [(wt[:, kc, jj * 128:(jj + 1) * 128], amixT[:, kc, c0 - TOK:c0 - TOK + wd])
                                                     for kc in range(8)], [b_amixT, bw])
                        S.op("dve", lambda e, pt=pt, jj=jj, c0=c0, wd=wd: e.tensor_tensor(
                            out=M1[:, jj, c0 - TOK:c0 - TOK + wd], in0=pt[:, 0:wd], in1=sgA[:, jj, c0 - TOK:c0 - TOK + wd], op=ALU.mult),
                            reads=[pbuf, b_sgA], writes=[b_M1])
                gate_block()
                wt, bw = w_next()
                for jj in range(4):
                    for (c0, wd) in PASSES:
                        pt, pbuf = bank("A")
                        mm_group(pt[:, 0:wd], pbuf, [(wt[:, kc, jj * 128:(jj + 1) * 128], bmixT[:, kc, c0 - TOK:c0 - TOK + wd])
                                                     for kc in range(4)], [b_bmixT, bw])
                        S.op("dve", lambda e, pt=pt, jj=jj, c0=c0, wd=wd: e.tensor_tensor(
                            out=mtmp[:, 0:wd], in0=pt[:, 0:wd], in1=sgB[:, jj, c0 - TOK:c0 - TOK + wd], op=ALU.mult),
                            reads=[pbuf, b_sgB], writes=[b_mtmp])
                        S.op("dve", lambda e, cb=cb, jj=jj, c0=c0, wd=wd: e.tensor_tensor(
                            out=mergedT[:, cb * 4 + jj, c0 - TOK:c0 - TOK + wd], in0=mtmp[:, 0:wd],
                            in1=M1[:, jj, c0 - TOK:c0 - TOK + wd], op=ALU.add),
                            reads=[b_mtmp, b_M1], writes=[b_mergedT])

            S.barrier()
            for r in (r_hT, r_amixT, r_bmixT, r_sgA, r_M1, r_mtmp):
                AR.release(r)

            if STOP < 4:
                return
            X2, b_X2, r_X2 = sb("X2", [128, 9, D], F32)
            bX2 = [Buf("X2_%d" % i) for i in range(9)]
            xpc = [sb("xpc%d" % i, [128, 512], F32) for i in range(3)]
            rows_of = [128] * 8 + [NS]
            k = 0

            def load_xpiece(k):
                cb, tt = divmod(k, 9)
                t, bf, _ = xpc[k % 3]
                rows = rows_of[tt]
                src = xloc[TOK + tt * 128:TOK + (tt + 1) * 128, cb * 512:(cb + 1) * 512] if tt < 8 else xs[:, cb * 512:(cb + 1) * 512]
                S.dma("sp", lambda e: e.dma_start(out=t[0:rows, :], in_=src), writes=[bf])

            load_xpiece(0); load_xpiece(1)
            for cb in range(4):
                wt, bw = w_next()
                for tt in range(9):
                    if k + 2 < 36:
                        load_xpiece(k + 2)
                    rows = rows_of[tt]
                    t, bf, _ = xpc[k % 3]
                    pt, pbuf = bank("A")
                    mm_group(pt[0:rows, :], pbuf, [(mergedT[:, kc, tt * 128:tt * 128 + rows], wt[:, kc, :]) for kc in range(16)],
                             [b_mergedT, bw])
                    S.op("dve", lambda e, pt=pt, t=t, tt=tt, cb=cb, rows=rows: e.tensor_tensor(
                        out=X2[0:rows, tt, cb * 512:(cb + 1) * 512], in0=t[0:rows, :], in1=pt[0:rows, :], op=ALU.add),
                        reads=[pbuf, bf], writes=[bX2[tt]])
                    k += 1
            S.barrier()
            AR.release(r_mergedT)
            for i in range(3):
                AR.release(xpc[i][2])

            if STOP < 5:
                return
            h2T, b_h2T, r_h2T = sb("h2T", [128, 16, TOK + NS], BF16)
            rstd2, b_rstd2, _ = sb("rstd2", [128, 9], F32)
            xnb = [sb("xnb%d" % i, [128, D], BF16) for i in range(2)]
            SQ["t"], SQ["b"], SQ["r"] = sb("sqjunk", [128, D], BF16)
            for tt in range(9):
                rows = rows_of[tt]
                rs = rms_stats(X2[0:rows, tt, :], rows, bX2[tt], tt % 2)
                S.op("dve", lambda e, rs=rs, tt=tt, rows=rows: e.tensor_copy(out=rstd2[0:rows, tt:tt + 1], in_=rs),
                     reads=[b_stat], writes=[b_rstd2])
                xn, bxn, _ = xnb[tt % 2]
                S.op("act", lambda e, xn=xn, rs=rs, tt=tt, rows=rows: e.activation(
                    out=xn[0:rows, :], in_=X2[0:rows, tt, :], func=AF.Copy, scale=rs), reads=[bX2[tt], b_stat], writes=[bxn])
                transpose_to(h2T, b_h2T, tt * 128, xn, bxn, rows, 1)

            if STOP < 6:
                return
            skF, b_skF, r_skF = sb("skF", [128, 2, 128], F32)
            skT, b_skT, _ = sb("skT", [128, 2, 128], BF16)
            S.dma("sp", lambda e: e.dma_start(out=skF[:], in_=subk.rearrange("j k d -> k j d")), writes=[b_skF])
            pt, pbuf = bank("C")
            for j in range(2):
                S.op("pe", lambda e, j=j, pt=pt: e.transpose(out=pt[:, j * 128:(j + 1) * 128], in_=skF[:, j, :], identity=identF[:]),
                     reads=[b_skF, b_identF], writes=[pbuf], signal=(j == 1))
            S.op("act", lambda e, pt=pt: e.copy(out=skT[:], in_=pt[:, 0:256].rearrange("p (j k) -> p j k", j=2)),
                 reads=[pbuf], writes=[b_skT])
            T1, b_T1, r_T1 = sb("T1", [128, 9, 16, 16], F32)
            I1, b_I1, r_I1 = sb("I1", [128, 9, 16, 16], U32)
            qc, b_qc, r_qc = sb("qc", [128, TOK + NS], BF16)
            scs = [sb("scs%d" % i, [128, 128], F32) for i in range(2)]
            sc2, b_sc2, r_sc2 = sb("sc2", [128, 128], F32)
            k = 0
            for cb in range(4):
                wt, bw = w_next()
                for jj in range(4):
                    c = cb * 4 + jj
                    for (c0, wd) in PASSES:
                        pt, pbuf = bank("A")
                        mm_group(pt[:, 0:wd], pbuf, [(wt[:, kc, jj * 128:(jj + 1) * 128], h2T[:, kc, c0 - TOK:c0 - TOK + wd])
                                                     for kc in range(16)], [b_h2T, bw])
                        S.op("act", lambda e, pt=pt, c0=c0, wd=wd: e.copy(out=qc[:, c0 - TOK:c0 - TOK + wd], in_=pt[:, 0:wd]),
                             reads=[pbuf], writes=[b_qc])
                    for tt in range(9):
                        rows = rows_of[tt]
                        pt, pbuf = bank("B") if tt % 2 else bank("C")
                        S.op("pe", lambda e, pt=pt, tt=tt, rows=rows, c=c: e.matmul(
                            pt[0:rows, 0:128], lhsT=qc[:, tt * 128:tt * 128 + rows], rhs=skT[:, c % 2, :], start=True, stop=True),
                            reads=[b_qc, b_skT], writes=[pbuf])
                        sc, b_sc, _ = scs[k % 2]; k += 1
                        S.op("act", lambda e, pt=pt, sc=sc, rows=rows: e.copy(out=sc[0:rows, :], in_=pt[0:rows, 0:128]),
                             reads=[pbuf], writes=[b_sc])
                        S.op("dve", lambda e, sc=sc, rows=rows, tt=tt, c=c: e.max(out=T1[0:rows, tt, c, 0:8], in_=sc[0:rows, :]),
                             reads=[b_sc], writes=[b_T1])
                        S.op("dve", lambda e, sc=sc, rows=rows, tt=tt, c=c: e.max_index(
                            out=I1[0:rows, tt, c, 0:8], in_max=T1[0:rows, tt, c, 0:8], in_values=sc[0:rows, :]),
                            reads=[b_sc, b_T1], writes=[b_I1])
                        S.op("dve", lambda e, sc=sc, rows=rows, tt=tt, c=c: e.match_replace(
                            out=sc2[0:rows, :], in_to_replace=T1[0:rows, tt, c, 0:8], in_values=sc[0:rows, :], imm_value=-3.0e38),
                            reads=[b_sc, b_T1], writes=[b_sc2])
                        S.op("dve", lambda e, rows=rows, tt=tt, c=c: e.max(out=T1[0:rows, tt, c, 8:16], in_=sc2[0:rows, :]),
                             reads=[b_sc2], writes=[b_T1])
                        S.op("dve", lambda e, rows=rows, tt=tt, c=c: e.max_index(
                            out=I1[0:rows, tt, c, 8:16], in_max=T1[0:rows, tt, c, 8:16], in_values=sc2[0:rows, :]),
                            reads=[b_sc2, b_T1], writes=[b_I1])
            S.barrier()
            for r in (r_skF, r_qc, scs[0][2], scs[1][2], r_sc2, r_h2T, xnb[0][2], xnb[1][2], SQ["r"]):
                AR.release(r)

            if STOP < 7:
                return
            tab_issue(len(tab_jobs))
            cand, b_cand, r_cand = sb("cand", [128, 8, 256], F32)
            cand2, b_cand2, r_cand2 = sb("cand2", [128, 256], F32)
            ts, b_ts, r_ts = sb("ts", [128, 8, 16], F32)
            ic, b_ic, r_ic = sb("ic", [128, 8, 16], U32)
            icw, b_icw, r_icw = sb("icw", [128, 2, 8, 16], U32)
            icf, b_icf, r_icf = sb("icf", [128, 2, 8, 16], F32)
            i1f, b_i1f, r_i1f = sb("i1f", [128, 16, 16], F32)
            iota16, b_iota16, r_iota16 = sb("iota16", [128, 16], F32)
            oh, b_oh = cand[:].rearrange("p h (a b) -> p h a b", a=16), b_cand
            ef, b_ef, r_ef = sb("ef", [128, 2, 8, 16], F32)
            gs2, b_gs2, r_gs2 = sb("gs2", [128, 16], F32)
            eidx, b_eidx, _ = sb("eidx", [128, 9, 128], I32, top=True)
            gsm, b_gsm, _ = sb("gsm", [128, 9, 128], F32, top=True)
            S.op("pool", lambda e: e.iota(iota16[:], pattern=[[1, 16]], base=0, channel_multiplier=0,
                                          allow_small_or_imprecise_dtypes=True), writes=[b_iota16])
            S.op("pool", lambda e: e.memset(eidx[:], 0), writes=[b_eidx])
            for tt in range(9):
                rows = rows_of[tt]
                T = T1[0:rows, tt].rearrange("p (h j) k -> p h j k", j=2)
                G = gsm[0:rows, tt, :].rearrange("p (h k) -> p h k", h=8)
                S.op("dve", lambda e, T=T, rows=rows: e.tensor_tensor(
                    out=cand[0:rows].rearrange("p h (a b) -> p h a b", a=16),
                    in0=T[:, :, 0, :].unsqueeze(3).broadcast_to([rows, 8, 16, 16]),
                    in1=T[:, :, 1, :].unsqueeze(2).broadcast_to([rows, 8, 16, 16]), op=ALU.add),
                    reads=[b_T1], writes=[b_cand])
                for h in range(8):
                    S.op("dve", lambda e, h=h, rows=rows: e.max(out=ts[0:rows, h, 0:8], in_=cand[0:rows, h, :]),
                         reads=[b_cand], writes=[b_ts])
                    S.op("dve", lambda e, h=h, rows=rows: e.max_index(out=ic[0:rows, h, 0:8], in_max=ts[0:rows, h, 0:8],
                                                                   in_values=cand[0:rows, h, :]),
                         reads=[b_cand, b_ts], writes=[b_ic])
                    S.op("dve", lambda e, h=h, rows=rows: e.match_replace(out=cand2[0:rows, :], in_to_replace=ts[0:rows, h, 0:8],
                                                                       in_values=cand[0:rows, h, :], imm_value=-3.0e38),
                         reads=[b_cand, b_ts], writes=[b_cand2])
                    S.op("dve", lambda e, h=h, rows=rows: e.max(out=ts[0:rows, h, 8:16], in_=cand2[0:rows, :]),
                         reads=[b_cand2], writes=[b_ts])
                    S.op("dve", lambda e, h=h, rows=rows: e.max_index(out=ic[0:rows, h, 8:16], in_max=ts[0:rows, h, 8:16],
                                                                   in_values=cand2[0:rows, :]),
                         reads=[b_cand2, b_ts], writes=[b_ic])
                S.op("dve", lambda e, rows=rows: e.tensor_single_scalar(out=icw[0:rows, 0], in_=ic[0:rows], scalar=c4[0:rows, 0:1],
                                                                       op=ALU.logical_shift_right), reads=[b_ic, b_c4], writes=[b_icw])
                S.op("dve", lambda e, rows=rows: e.tensor_single_scalar(out=icw[0:rows, 1], in_=ic[0:rows], scalar=c4[0:rows, 1:2],
                                                                       op=ALU.bitwise_and), reads=[b_ic, b_c4], writes=[b_icw])
                S.op("dve", lambda e, rows=rows: e.tensor_copy(out=icf[0:rows], in_=icw[0:rows]), reads=[b_icw], writes=[b_icf])
                S.op("dve", lambda e, rows=rows, tt=tt: e.tensor_copy(out=i1f[0:rows], in_=I1[0:rows, tt]), reads=[b_I1], writes=[b_i1f])
                I = i1f[0:rows].rearrange("p (h j) k -> p h j k", j=2)
                for side in range(2):
                    S.op("dve", lambda e, side=side, rows=rows: e.tensor_tensor(
                        out=oh[0:rows], in0=icf[0:rows, side].unsqueeze(3).broadcast_to([rows, 8, 16, 16]),
                        in1=iota16[0:rows].unsqueeze(1).unsqueeze(1).broadcast_to([rows, 8, 16, 16]), op=ALU.is_equal),
                        reads=[b_icf, b_iota16], writes=[b_oh])
                    S.op("dve", lambda e, side=side, rows=rows, I=I: e.tensor_tensor(
                        out=oh[0:rows], in0=oh[0:rows], in1=I[:, :, side, :].unsqueeze(2).broadcast_to([rows, 8, 16, 16]),
                        op=ALU.mult), reads=[b_oh, b_i1f], writes=[b_oh])
                    S.op("dve", lambda e, side=side, rows=rows: e.tensor_reduce(out=ef[0:rows, side], in_=oh[0:rows], axis=AX.X,
                                                                             op=ALU.add), reads=[b_oh], writes=[b_ef])
                S.op("dve", lambda e, rows=rows: e.scalar_tensor_tensor(
                    out=ef[0:rows, 0], in0=ef[0:rows, 0], scalar=128.0, in1=ef[0:rows, 1], op0=ALU.mult, op1=ALU.add),
                    reads=[b_ef], writes=[b_ef])
                S.op("dve", lambda e, rows=rows, tt=tt: e.tensor_copy(out=eidx[0:rows, tt, :].rearrange("p (h k) -> p h k", h=8),
                                                                   in_=ef[0:rows, 0]), reads=[b_ef], writes=[b_eidx])
                S.op("dve", lambda e, rows=rows, G=G: e.tensor_tensor(
                    out=G, in0=ts[0:rows], in1=ts[0:rows, :, 0:1].broadcast_to([rows, 8, 16]), op=ALU.subtract),
                    reads=[b_ts], writes=[b_gsm])
                S.op("act", lambda e, G=G: e.activation(out=G, in_=G, func=AF.Exp), reads=[b_gsm], writes=[b_gsm])
                S.op("dve", lambda e, rows=rows, G=G: e.tensor_reduce(out=gs2[0:rows, 0:8], in_=G, axis=AX.X, op=ALU.add),
                     reads=[b_gsm], writes=[b_gs2])
                S.op("dve", lambda e, rows=rows: e.reciprocal(out=gs2[0:rows, 8:16], in_=gs2[0:rows, 0:8]), reads=[b_gs2], writes=[b_gs2])
                S.op("dve", lambda e, rows=rows, G=G: e.tensor_tensor(
                    out=G, in0=G, in1=gs2[0:rows, 8:16].unsqueeze(2).broadcast_to([rows, 8, 16]), op=ALU.mult),
                    reads=[b_gsm, b_gs2], writes=[b_gsm])
            S.barrier()
            for r in (r_cand, r_cand2, r_ts, r_ic, r_icw, r_icf, r_i1f, r_iota16, r_ef, r_gs2, r_T1, r_I1):
                AR.release(r)

            gffn_b, b_gffn_b, r_gffn_b = sb("gffn_b", [128, D], F32)
            S.dma("sp", lambda e: e.dma_start(out=gffn_b[:], in_=gvec[1, :].partition_broadcast(128)), writes=[b_gffn_b])
            actp, _, r_actp = sb("actp", [128, 128], F32)
            coef, b_coef, r_coef = sb("coef", [128, 2, 128], F32)
            b_actps = [Buf("actp%d" % i) for i in range(4)]
            h2t, b_h2t, r_h2t = sb("h2t", [128, D], F32)
            NG = 6
            LOOK = NG - 2
            ug = [sb("ug%d" % i, [128, 2, D], BF16) for i in range(NG)]
            dgt = [sb("dgt%d" % i, [128, 128], BF16) for i in range(4)]
            pc_rows = pc16.rearrange("e j d -> e (j d)")
            jobs = [(tt, sidx) for tt in range(9) for sidx in range(128)]
            gstate = {"issued": 0, "dg": 0}
            gbuf = {}

            def g_issue_upto(n):
                while gstate["issued"] < min(n, len(jobs)):
                    ji = gstate["issued"]
                    tt, sidx = jobs[ji]
                    t, bf, _ = ug[ji % NG]
                    S.dma("pool", lambda e, t=t, tt=tt, sidx=sidx: e.indirect_dma_start(
                        out=t[:].rearrange("p j d -> p (j d)"), out_offset=None, in_=pc_rows,
                        in_offset=bass.IndirectOffsetOnAxis(ap=eidx[:, tt, sidx:sidx + 1], axis=0)),
                        reads=[b_eidx, b_tab], writes=[bf])
                    gbuf[ji] = (t, bf)
                    gstate["issued"] += 1

            for ji, (tt, sidx) in enumerate(jobs):
                rows = rows_of[tt]
                if sidx == 0:
                    S.op("dve", lambda e, rows=rows, tt=tt: e.scalar_tensor_tensor(
                        out=h2t[0:rows, :], in0=X2[0:rows, tt, :], scalar=rstd2[0:rows, tt:tt + 1], in1=gffn_b[0:rows, :],
                        op0=ALU.mult, op1=ALU.mult), reads=[bX2[tt], b_rstd2, b_gffn_b], writes=[b_h2t])
                g_issue_upto(ji + 1 + LOOK)
                t, bf = gbuf.pop(ji)
                b_ap = b_actps[sidx % 4]
                S.op("dve", lambda e, t=t, sidx=sidx, rows=rows: e.scalar_tensor_tensor(
                    out=t[0:rows, 0, :], in0=t[0:rows, 0, :], scalar=1.0, in1=h2t[0:rows, :], op0=ALU.mult, op1=ALU.mult,
                    accum_out=actp[0:rows, sidx:sidx + 1]), reads=[b_h2t], writes=[bf, b_ap])
                S.op("act", lambda e, rows=rows, sidx=sidx: e.activation(out=coef[0:rows, 0, sidx:sidx + 1], in_=actp[0:rows, sidx:sidx + 1],
                                                                    func=AF.Gelu_apprx_tanh), reads=[b_ap], writes=[b_coef])
                S.op("act", lambda e, rows=rows, sidx=sidx, tt=tt: e.activation(
                    out=coef[0:rows, 1, sidx:sidx + 1], in_=coef[0:rows, 0, sidx:sidx + 1], func=AF.Copy,
                    scale=gsm[0:rows, tt, sidx:sidx + 1]), reads=[b_coef, b_gsm], writes=[b_coef])
                dg_t, b_dg, _ = dgt[gstate["dg"] % 4]; gstate["dg"] += 1
                S.op("act", lambda e, dg_t=dg_t, rows=rows, sidx=sidx: e.activation(
                    out=dg_t[0:rows, 0:rows], in_=identB[0:rows, 0:rows], func=AF.Copy, scale=coef[0:rows, 1, sidx:sidx + 1]),
                    reads=[b_identB, b_coef], writes=[b_dg])
                first, last = (sidx == 0), (sidx == 127)
                for c in range(4):
                    pt, pbuf = PB[(tt % 2) * 4 + c]
                    S.op("pe", lambda e, pt=pt, c=c, t=t, dg_t=dg_t, rows=rows, first=first, last=last: e.matmul(
                        pt[0:rows, :], lhsT=dg_t[0:rows, 0:rows], rhs=t[0:rows, 1, c * 512:(c + 1) * 512], start=first, stop=last),
                        reads=[b_dg, bf], writes=[pbuf], signal=(c == 3))
                if last:
                    for c in range(4):
                        pt, pbuf = PB[(tt % 2) * 4 + c]
                        S.op("dve", lambda e, pt=pt, c=c, rows=rows, tt=tt: e.tensor_tensor(
                            out=X2[0:rows, tt, c * 512:(c + 1) * 512], in0=X2[0:rows, tt, c * 512:(c + 1) * 512],
                            in1=pt[0:rows, :], op=ALU.add), reads=[pbuf, bX2[tt]], writes=[bX2[tt]])

            if STOP < 8:
                return
            S.barrier()
            for r in [r_gffn_b, r_h2t, r_actp, r_coef] + [u[2] for u in ug] + [d_[2] for d_ in dgt]:
                AR.release(r)
            x3T, b_x3T, r_x3T = sb("x3T", [128, 16, TOK + NS], BF16)
            xnb = [sb("xnb%d" % i, [128, D], BF16) for i in range(2)]
            pT_, b_pT, _ = sb("pT", [128, 2, TOK + NS], BF16)
            pst, b_pst, _ = sb("pst", [128, 256], F32)
            psb, b_psb, _ = sb("psb", [128, 256], BF16)
            wpl, b_wpl, _ = sb("wpl", [128, 2, D], BF16)
            S.dma("pool", lambda e: e.dma_start(out=wpl[:], in_=w_ple.rearrange("(k p) c -> p k c", p=128)), writes=[b_wpl])
            for tt in range(9):
                rows = rows_of[tt]
                xn, bxn, _ = xnb[tt % 2]
                S.op("act", lambda e, xn=xn, tt=tt, rows=rows: e.copy(out=xn[0:rows, :], in_=X2[0:rows, tt, :]),
                     reads=[bX2[tt]], writes=[bxn])
                transpose_to(x3T, b_x3T, tt * 128, xn, bxn, rows, None)
                src = ploc[tt * 128:(tt + 1) * 128, :] if tt < 8 else psm
                S.dma("sp", lambda e, src=src, rows=rows: e.dma_start(out=pst[0:rows, 0:256], in_=src), writes=[b_pst])
                psbv = psb
                S.op("act", lambda e, rows=rows, psbv=psbv: e.copy(out=psbv[0:rows, 0:256], in_=pst[0:rows, 0:256]),
                     reads=[b_pst], writes=[b_psb])
                pt2, pbuf2 = bank("C")
                ptb = pt2[:].bitcast(BF16)
                for j in range(2):
                    S.op("pe", lambda e, j=j, ptb=ptb, rows=rows, psbv=psbv: e.transpose(
                        out=ptb[:, j * 128:j * 128 + rows], in_=psbv[0:rows, j * 128:(j + 1) * 128], identity=identB[0:rows, 0:rows]),
                        reads=[b_psb, b_identB], writes=[pbuf2], signal=(j == 1))
                S.op("dve", lambda e, ptb=ptb, tt=tt, rows=rows: e.tensor_copy(
                    out=pT_[:, :, tt * 128:tt * 128 + rows], in_=ptb[:, 0:256].rearrange("p (j t) -> p j t", j=2)[:, :, 0:rows]),
                    reads=[pbuf2], writes=[b_pT])
            sig, b_sig, _ = sb("sig", [128, 512], F32)
            for cb in range(4):
                wg, bwg = w_next()
                for tt in range(9):
                    rows = rows_of[tt]
                    pg, pbg = bank("A")
                    mm_group(pg[0:rows, :], pbg, [(x3T[:, kc, tt * 128:tt * 128 + rows], wg[:, kc, :]) for kc in range(16)],
                             [b_x3T, bwg])
                    pe_, pbe = bank("A")
                    mm_group(pe_[0:rows, :], pbe, [(pT_[:, kc, tt * 128:tt * 128 + rows], wpl[:, kc, cb * 512:(cb + 1) * 512])
                                                   for kc in range(2)], [b_pT, b_wpl])
                    S.op("act", lambda e, pg=pg, rows=rows: e.activation(out=sig[0:rows, 0:512], in_=pg[0:rows, :], func=AF.Sigmoid),
                         reads=[pbg], writes=[b_sig])
                    S.op("dve", lambda e, pe_=pe_, rows=rows: e.tensor_tensor(out=sig[0:rows, 0:512], in0=sig[0:rows, 0:512],
                                                                             in1=pe_[0:rows, :], op=ALU.mult),
                         reads=[pbe, b_sig], writes=[b_sig])
                    S.op("dve", lambda e, tt=tt, cb=cb, rows=rows: e.tensor_tensor(
                        out=X2[0:rows, tt, cb * 512:(cb + 1) * 512], in0=X2[0:rows, tt, cb * 512:(cb + 1) * 512],
                        in1=sig[0:rows, 0:512], op=ALU.add), reads=[b_sig, bX2[tt]], writes=[bX2[tt]])

            if STOP < 9:
                return
            S.barrier()
            AR.release(r_x3T)
            gfin_b, b_gfin_b, _ = sb("gfin_b", [128, D], F32)
            S.dma("sp", lambda e: e.dma_start(out=gfin_b[:], in_=gvec[2, :].partition_broadcast(128)), writes=[b_gfin_b])
            SQ["t"], SQ["b"], SQ["r"] = sb("sqjunk", [128, D], BF16)
            xst = [sb("yo%d" % i, [128, D], F32) for i in range(2)]
            for tt in range(9):
                rows = rows_of[tt]
                rs = rms_stats(X2[0:rows, tt, :], rows, bX2[tt], tt % 2)
                yo, b_yo, _ = xst[tt % 2]
                S.op("dve", lambda e, yo=yo, rs=rs, tt=tt, rows=rows: e.scalar_tensor_tensor(
                    out=yo[0:rows, :], in0=X2[0:rows, tt, :], scalar=rs, in1=gfin_b[0:rows, :], op0=ALU.mult, op1=ALU.mult),
                    reads=[bX2[tt], b_stat, b_gfin_b], writes=[b_yo])
                dst = y[tt * 128:(tt + 1) * 128, :] if tt < 8 else ys
                S.dma("sp", lambda e, yo=yo, dst=dst, rows=rows: e.dma_start(out=dst, in_=yo[0:rows, :]), reads=[b_yo], dbuf=outb)

        phases()
        S.barrier()
        build_nc.sbuf_base = (nc.sbuf_base, nc.sbuf_top)
        S.wait_all("sp", [outb])
        S._wait("sp", (outb.sem, outb.cnt, "d_outb"))
        build_nc.stats = dict(ninstr=dict(S.ninstr), nsem=5 + len(S.dbufs))
    return nc


def _rope_rows(pos):
    half = 16
    inv = (np.float32(500000.0) ** (-(np.arange(half, dtype=np.float32)) / np.float32(half))).astype(np.float32)
    ang = pos.astype(np.float32)[:, None] * inv[None, :]
    c, s = np.cos(ang).astype(np.float32), np.sin(ang).astype(np.float32)
    return np.concatenate([c, c, -s, s], axis=1).astype(np.float32)


def _consts(half):
    pos = np.maximum(np.arange(2 * TOK) - TOK + TOK * half, 0)
    tab = _rope_rows(pos)
    rope = np.zeros((128, 56, 64), np.float32)
    p = np.arange(128)
    for g, d in enumerate(DIL):
        tpr = (2 * TOK // d) // 128
        for n in range(16):
            r, i0 = n // tpr, (n % tpr) * 128
            rope[:, g * 16 + n, :] = tab[(i0 + p) * d + r]
    for j in range(8):
        r = 2 * j + (p >= 64)
        i = 64 + (p % 64)
        rope[:, 48 + j, :] = tab[i * 16 + r]
    ropes = np.repeat(_rope_rows(np.array([2048])), NS, axis=0)
    cb = NEG if half == 0 else 0.0
    pp, ff = np.meshgrid(np.arange(128), np.arange(128), indexing="ij")
    m = np.zeros((128, 5, 128), np.float32)
    m[:, 0, :] = np.where(ff <= pp, 0.0, NEG)
    m[:, 1, :] = np.where(pp <= ff, 0.0, NEG)
    m[:, 2, :] = m[:, 0, :] + cb
    m[:, 3, :] = cb
    m[:, 4, :] = np.where(pp < 64, cb, np.where(pp - 64 <= ff, 0.0, NEG))
    return rope, ropes, m


def _in_maps(x_prompt, x_sample, cache_kv_w128, cache_kv_w512, cache_kv_w2048, p_prompt, p_sample, g_mix, w_in,
             sgu_ln_g, sgu_ln_b, w_s, b_s, w_a_out, w_b_out, w_o, g_ffn, peer_w_q, peer_sub_k1, peer_sub_k2,
             peer_u, peer_v, w_ple, w_ple_gate, g_final, cores=None):
    f = lambda a: np.ascontiguousarray(np.asarray(a, dtype=np.float32))
    shared = {
        "w_in": f(w_in[0]), "w_a_out": f(w_a_out[0]), "w_b_out": f(w_b_out[0]), "w_o": f(w_o[0]), "w_q": f(peer_w_q[0]),
        "w_pg": f(w_ple_gate[0]), "w_ple": f(w_ple[0]), "peer_u": f(peer_u[0]), "peer_v": f(peer_v[0]),
        "w_s": f(w_s[0]), "b_s": f(b_s[0]), "subk": f(np.stack([peer_sub_k1[0], peer_sub_k2[0]])),
        "gvec": f(np.stack([g_mix[0], g_ffn[0], g_final])), "lnv": f(np.stack([sgu_ln_g[0], sgu_ln_b[0]])),
    }
    maps = []
    for c in (range(NCORES) if cores is None else cores):
        b, half = c // 2, c % 2
        own = x_prompt[b, half * TOK:(half + 1) * TOK]
        ctx = x_prompt[b, 0:TOK]
        sl = slice(c * NS, (c + 1) * NS)
        caches = np.stack([
            np.asarray(cache_kv_w128[0, sl]).reshape(NS, 128, 1024),
            np.asarray(cache_kv_w512[0, sl, 0::4]).reshape(NS, 128, 1024),
            np.asarray(cache_kv_w2048[0, sl, 0::16]).reshape(NS, 128, 1024)])
        rope, ropes, m = _consts(half)
        d = dict(shared)
        d.update({"xloc": f(np.concatenate([ctx, own], axis=0)), "xs": f(x_sample[sl, 0]),
                  "ploc": f(p_prompt[0, b, half * TOK:(half + 1) * TOK]), "psm": f(p_sample[0, sl, 0]),
                  "cache": f(caches), "rope": rope, "ropes": ropes, "masks": m})
        maps.append(d)
    return maps


def _assemble(res):
    y = np.zeros((4, 2 * TOK, D), np.float32)
    ysm = np.zeros((128, 1, D), np.float32)
    k0 = np.zeros((1, 4, 128, 2, 4, 128), np.float32)
    k1 = np.zeros((1, 4, 512, 2, 4, 128), np.float32)
    k2 = np.zeros((1, 4, 2 * TOK, 2, 4, 128), np.float32)
    ks = [np.zeros((1, 128, 1, 2, 4, 128), np.float32) for _ in range(3)]
    sg = np.zeros((1, 128, 1, A_W), np.float32)
    for c, r in enumerate(res):
        b, half = c // 2, c % 2
        y[b, half * TOK:(half + 1) * TOK] = r["y"]
        ysm[c * NS:(c + 1) * NS, 0] = r["ys"]
        k2[0, b, half * TOK:(half + 1) * TOK] = r["kv2"].reshape(TOK, 2, 4, 128)
        if half == 1:
            k0[0, b] = r["kv0"].reshape(128, 2, 4, 128)
            k1[0, b] = r["kv1"].reshape(512, 2, 4, 128)
        for g in range(3):
            ks[g][0, c * NS:(c + 1) * NS, 0] = r["kvs"][g].reshape(NS, 2, 4, 128)
        sg[0, c * NS:(c + 1) * NS, 0] = r["sguv"]
    return (y, ysm, k0, k1, k2, ks[0], ks[1], ks[2], sg)


def kernel(**inputs):
    maps = _in_maps(**inputs)
    nc = build_nc()
    res = run_bass_kernel_spmd(nc, maps, core_ids=list(range(NCORES)))
    return _assemble(res.results)
```

```python
import contextlib
import os
import math
import numpy as np
import concourse.bass as bass
import concourse.mybir as mybir
from concourse.bass_utils import run_bass_kernel_spmd

F32 = mybir.dt.float32
BF16 = mybir.dt.bfloat16
I32 = mybir.dt.int32
U32 = mybir.dt.uint32
AF = mybir.ActivationFunctionType
ALU = mybir.AluOpType
AX = mybir.AxisListType
DTSIZE = {F32: 4, BF16: 2, I32: 4, U32: 4}

D = 2048
NCORES = 8
TOK = 1024
NS = 16
NCOL = 2 * TOK + NS
EPS = 1e-6
A_W = 1024
IN_COLS = 10752
C_UA, C_VA, C_Q, C_K, C_V, C_GA, C_GB = 0, 1024, 2048, 3584, 5120, 6656, 8704
DIL = (1, 4, 16)
SCALE = 128 ** -0.5
NEG = -30000.0
NEXP = 16384
PASSES = ((TOK, 512), (TOK + 512, 512), (2 * TOK, NS))


class Buf:
    __slots__ = ("name", "w", "r", "sem", "cnt", "excl")

    def __init__(self, name, excl=False):
        self.name = name
        self.excl = excl
        self.w = None
        self.r = {}
        self.sem = None
        self.cnt = 0


class Sched:
    def __init__(self, nc, stack):
        self.nc = nc
        self.stack = stack
        self.eng = {"pe": nc.tensor, "act": nc.scalar, "dve": nc.vector,
                    "pool": nc.gpsimd, "sp": nc.sync}
        self.sem = {k: stack.enter_context(nc.semaphore("s_" + k)) for k in self.eng}
        self.count = {k: 0 for k in self.eng}
        self.seen = {k: {} for k in self.eng}
        self.dbufs = []
        self.ninstr = {k: 0 for k in self.eng}

    def _wait(self, e, tok):
        if tok is None:
            return
        sem, val, key = tok
        if key == "pe" and e == "pe":
            return
        if self.seen[e].get(key, 0) >= val:
            return
        self.eng[e].wait_ge(sem, val)
        self.seen[e][key] = val

    def _deps(self, e, reads, writes):
        for b in reads:
            self._wait(e, b.w)
        for b in writes:
            self._wait(e, b.w)
            for t in b.r.values():
                self._wait(e, t)

    def _commit(self, tok, reads, writes):
        for b in reads:
            b.r[tok[2]] = tok
        for b in writes:
            b.w = tok
            b.r = {}

    def op(self, e, fn, reads=(), writes=(), signal=True):
        if any(b.excl for b in reads):
            writes = list(writes) + [b for b in reads if b.excl]
            reads = [b for b in reads if not b.excl]
        self._deps(e, reads, writes)
        ins = fn(self.eng[e])
        self.ninstr[e] += 1
        if signal:
            self.count[e] += 1
            ins.then_inc(self.sem[e], 1)
            tok = (self.sem[e], self.count[e], e)
        else:
            tok = (self.sem[e], self.count[e] + 1, e)
        self._commit(tok, reads, writes)
        return tok

    def dma(self, q, fn, reads=(), writes=(), dbuf=None):
        self._deps(q, reads, writes)
        if dbuf is None:
            dbuf = writes[0] if writes else reads[0]
        if dbuf.sem is None:
            dbuf.sem = self.stack.enter_context(self.nc.semaphore("d_" + dbuf.name))
            self.dbufs.append(dbuf)
        ins = fn(self.eng[q])
        dbuf.cnt += 16
        ins.then_inc(dbuf.sem, 16)
        tok = (dbuf.sem, dbuf.cnt, "d_" + dbuf.name)
        self._commit(tok, reads, writes)
        self.ninstr[q] += 1
        return tok

    def wait_all(self, e, bufs):
        for b in bufs:
            self._wait(e, b.w)
            for t in b.r.values():
                self._wait(e, t)

    def barrier(self):
        for e in self.eng:
            for x in ("pe", "act", "dve", "pool"):
                if x != e and self.count[x] > 0:
                    self._wait(e, (self.sem[x], self.count[x], x))
            for b in self.dbufs:
                if b.cnt > 0:
                    self._wait(e, (b.sem, b.cnt, "d_" + b.name))


class Arena:
    def __init__(self, nc, lo, hi):
        self.nc = nc
        self.free = [(lo, hi)]
        self.n = 0
        self.peak = 0
        self.hi = hi

    def alloc(self, name, shape, dt, top=False):
        nb = int(np.prod(shape[1:])) * DTSIZE[dt]
        nb = (nb + 63) // 64 * 64
        order = range(len(self.free) - 1, -1, -1) if top else range(len(self.free))
        for i in order:
            a, b = self.free[i]
            if b - a >= nb:
                if top:
                    off = b - nb
                    self.free[i] = (a, off)
                else:
                    off = a
                    self.free[i] = (a + nb, b)
                if self.free[i][0] == self.free[i][1]:
                    del self.free[i]
                self.n += 1
                used = self.hi - sum(y - x for x, y in self.free)
                self.peak = max(self.peak, used)
                h = self.nc.alloc_sbuf_tensor_at("%s_%d" % (name, self.n), list(shape), dt, offset=off)
                return h, (off, off + nb)
        raise RuntimeError("SBUF arena exhausted allocating %s %s (free=%s)" % (name, shape, self.free))

    def release(self, region):
        self.free.append(region)
        self.free.sort()
        merged = []
        for a, b in self.free:
            if merged and merged[-1][1] == a:
                merged[-1] = (merged[-1][0], b)
            else:
                merged.append((a, b))
        self.free = merged


def build_nc():
    nc = bass.Bass("TRN2", target_bir_lowering=False)
    SKIP = set(os.environ.get('KSKIP', '').split(','))
    NEXP_ = NEXP if int(os.environ.get('KSTOP', '99')) >= 7 else 128
    di = lambda name, shape, dt=F32: nc.dram_tensor(name, list(shape), dt, kind="ExternalInput").ap()
    do = lambda name, shape, dt=F32: nc.dram_tensor(name, list(shape), dt, kind="ExternalOutput").ap()

    xloc = di("xloc", [2 * TOK, D]); xs = di("xs", [NS, D])
    ploc = di("ploc", [TOK, 256]); psm = di("psm", [NS, 256])
    cache = di("cache", [3, NS, 128, 1024])
    w_in = di("w_in", [D, IN_COLS]); w_a_out = di("w_a_out", [A_W, D]); w_b_out = di("w_b_out", [512, D])
    w_o = di("w_o", [D, D]); w_q = di("w_q", [D, D]); w_pg = di("w_pg", [D, D]); w_ple = di("w_ple", [256, D])
    peer_u = di("peer_u", [NEXP_, D]); peer_v = di("peer_v", [NEXP_, D])
    w_s = di("w_s", [8, 128, 128]); b_s = di("b_s", [8, 128]); subk = di("subk", [2, 128, 128])
    gvec = di("gvec", [3, D]); lnv = di("lnv", [2, A_W])
    rope = di("rope", [128, 56, 64]); ropes = di("ropes", [NS, 64]); masks = di("masks", [128, 5, 128])

    y = do("y", [TOK, D]); ys = do("ys", [NS, D])
    kv0 = do("kv0", [128, 1024]); kv1 = do("kv1", [512, 1024]); kv2 = do("kv2", [TOK, 1024])
    kvs = do("kvs", [3, NS, 1024]); sguv = do("sguv", [NS, A_W])
    kvout = (kv0, kv1, kv2)
    pc16 = nc.dram_tensor("pc16", [NEXP_, 2, D], BF16, kind="Internal").ap()

    with contextlib.ExitStack() as st:
        S = Sched(nc, st)
        AR = Arena(nc, 16512, 229344)
        outb = Buf("outb")

        def sb(name, shape, dt, top=False):
            h, reg = AR.alloc(name, shape, dt, top)
            return h, Buf(name), reg

        PB = []
        for i in range(8):
            t = nc.alloc_psum_tensor("pb%d" % i, [128, 512], F32)
            PB.append((t, Buf("pb%d" % i, excl=True)))
        rr = {"A": 0, "B": 0, "C": 0}
        pools = {"A": (0, 1, 2, 3), "B": (4, 5), "C": (6, 7)}

        def bank(pool):
            ids = pools[pool]
            i = ids[rr[pool] % len(ids)]
            rr[pool] += 1
            return PB[i]

        def mm_group(out_ap, pbuf, pairs, reads):
            n = len(pairs)
            for i, (l, r) in enumerate(pairs):
                S.op("pe", lambda e, l=l, r=r, i=i: e.matmul(out_ap, lhsT=l, rhs=r, start=(i == 0), stop=(i == n - 1)),
                     reads=reads, writes=[pbuf], signal=(i == n - 1))

        identF, b_identF, _ = sb("identF", [128, 128], F32)
        identB, b_identB, _ = sb("identB", [128, 128], BF16)
        onesB, b_onesB, _ = sb("onesB", [128, 128], BF16)
        S.op("pool", lambda e: e.memset(identF[:], 1.0), writes=[b_identF])
        S.op("pool", lambda e: e.affine_select(out=identF[:], in_=identF[:], pattern=[[-1, 128]],
                                               compare_op=ALU.is_equal, fill=0.0, base=0, channel_multiplier=1),
             reads=[b_identF], writes=[b_identF])
        S.op("pool", lambda e: e.tensor_copy(out=identB[:], in_=identF[:]), reads=[b_identF], writes=[b_identB])
        S.op("pool", lambda e: e.memset(onesB[:], 1.0), writes=[b_onesB])

        gcol, b_gcol, _ = sb("gcol", [128, 2, 16], F32)
        grow, b_grow, r_grow = sb("grow", [16, 2, 128], F32)
        S.dma("sp", lambda e: e.dma_start(out=grow[:], in_=gvec[0:2, :].rearrange("g (k p) -> k g p", p=128)),
              writes=[b_grow])
        for gi in range(2):
            pt, pbuf = bank("C")
            S.op("pe", lambda e, gi=gi, pt=pt: e.transpose(out=pt[:, 0:16], in_=grow[:, gi, :], identity=identF[0:16, 0:16]),
                 reads=[b_grow, b_identF], writes=[pbuf])
            S.op("act", lambda e, gi=gi, pt=pt: e.copy(out=gcol[:, gi, :], in_=pt[:, 0:16]), reads=[pbuf], writes=[b_gcol])
        maskB, b_maskB, _ = sb("maskB", [128, 5, 128], BF16)
        S.dma("pool", lambda e: e.dma_start(out=maskB[:], in_=masks), writes=[b_maskB])
        c4, b_c4, _ = sb("c4", [128, 2], U32)
        S.op("pool", lambda e: e.memset(c4[:, 0:1], 4), writes=[b_c4])
        S.op("pool", lambda e: e.memset(c4[:, 1:2], 15), writes=[b_c4])
        ropeS, b_ropeS, _ = sb("ropeS", [NS, 64], F32)
        S.dma("sp", lambda e: e.dma_start(out=ropeS[:], in_=ropes), writes=[b_ropeS])

        NW = 2
        wslot = [sb("wslot%d" % i, [128, 16, 512], BF16) for i in range(NW)]
        wq = []
        wstate = {"issued": 0, "used": 0}

        def w_plan(blocks):
            wq.extend(blocks)

        b_tab = Buf("ptab")
        TCH = 1024 if NEXP_ >= 1024 else NEXP_
        tab_jobs = [(src, j, r0) for r0 in range(0, NEXP_, TCH) for (src, j) in ((peer_u, 0), (peer_v, 1))]

        def tab_issue(k=1):
            for _ in range(k):
                if not tab_jobs:
                    return
                src, j, r0 = tab_jobs.pop(0)
                tok = S.dma("pool", lambda e: e.dma_start(out=pc16[r0:r0 + TCH, j, :], in_=src[r0:r0 + TCH, :]), dbuf=b_tab)
                b_tab.w = tok

        def w_issue_upto(n):
            while wstate["issued"] < min(n, len(wq)):
                i = wstate["issued"]
                ap = wq[i]
                K, C = ap.shape
                kc = K // 128
                t, bf, _ = wslot[i % NW]
                S.dma("pool", lambda e, t=t, ap=ap, kc=kc, C=C: e.dma_start(
                    out=t[:, 0:kc, 0:C], in_=ap.rearrange("(k p) c -> p k c", p=128)), writes=[bf])
                wstate["issued"] += 1
                tab_issue(1)

        def w_next(issue=True):
            i = wstate["used"]
            if issue:
                w_issue_upto(i + NW)
            wstate["used"] += 1
            t, bf, _ = wslot[i % NW]
            return t, bf

        xst = [sb("xst%d" % i, [128, D], F32) for i in range(2)]
        xnb = [sb("xnb%d" % i, [128, D], BF16) for i in range(2)]
        SQ = {}
        SQ["t"], SQ["b"], SQ["r"] = sb("sqjunk", [128, D], BF16)
        stat, b_stat, _ = sb("stat", [128, 8], F32)

        def rms_stats(src_ap, rows, b_src, slot):
            ss = stat[0:rows, slot * 2:slot * 2 + 1]
            rs = stat[0:rows, slot * 2 + 1:slot * 2 + 2]
            sq_junk, b_sq_junk = SQ["t"], SQ["b"]
            S.op("act", lambda e: e.activation(out=sq_junk[0:rows, :], in_=src_ap, func=AF.Square, accum_out=ss),
                 reads=[b_src], writes=[b_sq_junk, b_stat])
            S.op("dve", lambda e: e.tensor_scalar(out=ss, in0=ss, scalar1=1.0 / D, scalar2=EPS, op0=ALU.mult, op1=ALU.add),
                 reads=[b_stat], writes=[b_stat])
            S.op("act", lambda e: e.sqrt(out=ss, in_=ss), reads=[b_stat], writes=[b_stat])
            S.op("dve", lambda e: e.reciprocal(out=rs, in_=ss), reads=[b_stat], writes=[b_stat])
            return rs

        def transpose_to(dstT, b_dstT, col0, src_bf, b_src, rows, gsel):
            for half in range(2):
                pt, pbuf = bank("B")
                ptb = pt[:].bitcast(BF16)
                for j in range(8):
                    kc = half * 8 + j
                    S.op("pe", lambda e, kc=kc, j=j, ptb=ptb: e.transpose(
                        out=ptb[:, j * 128:j * 128 + rows], in_=src_bf[0:rows, kc * 128:(kc + 1) * 128],
                        identity=identB[0:rows, 0:rows]),
                        reads=[b_src, b_identB], writes=[pbuf], signal=(j == 7))
                for j in range(8):
                    kc = half * 8 + j
                    eng = "dve" if half == 0 else "act"
                    if gsel is None:
                        if eng == "dve":
                            S.op("dve", lambda e, kc=kc, j=j, ptb=ptb: e.tensor_copy(
                                out=dstT[:, kc, col0:col0 + rows], in_=ptb[:, j * 128:j * 128 + rows]),
                                reads=[pbuf], writes=[b_dstT])
                        else:
                            S.op("act", lambda e, kc=kc, j=j, ptb=ptb: e.copy(
                                out=dstT[:, kc, col0:col0 + rows], in_=ptb[:, j * 128:j * 128 + rows]),
                                reads=[pbuf], writes=[b_dstT])
                    elif eng == "dve":
                        S.op("dve", lambda e, kc=kc, j=j, ptb=ptb: e.tensor_scalar(
                            out=dstT[:, kc, col0:col0 + rows], in0=ptb[:, j * 128:j * 128 + rows],
                            scalar1=gcol[:, gsel, kc:kc + 1], scalar2=None, op0=ALU.mult),
                            reads=[pbuf, b_gcol], writes=[b_dstT])
                    else:
                        S.op("act", lambda e, kc=kc, j=j, ptb=ptb: e.activation(
                            out=dstT[:, kc, col0:col0 + rows], in_=ptb[:, j * 128:j * 128 + rows],
                            func=AF.Copy, scale=gcol[:, gsel, kc:kc + 1]),
                            reads=[pbuf, b_gcol], writes=[b_dstT])

        STOP = int(os.environ.get('KSTOP', '99'))
        SUB = int(os.environ.get('KSUB', '99'))

        def phases():
            nonlocal xst, xnb
            if STOP < 0:
                return
            hT, b_hT, r_hT = sb("hT", [128, 16, NCOL], BF16)
            plan = []
            for g in range(3):
                plan += [w_in[:, C_K + g * 512:C_K + (g + 1) * 512], w_in[:, C_V + g * 512:C_V + (g + 1) * 512],
                         w_in[:, C_Q + g * 512:C_Q + (g + 1) * 512]]
            plan += [w_in[:, C_VA:C_VA + 512], w_in[:, C_VA + 512:C_VA + 1024]]
            plan += [w_in[:, C_UA:C_UA + 512], w_in[:, C_UA + 512:C_UA + 1024]]
            for cb in range(4):
                plan += [w_in[:, C_GA + cb * 512:C_GA + (cb + 1) * 512], w_a_out[:, cb * 512:(cb + 1) * 512],
                         w_in[:, C_GB + cb * 512:C_GB + (cb + 1) * 512], w_b_out[:, cb * 512:(cb + 1) * 512]]
            for cb in range(4):
                plan += [w_o[:, cb * 512:(cb + 1) * 512]]
            for cb in range(4):
                plan += [w_q[:, cb * 512:(cb + 1) * 512]]
            for cb in range(4):
                plan += [w_pg[:, cb * 512:(cb + 1) * 512]]
            w_plan(plan)
            w_issue_upto(NW)

            tiles = [(xloc[n * 128:(n + 1) * 128, :], 128, n * 128) for n in range(16)] + [(xs, NS, 2 * TOK)]

            def load_x(i):
                src, rows, _ = tiles[i]
                t, bf, _ = xst[i % 2]
                S.dma("sp", lambda e: e.dma_start(out=t[0:rows, :], in_=src), writes=[bf])

            load_x(0)
            for i, (src, rows, col0) in enumerate(tiles):
                if i + 1 < len(tiles):
                    load_x(i + 1)
                t, bf, _ = xst[i % 2]
                xn, bxn, _ = xnb[i % 2]
                rs = rms_stats(t[0:rows, :], rows, bf, i % 2)
                S.op("act", lambda e, t=t, xn=xn, rs=rs, rows=rows: e.activation(
                    out=xn[0:rows, :], in_=t[0:rows, :], func=AF.Copy, scale=rs), reads=[bf, b_stat], writes=[bxn])
                transpose_to(hT, b_hT, col0, xn, bxn, rows, 0)
            S.barrier()
            for r in (xst[0][2], xst[1][2], xnb[0][2], xnb[1][2], SQ["r"], r_grow):
                AR.release(r)

            if STOP < 1:
                return
            ACC, b_ACC, r_ACC = sb("ACC", [128, 2, 4, TOK], F32)
            KT, b_KT, r_KT = sb("KT", [128, 4, 2 * TOK], BF16)
            QT, b_QT, r_QT = sb("QT", [128, 4, TOK], BF16)
            VG, b_VG, r_VG = sb("VG", [128, 16, 512], BF16)
            kf = [sb("kf%d" % i, [128, 512], F32) for i in range(2)]
            kb = [sb("kb%d" % i, [128, 512], BF16) for i in range(2)]
            rtmp, b_rtmp, r_rtmp = sb("rtmp", [128, 2, 4, 32], F32)
            PT = [sb("PT%d" % i, [128, 2, 128], BF16) for i in range(2)]
            qkv_c, b_qkv_c, r_qkv_c = sb("qkv_c", [128, 2, 512], F32)
            stg = [sb("stg%d" % i, [NS, 512], F32) for i in range(2)]
            ropeG, b_ropeG, r_ropeG = sb("ropeG", [128, 16, 64], F32)
            ropeQ2, b_ropeQ2, r_ropeQ2 = sb("ropeQ2", [128, 8, 64], F32)
            S.dma("sp", lambda e: e.dma_start(out=ropeQ2[:], in_=rope[:, 48:56, :]), writes=[b_ropeQ2])
            cnt = {"kf": 0, "kb": 0, "pt": 0, "stg": 0}

            def stash_sample(pt, pbuf, which, g):
                blk = which * 3 + g
                if "stash" in SKIP:
                    return
                t, bt, _ = stg[cnt["stg"] % 2]; cnt["stg"] += 1
                S.op("act", lambda e: e.copy(out=t[:], in_=pt[0:NS, :]), reads=[pbuf], writes=[bt])
                j, slot = blk % 8, blk // 8
                S.dma("sp", lambda e: e.dma_start(out=qkv_c[16 * j:16 * j + 16, slot, :], in_=t[:]), reads=[bt], writes=[b_qkv_c])

            def rope_apply(t, bt, rows, tab_ap, b_tab):
                x4 = t[0:rows, :].rearrange("p (h d) -> p h d", h=4)
                cc = tab_ap[:, 0:32].unsqueeze(1).broadcast_to([rows, 4, 32])
                s1 = tab_ap[:, 32:48].unsqueeze(1).broadcast_to([rows, 4, 16])
                s2 = tab_ap[:, 48:64].unsqueeze(1).broadcast_to([rows, 4, 16])
                A = rtmp[0:rows, 0]
                B = rtmp[0:rows, 1]
                if "rope" in SKIP:
                    return
                S.op("dve", lambda e: e.tensor_tensor(out=A, in0=x4[:, :, 0:32], in1=cc, op=ALU.mult),
                     reads=[bt, b_tab], writes=[b_rtmp])
                S.op("dve", lambda e: e.tensor_tensor(out=B[:, :, 0:16], in0=x4[:, :, 16:32], in1=s1, op=ALU.mult),
                     reads=[bt, b_tab], writes=[b_rtmp])
                S.op("dve", lambda e: e.tensor_tensor(out=B[:, :, 16:32], in0=x4[:, :, 0:16], in1=s2, op=ALU.mult),
                     reads=[bt, b_tab], writes=[b_rtmp])
                S.op("dve", lambda e: e.tensor_tensor(out=x4[:, :, 0:32], in0=A, in1=B, op=ALU.add),
                     reads=[b_rtmp], writes=[bt])

            def gtile_cols(g, n):
                d = DIL[g]
                tpr = (2 * TOK // d) // 128
                r, i0 = n // tpr, (n % tpr) * 128
                start = i0 * d + r
                return slice(start, start + 127 * d + 1, d), r, i0

            first_group = True
            for g in range(3):
                d = DIL[g]
                L = 2 * TOK // d
                tpr = L // 128
                Lq = TOK // d
                if g == 0:
                    ktiles = list(range(7, 16))
                elif g == 1:
                    ktiles = [n for n in range(16) if n % 4 >= 1]
                else:
                    ktiles = list(range(16))
                S.dma("sp", lambda e, g=g: e.dma_start(out=ropeG[:], in_=rope[:, g * 16:(g + 1) * 16, :]), writes=[b_ropeG])
                wt, bw = w_next()
                for n in ktiles + ["s"]:
                    pt, pbuf = bank("A")
                    if n == "s":
                        rows = NS
                        lhs = lambda kc: hT[:, kc, 2 * TOK:2 * TOK + NS]
                    else:
                        rows = 128
                        sl, r, i0 = gtile_cols(g, n)
                        lhs = lambda kc, sl=sl: hT[:, kc, sl]
                    mm_group(pt[0:rows, :], pbuf, [(lhs(kc), wt[:, kc, :]) for kc in range(16)], [b_hT, bw])
                    if n == "s":
                        stash_sample(pt, pbuf, 1, g)
                        continue
                    t, bt, _ = kf[cnt["kf"] % 2]; cnt["kf"] += 1
                    S.op("act", lambda e, pt=pt, t=t: e.copy(out=t[:], in_=pt[:]), reads=[pbuf], writes=[bt])
                    rope_apply(t, bt, 128, ropeG[:, n, :], b_ropeG)
                    if "kvout" in SKIP:
                        pass
                    elif g == 0 and n == 15:
                        S.dma("sp", lambda e, t=t: e.dma_start(out=kv0[:, 0:512], in_=t[:]), reads=[bt], dbuf=outb)
                    elif g == 1 and n % 4 == 3:
                        S.dma("sp", lambda e, t=t, r=r: e.dma_start(out=kv1[r:512:4, 0:512], in_=t[:]), reads=[bt], dbuf=outb)
                    elif g == 2:
                        S.dma("sp", lambda e, t=t, r=r: e.dma_start(out=kv2[r:TOK:16, 0:512], in_=t[64:128, :]), reads=[bt], dbuf=outb)
                    tb, btb, _ = kb[cnt["kb"] % 2]; cnt["kb"] += 1
                    S.op("act", lambda e, t=t, tb=tb: e.copy(out=tb[:], in_=t[:]), reads=[bt], writes=[btb])
                    if "ktr" in SKIP:
                        continue
                    pt2, pbuf2 = bank("B")
                    ptb = pt2[:].bitcast(BF16)
                    for h in range(4):
                        S.op("pe", lambda e, h=h, ptb=ptb, tb=tb: e.transpose(out=ptb[:, h * 128:(h + 1) * 128],
                                                                        in_=tb[:, h * 128:(h + 1) * 128], identity=identB[:]),
                             reads=[btb, b_identB], writes=[pbuf2], signal=(h == 3))
                    S.op("dve", lambda e, ptb=ptb, n=n: e.tensor_copy(out=KT[:, :, n * 128:(n + 1) * 128],
                                                                 in_=ptb[:, 0:512].rearrange("p (h t) -> p h t", h=4)),
                         reads=[pbuf2], writes=[b_KT])
                if SUB < 0:
                    return
                wt, bw = w_next()
                for n in ktiles + ["s"]:
                    pt, pbuf = bank("A")
                    if n == "s":
                        mm_group(pt[0:NS, :], pbuf, [(hT[:, kc, 2 * TOK:2 * TOK + NS], wt[:, kc, :]) for kc in range(16)], [b_hT, bw])
                        stash_sample(pt, pbuf, 2, g)
                        continue
                    sl, r, i0 = gtile_cols(g, n)
                    mm_group(pt[:, :], pbuf, [(hT[:, kc, sl], wt[:, kc, :]) for kc in range(16)], [b_hT, bw])
                    own_out = ((g == 0 and n == 15) or (g == 1 and n % 4 == 3) or (g == 2)) and "kvout" not in SKIP
                    if own_out:
                        t, bt, _ = kf[cnt["kf"] % 2]; cnt["kf"] += 1
                        S.op("act", lambda e, pt=pt, t=t: e.copy(out=t[:], in_=pt[:]), reads=[pbuf], writes=[bt])
                        if g == 0:
                            S.dma("sp", lambda e, t=t: e.dma_start(out=kv0[:, 512:1024], in_=t[:]), reads=[bt], dbuf=outb)
                        elif g == 1:
                            S.dma("sp", lambda e, t=t, r=r: e.dma_start(out=kv1[r:512:4, 512:1024], in_=t[:]), reads=[bt], dbuf=outb)
                        else:
                            S.dma("sp", lambda e, t=t, r=r: e.dma_start(out=kv2[r:TOK:16, 512:1024], in_=t[64:128, :]),
                                  reads=[bt], dbuf=outb)
                    if own_out:
                        S.op("dve", lambda e, t=t, n=n: e.tensor_copy(out=VG[:, n, :], in_=t[:]), reads=[bt], writes=[b_VG])
                    else:
                        S.op("dve", lambda e, pt=pt, n=n: e.tensor_copy(out=VG[:, n, :], in_=pt[:]), reads=[pbuf], writes=[b_VG])
                wt, bw = w_next()
                if g == 0:
                    qtiles = [(n, gtile_cols(0, n)[0], ropeG[:, n, :], (n - 8) * 128) for n in range(8, 16)]
                elif g == 1:
                    qtiles = []
                    for n in range(16):
                        if n % 4 >= 2:
                            sl, r, i0 = gtile_cols(1, n)
                            qtiles.append((n, sl, ropeG[:, n, :], r * Lq + (i0 - Lq)))
                else:
                    qtiles = []
                    for r0 in range(0, 16, 2):
                        qtiles.append((r0, None, ropeQ2[:, r0 // 2, :], r0 * 64))
                for (n, sl, tab, qc0) in qtiles + [("s", None, None, None)]:
                    pt, pbuf = bank("A")
                    if n == "s":
                        mm_group(pt[0:NS, :], pbuf, [(hT[:, kc, 2 * TOK:2 * TOK + NS], wt[:, kc, :]) for kc in range(16)], [b_hT, bw])
                        stash_sample(pt, pbuf, 0, g)
                        continue
                    if g == 2:
                        for hf in range(2):
                            c0 = TOK + n + hf
                            mm_group(pt[hf * 64:(hf + 1) * 64, :], pbuf,
                                     [(hT[:, kc, c0:c0 + 63 * 16 + 1:16], wt[:, kc, :]) for kc in range(16)], [b_hT, bw])
                    else:
                        mm_group(pt[:, :], pbuf, [(hT[:, kc, sl], wt[:, kc, :]) for kc in range(16)], [b_hT, bw])
                    t, bt, _ = kf[cnt["kf"] % 2]; cnt["kf"] += 1
                    S.op("act", lambda e, pt=pt, t=t: e.copy(out=t[:], in_=pt[:]), reads=[pbuf], writes=[bt])
                    rope_apply(t, bt, 128, tab, b_ropeQ2 if g == 2 else b_ropeG)
                    tb, btb, _ = kb[cnt["kb"] % 2]; cnt["kb"] += 1
                    S.op("act", lambda e, t=t, tb=tb: e.copy(out=tb[:], in_=t[:]), reads=[bt], writes=[btb])
                    pt2, pbuf2 = bank("B")
                    ptb = pt2[:].bitcast(BF16)
                    for h in range(4):
                        S.op("pe", lambda e, h=h, ptb=ptb, tb=tb: e.transpose(out=ptb[:, h * 128:(h + 1) * 128],
                                                                        in_=tb[:, h * 128:(h + 1) * 128], identity=identB[:]),
                             reads=[btb, b_identB], writes=[pbuf2], signal=(h == 3))
                    S.op("dve", lambda e, ptb=ptb, qc0=qc0: e.tensor_copy(out=QT[:, :, qc0:qc0 + 128],
                                                                     in_=ptb[:, 0:512].rearrange("p (h t) -> p h t", h=4)),
                         reads=[pbuf2], writes=[b_QT])
                if SUB < 1 + 2 * g:
                    return
                TQ = 128 if g < 2 else 64
                for h in range(4):
                    for r in range(d):
                        for qt in range(Lq // TQ):
                            qc0 = r * Lq + qt * TQ
                            i0q = Lq + qt * TQ
                            if g < 2:
                                kprev = r * L + i0q - 128
                                blocks = [(kprev, 128, (kprev // 128), maskB[:, 2 if qt == 0 else 0, :]),
                                          (r * L + i0q, 128, (r * L + i0q) // 128, maskB[:, 1, :])]
                            else:
                                blocks = [(r * L, 128, r, maskB[:, 4, 0:64])]
                            nb = len(blocks)
                            ps_s, pb_s = bank("A")
                            for bi, (kc0, nk, vt, mk) in enumerate(blocks):
                                o = ps_s[0:nk, bi * 128:bi * 128 + TQ]
                                S.op("pe", lambda e, o=o, kc0=kc0, nk=nk, h=h, qc0=qc0: e.matmul(
                                    o, lhsT=KT[:, h, kc0:kc0 + nk], rhs=QT[:, h, qc0:qc0 + TQ], start=True, stop=False),
                                    reads=[b_KT, b_QT], writes=[pb_s], signal=False)
                                S.op("pe", lambda e, o=o, nk=nk, mk=mk: e.matmul(
                                    o, lhsT=identB[0:nk, 0:nk], rhs=mk, start=False, stop=True),
                                    reads=[b_identB, b_maskB], writes=[pb_s], signal=(bi == nb - 1))
                            p_t, b_p, _ = PT[cnt["pt"] % 2]; cnt["pt"] += 1
                            S.op("act", lambda e, ps_s=ps_s, p_t=p_t, nb=nb: e.activation(
                                out=p_t[:, 0:nb, 0:TQ], in_=ps_s[:].rearrange("p (b t) -> p b t", b=4)[:, 0:nb, 0:TQ],
                                func=AF.Exp, scale=SCALE), reads=[pb_s], writes=[b_p])
                            ps_o, pb_o = bank("B") if (cnt["pt"] % 2) else bank("C")
                            mm_group(ps_o[:, 0:TQ], pb_o, [(VG[:, vt, h * 128:(h + 1) * 128], p_t[:, bi, 0:TQ])
                                                          for bi, (kc0, nk, vt, mk) in enumerate(blocks)], [b_VG, b_p])
                            mm_group(ps_o[:, 128:128 + TQ], pb_o, [(onesB[:, :], p_t[:, bi, 0:TQ]) for bi in range(nb)],
                                     [b_onesB, b_p])
                            nat = slice(qt * TQ * d + r, qt * TQ * d + r + (TQ - 1) * d + 1, d)
                            src = ps_o[:].rearrange("p (b t) -> p b t", b=4)[:, 0:2, 0:TQ]
                            if first_group:
                                S.op("dve", lambda e, src=src, h=h, nat=nat: e.tensor_copy(out=ACC[:, :, h, nat], in_=src),
                                     reads=[pb_o], writes=[b_ACC])
                            else:
                                S.op("dve", lambda e, src=src, h=h, nat=nat: e.tensor_tensor(
                                    out=ACC[:, :, h, nat], in0=ACC[:, :, h, nat], in1=src, op=ALU.add),
                                    reads=[pb_o, b_ACC], writes=[b_ACC])
                first_group = False
                if SUB < 2 + 2 * g:
                    return
            S.barrier()
            for r in (r_KT, r_QT, r_VG, r_ropeG, r_ropeQ2, kf[0][2], kf[1][2], kb[0][2], kb[1][2], PT[0][2], PT[1][2]):
                AR.release(r)
            bmixT, b_bmixT, r_bmixT = sb("bmixT", [128, 4, TOK + NS], BF16)
            S.op("dve", lambda e: e.reciprocal(out=ACC[:, 1], in_=ACC[:, 1]), reads=[b_ACC], writes=[b_ACC])
            S.op("dve", lambda e: e.tensor_tensor(out=bmixT[:, :, 0:TOK], in0=ACC[:, 0], in1=ACC[:, 1], op=ALU.mult),
                 reads=[b_ACC], writes=[b_bmixT])
            qkv_s, b_qkv_s, r_qkv_s = sb("qkv_s", [NS, 3, 3, 512], F32)
            for which in range(3):
                for g in range(3):
                    blk = which * 3 + g
                    j, slot = blk % 8, blk // 8
                    S.dma("sp", lambda e, which=which, g=g, j=j, slot=slot: e.dma_start(
                        out=qkv_s[:, which, g, :], in_=qkv_c[16 * j:16 * j + 16, slot, :]), reads=[b_qkv_c], writes=[b_qkv_s])

            if SUB < 7:
                return
            for which in (0, 1):
                for g in range(3):
                    x4 = qkv_s[:, which, g, :].rearrange("p (h d) -> p h d", h=4)
                    cc = ropeS[:, 0:32].unsqueeze(1).broadcast_to([NS, 4, 32])
                    s1 = ropeS[:, 32:48].unsqueeze(1).broadcast_to([NS, 4, 16])
                    s2 = ropeS[:, 48:64].unsqueeze(1).broadcast_to([NS, 4, 16])
                    A = rtmp[0:NS, 0]
                    B = rtmp[0:NS, 1]
                    S.op("dve", lambda e, x4=x4, A=A, cc=cc: e.tensor_tensor(out=A, in0=x4[:, :, 0:32], in1=cc, op=ALU.mult),
                         reads=[b_qkv_s, b_ropeS], writes=[b_rtmp])
                    S.op("dve", lambda e, x4=x4, B=B, s1=s1: e.tensor_tensor(out=B[:, :, 0:16], in0=x4[:, :, 16:32], in1=s1, op=ALU.mult),
                         reads=[b_qkv_s, b_ropeS], writes=[b_rtmp])
                    S.op("dve", lambda e, x4=x4, B=B, s2=s2: e.tensor_tensor(out=B[:, :, 16:32], in0=x4[:, :, 0:16], in1=s2, op=ALU.mult),
                         reads=[b_qkv_s, b_ropeS], writes=[b_rtmp])
                    S.op("dve", lambda e, x4=x4, A=A, B=B: e.tensor_tensor(out=x4[:, :, 0:32], in0=A, in1=B, op=ALU.add),
                         reads=[b_rtmp], writes=[b_qkv_s])
            for g in range(3):
                S.dma("sp", lambda e, g=g: e.dma_start(out=kvs[g, :, 0:512], in_=qkv_s[:, 1, g, :]), reads=[b_qkv_s], dbuf=outb)
                S.dma("sp", lambda e, g=g: e.dma_start(out=kvs[g, :, 512:1024], in_=qkv_s[:, 2, g, :]), reads=[b_qkv_s], dbuf=outb)

            CK = [sb("CK%d" % i, [128, 1024], F32) for i in range(3)]
            SEL, b_SEL, r_SEL = sb("SEL", [NS, NS, 128], F32)
            SELT, b_SELT, r_SELT = sb("SELT", [128, NS, NS], F32)
            S.op("pool", lambda e: e.memset(SEL[:], 1.0), writes=[b_SEL])
            S.op("pool", lambda e: e.affine_select(out=SEL[:], in_=SEL[:], pattern=[[1, NS], [0, 128]], compare_op=ALU.is_equal,
                                                   fill=0.0, base=0, channel_multiplier=-1), reads=[b_SEL], writes=[b_SEL])
            S.op("pool", lambda e: e.memset(SELT[:], 1.0), writes=[b_SELT])
            S.op("pool", lambda e: e.affine_select(out=SELT[:], in_=SELT[:], pattern=[[1, NS], [-1, NS]], compare_op=ALU.is_equal,
                                                   fill=0.0, base=0, channel_multiplier=0), reads=[b_SELT], writes=[b_SELT])
            sprods = [sb("sprod%d" % i, [128, 512], F32) for i in range(3)]
            sscs = [sb("ssc%d" % i, [128, 8], F32) for i in range(3)]
            snew, b_snew, r_snew = sb("snew", [NS, 3, 512], F32)
            sn_s, b_sn_s, r_sn_s = sb("sn_s", [NS, 3, 8], F32)
            so, b_so, r_so = sb("so", [NS, 516], F32)
            sob, b_sob, r_sob = sb("sob", [NS, 512], BF16)
            ps_os, pb_os = PB[6]
            ps_ds, pb_ds = PB[7]
            S.op("dve", lambda e: e.tensor_tensor(out=snew[:], in0=qkv_s[:, 0], in1=qkv_s[:, 1], op=ALU.mult),
                 reads=[b_qkv_s], writes=[b_snew])
            S.op("dve", lambda e: e.tensor_reduce(out=sn_s[:, :, 0:4], in_=snew[:].rearrange("p g (h d) -> p g h d", h=4),
                                                  axis=AX.X, op=ALU.add), reads=[b_snew], writes=[b_sn_s])
            S.op("act", lambda e: e.activation(out=sn_s[:, :, 4:8], in_=sn_s[:, :, 0:4], func=AF.Exp, scale=SCALE),
                 reads=[b_sn_s], writes=[b_sn_s])
            S.op("dve", lambda e: e.tensor_tensor(
                out=snew[:].rearrange("p g (h d) -> p g h d", h=4), in0=qkv_s[:, 2].rearrange("p g (h d) -> p g h d", h=4),
                in1=sn_s[:, :, 4:8].unsqueeze(3).broadcast_to([NS, 3, 4, 128]), op=ALU.mult),
                reads=[b_qkv_s, b_sn_s], writes=[b_snew])
            k = 0
            for n in range(NS):
                for g in range(3):
                    ck, b_ck, _ = CK[k % 3]
                    sprod, b_sprod, _ = sprods[k % 3]
                    ssc, b_ssc, _ = sscs[k % 3]
                    S.dma("sp", lambda e, ck=ck, g=g, n=n: e.dma_start(out=ck[:], in_=cache[g, n]), writes=[b_ck])
                    pq, pbq = bank("A")
                    S.op("pe", lambda e, pq=pq, n=n, g=g: e.matmul(pq[:, :], lhsT=SEL[:, n, :], rhs=qkv_s[:, 0, g, :],
                                                               start=True, stop=True), reads=[b_SEL, b_qkv_s], writes=[pbq])
                    S.op("dve", lambda e, ck=ck, pq=pq, sprod=sprod: e.tensor_tensor(out=sprod[:], in0=ck[:, 0:512], in1=pq[:, :], op=ALU.mult),
                         reads=[b_ck, pbq], writes=[b_sprod])
                    S.op("dve", lambda e, sprod=sprod, ssc=ssc: e.tensor_reduce(out=ssc[:, 0:4], in_=sprod[:].rearrange("p (h d) -> p h d", h=4),
                                                          axis=AX.X, op=ALU.add), reads=[b_sprod], writes=[b_ssc])
                    S.op("act", lambda e, ssc=ssc: e.activation(out=ssc[:, 4:8], in_=ssc[:, 0:4], func=AF.Exp, scale=SCALE),
                         reads=[b_ssc], writes=[b_ssc])
                    S.op("dve", lambda e, ck=ck, sprod=sprod, ssc=ssc: e.tensor_tensor(
                        out=sprod[:].rearrange("p (h d) -> p h d", h=4), in0=ck[:, 512:1024].rearrange("p (h d) -> p h d", h=4),
                        in1=ssc[:, 4:8].unsqueeze(2).broadcast_to([128, 4, 128]), op=ALU.mult),
                        reads=[b_ck, b_ssc], writes=[b_sprod])
                    first, last = (k == 0), (k == NS * 3 - 1)
                    S.op("pe", lambda e, n=n, first=first, last=last, sprod=sprod: e.matmul(ps_os[0:NS, :], lhsT=SELT[:, n, :], rhs=sprod[:],
                                                                          start=first, stop=last),
                         reads=[b_SELT, b_sprod], writes=[pb_os], signal=True)
                    S.op("pe", lambda e, n=n, first=first, last=last, ssc=ssc: e.matmul(ps_ds[0:NS, 0:4], lhsT=SELT[:, n, :], rhs=ssc[:, 4:8],
                                                                          start=first, stop=last),
                         reads=[b_SELT, b_ssc], writes=[pb_ds], signal=True)
                    k += 1
            S.op("dve", lambda e: e.tensor_tensor(out=so[:, 0:512], in0=snew[:, 0, :], in1=snew[:, 1, :], op=ALU.add),
                 reads=[b_snew], writes=[b_so])
            S.op("dve", lambda e: e.tensor_tensor(out=so[:, 0:512], in0=so[:, 0:512], in1=snew[:, 2, :], op=ALU.add),
                 reads=[b_snew, b_so], writes=[b_so])
            S.op("dve", lambda e: e.tensor_tensor(out=so[:, 0:512], in0=so[:, 0:512], in1=ps_os[0:NS, :], op=ALU.add),
                 reads=[pb_os, b_so], writes=[b_so])
            S.op("dve", lambda e: e.tensor_tensor(out=so[:, 512:516], in0=sn_s[:, 0, 4:8], in1=sn_s[:, 1, 4:8], op=ALU.add),
                 reads=[b_sn_s], writes=[b_so])
            S.op("dve", lambda e: e.tensor_tensor(out=so[:, 512:516], in0=so[:, 512:516], in1=sn_s[:, 2, 4:8], op=ALU.add),
                 reads=[b_sn_s, b_so], writes=[b_so])
            S.op("dve", lambda e: e.tensor_tensor(out=so[:, 512:516], in0=so[:, 512:516], in1=ps_ds[0:NS, 0:4], op=ALU.add),
                 reads=[pb_ds, b_so], writes=[b_so])
            S.op("dve", lambda e: e.reciprocal(out=so[:, 512:516], in_=so[:, 512:516]), reads=[b_so], writes=[b_so])
            S.op("dve", lambda e: e.tensor_tensor(out=sob[:].rearrange("p (h d) -> p h d", h=4),
                                                  in0=so[:, 0:512].rearrange("p (h d) -> p h d", h=4),
                                                  in1=so[:, 512:516].unsqueeze(2).broadcast_to([NS, 4, 128]), op=ALU.mult),
                 reads=[b_so], writes=[b_sob])
            pt2, pbuf2 = bank("B")
            ptb = pt2[:].bitcast(BF16)
            for h in range(4):
                S.op("pe", lambda e, h=h, ptb=ptb: e.transpose(out=ptb[:, h * NS:(h + 1) * NS], in_=sob[:, h * 128:(h + 1) * 128],
                                                          identity=identB[0:NS, 0:NS]),
                     reads=[b_sob, b_identB], writes=[pbuf2], signal=(h == 3))
            S.op("dve", lambda e, ptb=ptb: e.tensor_copy(out=bmixT[:, :, TOK:TOK + NS],
                                                    in_=ptb[:, 0:4 * NS].rearrange("p (h t) -> p h t", h=4)),
                 reads=[pbuf2], writes=[b_bmixT])

            S.barrier()
            for r in [r_ACC, r_rtmp, r_qkv_s, r_qkv_c, stg[0][2], stg[1][2], r_SEL, r_SELT, r_snew, r_sn_s,
                      r_so, r_sob, CK[0][2], CK[1][2], CK[2][2]] + [x[2] for x in sprods] + [x[2] for x in sscs]:
                AR.release(r)

            if STOP < 2:
                return
            amixT, b_amixT, r_amixT = sb("amixT", [128, 8, TOK + NS], BF16)
            vn, b_vn, r_vn = sb("vn", [128, 9, A_W], BF16)
            gv = [sb("gv%d" % i, [128, A_W], F32) for i in range(2)]
            lng, b_lng, r_lng = sb("lng", [128, A_W], F32)
            lnb, b_lnb, r_lnb = sb("lnb", [128, A_W], F32)
            S.dma("sp", lambda e: e.dma_start(out=lng[:], in_=lnv[0, :].partition_broadcast(128)), writes=[b_lng])
            S.dma("sp", lambda e: e.dma_start(out=lnb[:], in_=lnv[1, :].partition_broadcast(128)), writes=[b_lnb])
            bns, b_bns, r_bns = sb("bns", [128, 2, 8], F32)
            wsT, b_wsT, r_wsT = sb("wsT", [128, 8, 128], BF16)
            wsF, b_wsF, r_wsF = sb("wsF", [128, 8, 128], F32)
            bsb, b_bsb, r_bsb = sb("bsb", [128, 8, 128], F32)
            bs0, b_bs0, r_bs0 = sb("bs0", [128, 8], F32)
            ws00, b_ws00, r_ws00 = sb("ws00", [16, 8], F32)
            dg, b_dg, r_dg = sb("dg", [16, 8, 16], BF16)
            sgt, b_sgt, r_sgt = sb("sgt", [128, 512], F32)
            S.dma("sp", lambda e: e.dma_start(out=wsF[:], in_=w_s.rearrange("g t s -> t g s")), writes=[b_wsF])
            S.dma("sp", lambda e: e.dma_start(out=bsb[:], in_=b_s.rearrange("g t -> (g t)").partition_broadcast(128)
                                              .rearrange("p (g t) -> p g t", g=8)), writes=[b_bsb])
            S.dma("sp", lambda e: e.dma_start(out=ws00[:], in_=w_s[:, 0, 0].partition_broadcast(16),
                                              allow_slow_non_contiguous=True), writes=[b_ws00])
            wsM, b_wsM, r_wsM = sb("wsM", [128, 8, 128], F32)
            for half in range(2):
                pt, pbuf = bank("C")
                for j in range(4):
                    gi = half * 4 + j
                    S.op("pe", lambda e, gi=gi, j=j, pt=pt: e.transpose(out=pt[:, j * 128:(j + 1) * 128], in_=wsF[:, gi, :],
                                                                   identity=identF[:]),
                         reads=[b_wsF, b_identF], writes=[pbuf], signal=(j == 3))
                S.op("act", lambda e, half=half, pt=pt: e.copy(out=wsM[:, half * 4:half * 4 + 4, :],
                                                           in_=pt[:].rearrange("p (g t) -> p g t", g=4)),
                     reads=[pbuf], writes=[b_wsM])
            S.op("pool", lambda e: e.affine_select(out=wsT[:], in_=wsM[:], pattern=[[0, 8], [1, 128]],
                                                   compare_op=ALU.is_ge, fill=0.0, base=0, channel_multiplier=-1),
                 reads=[b_wsM], writes=[b_wsT])
            S.op("dve", lambda e: e.tensor_tensor(out=dg[:], in0=identF[0:16, 0:16].unsqueeze(1).broadcast_to([16, 8, 16]),
                                                  in1=ws00[:].unsqueeze(2).broadcast_to([16, 8, 16]), op=ALU.mult),
                 reads=[b_identF, b_ws00], writes=[b_dg])

            wva0, bwva0 = w_next()
            wva1, bwva1 = w_next(issue=False)
            own_tiles = [(TOK + n * 128, 128) for n in range(8)] + [(2 * TOK, NS)]
            for ti, (c0, rows) in enumerate(own_tiles):
                g_t, b_g, _ = gv[ti % 2]
                for blk, (wt, bw) in enumerate(((wva0, bwva0), (wva1, bwva1))):
                    pt, pbuf = bank("A")
                    mm_group(pt[0:rows, :], pbuf, [(hT[:, kc, c0:c0 + rows], wt[:, kc, :]) for kc in range(16)], [b_hT, bw])
                    S.op("act", lambda e, pt=pt, blk=blk, g_t=g_t, rows=rows: e.activation(
                        out=g_t[0:rows, blk * 512:(blk + 1) * 512], in_=pt[0:rows, :], func=AF.Gelu_apprx_tanh),
                        reads=[pbuf], writes=[b_g])
                for blk in range(2):
                    S.op("dve", lambda e, blk=blk, g_t=g_t, rows=rows: e.bn_stats(
                        out=bns[0:rows, blk, 0:6], in_=g_t[0:rows, blk * 512:(blk + 1) * 512]), reads=[b_g], writes=[b_bns])
                mv = bns[0:rows, 0, 6:8]
                S.op("dve", lambda e, rows=rows, mv=mv: e.bn_aggr(out=mv, in_=bns[0:rows, :, 0:6]), reads=[b_bns], writes=[b_bns])
                sd = bns[0:rows, 1, 6:7]
                rsd = bns[0:rows, 1, 7:8]
                S.op("dve", lambda e, rows=rows, sd=sd: e.tensor_scalar(out=sd, in0=bns[0:rows, 0, 7:8], scalar1=EPS, scalar2=None,
                                                                     op0=ALU.add), reads=[b_bns], writes=[b_bns])
                S.op("act", lambda e, sd=sd: e.sqrt(out=sd, in_=sd), reads=[b_bns], writes=[b_bns])
                S.op("dve", lambda e, sd=sd, rsd=rsd: e.reciprocal(out=rsd, in_=sd), reads=[b_bns], writes=[b_bns])
                S.op("dve", lambda e, g_t=g_t, rows=rows, rsd=rsd: e.tensor_scalar(
                    out=g_t[0:rows, :], in0=g_t[0:rows, :], scalar1=bns[0:rows, 0, 6:7], scalar2=rsd,
                    op0=ALU.subtract, op1=ALU.mult), reads=[b_g, b_bns], writes=[b_g])
                S.op("dve", lambda e, g_t=g_t, rows=rows: e.tensor_tensor(out=g_t[0:rows, :], in0=g_t[0:rows, :], in1=lng[0:rows, :],
                                                                      op=ALU.mult), reads=[b_g, b_lng], writes=[b_g])
                if rows == 128:
                    S.op("dve", lambda e, g_t=g_t, ti=ti: e.tensor_tensor(out=vn[:, ti, :], in0=g_t[:], in1=lnb[:], op=ALU.add),
                         reads=[b_g, b_lnb], writes=[b_vn])
                else:
                    S.op("dve", lambda e, g_t=g_t, rows=rows: e.tensor_tensor(out=g_t[0:rows, :], in0=g_t[0:rows, :],
                                                                          in1=lnb[0:rows, :], op=ALU.add),
                         reads=[b_g, b_lnb], writes=[b_g])
                    S.dma("sp", lambda e, g_t=g_t, rows=rows: e.dma_start(out=sguv, in_=g_t[0:rows, :]), reads=[b_g], dbuf=outb)
                    S.op("act", lambda e, g_t=g_t, rows=rows, ti=ti: e.copy(out=vn[0:rows, ti, :], in_=g_t[0:rows, :]),
                         reads=[b_g], writes=[b_vn])

            for blk in range(2):
                wt, bw = w_next()
                for jj in range(4):
                    j = blk * 4 + jj
                    for (c0, wd) in PASSES:
                        pt, pbuf = bank("A")
                        mm_group(pt[:, 0:wd], pbuf, [(wt[:, kc, jj * 128:(jj + 1) * 128], hT[:, kc, c0:c0 + wd]) for kc in range(16)],
                                 [b_hT, bw])
                        S.op("act", lambda e, pt=pt, j=j, c0=c0, wd=wd: e.activation(
                            out=amixT[:, j, c0 - TOK:c0 - TOK + wd], in_=pt[:, 0:wd], func=AF.Gelu_apprx_tanh),
                            reads=[pbuf], writes=[b_amixT])

            for tt in range(8):
                for half in range(2):
                    pt, pbuf = bank("A")
                    for jj in range(4):
                        gi = half * 4 + jj
                        S.op("pe", lambda e, pt=pt, jj=jj, gi=gi, tt=tt: e.matmul(
                            pt[:, jj * 128:(jj + 1) * 128], lhsT=vn[:, tt, gi * 128:(gi + 1) * 128], rhs=wsT[:, gi, :],
                            start=True, stop=True), reads=[b_vn, b_wsT], writes=[pbuf], signal=(jj == 3))
                    S.op("dve", lambda e, pt=pt, half=half: e.tensor_tensor(
                        out=sgt[:].rearrange("p (g t) -> p g t", g=4), in0=pt[:].rearrange("p (g t) -> p g t", g=4),
                        in1=bsb[:, half * 4:half * 4 + 4, :], op=ALU.add), reads=[pbuf, b_bsb], writes=[b_sgt])
                    S.op("dve", lambda e, half=half, tt=tt: e.tensor_tensor(
                        out=amixT[:, half * 4:half * 4 + 4, tt * 128:(tt + 1) * 128],
                        in0=amixT[:, half * 4:half * 4 + 4, tt * 128:(tt + 1) * 128],
                        in1=sgt[:].rearrange("p (g t) -> p g t", g=4), op=ALU.mult), reads=[b_sgt, b_amixT], writes=[b_amixT])
            pt, pbuf = bank("A")
            for gi in range(8):
                S.op("pe", lambda e, pt=pt, gi=gi: e.matmul(pt[:, gi * NS:(gi + 1) * NS], lhsT=vn[0:NS, 8, gi * 128:(gi + 1) * 128],
                                                       rhs=dg[:, gi, :], start=True, stop=True),
                     reads=[b_vn, b_dg], writes=[pbuf], signal=(gi == 7))
            S.op("dve", lambda e, pt=pt: e.tensor_tensor(
                out=sgt[:, 0:128].rearrange("p (g t) -> p g t", g=8), in0=pt[:, 0:128].rearrange("p (g t) -> p g t", g=8),
                in1=bsb[:, :, 0:1].broadcast_to([128, 8, NS]), op=ALU.add), reads=[pbuf, b_bsb], writes=[b_sgt])
            S.op("dve", lambda e: e.tensor_tensor(out=amixT[:, :, TOK:TOK + NS], in0=amixT[:, :, TOK:TOK + NS],
                                                  in1=sgt[:, 0:128].rearrange("p (g t) -> p g t", g=8), op=ALU.mult),
                 reads=[b_sgt, b_amixT], writes=[b_amixT])

            S.barrier()
            for r in (r_vn, gv[0][2], gv[1][2], r_lng, r_lnb, r_bns, r_wsT, r_wsF, r_bsb, r_bs0, r_ws00, r_dg, r_sgt, r_wsM):
                AR.release(r)

            if STOP < 3:
                return
            mergedT, b_mergedT, r_mergedT = sb("mergedT", [128, 16, TOK + NS], BF16, top=True)
            sgA, b_sgA, r_sgA = sb("sgA", [128, 4, TOK + NS], BF16)
            sgB, b_sgB = sgA, b_sgA
            M1, b_M1, r_M1 = sb("M1", [128, 4, TOK + NS], F32)
            mtmp, b_mtmp, r_mtmp = sb("mtmp", [128, 512], F32)
            def gate_block():
                wt, bw = w_next()
                for jj in range(4):
                    for (c0, wd) in PASSES:
                        pt, pbuf = bank("A")
                        mm_group(pt[:, 0:wd], pbuf, [(wt[:, kc, jj * 128:(jj + 1) * 128], hT[:, kc, c0:c0 + wd]) for kc in range(16)],
                                 [b_hT, bw])
                        S.op("act", lambda e, pt=pt, jj=jj, c0=c0, wd=wd: e.activation(
                            out=sgA[:, jj, c0 - TOK:c0 - TOK + wd], in_=pt[:, 0:wd], func=AF.Sigmoid),
                            reads=[pbuf], writes=[b_sgA])

            for cb in range(4):
                gate_block()
                wt, bw = w_next()
                for jj in range(4):
                    for (c0, wd) in PASSES:
                        pt, pbuf = bank("A")
                        mm_group(pt[:, 0:wd], pbuf, [(wt[:, kc, jj * 128:(jj + 1) * 128], amixT[:, kc, c0 - TOK:c0 - TOK + wd])
                                                     for kc in range(8)], [b_amixT, bw])
                        S.op("dve", lambda e, pt=pt, jj=jj, c0=c0, wd=wd: e.tensor_tensor(
                            out=M1[:, jj, c0 - TOK:c0 - TOK + wd], in0=pt[:, 0:wd], in1=sgA[:, jj, c0 - TOK:c0 - TOK + wd], op=ALU.mult),
                            reads=[pbuf, b_sgA], writes=[b_M1])
                gate_block()
                wt, bw = w_next()
                for jj in range(4):
                    for (c0, wd) in PASSES:
                        pt, pbuf = bank("A")
                        mm_group(pt[:, 0:wd], pbuf, [(wt[:, kc, jj * 128:(jj + 1) * 128], bmixT[:, kc, c0 - TOK:c0 - TOK + wd])
                                                     for kc in range(4)], [b_bmixT, bw])
                        S.op("dve", lambda e, pt=pt, jj=jj, c0=c0, wd=wd: e.tensor_tensor(
                            out=mtmp[:, 0:wd], in0=pt[:, 0:wd], in1=sgB[:, jj, c0 - TOK:c0 - TOK + wd], op=ALU.mult),
                            reads=[pbuf, b_sgB], writes=[b_mtmp])
                        S.op("dve", lambda e, cb=cb, jj=jj, c0=c0, wd=wd: e.tensor_tensor(
                            out=mergedT[:, cb * 4 + jj, c0 - TOK:c0 - TOK + wd], in0=mtmp[:, 0:wd],
                            in1=M1[:, jj, c0 - TOK:c0 - TOK + wd], op=ALU.add),
                            reads=[b_mtmp, b_M1], writes=[b_mergedT])

            S.barrier()
            for r in (r_hT, r_amixT, r_bmixT, r_sgA, r_M1, r_mtmp):
                AR.release(r)

            if STOP < 4:
                return
            X2, b_X2, r_X2 = sb("X2", [128, 9, D], F32)
            bX2 = [Buf("X2_%d" % i) for i in range(9)]
            xpc = [sb("xpc%d" % i, [128, 512], F32) for i in range(3)]
            rows_of = [128] * 8 + [NS]
            k = 0

            def load_xpiece(k):
                cb, tt = divmod(k, 9)
                t, bf, _ = xpc[k % 3]
                rows = rows_of[tt]
                src = xloc[TOK + tt * 128:TOK + (tt + 1) * 128, cb * 512:(cb + 1) * 512] if tt < 8 else xs[:, cb * 512:(cb + 1) * 512]
                S.dma("sp", lambda e: e.dma_start(out=t[0:rows, :], in_=src), writes=[bf])

            load_xpiece(0); load_xpiece(1)
            for cb in range(4):
                wt, bw = w_next()
                for tt in range(9):
                    if k + 2 < 36:
                        load_xpiece(k + 2)
                    rows = rows_of[tt]
                    t, bf, _ = xpc[k % 3]
                    pt, pbuf = bank("A")
                    mm_group(pt[0:rows, :], pbuf, [(mergedT[:, kc, tt * 128:tt * 128 + rows], wt[:, kc, :]) for kc in range(16)],
                             [b_mergedT, bw])
                    S.op("dve", lambda e, pt=pt, t=t, tt=tt, cb=cb, rows=rows: e.tensor_tensor(
                        out=X2[0:rows, tt, cb * 512:(cb + 1) * 512], in0=t[0:rows, :], in1=pt[0:rows, :], op=ALU.add),
                        reads=[pbuf, bf], writes=[bX2[tt]])
                    k += 1
            S.barrier()
            AR.release(r_mergedT)
            for i in range(3):
                AR.release(xpc[i][2])

            if STOP < 5:
                return
            h2T, b_h2T, r_h2T = sb("h2T", [128, 16, TOK + NS], BF16)
            rstd2, b_rstd2, _ = sb("rstd2", [128, 9], F32)
            xnb = [sb("xnb%d" % i, [128, D], BF16) for i in range(2)]
            SQ["t"], SQ["b"], SQ["r"] = sb("sqjunk", [128, D], BF16)
            for tt in range(9):
                rows = rows_of[tt]
                rs = rms_stats(X2[0:rows, tt, :], rows, bX2[tt], tt % 2)
                S.op("dve", lambda e, rs=rs, tt=tt, rows=rows: e.tensor_copy(out=rstd2[0:rows, tt:tt + 1], in_=rs),
                     reads=[b_stat], writes=[b_rstd2])
                xn, bxn, _ = xnb[tt % 2]
                S.op("act", lambda e, xn=xn, rs=rs, tt=tt, rows=rows: e.activation(
                    out=xn[0:rows, :], in_=X2[0:rows, tt, :], func=AF.Copy, scale=rs), reads=[bX2[tt], b_stat], writes=[bxn])
                transpose_to(h2T, b_h2T, tt * 128, xn, bxn, rows, 1)

            if STOP < 6:
                return
            skF, b_skF, r_skF = sb("skF", [128, 2, 128], F32)
            skT, b_skT, _ = sb("skT", [128, 2, 128], BF16)
            S.dma("sp", lambda e: e.dma_start(out=skF[:], in_=subk.rearrange("j k d -> k j d")), writes=[b_skF])
            pt, pbuf = bank("C")
            for j in range(2):
                S.op("pe", lambda e, j=j, pt=pt: e.transpose(out=pt[:, j * 128:(j + 1) * 128], in_=skF[:, j, :], identity=identF[:]),
                     reads=[b_skF, b_identF], writes=[pbuf], signal=(j == 1))
            S.op("act", lambda e, pt=pt: e.copy(out=skT[:], in_=pt[:, 0:256].rearrange("p (j k) -> p j k", j=2)),
                 reads=[pbuf], writes=[b_skT])
            T1, b_T1, r_T1 = sb("T1", [128, 9, 16, 16], F32)
            I1, b_I1, r_I1 = sb("I1", [128, 9, 16, 16], U32)
            qc, b_qc, r_qc = sb("qc", [128, TOK + NS], BF16)
            scs = [sb("scs%d" % i, [128, 128], F32) for i in range(2)]
            sc2, b_sc2, r_sc2 = sb("sc2", [128, 128], F32)
            k = 0
            for cb in range(4):
                wt, bw = w_next(issue=(cb < 3))
                for jj in range(4):
                    c = cb * 4 + jj
                    for (c0, wd) in PASSES:
                        pt, pbuf = bank("A")
                        mm_group(pt[:, 0:wd], pbuf, [(wt[:, kc, jj * 128:(jj + 1) * 128], h2T[:, kc, c0 - TOK:c0 - TOK + wd])
                                                     for kc in range(16)], [b_h2T, bw])
                        S.op("act", lambda e, pt=pt, c0=c0, wd=wd: e.copy(out=qc[:, c0 - TOK:c0 - TOK + wd], in_=pt[:, 0:wd]),
                             reads=[pbuf], writes=[b_qc])
                    for tt in range(9):
                        rows = rows_of[tt]
                        pt, pbuf = bank("B") if tt % 2 else bank("C")
                        S.op("pe", lambda e, pt=pt, tt=tt, rows=rows, c=c: e.matmul(
                            pt[0:rows, 0:128], lhsT=qc[:, tt * 128:tt * 128 + rows], rhs=skT[:, c % 2, :], start=True, stop=True),
                            reads=[b_qc, b_skT], writes=[pbuf])
                        sc, b_sc, _ = scs[k % 2]; k += 1
                        S.op("act", lambda e, pt=pt, sc=sc, rows=rows: e.copy(out=sc[0:rows, :], in_=pt[0:rows, 0:128]),
                             reads=[pbuf], writes=[b_sc])
                        S.op("dve", lambda e, sc=sc, rows=rows, tt=tt, c=c: e.max(out=T1[0:rows, tt, c, 0:8], in_=sc[0:rows, :]),
                             reads=[b_sc], writes=[b_T1])
                        S.op("dve", lambda e, sc=sc, rows=rows, tt=tt, c=c: e.max_index(
                            out=I1[0:rows, tt, c, 0:8], in_max=T1[0:rows, tt, c, 0:8], in_values=sc[0:rows, :]),
                            reads=[b_sc, b_T1], writes=[b_I1])
                        S.op("dve", lambda e, sc=sc, rows=rows, tt=tt, c=c: e.match_replace(
                            out=sc2[0:rows, :], in_to_replace=T1[0:rows, tt, c, 0:8], in_values=sc[0:rows, :], imm_value=-3.0e38),
                            reads=[b_sc, b_T1], writes=[b_sc2])
                        S.op("dve", lambda e, rows=rows, tt=tt, c=c: e.max(out=T1[0:rows, tt, c, 8:16], in_=sc2[0:rows, :]),
                             reads=[b_sc2], writes=[b_T1])
                        S.op("dve", lambda e, rows=rows, tt=tt, c=c: e.max_index(
                            out=I1[0:rows, tt, c, 8:16], in_max=T1[0:rows, tt, c, 8:16], in_values=sc2[0:rows, :]),
                            reads=[b_sc2, b_T1], writes=[b_I1])
            S.barrier()
            for r in (r_skF, r_qc, scs[0][2], scs[1][2], r_sc2, r_h2T, xnb[0][2], xnb[1][2], SQ["r"]):
                AR.release(r)

            if STOP < 7:
                return
            tab_issue(len(tab_jobs))
            cand, b_cand, r_cand = sb("cand", [128, 8, 256], F32)
            cand2, b_cand2, r_cand2 = sb("cand2", [128, 256], F32)
            ts, b_ts, r_ts = sb("ts", [128, 8, 16], F32)
            ic, b_ic, r_ic = sb("ic", [128, 8, 16], U32)
            icw, b_icw, r_icw = sb("icw", [128, 2, 8, 16], U32)
            icf, b_icf, r_icf = sb("icf", [128, 2, 8, 16], F32)
            i1f, b_i1f, r_i1f = sb("i1f", [128, 16, 16], F32)
            iota16, b_iota16, r_iota16 = sb("iota16", [128, 16], F32)
            oh, b_oh = cand[:].rearrange("p h (a b) -> p h a b", a=16), b_cand
            ef, b_ef, r_ef = sb("ef", [128, 2, 8, 16], F32)
            gs2, b_gs2, r_gs2 = sb("gs2", [128, 16], F32)
            eidx, _, _ = sb("eidx", [128, 9, 128], I32, top=True)
            gsm, _, _ = sb("gsm", [128, 9, 128], F32, top=True)
            b_eidxs = [Buf("eidx%d" % i) for i in range(9)]
            b_gsms = [Buf("gsm%d" % i) for i in range(9)]
            sel_ops = [[] for _ in range(9)]
            S.op("pool", lambda e: e.iota(iota16[:], pattern=[[1, 16]], base=0, channel_multiplier=0,
                                          allow_small_or_imprecise_dtypes=True), writes=[b_iota16])
            S.op("pool", lambda e: e.memset(eidx[:], 0), writes=b_eidxs)
            for tt in range(9):
                rows = rows_of[tt]
                b_eidx, b_gsm = b_eidxs[tt], b_gsms[tt]
                DEF = lambda eng, fn, reads=(), writes=(), _l=sel_ops[tt]: _l.append((eng, fn, list(reads), list(writes)))
                T = T1[0:rows, tt].rearrange("p (h j) k -> p h j k", j=2)
                G = gsm[0:rows, tt, :].rearrange("p (h k) -> p h k", h=8)
                DEF("dve", lambda e, T=T, rows=rows: e.tensor_tensor(
                    out=cand[0:rows].rearrange("p h (a b) -> p h a b", a=16),
                    in0=T[:, :, 0, :].unsqueeze(3).broadcast_to([rows, 8, 16, 16]),
                    in1=T[:, :, 1, :].unsqueeze(2).broadcast_to([rows, 8, 16, 16]), op=ALU.add),
                    reads=[b_T1], writes=[b_cand])
                for h in range(8):
                    DEF("dve", lambda e, h=h, rows=rows: e.max(out=ts[0:rows, h, 0:8], in_=cand[0:rows, h, :]),
                         reads=[b_cand], writes=[b_ts])
                    DEF("dve", lambda e, h=h, rows=rows: e.max_index(out=ic[0:rows, h, 0:8], in_max=ts[0:rows, h, 0:8],
                                                                   in_values=cand[0:rows, h, :]),
                         reads=[b_cand, b_ts], writes=[b_ic])
                    DEF("dve", lambda e, h=h, rows=rows: e.match_replace(out=cand2[0:rows, :], in_to_replace=ts[0:rows, h, 0:8],
                                                                       in_values=cand[0:rows, h, :], imm_value=-3.0e38),
                         reads=[b_cand, b_ts], writes=[b_cand2])
                    DEF("dve", lambda e, h=h, rows=rows: e.max(out=ts[0:rows, h, 8:16], in_=cand2[0:rows, :]),
                         reads=[b_cand2], writes=[b_ts])
                    DEF("dve", lambda e, h=h, rows=rows: e.max_index(out=ic[0:rows, h, 8:16], in_max=ts[0:rows, h, 8:16],
                                                                   in_values=cand2[0:rows, :]),
                         reads=[b_cand2, b_ts], writes=[b_ic])
                DEF("dve", lambda e, rows=rows: e.tensor_single_scalar(out=icw[0:rows, 0], in_=ic[0:rows], scalar=c4[0:rows, 0:1],
                                                                       op=ALU.logical_shift_right), reads=[b_ic, b_c4], writes=[b_icw])
                DEF("dve", lambda e, rows=rows: e.tensor_single_scalar(out=icw[0:rows, 1], in_=ic[0:rows], scalar=c4[0:rows, 1:2],
                                                                       op=ALU.bitwise_and), reads=[b_ic, b_c4], writes=[b_icw])
                DEF("dve", lambda e, rows=rows: e.tensor_copy(out=icf[0:rows], in_=icw[0:rows]), reads=[b_icw], writes=[b_icf])
                DEF("dve", lambda e, rows=rows, tt=tt: e.tensor_copy(out=i1f[0:rows], in_=I1[0:rows, tt]), reads=[b_I1], writes=[b_i1f])
                I = i1f[0:rows].rearrange("p (h j) k -> p h j k", j=2)
                for side in range(2):
                    DEF("dve", lambda e, side=side, rows=rows: e.tensor_tensor(
                        out=oh[0:rows], in0=icf[0:rows, side].unsqueeze(3).broadcast_to([rows, 8, 16, 16]),
                        in1=iota16[0:rows].unsqueeze(1).unsqueeze(1).broadcast_to([rows, 8, 16, 16]), op=ALU.is_equal),
                        reads=[b_icf, b_iota16], writes=[b_oh])
                    DEF("dve", lambda e, side=side, rows=rows, I=I: e.tensor_tensor(
                        out=oh[0:rows], in0=oh[0:rows], in1=I[:, :, side, :].unsqueeze(2).broadcast_to([rows, 8, 16, 16]),
                        op=ALU.mult), reads=[b_oh, b_i1f], writes=[b_oh])
                    DEF("dve", lambda e, side=side, rows=rows: e.tensor_reduce(out=ef[0:rows, side], in_=oh[0:rows], axis=AX.X,
                                                                             op=ALU.add), reads=[b_oh], writes=[b_ef])
                DEF("dve", lambda e, rows=rows: e.scalar_tensor_tensor(
                    out=ef[0:rows, 0], in0=ef[0:rows, 0], scalar=128.0, in1=ef[0:rows, 1], op0=ALU.mult, op1=ALU.add),
                    reads=[b_ef], writes=[b_ef])
                DEF("dve", lambda e, rows=rows, tt=tt: e.tensor_copy(out=eidx[0:rows, tt, :].rearrange("p (h k) -> p h k", h=8),
                                                                   in_=ef[0:rows, 0]), reads=[b_ef], writes=[b_eidx])
                DEF("dve", lambda e, rows=rows, G=G: e.tensor_tensor(
                    out=G, in0=ts[0:rows], in1=ts[0:rows, :, 0:1].broadcast_to([rows, 8, 16]), op=ALU.subtract),
                    reads=[b_ts], writes=[b_gsm])
                DEF("act", lambda e, G=G: e.activation(out=G, in_=G, func=AF.Exp), reads=[b_gsm], writes=[b_gsm])
                DEF("dve", lambda e, rows=rows, G=G: e.tensor_reduce(out=gs2[0:rows, 0:8], in_=G, axis=AX.X, op=ALU.add),
                     reads=[b_gsm], writes=[b_gs2])
                DEF("dve", lambda e, rows=rows: e.reciprocal(out=gs2[0:rows, 8:16], in_=gs2[0:rows, 0:8]), reads=[b_gs2], writes=[b_gs2])
                DEF("dve", lambda e, rows=rows, G=G: e.tensor_tensor(
                    out=G, in0=G, in1=gs2[0:rows, 8:16].unsqueeze(2).broadcast_to([rows, 8, 16]), op=ALU.mult),
                    reads=[b_gsm, b_gs2], writes=[b_gsm])
            for o in sel_ops[0]:
                S.op(*o)
            sel_ops[0] = []

            gffn_b, b_gffn_b, r_gffn_b = sb("gffn_b", [128, D], F32)
            S.dma("sp", lambda e: e.dma_start(out=gffn_b[:], in_=gvec[1, :].partition_broadcast(128)), writes=[b_gffn_b])
            actp, _, r_actp = sb("actp", [128, 128], F32)
            coef, b_coef, r_coef = sb("coef", [128, 2, 128], F32)
            b_actps = [Buf("actp%d" % i) for i in range(4)]
            h2t, b_h2t, r_h2t = sb("h2t", [128, D], F32)
            NG = 8
            LOOK = NG - 2
            ug = [sb("ug%d" % i, [128, 2, D], BF16) for i in range(NG - 4)]
            for wi in range(2):
                wa, wb_ = wslot[wi][2]
                for hh in range(2):
                    h_ = nc.alloc_sbuf_tensor_at("ugw%d_%d" % (wi, hh), [128, 2, D], BF16, offset=wa + hh * 8192)
                    ug.append((h_, Buf("ugw%d_%d" % (wi, hh)), None))
            dgt = [sb("dgt%d" % i, [128, 128], BF16) for i in range(4)]
            pc_rows = pc16.rearrange("e j d -> e (j d)")
            jobs = [(tt, sidx) for tt in range(9) for sidx in range(128)]
            gstate = {"issued": 0, "dg": 0}
            gbuf = {}

            def g_issue_upto(n):
                while gstate["issued"] < min(n, len(jobs)):
                    ji = gstate["issued"]
                    tt, sidx = jobs[ji]
                    t, bf, _ = ug[ji % NG]
                    S.dma("pool", lambda e, t=t, tt=tt, sidx=sidx: e.indirect_dma_start(
                        out=t[:].rearrange("p j d -> p (j d)"), out_offset=None, in_=pc_rows,
                        in_offset=bass.IndirectOffsetOnAxis(ap=eidx[:, tt, sidx:sidx + 1], axis=0)),
                        reads=[b_eidxs[tt], b_tab], writes=[bf])
                    gbuf[ji] = (t, bf)
                    gstate["issued"] += 1

            for ji, (tt, sidx) in enumerate(jobs):
                rows = rows_of[tt]
                if sidx == 0:
                    S.op("dve", lambda e, rows=rows, tt=tt: e.scalar_tensor_tensor(
                        out=h2t[0:rows, :], in0=X2[0:rows, tt, :], scalar=rstd2[0:rows, tt:tt + 1], in1=gffn_b[0:rows, :],
                        op0=ALU.mult, op1=ALU.mult), reads=[bX2[tt], b_rstd2, b_gffn_b], writes=[b_h2t])
                if tt + 1 < 9:
                    nxt = sel_ops[tt + 1]
                    take = len(nxt) if sidx >= 127 - LOOK - 1 else min(1, len(nxt))
                    for o in nxt[:take]:
                        S.op(*o)
                    del nxt[:take]
                g_issue_upto(ji + 1 + LOOK)
                t, bf = gbuf.pop(ji)
                b_ap = b_actps[sidx % 4]
                S.op("dve", lambda e, t=t, sidx=sidx, rows=rows: e.scalar_tensor_tensor(
                    out=t[0:rows, 0, :], in0=t[0:rows, 0, :], scalar=1.0, in1=h2t[0:rows, :], op0=ALU.mult, op1=ALU.mult,
                    accum_out=actp[0:rows, sidx:sidx + 1]), reads=[b_h2t], writes=[bf, b_ap])
                S.op("act", lambda e, rows=rows, sidx=sidx: e.activation(out=coef[0:rows, 0, sidx:sidx + 1], in_=actp[0:rows, sidx:sidx + 1],
                                                                    func=AF.Gelu_apprx_tanh), reads=[b_ap], writes=[b_coef])
                S.op("act", lambda e, rows=rows, sidx=sidx, tt=tt: e.activation(
                    out=coef[0:rows, 1, sidx:sidx + 1], in_=coef[0:rows, 0, sidx:sidx + 1], func=AF.Copy,
                    scale=gsm[0:rows, tt, sidx:sidx + 1]), reads=[b_coef, b_gsms[tt]], writes=[b_coef])
                dg_t, b_dg, _ = dgt[gstate["dg"] % 4]; gstate["dg"] += 1
                S.op("act", lambda e, dg_t=dg_t, rows=rows, sidx=sidx: e.activation(
                    out=dg_t[0:rows, 0:rows], in_=identB[0:rows, 0:rows], func=AF.Copy, scale=coef[0:rows, 1, sidx:sidx + 1]),
                    reads=[b_identB, b_coef], writes=[b_dg])
                first, last = (sidx == 0), (sidx == 127)
                for c in range(4):
                    pt, pbuf = PB[(tt % 2) * 4 + c]
                    S.op("pe", lambda e, pt=pt, c=c, t=t, dg_t=dg_t, rows=rows, first=first, last=last: e.matmul(
                        pt[0:rows, :], lhsT=dg_t[0:rows, 0:rows], rhs=t[0:rows, 1, c * 512:(c + 1) * 512], start=first, stop=last),
                        reads=[b_dg, bf], writes=[pbuf], signal=(c == 3))
                if last:
                    for c in range(4):
                        pt, pbuf = PB[(tt % 2) * 4 + c]
                        S.op("dve", lambda e, pt=pt, c=c, rows=rows, tt=tt: e.tensor_tensor(
                            out=X2[0:rows, tt, c * 512:(c + 1) * 512], in0=X2[0:rows, tt, c * 512:(c + 1) * 512],
                            in1=pt[0:rows, :], op=ALU.add), reads=[pbuf, bX2[tt]], writes=[bX2[tt]])

            if STOP < 8:
                return
            S.barrier()
            for r in [r_gffn_b, r_h2t, r_actp, r_coef, r_cand, r_cand2, r_ts, r_ic, r_icw, r_icf, r_i1f, r_iota16, r_ef, r_gs2,
                      r_T1, r_I1] + [u[2] for u in ug if u[2] is not None] + [d_[2] for d_ in dgt]:
                AR.release(r)
            x3T, b_x3T, r_x3T = sb("x3T", [128, 16, TOK + NS], BF16)
            xnb = [sb("xnb%d" % i, [128, D], BF16) for i in range(2)]
            pT_, b_pT, _ = sb("pT", [128, 2, TOK + NS], BF16)
            pst, b_pst, _ = sb("pst", [128, 256], F32)
            psb, b_psb, _ = sb("psb", [128, 256], BF16)
            wpl, b_wpl, _ = sb("wpl", [128, 2, D], BF16)
            S.dma("pool", lambda e: e.dma_start(out=wpl[:], in_=w_ple.rearrange("(k p) c -> p k c", p=128)), writes=[b_wpl])
            for tt in range(9):
                rows = rows_of[tt]
                xn, bxn, _ = xnb[tt % 2]
                S.op("act", lambda e, xn=xn, tt=tt, rows=rows: e.copy(out=xn[0:rows, :], in_=X2[0:rows, tt, :]),
                     reads=[bX2[tt]], writes=[bxn])
                transpose_to(x3T, b_x3T, tt * 128, xn, bxn, rows, None)
                src = ploc[tt * 128:(tt + 1) * 128, :] if tt < 8 else psm
                S.dma("sp", lambda e, src=src, rows=rows: e.dma_start(out=pst[0:rows, 0:256], in_=src), writes=[b_pst])
                psbv = psb
                S.op("act", lambda e, rows=rows, psbv=psbv: e.copy(out=psbv[0:rows, 0:256], in_=pst[0:rows, 0:256]),
                     reads=[b_pst], writes=[b_psb])
                pt2, pbuf2 = bank("C")
                ptb = pt2[:].bitcast(BF16)
                for j in range(2):
                    S.op("pe", lambda e, j=j, ptb=ptb, rows=rows, psbv=psbv: e.transpose(
                        out=ptb[:, j * 128:j * 128 + rows], in_=psbv[0:rows, j * 128:(j + 1) * 128], identity=identB[0:rows, 0:rows]),
                        reads=[b_psb, b_identB], writes=[pbuf2], signal=(j == 1))
                S.op("dve", lambda e, ptb=ptb, tt=tt, rows=rows: e.tensor_copy(
                    out=pT_[:, :, tt * 128:tt * 128 + rows], in_=ptb[:, 0:256].rearrange("p (j t) -> p j t", j=2)[:, :, 0:rows]),
                    reads=[pbuf2], writes=[b_pT])
            sig, b_sig, _ = sb("sig", [128, 512], F32)
            for cb in range(4):
                wg, bwg = w_next()
                for tt in range(9):
                    rows = rows_of[tt]
                    pg, pbg = bank("A")
                    mm_group(pg[0:rows, :], pbg, [(x3T[:, kc, tt * 128:tt * 128 + rows], wg[:, kc, :]) for kc in range(16)],
                             [b_x3T, bwg])
                    pe_, pbe = bank("A")
                    mm_group(pe_[0:rows, :], pbe, [(pT_[:, kc, tt * 128:tt * 128 + rows], wpl[:, kc, cb * 512:(cb + 1) * 512])
                                                   for kc in range(2)], [b_pT, b_wpl])
                    S.op("act", lambda e, pg=pg, rows=rows: e.activation(out=sig[0:rows, 0:512], in_=pg[0:rows, :], func=AF.Sigmoid),
                         reads=[pbg], writes=[b_sig])
                    S.op("dve", lambda e, pe_=pe_, rows=rows: e.tensor_tensor(out=sig[0:rows, 0:512], in0=sig[0:rows, 0:512],
                                                                             in1=pe_[0:rows, :], op=ALU.mult),
                         reads=[pbe, b_sig], writes=[b_sig])
                    S.op("dve", lambda e, tt=tt, cb=cb, rows=rows: e.tensor_tensor(
                        out=X2[0:rows, tt, cb * 512:(cb + 1) * 512], in0=X2[0:rows, tt, cb * 512:(cb + 1) * 512],
                        in1=sig[0:rows, 0:512], op=ALU.add), reads=[b_sig, bX2[tt]], writes=[bX2[tt]])

            if STOP < 9:
                return
            S.barrier()
            AR.release(r_x3T)
            gfin_b, b_gfin_b, _ = sb("gfin_b", [128, D], F32)
            S.dma("sp", lambda e: e.dma_start(out=gfin_b[:], in_=gvec[2, :].partition_broadcast(128)), writes=[b_gfin_b])
            SQ["t"], SQ["b"], SQ["r"] = sb("sqjunk", [128, D], BF16)
            xst = [sb("yo%d" % i, [128, D], F32) for i in range(2)]
            for tt in range(9):
                rows = rows_of[tt]
                rs = rms_stats(X2[0:rows, tt, :], rows, bX2[tt], tt % 2)
                yo, b_yo, _ = xst[tt % 2]
                S.op("dve", lambda e, yo=yo, rs=rs, tt=tt, rows=rows: e.scalar_tensor_tensor(
                    out=yo[0:rows, :], in0=X2[0:rows, tt, :], scalar=rs, in1=gfin_b[0:rows, :], op0=ALU.mult, op1=ALU.mult),
                    reads=[bX2[tt], b_stat, b_gfin_b], writes=[b_yo])
                dst = y[tt * 128:(tt + 1) * 128, :] if tt < 8 else ys
                S.dma("sp", lambda e, yo=yo, dst=dst, rows=rows: e.dma_start(out=dst, in_=yo[0:rows, :]), reads=[b_yo], dbuf=outb)

        phases()
        S.barrier()
        build_nc.sbuf_base = (nc.sbuf_base, nc.sbuf_top)
        S.wait_all("sp", [outb])
        S._wait("sp", (outb.sem, outb.cnt, "d_outb"))
        build_nc.stats = dict(ninstr=dict(S.ninstr), nsem=5 + len(S.dbufs))
    return nc


def _rope_rows(pos):
    half = 16
    inv = (np.float32(500000.0) ** (-(np.arange(half, dtype=np.float32)) / np.float32(half))).astype(np.float32)
    ang = pos.astype(np.float32)[:, None] * inv[None, :]
    c, s = np.cos(ang).astype(np.float32), np.sin(ang).astype(np.float32)
    return np.concatenate([c, c, -s, s], axis=1).astype(np.float32)


def _consts(half):
    pos = np.maximum(np.arange(2 * TOK) - TOK + TOK * half, 0)
    tab = _rope_rows(pos)
    rope = np.zeros((128, 56, 64), np.float32)
    p = np.arange(128)
    for g, d in enumerate(DIL):
        tpr = (2 * TOK // d) // 128
        for n in range(16):
            r, i0 = n // tpr, (n % tpr) * 128
            rope[:, g * 16 + n, :] = tab[(i0 + p) * d + r]
    for j in range(8):
        r = 2 * j + (p >= 64)
        i = 64 + (p % 64)
        rope[:, 48 + j, :] = tab[i * 16 + r]
    ropes = np.repeat(_rope_rows(np.array([2048])), NS, axis=0)
    cb = NEG if half == 0 else 0.0
    pp, ff = np.meshgrid(np.arange(128), np.arange(128), indexing="ij")
    m = np.zeros((128, 5, 128), np.float32)
    m[:, 0, :] = np.where(ff <= pp, 0.0, NEG)
    m[:, 1, :] = np.where(pp <= ff, 0.0, NEG)
    m[:, 2, :] = m[:, 0, :] + cb
    m[:, 3, :] = cb
    m[:, 4, :] = np.where(pp < 64, cb, np.where(pp - 64 <= ff, 0.0, NEG))
    return rope, ropes, m


def _in_maps(x_prompt, x_sample, cache_kv_w128, cache_kv_w512, cache_kv_w2048, p_prompt, p_sample, g_mix, w_in,
             sgu_ln_g, sgu_ln_b, w_s, b_s, w_a_out, w_b_out, w_o, g_ffn, peer_w_q, peer_sub_k1, peer_sub_k2,
             peer_u, peer_v, w_ple, w_ple_gate, g_final, cores=None):
    f = lambda a: np.ascontiguousarray(np.asarray(a, dtype=np.float32))
    shared = {
        "w_in": f(w_in[0]), "w_a_out": f(w_a_out[0]), "w_b_out": f(w_b_out[0]), "w_o": f(w_o[0]), "w_q": f(peer_w_q[0]),
        "w_pg": f(w_ple_gate[0]), "w_ple": f(w_ple[0]), "peer_u": f(peer_u[0]), "peer_v": f(peer_v[0]),
        "w_s": f(w_s[0]), "b_s": f(b_s[0]), "subk": f(np.stack([peer_sub_k1[0], peer_sub_k2[0]])),
        "gvec": f(np.stack([g_mix[0], g_ffn[0], g_final])), "lnv": f(np.stack([sgu_ln_g[0], sgu_ln_b[0]])),
    }
    maps = []
    for c in (range(NCORES) if cores is None else cores):
        b, half = c // 2, c % 2
        own = x_prompt[b, half * TOK:(half + 1) * TOK]
        ctx = x_prompt[b, 0:TOK]
        sl = slice(c * NS, (c + 1) * NS)
        caches = np.stack([
            np.asarray(cache_kv_w128[0, sl]).reshape(NS, 128, 1024),
            np.asarray(cache_kv_w512[0, sl, 0::4]).reshape(NS, 128, 1024),
            np.asarray(cache_kv_w2048[0, sl, 0::16]).reshape(NS, 128, 1024)])
        rope, ropes, m = _consts(half)
        d = dict(shared)
        d.update({"xloc": f(np.concatenate([ctx, own], axis=0)), "xs": f(x_sample[sl, 0]),
                  "ploc": f(p_prompt[0, b, half * TOK:(half + 1) * TOK]), "psm": f(p_sample[0, sl, 0]),
                  "cache": f(caches), "rope": rope, "ropes": ropes, "masks": m})
        maps.append(d)
    return maps


def _assemble(res):
    y = np.zeros((4, 2 * TOK, D), np.float32)
    ysm = np.zeros((128, 1, D), np.float32)
    k0 = np.zeros((1, 4, 128, 2, 4, 128), np.float32)
    k1 = np.zeros((1, 4, 512, 2, 4, 128), np.float32)
    k2 = np.zeros((1, 4, 2 * TOK, 2, 4, 128), np.float32)
    ks = [np.zeros((1, 128, 1, 2, 4, 128), np.float32) for _ in range(3)]
    sg = np.zeros((1, 128, 1, A_W), np.float32)
    for c, r in enumerate(res):
        b, half = c // 2, c % 2
        y[b, half * TOK:(half + 1) * TOK] = r["y"]
        ysm[c * NS:(c + 1) * NS, 0] = r["ys"]
        k2[0, b, half * TOK:(half + 1) * TOK] = r["kv2"].reshape(TOK, 2, 4, 128)
        if half == 1:
            k0[0, b] = r["kv0"].reshape(128, 2, 4, 128)
            k1[0, b] = r["kv1"].reshape(512, 2, 4, 128)
        for g in range(3):
            ks[g][0, c * NS:(c + 1) * NS, 0] = r["kvs"][g].reshape(NS, 2, 4, 128)
        sg[0, c * NS:(c + 1) * NS, 0] = r["sguv"]
    return (y, ysm, k0, k1, k2, ks[0], ks[1], ks[2], sg)


def kernel(**inputs):
    maps = _in_maps(**inputs)
    nc = build_nc()
    res = run_bass_kernel_spmd(nc, maps, core_ids=list(range(NCORES)))
    return _assemble(res.results)
```

```python
import contextlib
import os
import math
import numpy as np
import concourse.bass as bass
import concourse.mybir as mybir
from concourse.bass_utils import run_bass_kernel_spmd

F32 = mybir.dt.float32
BF16 = mybir.dt.bfloat16
I32 = mybir.dt.int32
U32 = mybir.dt.uint32
AF = mybir.ActivationFunctionType
ALU = mybir.AluOpType
AX = mybir.AxisListType
DTSIZE = {F32: 4, BF16: 2, I32: 4, U32: 4}

D = 2048
NCORES = 8
TOK = 1024
NS = 16
NCOL = 2 * TOK + NS
EPS = 1e-6
A_W = 1024
IN_COLS = 10752
C_UA, C_VA, C_Q, C_K, C_V, C_GA, C_GB = 0, 1024, 2048, 3584, 5120, 6656, 8704
DIL = (1, 4, 16)
SCALE = 128 ** -0.5
NEG = -30000.0
NEXP = 16384
PASSES = ((TOK, 512), (TOK + 512, 512), (2 * TOK, NS))


class Buf:
    __slots__ = ("name", "w", "r", "sem", "cnt", "excl")

    def __init__(self, name, excl=False):
        self.name = name
        self.excl = excl
        self.w = None
        self.r = {}
        self.sem = None
        self.cnt = 0


class Sched:
    def __init__(self, nc, stack):
        self.nc = nc
        self.stack = stack
        self.eng = {"pe": nc.tensor, "act": nc.scalar, "dve": nc.vector,
                    "pool": nc.gpsimd, "sp": nc.sync}
        self.sem = {k: stack.enter_context(nc.semaphore("s_" + k)) for k in self.eng}
        self.count = {k: 0 for k in self.eng}
        self.seen = {k: {} for k in self.eng}
        self.dbufs = []
        self.ninstr = {k: 0 for k in self.eng}

    def _wait(self, e, tok):
        if tok is None:
            return
        sem, val, key = tok
        if key == "pe" and e == "pe":
            return
        if self.seen[e].get(key, 0) >= val:
            return
        self.eng[e].wait_ge(sem, val)
        self.seen[e][key] = val

    def _deps(self, e, reads, writes):
        for b in reads:
            self._wait(e, b.w)
        for b in writes:
            self._wait(e, b.w)
            for t in b.r.values():
                self._wait(e, t)

    def _commit(self, tok, reads, writes):
        for b in reads:
            b.r[tok[2]] = tok
        for b in writes:
            b.w = tok
            b.r = {}

    def op(self, e, fn, reads=(), writes=(), signal=True):
        if any(b.excl for b in reads):
            writes = list(writes) + [b for b in reads if b.excl]
            reads = [b for b in reads if not b.excl]
        self._deps(e, reads, writes)
        ins = fn(self.eng[e])
        self.ninstr[e] += 1
        if signal:
            self.count[e] += 1
            ins.then_inc(self.sem[e], 1)
            tok = (self.sem[e], self.count[e], e)
        else:
            tok = (self.sem[e], self.count[e] + 1, e)
        self._commit(tok, reads, writes)
        return tok

    def dma(self, q, fn, reads=(), writes=(), dbuf=None):
        self._deps(q, reads, writes)
        if dbuf is None:
            dbuf = writes[0] if writes else reads[0]
        if dbuf.sem is None:
            dbuf.sem = self.stack.enter_context(self.nc.semaphore("d_" + dbuf.name))
            self.dbufs.append(dbuf)
        ins = fn(self.eng[q])
        dbuf.cnt += 16
        ins.then_inc(dbuf.sem, 16)
        tok = (dbuf.sem, dbuf.cnt, "d_" + dbuf.name)
        self._commit(tok, reads, writes)
        self.ninstr[q] += 1
        return tok

    def wait_all(self, e, bufs):
        for b in bufs:
            self._wait(e, b.w)
            for t in b.r.values():
                self._wait(e, t)

    def barrier(self):
        for e in self.eng:
            for x in ("pe", "act", "dve", "pool"):
                if x != e and self.count[x] > 0:
                    self._wait(e, (self.sem[x], self.count[x], x))
            for b in self.dbufs:
                if b.cnt > 0:
                    self._wait(e, (b.sem, b.cnt, "d_" + b.name))


class Arena:
    def __init__(self, nc, lo, hi):
        self.nc = nc
        self.free = [(lo, hi)]
        self.n = 0
        self.peak = 0
        self.hi = hi

    def alloc(self, name, shape, dt, top=False):
        nb = int(np.prod(shape[1:])) * DTSIZE[dt]
        nb = (nb + 63) // 64 * 64
        order = range(len(self.free) - 1, -1, -1) if top else range(len(self.free))
        for i in order:
            a, b = self.free[i]
            if b - a >= nb:
                if top:
                    off = b - nb
                    self.free[i] = (a, off)
                else:
                    off = a
                    self.free[i] = (a + nb, b)
                if self.free[i][0] == self.free[i][1]:
                    del self.free[i]
                self.n += 1
                used = self.hi - sum(y - x for x, y in self.free)
                self.peak = max(self.peak, used)
                h = self.nc.alloc_sbuf_tensor_at("%s_%d" % (name, self.n), list(shape), dt, offset=off)
                return h, (off, off + nb)
        raise RuntimeError("SBUF arena exhausted allocating %s %s (free=%s)" % (name, shape, self.free))

    def release(self, region):
        self.free.append(region)
        self.free.sort()
        merged = []
        for a, b in self.free:
            if merged and merged[-1][1] == a:
                merged[-1] = (merged[-1][0], b)
            else:
                merged.append((a, b))
        self.free = merged


def build_nc():
    nc = bass.Bass("TRN2", target_bir_lowering=False)
    SKIP = set(os.environ.get('KSKIP', '').split(','))
    NEXP_ = NEXP if int(os.environ.get('KSTOP', '99')) >= 7 else 128
    di = lambda name, shape, dt=F32: nc.dram_tensor(name, list(shape), dt, kind="ExternalInput").ap()
    do = lambda name, shape, dt=F32: nc.dram_tensor(name, list(shape), dt, kind="ExternalOutput").ap()

    xloc = di("xloc", [2 * TOK, D]); xs = di("xs", [NS, D])
    ploc = di("ploc", [TOK, 256]); psm = di("psm", [NS, 256])
    cache = di("cache", [3, NS, 128, 1024])
    w_in = di("w_in", [D, IN_COLS]); w_a_out = di("w_a_out", [A_W, D]); w_b_out = di("w_b_out", [512, D])
    w_o = di("w_o", [D, D]); w_q = di("w_q", [D, D]); w_pg = di("w_pg", [D, D]); w_ple = di("w_ple", [256, D])
    peer_u = di("peer_u", [NEXP_, D]); peer_v = di("peer_v", [NEXP_, D])
    w_s = di("w_s", [8, 128, 128]); b_s = di("b_s", [8, 128]); subk = di("subk", [2, 128, 128])
    gvec = di("gvec", [3, D]); lnv = di("lnv", [2, A_W])
    rope = di("rope", [128, 56, 64]); ropes = di("ropes", [NS, 64]); masks = di("masks", [128, 5, 128])

    y = do("y", [TOK, D]); ys = do("ys", [NS, D])
    kv0 = do("kv0", [128, 1024]); kv1 = do("kv1", [512, 1024]); kv2 = do("kv2", [TOK, 1024])
    kvs = do("kvs", [3, NS, 1024]); sguv = do("sguv", [NS, A_W])
    kvout = (kv0, kv1, kv2)
    pc16 = nc.dram_tensor("pc16", [NEXP_, 2, D], BF16, kind="Internal").ap()

    with contextlib.ExitStack() as st:
        S = Sched(nc, st)
        AR = Arena(nc, 16512, 229344)
        outb = Buf("outb")

        def sb(name, shape, dt, top=False):
            h, reg = AR.alloc(name, shape, dt, top)
            return h, Buf(name), reg

        PB = []
        for i in range(8):
            t = nc.alloc_psum_tensor("pb%d" % i, [128, 512], F32)
            PB.append((t, Buf("pb%d" % i, excl=True)))
        rr = {"A": 0, "B": 0, "C": 0}
        pools = {"A": (0, 1, 2, 3), "B": (4, 5), "C": (6, 7)}

        def bank(pool):
            ids = pools[pool]
            i = ids[rr[pool] % len(ids)]
            rr[pool] += 1
            return PB[i]

        def mm_group(out_ap, pbuf, pairs, reads):
            n = len(pairs)
            for i, (l, r) in enumerate(pairs):
                S.op("pe", lambda e, l=l, r=r, i=i: e.matmul(out_ap, lhsT=l, rhs=r, start=(i == 0), stop=(i == n - 1)),
                     reads=reads, writes=[pbuf], signal=(i == n - 1))

        identF, b_identF, _ = sb("identF", [128, 128], F32)
        identB, b_identB, _ = sb("identB", [128, 128], BF16)
        onesB, b_onesB, _ = sb("onesB", [128, 128], BF16)
        S.op("pool", lambda e: e.memset(identF[:], 1.0), writes=[b_identF])
        S.op("pool", lambda e: e.affine_select(out=identF[:], in_=identF[:], pattern=[[-1, 128]],
                                               compare_op=ALU.is_equal, fill=0.0, base=0, channel_multiplier=1),
             reads=[b_identF], writes=[b_identF])
        S.op("pool", lambda e: e.tensor_copy(out=identB[:], in_=identF[:]), reads=[b_identF], writes=[b_identB])
        S.op("pool", lambda e: e.memset(onesB[:], 1.0), writes=[b_onesB])

        gcol, b_gcol, _ = sb("gcol", [128, 2, 16], F32)
        grow, b_grow, r_grow = sb("grow", [16, 2, 128], F32)
        S.dma("sp", lambda e: e.dma_start(out=grow[:], in_=gvec[0:2, :].rearrange("g (k p) -> k g p", p=128)),
              writes=[b_grow])
        for gi in range(2):
            pt, pbuf = bank("C")
            S.op("pe", lambda e, gi=gi, pt=pt: e.transpose(out=pt[:, 0:16], in_=grow[:, gi, :], identity=identF[0:16, 0:16]),
                 reads=[b_grow, b_identF], writes=[pbuf])
            S.op("act", lambda e, gi=gi, pt=pt: e.copy(out=gcol[:, gi, :], in_=pt[:, 0:16]), reads=[pbuf], writes=[b_gcol])
        maskB, b_maskB, _ = sb("maskB", [128, 5, 128], BF16)
        S.dma("pool", lambda e: e.dma_start(out=maskB[:], in_=masks), writes=[b_maskB])
        c4, b_c4, _ = sb("c4", [128, 2], U32)
        S.op("pool", lambda e: e.memset(c4[:, 0:1], 4), writes=[b_c4])
        S.op("pool", lambda e: e.memset(c4[:, 1:2], 15), writes=[b_c4])
        ropeS, b_ropeS, _ = sb("ropeS", [NS, 64], F32)
        S.dma("sp", lambda e: e.dma_start(out=ropeS[:], in_=ropes), writes=[b_ropeS])

        NW = 2
        wslot = [sb("wslot%d" % i, [128, 16, 512], BF16) for i in range(NW)]
        wq = []
        wstate = {"issued": 0, "used": 0}

        def w_plan(blocks):
            wq.extend(blocks)

        b_tab = Buf("ptab")
        TCH = 1024 if NEXP_ >= 1024 else NEXP_
        tab_jobs = [(src, j, r0) for r0 in range(0, NEXP_, TCH) for (src, j) in ((peer_u, 0), (peer_v, 1))]

        def tab_issue(k=1):
            for _ in range(k):
                if not tab_jobs:
                    return
                src, j, r0 = tab_jobs.pop(0)
                tok = S.dma("pool", lambda e: e.dma_start(out=pc16[r0:r0 + TCH, j, :], in_=src[r0:r0 + TCH, :]), dbuf=b_tab)
                b_tab.w = tok

        def w_issue_upto(n):
            while wstate["issued"] < min(n, len(wq)):
                i = wstate["issued"]
                ap = wq[i]
                K, C = ap.shape
                kc = K // 128
                t, bf, _ = wslot[i % NW]
                S.dma("pool", lambda e, t=t, ap=ap, kc=kc, C=C: e.dma_start(
                    out=t[:, 0:kc, 0:C], in_=ap.rearrange("(k p) c -> p k c", p=128)), writes=[bf])
                wstate["issued"] += 1
                tab_issue(2)

        def w_next(issue=True):
            i = wstate["used"]
            if issue:
                w_issue_upto(i + NW)
            wstate["used"] += 1
            t, bf, _ = wslot[i % NW]
            return t, bf

        xst = [sb("xst%d" % i, [128, D], F32) for i in range(2)]
        xnb = [sb("xnb%d" % i, [128, D], BF16) for i in range(2)]
        SQ = {}
        SQ["t"], SQ["b"], SQ["r"] = sb("sqjunk", [128, D], BF16)
        stat, b_stat, _ = sb("stat", [128, 8], F32)

        def rms_stats(src_ap, rows, b_src, slot):
            ss = stat[0:rows, slot * 2:slot * 2 + 1]
            rs = stat[0:rows, slot * 2 + 1:slot * 2 + 2]
            sq_junk, b_sq_junk = SQ["t"], SQ["b"]
            S.op("act", lambda e: e.activation(out=sq_junk[0:rows, :], in_=src_ap, func=AF.Square, accum_out=ss),
                 reads=[b_src], writes=[b_sq_junk, b_stat])
            S.op("dve", lambda e: e.tensor_scalar(out=ss, in0=ss, scalar1=1.0 / D, scalar2=EPS, op0=ALU.mult, op1=ALU.add),
                 reads=[b_stat], writes=[b_stat])
            S.op("act", lambda e: e.sqrt(out=ss, in_=ss), reads=[b_stat], writes=[b_stat])
            S.op("dve", lambda e: e.reciprocal(out=rs, in_=ss), reads=[b_stat], writes=[b_stat])
            return rs

        def transpose_to(dstT, b_dstT, col0, src_bf, b_src, rows, gsel):
            for half in range(2):
                pt, pbuf = bank("B")
                ptb = pt[:].bitcast(BF16)
                for j in range(8):
                    kc = half * 8 + j
                    S.op("pe", lambda e, kc=kc, j=j, ptb=ptb: e.transpose(
                        out=ptb[:, j * 128:j * 128 + rows], in_=src_bf[0:rows, kc * 128:(kc + 1) * 128],
                        identity=identB[0:rows, 0:rows]),
                        reads=[b_src, b_identB], writes=[pbuf], signal=(j == 7))
                for j in range(8):
                    kc = half * 8 + j
                    eng = "dve" if half == 0 else "act"
                    if gsel is None:
                        if eng == "dve":
                            S.op("dve", lambda e, kc=kc, j=j, ptb=ptb: e.tensor_copy(
                                out=dstT[:, kc, col0:col0 + rows], in_=ptb[:, j * 128:j * 128 + rows]),
                                reads=[pbuf], writes=[b_dstT])
                        else:
                            S.op("act", lambda e, kc=kc, j=j, ptb=ptb: e.copy(
                                out=dstT[:, kc, col0:col0 + rows], in_=ptb[:, j * 128:j * 128 + rows]),
                                reads=[pbuf], writes=[b_dstT])
                    elif eng == "dve":
                        S.op("dve", lambda e, kc=kc, j=j, ptb=ptb: e.tensor_scalar(
                            out=dstT[:, kc, col0:col0 + rows], in0=ptb[:, j * 128:j * 128 + rows],
                            scalar1=gcol[:, gsel, kc:kc + 1], scalar2=None, op0=ALU.mult),
                            reads=[pbuf, b_gcol], writes=[b_dstT])
                    else:
                        S.op("act", lambda e, kc=kc, j=j, ptb=ptb: e.activation(
                            out=dstT[:, kc, col0:col0 + rows], in_=ptb[:, j * 128:j * 128 + rows],
                            func=AF.Copy, scale=gcol[:, gsel, kc:kc + 1]),
                            reads=[pbuf, b_gcol], writes=[b_dstT])

        STOP = int(os.environ.get('KSTOP', '99'))
        SUB = int(os.environ.get('KSUB', '99'))

        def phases():
            nonlocal xst, xnb
            if STOP < 0:
                return
            hT, b_hT, r_hT = sb("hT", [128, 16, NCOL], BF16)
            plan = []
            for g in range(3):
                plan += [w_in[:, C_K + g * 512:C_K + (g + 1) * 512], w_in[:, C_V + g * 512:C_V + (g + 1) * 512],
                         w_in[:, C_Q + g * 512:C_Q + (g + 1) * 512]]
            plan += [w_in[:, C_VA:C_VA + 512], w_in[:, C_VA + 512:C_VA + 1024]]
            plan += [w_in[:, C_UA:C_UA + 512], w_in[:, C_UA + 512:C_UA + 1024]]
            for cb in range(4):
                plan += [w_in[:, C_GA + cb * 512:C_GA + (cb + 1) * 512], w_a_out[:, cb * 512:(cb + 1) * 512],
                         w_in[:, C_GB + cb * 512:C_GB + (cb + 1) * 512], w_b_out[:, cb * 512:(cb + 1) * 512]]
            for cb in range(4):
                plan += [w_o[:, cb * 512:(cb + 1) * 512]]
            for cb in range(4):
                plan += [w_q[:, cb * 512:(cb + 1) * 512]]
            for cb in range(4):
                plan += [w_pg[:, cb * 512:(cb + 1) * 512]]
            w_plan(plan)
            w_issue_upto(NW)

            tiles = [(xloc[n * 128:(n + 1) * 128, :], 128, n * 128) for n in range(16)] + [(xs, NS, 2 * TOK)]

            def load_x(i):
                src, rows, _ = tiles[i]
                t, bf, _ = xst[i % 2]
                S.dma("sp", lambda e: e.dma_start(out=t[0:rows, :], in_=src), writes=[bf])

            load_x(0)
            for i, (src, rows, col0) in enumerate(tiles):
                if i + 1 < len(tiles):
                    load_x(i + 1)
                t, bf, _ = xst[i % 2]
                xn, bxn, _ = xnb[i % 2]
                rs = rms_stats(t[0:rows, :], rows, bf, i % 2)
                S.op("act", lambda e, t=t, xn=xn, rs=rs, rows=rows: e.activation(
                    out=xn[0:rows, :], in_=t[0:rows, :], func=AF.Copy, scale=rs), reads=[bf, b_stat], writes=[bxn])
                transpose_to(hT, b_hT, col0, xn, bxn, rows, 0)
            S.barrier()
            for r in (xst[0][2], xst[1][2], xnb[0][2], xnb[1][2], SQ["r"], r_grow):
                AR.release(r)

            if STOP < 1:
                return
            ACC, b_ACC, r_ACC = sb("ACC", [128, 2, 4, TOK], F32)
            KT, b_KT, r_KT = sb("KT", [128, 4, 2 * TOK], BF16)
            QT, b_QT, r_QT = sb("QT", [128, 4, TOK], BF16)
            VG, b_VG, r_VG = sb("VG", [128, 16, 512], BF16)
            kf = [sb("kf%d" % i, [128, 512], F32) for i in range(2)]
            kb = [sb("kb%d" % i, [128, 512], BF16) for i in range(2)]
            rtmp, b_rtmp, r_rtmp = sb("rtmp", [128, 2, 4, 32], F32)
            PT = [sb("PT%d" % i, [128, 2, 128], BF16) for i in range(2)]
            qkv_c, b_qkv_c, r_qkv_c = sb("qkv_c", [128, 2, 512], F32)
            stg = [sb("stg%d" % i, [NS, 512], F32) for i in range(2)]
            ropeG, b_ropeG, r_ropeG = sb("ropeG", [128, 16, 64], F32)
            ropeQ2, b_ropeQ2, r_ropeQ2 = sb("ropeQ2", [128, 8, 64], F32)
            S.dma("sp", lambda e: e.dma_start(out=ropeQ2[:], in_=rope[:, 48:56, :]), writes=[b_ropeQ2])
            cnt = {"kf": 0, "kb": 0, "pt": 0, "stg": 0}

            def stash_sample(pt, pbuf, which, g):
                blk = which * 3 + g
                if "stash" in SKIP:
                    return
                t, bt, _ = stg[cnt["stg"] % 2]; cnt["stg"] += 1
                S.op("act", lambda e: e.copy(out=t[:], in_=pt[0:NS, :]), reads=[pbuf], writes=[bt])
                j, slot = blk % 8, blk // 8
                S.dma("sp", lambda e: e.dma_start(out=qkv_c[16 * j:16 * j + 16, slot, :], in_=t[:]), reads=[bt], writes=[b_qkv_c])

            def rope_apply(t, bt, rows, tab_ap, b_tab):
                x4 = t[0:rows, :].rearrange("p (h d) -> p h d", h=4)
                cc = tab_ap[:, 0:32].unsqueeze(1).broadcast_to([rows, 4, 32])
                s1 = tab_ap[:, 32:48].unsqueeze(1).broadcast_to([rows, 4, 16])
                s2 = tab_ap[:, 48:64].unsqueeze(1).broadcast_to([rows, 4, 16])
                A = rtmp[0:rows, 0]
                B = rtmp[0:rows, 1]
                if "rope" in SKIP:
                    return
                S.op("dve", lambda e: e.tensor_tensor(out=A, in0=x4[:, :, 0:32], in1=cc, op=ALU.mult),
                     reads=[bt, b_tab], writes=[b_rtmp])
                S.op("dve", lambda e: e.tensor_tensor(out=B[:, :, 0:16], in0=x4[:, :, 16:32], in1=s1, op=ALU.mult),
                     reads=[bt, b_tab], writes=[b_rtmp])
                S.op("dve", lambda e: e.tensor_tensor(out=B[:, :, 16:32], in0=x4[:, :, 0:16], in1=s2, op=ALU.mult),
                     reads=[bt, b_tab], writes=[b_rtmp])
                S.op("dve", lambda e: e.tensor_tensor(out=x4[:, :, 0:32], in0=A, in1=B, op=ALU.add),
                     reads=[b_rtmp], writes=[bt])

            def gtile_cols(g, n):
                d = DIL[g]
                tpr = (2 * TOK // d) // 128
                r, i0 = n // tpr, (n % tpr) * 128
                start = i0 * d + r
                return slice(start, start + 127 * d + 1, d), r, i0

            first_group = True
            for g in range(3):
                d = DIL[g]
                L = 2 * TOK // d
                tpr = L // 128
                Lq = TOK // d
                if g == 0:
                    ktiles = list(range(7, 16))
                elif g == 1:
                    ktiles = [n for n in range(16) if n % 4 >= 1]
                else:
                    ktiles = list(range(16))
                S.dma("sp", lambda e, g=g: e.dma_start(out=ropeG[:], in_=rope[:, g * 16:(g + 1) * 16, :]), writes=[b_ropeG])
                wt, bw = w_next()
                for n in ktiles + ["s"]:
                    pt, pbuf = bank("A")
                    if n == "s":
                        rows = NS
                        lhs = lambda kc: hT[:, kc, 2 * TOK:2 * TOK + NS]
                    else:
                        rows = 128
                        sl, r, i0 = gtile_cols(g, n)
                        lhs = lambda kc, sl=sl: hT[:, kc, sl]
                    mm_group(pt[0:rows, :], pbuf, [(lhs(kc), wt[:, kc, :]) for kc in range(16)], [b_hT, bw])
                    if n == "s":
                        stash_sample(pt, pbuf, 1, g)
                        continue
                    t, bt, _ = kf[cnt["kf"] % 2]; cnt["kf"] += 1
                    S.op("act", lambda e, pt=pt, t=t: e.copy(out=t[:], in_=pt[:]), reads=[pbuf], writes=[bt])
                    rope_apply(t, bt, 128, ropeG[:, n, :], b_ropeG)
                    if "kvout" in SKIP:
                        pass
                    elif g == 0 and n == 15:
                        S.dma("sp", lambda e, t=t: e.dma_start(out=kv0[:, 0:512], in_=t[:]), reads=[bt], dbuf=outb)
                    elif g == 1 and n % 4 == 3:
                        S.dma("sp", lambda e, t=t, r=r: e.dma_start(out=kv1[r:512:4, 0:512], in_=t[:]), reads=[bt], dbuf=outb)
                    elif g == 2:
                        S.dma("sp", lambda e, t=t, r=r: e.dma_start(out=kv2[r:TOK:16, 0:512], in_=t[64:128, :]), reads=[bt], dbuf=outb)
                    tb, btb, _ = kb[cnt["kb"] % 2]; cnt["kb"] += 1
                    S.op("act", lambda e, t=t, tb=tb: e.copy(out=tb[:], in_=t[:]), reads=[bt], writes=[btb])
                    if "ktr" in SKIP:
                        continue
                    pt2, pbuf2 = bank("B")
                    ptb = pt2[:].bitcast(BF16)
                    for h in range(4):
                        S.op("pe", lambda e, h=h, ptb=ptb, tb=tb: e.transpose(out=ptb[:, h * 128:(h + 1) * 128],
                                                                        in_=tb[:, h * 128:(h + 1) * 128], identity=identB[:]),
                             reads=[btb, b_identB], writes=[pbuf2], signal=(h == 3))
                    S.op("dve", lambda e, ptb=ptb, n=n: e.tensor_copy(out=KT[:, :, n * 128:(n + 1) * 128],
                                                                 in_=ptb[:, 0:512].rearrange("p (h t) -> p h t", h=4)),
                         reads=[pbuf2], writes=[b_KT])
                if SUB < 0:
                    return
                wt, bw = w_next()
                for n in ktiles + ["s"]:
                    pt, pbuf = bank("A")
                    if n == "s":
                        mm_group(pt[0:NS, :], pbuf, [(hT[:, kc, 2 * TOK:2 * TOK + NS], wt[:, kc, :]) for kc in range(16)], [b_hT, bw])
                        stash_sample(pt, pbuf, 2, g)
                        continue
                    sl, r, i0 = gtile_cols(g, n)
                    mm_group(pt[:, :], pbuf, [(hT[:, kc, sl], wt[:, kc, :]) for kc in range(16)], [b_hT, bw])
                    own_out = ((g == 0 and n == 15) or (g == 1 and n % 4 == 3) or (g == 2)) and "kvout" not in SKIP
                    if own_out:
                        t, bt, _ = kf[cnt["kf"] % 2]; cnt["kf"] += 1
                        S.op("act", lambda e, pt=pt, t=t: e.copy(out=t[:], in_=pt[:]), reads=[pbuf], writes=[bt])
                        if g == 0:
                            S.dma("sp", lambda e, t=t: e.dma_start(out=kv0[:, 512:1024], in_=t[:]), reads=[bt], dbuf=outb)
                        elif g == 1:
                            S.dma("sp", lambda e, t=t, r=r: e.dma_start(out=kv1[r:512:4, 512:1024], in_=t[:]), reads=[bt], dbuf=outb)
                        else:
                            S.dma("sp", lambda e, t=t, r=r: e.dma_start(out=kv2[r:TOK:16, 512:1024], in_=t[64:128, :]),
                                  reads=[bt], dbuf=outb)
                    if own_out:
                        S.op("dve", lambda e, t=t, n=n: e.tensor_copy(out=VG[:, n, :], in_=t[:]), reads=[bt], writes=[b_VG])
                    else:
                        S.op("dve", lambda e, pt=pt, n=n: e.tensor_copy(out=VG[:, n, :], in_=pt[:]), reads=[pbuf], writes=[b_VG])
                wt, bw = w_next()
                if g == 0:
                    qtiles = [(n, gtile_cols(0, n)[0], ropeG[:, n, :], (n - 8) * 128) for n in range(8, 16)]
                elif g == 1:
                    qtiles = []
                    for n in range(16):
                        if n % 4 >= 2:
                            sl, r, i0 = gtile_cols(1, n)
                            qtiles.append((n, sl, ropeG[:, n, :], r * Lq + (i0 - Lq)))
                else:
                    qtiles = []
                    for r0 in range(0, 16, 2):
                        qtiles.append((r0, None, ropeQ2[:, r0 // 2, :], r0 * 64))
                for (n, sl, tab, qc0) in qtiles + [("s", None, None, None)]:
                    pt, pbuf = bank("A")
                    if n == "s":
                        mm_group(pt[0:NS, :], pbuf, [(hT[:, kc, 2 * TOK:2 * TOK + NS], wt[:, kc, :]) for kc in range(16)], [b_hT, bw])
                        stash_sample(pt, pbuf, 0, g)
                        continue
                    if g == 2:
                        for hf in range(2):
                            c0 = TOK + n + hf
                            mm_group(pt[hf * 64:(hf + 1) * 64, :], pbuf,
                                     [(hT[:, kc, c0:c0 + 63 * 16 + 1:16], wt[:, kc, :]) for kc in range(16)], [b_hT, bw])
                    else:
                        mm_group(pt[:, :], pbuf, [(hT[:, kc, sl], wt[:, kc, :]) for kc in range(16)], [b_hT, bw])
                    t, bt, _ = kf[cnt["kf"] % 2]; cnt["kf"] += 1
                    S.op("act", lambda e, pt=pt, t=t: e.copy(out=t[:], in_=pt[:]), reads=[pbuf], writes=[bt])
                    rope_apply(t, bt, 128, tab, b_ropeQ2 if g == 2 else b_ropeG)
                    tb, btb, _ = kb[cnt["kb"] % 2]; cnt["kb"] += 1
                    S.op("act", lambda e, t=t, tb=tb: e.copy(out=tb[:], in_=t[:]), reads=[bt], writes=[btb])
                    pt2, pbuf2 = bank("B")
                    ptb = pt2[:].bitcast(BF16)
                    for h in range(4):
                        S.op("pe", lambda e, h=h, ptb=ptb, tb=tb: e.transpose(out=ptb[:, h * 128:(h + 1) * 128],
                                                                        in_=tb[:, h * 128:(h + 1) * 128], identity=identB[:]),
                             reads=[btb, b_identB], writes=[pbuf2], signal=(h == 3))
                    S.op("dve", lambda e, ptb=ptb, qc0=qc0: e.tensor_copy(out=QT[:, :, qc0:qc0 + 128],
                                                                     in_=ptb[:, 0:512].rearrange("p (h t) -> p h t", h=4)),
                         reads=[pbuf2], writes=[b_QT])
                if SUB < 1 + 2 * g:
                    return
                TQ = 128 if g < 2 else 64
                for h in range(4):
                    for r in range(d):
                        for qt in range(Lq // TQ):
                            qc0 = r * Lq + qt * TQ
                            i0q = Lq + qt * TQ
                            if g < 2:
                                kprev = r * L + i0q - 128
                                blocks = [(kprev, 128, (kprev // 128), maskB[:, 2 if qt == 0 else 0, :]),
                                          (r * L + i0q, 128, (r * L + i0q) // 128, maskB[:, 1, :])]
                            else:
                                blocks = [(r * L, 128, r, maskB[:, 4, 0:64])]
                            nb = len(blocks)
                            ps_s, pb_s = bank("A")
                            for bi, (kc0, nk, vt, mk) in enumerate(blocks):
                                o = ps_s[0:nk, bi * 128:bi * 128 + TQ]
                                S.op("pe", lambda e, o=o, kc0=kc0, nk=nk, h=h, qc0=qc0: e.matmul(
                                    o, lhsT=KT[:, h, kc0:kc0 + nk], rhs=QT[:, h, qc0:qc0 + TQ], start=True, stop=False),
                                    reads=[b_KT, b_QT], writes=[pb_s], signal=False)
                                S.op("pe", lambda e, o=o, nk=nk, mk=mk: e.matmul(
                                    o, lhsT=identB[0:nk, 0:nk], rhs=mk, start=False, stop=True),
                                    reads=[b_identB, b_maskB], writes=[pb_s], signal=(bi == nb - 1))
                            p_t, b_p, _ = PT[cnt["pt"] % 2]; cnt["pt"] += 1
                            S.op("act", lambda e, ps_s=ps_s, p_t=p_t, nb=nb: e.activation(
                                out=p_t[:, 0:nb, 0:TQ], in_=ps_s[:].rearrange("p (b t) -> p b t", b=4)[:, 0:nb, 0:TQ],
                                func=AF.Exp, scale=SCALE), reads=[pb_s], writes=[b_p])
                            ps_o, pb_o = bank("B") if (cnt["pt"] % 2) else bank("C")
                            mm_group(ps_o[:, 0:TQ], pb_o, [(VG[:, vt, h * 128:(h + 1) * 128], p_t[:, bi, 0:TQ])
                                                          for bi, (kc0, nk, vt, mk) in enumerate(blocks)], [b_VG, b_p])
                            mm_group(ps_o[:, 128:128 + TQ], pb_o, [(onesB[:, :], p_t[:, bi, 0:TQ]) for bi in range(nb)],
                                     [b_onesB, b_p])
                            nat = slice(qt * TQ * d + r, qt * TQ * d + r + (TQ - 1) * d + 1, d)
                            src = ps_o[:].rearrange("p (b t) -> p b t", b=4)[:, 0:2, 0:TQ]
                            if first_group:
                                S.op("dve", lambda e, src=src, h=h, nat=nat: e.tensor_copy(out=ACC[:, :, h, nat], in_=src),
                                     reads=[pb_o], writes=[b_ACC])
                            else:
                                S.op("dve", lambda e, src=src, h=h, nat=nat: e.tensor_tensor(
                                    out=ACC[:, :, h, nat], in0=ACC[:, :, h, nat], in1=src, op=ALU.add),
                                    reads=[pb_o, b_ACC], writes=[b_ACC])
                first_group = False
                if SUB < 2 + 2 * g:
                    return
            S.barrier()
            for r in (r_KT, r_QT, r_VG, r_ropeG, r_ropeQ2, kf[0][2], kf[1][2], kb[0][2], kb[1][2], PT[0][2], PT[1][2]):
                AR.release(r)
            bmixT, b_bmixT, r_bmixT = sb("bmixT", [128, 4, TOK + NS], BF16)
            S.op("dve", lambda e: e.reciprocal(out=ACC[:, 1], in_=ACC[:, 1]), reads=[b_ACC], writes=[b_ACC])
            S.op("dve", lambda e: e.tensor_tensor(out=bmixT[:, :, 0:TOK], in0=ACC[:, 0], in1=ACC[:, 1], op=ALU.mult),
                 reads=[b_ACC], writes=[b_bmixT])
            qkv_s, b_qkv_s, r_qkv_s = sb("qkv_s", [NS, 3, 3, 512], F32)
            for which in range(3):
                for g in range(3):
                    blk = which * 3 + g
                    j, slot = blk % 8, blk // 8
                    S.dma("sp", lambda e, which=which, g=g, j=j, slot=slot: e.dma_start(
                        out=qkv_s[:, which, g, :], in_=qkv_c[16 * j:16 * j + 16, slot, :]), reads=[b_qkv_c], writes=[b_qkv_s])

            if SUB < 7:
                return
            for which in (0, 1):
                for g in range(3):
                    x4 = qkv_s[:, which, g, :].rearrange("p (h d) -> p h d", h=4)
                    cc = ropeS[:, 0:32].unsqueeze(1).broadcast_to([NS, 4, 32])
                    s1 = ropeS[:, 32:48].unsqueeze(1).broadcast_to([NS, 4, 16])
                    s2 = ropeS[:, 48:64].unsqueeze(1).broadcast_to([NS, 4, 16])
                    A = rtmp[0:NS, 0]
                    B = rtmp[0:NS, 1]
                    S.op("dve", lambda e, x4=x4, A=A, cc=cc: e.tensor_tensor(out=A, in0=x4[:, :, 0:32], in1=cc, op=ALU.mult),
                         reads=[b_qkv_s, b_ropeS], writes=[b_rtmp])
                    S.op("dve", lambda e, x4=x4, B=B, s1=s1: e.tensor_tensor(out=B[:, :, 0:16], in0=x4[:, :, 16:32], in1=s1, op=ALU.mult),
                         reads=[b_qkv_s, b_ropeS], writes=[b_rtmp])
                    S.op("dve", lambda e, x4=x4, B=B, s2=s2: e.tensor_tensor(out=B[:, :, 16:32], in0=x4[:, :, 0:16], in1=s2, op=ALU.mult),
                         reads=[b_qkv_s, b_ropeS], writes=[b_rtmp])
                    S.op("dve", lambda e, x4=x4, A=A, B=B: e.tensor_tensor(out=x4[:, :, 0:32], in0=A, in1=B, op=ALU.add),
                         reads=[b_rtmp], writes=[b_qkv_s])
            for g in range(3):
                S.dma("sp", lambda e, g=g: e.dma_start(out=kvs[g, :, 0:512], in_=qkv_s[:, 1, g, :]), reads=[b_qkv_s], dbuf=outb)
                S.dma("sp", lambda e, g=g: e.dma_start(out=kvs[g, :, 512:1024], in_=qkv_s[:, 2, g, :]), reads=[b_qkv_s], dbuf=outb)

            CK = [sb("CK%d" % i, [128, 1024], F32) for i in range(3)]
            SEL, b_SEL, r_SEL = sb("SEL", [NS, NS, 128], F32)
            SELT, b_SELT, r_SELT = sb("SELT", [128, NS, NS], F32)
            S.op("pool", lambda e: e.memset(SEL[:], 1.0), writes=[b_SEL])
            S.op("pool", lambda e: e.affine_select(out=SEL[:], in_=SEL[:], pattern=[[1, NS], [0, 128]], compare_op=ALU.is_equal,
                                                   fill=0.0, base=0, channel_multiplier=-1), reads=[b_SEL], writes=[b_SEL])
            S.op("pool", lambda e: e.memset(SELT[:], 1.0), writes=[b_SELT])
            S.op("pool", lambda e: e.affine_select(out=SELT[:], in_=SELT[:], pattern=[[1, NS], [-1, NS]], compare_op=ALU.is_equal,
                                                   fill=0.0, base=0, channel_multiplier=0), reads=[b_SELT], writes=[b_SELT])
            sprods = [sb("sprod%d" % i, [128, 512], F32) for i in range(3)]
            sscs = [sb("ssc%d" % i, [128, 8], F32) for i in range(3)]
            snew, b_snew, r_snew = sb("snew", [NS, 3, 512], F32)
            sn_s, b_sn_s, r_sn_s = sb("sn_s", [NS, 3, 8], F32)
            so, b_so, r_so = sb("so", [NS, 516], F32)
            sob, b_sob, r_sob = sb("sob", [NS, 512], BF16)
            ps_os, pb_os = PB[6]
            ps_ds, pb_ds = PB[7]
            S.op("dve", lambda e: e.tensor_tensor(out=snew[:], in0=qkv_s[:, 0], in1=qkv_s[:, 1], op=ALU.mult),
                 reads=[b_qkv_s], writes=[b_snew])
            S.op("dve", lambda e: e.tensor_reduce(out=sn_s[:, :, 0:4], in_=snew[:].rearrange("p g (h d) -> p g h d", h=4),
                                                  axis=AX.X, op=ALU.add), reads=[b_snew], writes=[b_sn_s])
            S.op("act", lambda e: e.activation(out=sn_s[:, :, 4:8], in_=sn_s[:, :, 0:4], func=AF.Exp, scale=SCALE),
                 reads=[b_sn_s], writes=[b_sn_s])
            S.op("dve", lambda e: e.tensor_tensor(
                out=snew[:].rearrange("p g (h d) -> p g h d", h=4), in0=qkv_s[:, 2].rearrange("p g (h d) -> p g h d", h=4),
                in1=sn_s[:, :, 4:8].unsqueeze(3).broadcast_to([NS, 3, 4, 128]), op=ALU.mult),
                reads=[b_qkv_s, b_sn_s], writes=[b_snew])
            k = 0
            for n in range(NS):
                for g in range(3):
                    ck, b_ck, _ = CK[k % 3]
                    sprod, b_sprod, _ = sprods[k % 3]
                    ssc, b_ssc, _ = sscs[k % 3]
                    S.dma("sp", lambda e, ck=ck, g=g, n=n: e.dma_start(out=ck[:], in_=cache[g, n]), writes=[b_ck])
                    pq, pbq = bank("A")
                    S.op("pe", lambda e, pq=pq, n=n, g=g: e.matmul(pq[:, :], lhsT=SEL[:, n, :], rhs=qkv_s[:, 0, g, :],
                                                               start=True, stop=True), reads=[b_SEL, b_qkv_s], writes=[pbq])
                    S.op("dve", lambda e, ck=ck, pq=pq, sprod=sprod: e.tensor_tensor(out=sprod[:], in0=ck[:, 0:512], in1=pq[:, :], op=ALU.mult),
                         reads=[b_ck, pbq], writes=[b_sprod])
                    S.op("dve", lambda e, sprod=sprod, ssc=ssc: e.tensor_reduce(out=ssc[:, 0:4], in_=sprod[:].rearrange("p (h d) -> p h d", h=4),
                                                          axis=AX.X, op=ALU.add), reads=[b_sprod], writes=[b_ssc])
                    S.op("act", lambda e, ssc=ssc: e.activation(out=ssc[:, 4:8], in_=ssc[:, 0:4], func=AF.Exp, scale=SCALE),
                         reads=[b_ssc], writes=[b_ssc])
                    S.op("dve", lambda e, ck=ck, sprod=sprod, ssc=ssc: e.tensor_tensor(
                        out=sprod[:].rearrange("p (h d) -> p h d", h=4), in0=ck[:, 512:1024].rearrange("p (h d) -> p h d", h=4),
                        in1=ssc[:, 4:8].unsqueeze(2).broadcast_to([128, 4, 128]), op=ALU.mult),
                        reads=[b_ck, b_ssc], writes=[b_sprod])
                    first, last = (k == 0), (k == NS * 3 - 1)
                    S.op("pe", lambda e, n=n, first=first, last=last, sprod=sprod: e.matmul(ps_os[0:NS, :], lhsT=SELT[:, n, :], rhs=sprod[:],
                                                                          start=first, stop=last),
                         reads=[b_SELT, b_sprod], writes=[pb_os], signal=True)
                    S.op("pe", lambda e, n=n, first=first, last=last, ssc=ssc: e.matmul(ps_ds[0:NS, 0:4], lhsT=SELT[:, n, :], rhs=ssc[:, 4:8],
                                                                          start=first, stop=last),
                         reads=[b_SELT, b_ssc], writes=[pb_ds], signal=True)
                    k += 1
            S.op("dve", lambda e: e.tensor_tensor(out=so[:, 0:512], in0=snew[:, 0, :], in1=snew[:, 1, :], op=ALU.add),
                 reads=[b_snew], writes=[b_so])
            S.op("dve", lambda e: e.tensor_tensor(out=so[:, 0:512], in0=so[:, 0:512], in1=snew[:, 2, :], op=ALU.add),
                 reads=[b_snew, b_so], writes=[b_so])
            S.op("dve", lambda e: e.tensor_tensor(out=so[:, 0:512], in0=so[:, 0:512], in1=ps_os[0:NS, :], op=ALU.add),
                 reads=[pb_os, b_so], writes=[b_so])
            S.op("dve", lambda e: e.tensor_tensor(out=so[:, 512:516], in0=sn_s[:, 0, 4:8], in1=sn_s[:, 1, 4:8], op=ALU.add),
                 reads=[b_sn_s], writes=[b_so])
            S.op("dve", lambda e: e.tensor_tensor(out=so[:, 512:516], in0=so[:, 512:516], in1=sn_s[:, 2, 4:8], op=ALU.add),
                 reads=[b_sn_s, b_so], writes=[b_so])
            S.op("dve", lambda e: e.tensor_tensor(out=so[:, 512:516], in0=so[:, 512:516], in1=ps_ds[0:NS, 0:4], op=ALU.add),
                 reads=[pb_ds, b_so], writes=[b_so])
            S.op("dve", lambda e: e.reciprocal(out=so[:, 512:516], in_=so[:, 512:516]), reads=[b_so], writes=[b_so])
            S.op("dve", lambda e: e.tensor_tensor(out=sob[:].rearrange("p (h d) -> p h d", h=4),
                                                  in0=so[:, 0:512].rearrange("p (h d) -> p h d", h=4),
                                                  in1=so[:, 512:516].unsqueeze(2).broadcast_to([NS, 4, 128]), op=ALU.mult),
                 reads=[b_so], writes=[b_sob])
            pt2, pbuf2 = bank("B")
            ptb = pt2[:].bitcast(BF16)
            for h in range(4):
                S.op("pe", lambda e, h=h, ptb=ptb: e.transpose(out=ptb[:, h * NS:(h + 1) * NS], in_=sob[:, h * 128:(h + 1) * 128],
                                                          identity=identB[0:NS, 0:NS]),
                     reads=[b_sob, b_identB], writes=[pbuf2], signal=(h == 3))
            S.op("dve", lambda e, ptb=ptb: e.tensor_copy(out=bmixT[:, :, TOK:TOK + NS],
                                                    in_=ptb[:, 0:4 * NS].rearrange("p (h t) -> p h t", h=4)),
                 reads=[pbuf2], writes=[b_bmixT])

            S.barrier()
            for r in [r_ACC, r_rtmp, r_qkv_s, r_qkv_c, stg[0][2], stg[1][2], r_SEL, r_SELT, r_snew, r_sn_s,
                      r_so, r_sob, CK[0][2], CK[1][2], CK[2][2]] + [x[2] for x in sprods] + [x[2] for x in sscs]:
                AR.release(r)

            if STOP < 2:
                return
            amixT, b_amixT, r_amixT = sb("amixT", [128, 8, TOK + NS], BF16)
            vn, b_vn, r_vn = sb("vn", [128, 9, A_W], BF16)
            gv = [sb("gv%d" % i, [128, A_W], F32) for i in range(2)]
            lng, b_lng, r_lng = sb("lng", [128, A_W], F32)
            lnb, b_lnb, r_lnb = sb("lnb", [128, A_W], F32)
            S.dma("sp", lambda e: e.dma_start(out=lng[:], in_=lnv[0, :].partition_broadcast(128)), writes=[b_lng])
            S.dma("sp", lambda e: e.dma_start(out=lnb[:], in_=lnv[1, :].partition_broadcast(128)), writes=[b_lnb])
            bns, b_bns, r_bns = sb("bns", [128, 2, 8], F32)
            wsT, b_wsT, r_wsT = sb("wsT", [128, 8, 128], BF16)
            wsF, b_wsF, r_wsF = sb("wsF", [128, 8, 128], F32)
            bsb, b_bsb, r_bsb = sb("bsb", [128, 8, 128], F32)
            bs0, b_bs0, r_bs0 = sb("bs0", [128, 8], F32)
            ws00, b_ws00, r_ws00 = sb("ws00", [16, 8], F32)
            dg, b_dg, r_dg = sb("dg", [16, 8, 16], BF16)
            sgt, b_sgt, r_sgt = sb("sgt", [128, 512], F32)
            S.dma("sp", lambda e: e.dma_start(out=wsF[:], in_=w_s.rearrange("g t s -> t g s")), writes=[b_wsF])
            S.dma("sp", lambda e: e.dma_start(out=bsb[:], in_=b_s.rearrange("g t -> (g t)").partition_broadcast(128)
                                              .rearrange("p (g t) -> p g t", g=8)), writes=[b_bsb])
            S.dma("sp", lambda e: e.dma_start(out=ws00[:], in_=w_s[:, 0, 0].partition_broadcast(16),
                                              allow_slow_non_contiguous=True), writes=[b_ws00])
            wsM, b_wsM, r_wsM = sb("wsM", [128, 8, 128], F32)
            for half in range(2):
                pt, pbuf = bank("C")
                for j in range(4):
                    gi = half * 4 + j
                    S.op("pe", lambda e, gi=gi, j=j, pt=pt: e.transpose(out=pt[:, j * 128:(j + 1) * 128], in_=wsF[:, gi, :],
                                                                   identity=identF[:]),
                         reads=[b_wsF, b_identF], writes=[pbuf], signal=(j == 3))
                S.op("act", lambda e, half=half, pt=pt: e.copy(out=wsM[:, half * 4:half * 4 + 4, :],
                                                           in_=pt[:].rearrange("p (g t) -> p g t", g=4)),
                     reads=[pbuf], writes=[b_wsM])
            S.op("pool", lambda e: e.affine_select(out=wsT[:], in_=wsM[:], pattern=[[0, 8], [1, 128]],
                                                   compare_op=ALU.is_ge, fill=0.0, base=0, channel_multiplier=-1),
                 reads=[b_wsM], writes=[b_wsT])
            S.op("dve", lambda e: e.tensor_tensor(out=dg[:], in0=identF[0:16, 0:16].unsqueeze(1).broadcast_to([16, 8, 16]),
                                                  in1=ws00[:].unsqueeze(2).broadcast_to([16, 8, 16]), op=ALU.mult),
                 reads=[b_identF, b_ws00], writes=[b_dg])

            wva0, bwva0 = w_next()
            wva1, bwva1 = w_next(issue=False)
            own_tiles = [(TOK + n * 128, 128) for n in range(8)] + [(2 * TOK, NS)]
            for ti, (c0, rows) in enumerate(own_tiles):
                g_t, b_g, _ = gv[ti % 2]
                for blk, (wt, bw) in enumerate(((wva0, bwva0), (wva1, bwva1))):
                    pt, pbuf = bank("A")
                    mm_group(pt[0:rows, :], pbuf, [(hT[:, kc, c0:c0 + rows], wt[:, kc, :]) for kc in range(16)], [b_hT, bw])
                    S.op("act", lambda e, pt=pt, blk=blk, g_t=g_t, rows=rows: e.activation(
                        out=g_t[0:rows, blk * 512:(blk + 1) * 512], in_=pt[0:rows, :], func=AF.Gelu_apprx_tanh),
                        reads=[pbuf], writes=[b_g])
                for blk in range(2):
                    S.op("dve", lambda e, blk=blk, g_t=g_t, rows=rows: e.bn_stats(
                        out=bns[0:rows, blk, 0:6], in_=g_t[0:rows, blk * 512:(blk + 1) * 512]), reads=[b_g], writes=[b_bns])
                mv = bns[0:rows, 0, 6:8]
                S.op("dve", lambda e, rows=rows, mv=mv: e.bn_aggr(out=mv, in_=bns[0:rows, :, 0:6]), reads=[b_bns], writes=[b_bns])
                sd = bns[0:rows, 1, 6:7]
                rsd = bns[0:rows, 1, 7:8]
                S.op("dve", lambda e, rows=rows, sd=sd: e.tensor_scalar(out=sd, in0=bns[0:rows, 0, 7:8], scalar1=EPS, scalar2=None,
                                                                     op0=ALU.add), reads=[b_bns], writes=[b_bns])
                S.op("act", lambda e, sd=sd: e.sqrt(out=sd, in_=sd), reads=[b_bns], writes=[b_bns])
                S.op("dve", lambda e, sd=sd, rsd=rsd: e.reciprocal(out=rsd, in_=sd), reads=[b_bns], writes=[b_bns])
                S.op("dve", lambda e, g_t=g_t, rows=rows, rsd=rsd: e.tensor_scalar(
                    out=g_t[0:rows, :], in0=g_t[0:rows, :], scalar1=bns[0:rows, 0, 6:7], scalar2=rsd,
                    op0=ALU.subtract, op1=ALU.mult), reads=[b_g, b_bns], writes=[b_g])
                S.op("dve", lambda e, g_t=g_t, rows=rows: e.tensor_tensor(out=g_t[0:rows, :], in0=g_t[0:rows, :], in1=lng[0:rows, :],
                                                                      op=ALU.mult), reads=[b_g, b_lng], writes=[b_g])
                if rows == 128:
                    S.op("dve", lambda e, g_t=g_t, ti=ti: e.tensor_tensor(out=vn[:, ti, :], in0=g_t[:], in1=lnb[:], op=ALU.add),
                         reads=[b_g, b_lnb], writes=[b_vn])
                else:
                    S.op("dve", lambda e, g_t=g_t, rows=rows: e.tensor_tensor(out=g_t[0:rows, :], in0=g_t[0:rows, :],
                                                                          in1=lnb[0:rows, :], op=ALU.add),
                         reads=[b_g, b_lnb], writes=[b_g])
                    S.dma("sp", lambda e, g_t=g_t, rows=rows: e.dma_start(out=sguv, in_=g_t[0:rows, :]), reads=[b_g], dbuf=outb)
                    S.op("act", lambda e, g_t=g_t, rows=rows, ti=ti: e.copy(out=vn[0:rows, ti, :], in_=g_t[0:rows, :]),
                         reads=[b_g], writes=[b_vn])

            for blk in range(2):
                wt, bw = w_next()
                for jj in range(4):
                    j = blk * 4 + jj
                    for (c0, wd) in PASSES:
                        pt, pbuf = bank("A")
                        mm_group(pt[:, 0:wd], pbuf, [(wt[:, kc, jj * 128:(jj + 1) * 128], hT[:, kc, c0:c0 + wd]) for kc in range(16)],
                                 [b_hT, bw])
                        S.op("act", lambda e, pt=pt, j=j, c0=c0, wd=wd: e.activation(
                            out=amixT[:, j, c0 - TOK:c0 - TOK + wd], in_=pt[:, 0:wd], func=AF.Gelu_apprx_tanh),
                            reads=[pbuf], writes=[b_amixT])

            for tt in range(8):
                for half in range(2):
                    pt, pbuf = bank("A")
                    for jj in range(4):
                        gi = half * 4 + jj
                        S.op("pe", lambda e, pt=pt, jj=jj, gi=gi, tt=tt: e.matmul(
                            pt[:, jj * 128:(jj + 1) * 128], lhsT=vn[:, tt, gi * 128:(gi + 1) * 128], rhs=wsT[:, gi, :],
                            start=True, stop=True), reads=[b_vn, b_wsT], writes=[pbuf], signal=(jj == 3))
                    S.op("dve", lambda e, pt=pt, half=half: e.tensor_tensor(
                        out=sgt[:].rearrange("p (g t) -> p g t", g=4), in0=pt[:].rearrange("p (g t) -> p g t", g=4),
                        in1=bsb[:, half * 4:half * 4 + 4, :], op=ALU.add), reads=[pbuf, b_bsb], writes=[b_sgt])
                    S.op("dve", lambda e, half=half, tt=tt: e.tensor_tensor(
                        out=amixT[:, half * 4:half * 4 + 4, tt * 128:(tt + 1) * 128],
                        in0=amixT[:, half * 4:half * 4 + 4, tt * 128:(tt + 1) * 128],
                        in1=sgt[:].rearrange("p (g t) -> p g t", g=4), op=ALU.mult), reads=[b_sgt, b_amixT], writes=[b_amixT])
            pt, pbuf = bank("A")
            for gi in range(8):
                S.op("pe", lambda e, pt=pt, gi=gi: e.matmul(pt[:, gi * NS:(gi + 1) * NS], lhsT=vn[0:NS, 8, gi * 128:(gi + 1) * 128],
                                                       rhs=dg[:, gi, :], start=True, stop=True),
                     reads=[b_vn, b_dg], writes=[pbuf], signal=(gi == 7))
            S.op("dve", lambda e, pt=pt: e.tensor_tensor(
                out=sgt[:, 0:128].rearrange("p (g t) -> p g t", g=8), in0=pt[:, 0:128].rearrange("p (g t) -> p g t", g=8),
                in1=bsb[:, :, 0:1].broadcast_to([128, 8, NS]), op=ALU.add), reads=[pbuf, b_bsb], writes=[b_sgt])
            S.op("dve", lambda e: e.tensor_tensor(out=amixT[:, :, TOK:TOK + NS], in0=amixT[:, :, TOK:TOK + NS],
                                                  in1=sgt[:, 0:128].rearrange("p (g t) -> p g t", g=8), op=ALU.mult),
                 reads=[b_sgt, b_amixT], writes=[b_amixT])

            S.barrier()
            for r in (r_vn, gv[0][2], gv[1][2], r_lng, r_lnb, r_bns, r_wsT, r_wsF, r_bsb, r_bs0, r_ws00, r_dg, r_sgt, r_wsM):
                AR.release(r)

            if STOP < 3:
                return
            mergedT, b_mergedT, r_mergedT = sb("mergedT", [128, 16, TOK + NS], BF16, top=True)
            sgA, b_sgA, r_sgA = sb("sgA", [128, 4, TOK + NS], BF16)
            sgB, b_sgB = sgA, b_sgA
            M1, b_M1, r_M1 = sb("M1", [128, 4, TOK + NS], F32)
            mtmp, b_mtmp, r_mtmp = sb("mtmp", [128, 512], F32)
            def gate_block():
                wt, bw = w_next()
                for jj in range(4):
                    for (c0, wd) in PASSES:
                        pt, pbuf = bank("A")
                        mm_group(pt[:, 0:wd], pbuf, [(wt[:, kc, jj * 128:(jj + 1) * 128], hT[:, kc, c0:c0 + wd]) for kc in range(16)],
                                 [b_hT, bw])
                        S.op("act", lambda e, pt=pt, jj=jj, c0=c0, wd=wd: e.activation(
                            out=sgA[:, jj, c0 - TOK:c0 - TOK + wd], in_=pt[:, 0:wd], func=AF.Sigmoid),
                            reads=[pbuf], writes=[b_sgA])

            for cb in range(4):
                gate_block()
                wt, bw = w_next()
                for jj in range(4):
                    for (c0, wd) in PASSES:
                        pt, pbuf = bank("A")
                        mm_group(pt[:, 0:wd], pbuf, [(wt[:, kc, jj * 128:(jj + 1) * 128], amixT[:, kc, c0 - TOK:c0 - TOK + wd])
                                                     for kc in range(8)], [b_amixT, bw])
                        S.op("dve", lambda e, pt=pt, jj=jj, c0=c0, wd=wd: e.tensor_tensor(
                            out=M1[:, jj, c0 - TOK:c0 - TOK + wd], in0=pt[:, 0:wd], in1=sgA[:, jj, c0 - TOK:c0 - TOK + wd], op=ALU.mult),
                            reads=[pbuf, b_sgA], writes=[b_M1])
                gate_block()
                wt, bw = w_next()
                for jj in range(4):
                    for (c0, wd) in PASSES:
                        pt, pbuf = bank("A")
                        mm_group(pt[:, 0:wd], pbuf, [(wt[:, kc, jj * 128:(jj + 1) * 128], bmixT[:, kc, c0 - TOK:c0 - TOK + wd])
                                                     for kc in range(4)], [b_bmixT, bw])
                        S.op("dve", lambda e, pt=pt, jj=jj, c0=c0, wd=wd: e.tensor_tensor(
                            out=mtmp[:, 0:wd], in0=pt[:, 0:wd], in1=sgB[:, jj, c0 - TOK:c0 - TOK + wd], op=ALU.mult),
                            reads=[pbuf, b_sgB], writes=[b_mtmp])
                        S.op("dve", lambda e, cb=cb, jj=jj, c0=c0, wd=wd: e.tensor_tensor(
                            out=mergedT[:, cb * 4 + jj, c0 - TOK:c0 - TOK + wd], in0=mtmp[:, 0:wd],
                            in1=M1[:, jj, c0 - TOK:c0 - TOK + wd], op=ALU.add),
                            reads=[b_mtmp, b_M1], writes=[b_mergedT])

            S.barrier()
            for r in (r_hT, r_amixT, r_bmixT, r_sgA, r_M1, r_mtmp):
                AR.release(r)

            if STOP < 4:
                return
            X2, b_X2, r_X2 = sb("X2", [128, 9, D], F32)
            bX2 = [Buf("X2_%d" % i) for i in range(9)]
            xpc = [sb("xpc%d" % i, [128, 512], F32) for i in range(3)]
            rows_of = [128] * 8 + [NS]
            k = 0

            def load_xpiece(k):
                cb, tt = divmod(k, 9)
                t, bf, _ = xpc[k % 3]
                rows = rows_of[tt]
                src = xloc[TOK + tt * 128:TOK + (tt + 1) * 128, cb * 512:(cb + 1) * 512] if tt < 8 else xs[:, cb * 512:(cb + 1) * 512]
                S.dma("sp", lambda e: e.dma_start(out=t[0:rows, :], in_=src), writes=[bf])

            load_xpiece(0); load_xpiece(1)
            for cb in range(4):
                wt, bw = w_next()
                for tt in range(9):
                    if k + 2 < 36:
                        load_xpiece(k + 2)
                    rows = rows_of[tt]
                    t, bf, _ = xpc[k % 3]
                    pt, pbuf = bank("A")
                    mm_group(pt[0:rows, :], pbuf, [(mergedT[:, kc, tt * 128:tt * 128 + rows], wt[:, kc, :]) for kc in range(16)],
                             [b_mergedT, bw])
                    S.op("dve", lambda e, pt=pt, t=t, tt=tt, cb=cb, rows=rows: e.tensor_tensor(
                        out=X2[0:rows, tt, cb * 512:(cb + 1) * 512], in0=t[0:rows, :], in1=pt[0:rows, :], op=ALU.add),
                        reads=[pbuf, bf], writes=[bX2[tt]])
                    k += 1
            S.barrier()
            AR.release(r_mergedT)
            for i in range(3):
                AR.release(xpc[i][2])

            if STOP < 5:
                return
            h2T, b_h2T, r_h2T = sb("h2T", [128, 16, TOK + NS], BF16)
            rstd2, b_rstd2, _ = sb("rstd2", [128, 9], F32)
            xnb = [sb("xnb%d" % i, [128, D], BF16) for i in range(2)]
            SQ["t"], SQ["b"], SQ["r"] = sb("sqjunk", [128, D], BF16)
            for tt in range(9):
                rows = rows_of[tt]
                rs = rms_stats(X2[0:rows, tt, :], rows, bX2[tt], tt % 2)
                S.op("dve", lambda e, rs=rs, tt=tt, rows=rows: e.tensor_copy(out=rstd2[0:rows, tt:tt + 1], in_=rs),
                     reads=[b_stat], writes=[b_rstd2])
                xn, bxn, _ = xnb[tt % 2]
                S.op("act", lambda e, xn=xn, rs=rs, tt=tt, rows=rows: e.activation(
                    out=xn[0:rows, :], in_=X2[0:rows, tt, :], func=AF.Copy, scale=rs), reads=[bX2[tt], b_stat], writes=[bxn])
                transpose_to(h2T, b_h2T, tt * 128, xn, bxn, rows, 1)

            if STOP < 6:
                return
            skF, b_skF, r_skF = sb("skF", [128, 2, 128], F32)
            skT, b_skT, _ = sb("skT", [128, 2, 128], BF16)
            S.dma("sp", lambda e: e.dma_start(out=skF[:], in_=subk.rearrange("j k d -> k j d")), writes=[b_skF])
            pt, pbuf = bank("C")
            for j in range(2):
                S.op("pe", lambda e, j=j, pt=pt: e.transpose(out=pt[:, j * 128:(j + 1) * 128], in_=skF[:, j, :], identity=identF[:]),
                     reads=[b_skF, b_identF], writes=[pbuf], signal=(j == 1))
            S.op("act", lambda e, pt=pt: e.copy(out=skT[:], in_=pt[:, 0:256].rearrange("p (j k) -> p j k", j=2)),
                 reads=[pbuf], writes=[b_skT])
            T1, b_T1, r_T1 = sb("T1", [128, 9, 16, 16], F32)
            I1, b_I1, r_I1 = sb("I1", [128, 9, 16, 16], U32)
            qc, b_qc, r_qc = sb("qc", [128, TOK + NS], BF16)
            scs = [sb("scs%d" % i, [128, 128], F32) for i in range(2)]
            sc2, b_sc2, r_sc2 = sb("sc2", [128, 128], F32)
            k = 0
            for cb in range(4):
                wt, bw = w_next(issue=(cb < 3))
                for jj in range(4):
                    c = cb * 4 + jj
                    for (c0, wd) in PASSES:
                        pt, pbuf = bank("A")
                        mm_group(pt[:, 0:wd], pbuf, [(wt[:, kc, jj * 128:(jj + 1) * 128], h2T[:, kc, c0 - TOK:c0 - TOK + wd])
                                                     for kc in range(16)], [b_h2T, bw])
                        S.op("act", lambda e, pt=pt, c0=c0, wd=wd: e.copy(out=qc[:, c0 - TOK:c0 - TOK + wd], in_=pt[:, 0:wd]),
                             reads=[pbuf], writes=[b_qc])
                    for tt in range(9):
                        rows = rows_of[tt]
                        pt, pbuf = bank("B") if tt % 2 else bank("C")
                        S.op("pe", lambda e, pt=pt, tt=tt, rows=rows, c=c: e.matmul(
                            pt[0:rows, 0:128], lhsT=qc[:, tt * 128:tt * 128 + rows], rhs=skT[:, c % 2, :], start=True, stop=True),
                            reads=[b_qc, b_skT], writes=[pbuf])
                        sc, b_sc, _ = scs[k % 2]; k += 1
                        S.op("act", lambda e, pt=pt, sc=sc, rows=rows: e.copy(out=sc[0:rows, :], in_=pt[0:rows, 0:128]),
                             reads=[pbuf], writes=[b_sc])
                        S.op("dve", lambda e, sc=sc, rows=rows, tt=tt, c=c: e.max(out=T1[0:rows, tt, c, 0:8], in_=sc[0:rows, :]),
                             reads=[b_sc], writes=[b_T1])
                        S.op("dve", lambda e, sc=sc, rows=rows, tt=tt, c=c: e.max_index(
                            out=I1[0:rows, tt, c, 0:8], in_max=T1[0:rows, tt, c, 0:8], in_values=sc[0:rows, :]),
                            reads=[b_sc, b_T1], writes=[b_I1])
                        S.op("dve", lambda e, sc=sc, rows=rows, tt=tt, c=c: e.match_replace(
                            out=sc2[0:rows, :], in_to_replace=T1[0:rows, tt, c, 0:8], in_values=sc[0:rows, :], imm_value=-3.0e38),
                            reads=[b_sc, b_T1], writes=[b_sc2])
                        S.op("dve", lambda e, rows=rows, tt=tt, c=c: e.max(out=T1[0:rows, tt, c, 8:16], in_=sc2[0:rows, :]),
                             reads=[b_sc2], writes=[b_T1])
                        S.op("dve", lambda e, rows=rows, tt=tt, c=c: e.max_index(
                            out=I1[0:rows, tt, c, 8:16], in_max=T1[0:rows, tt, c, 8:16], in_values=sc2[0:rows, :]),
                            reads=[b_sc2, b_T1], writes=[b_I1])
            S.barrier()
            for r in (r_skF, r_qc, scs[0][2], scs[1][2], r_sc2, r_h2T, xnb[0][2], xnb[1][2], SQ["r"]):
                AR.release(r)

            if STOP < 7:
                return
            tab_issue(len(tab_jobs))
            cand, b_cand, r_cand = sb("cand", [128, 8, 256], F32)
            cand2, b_cand2, r_cand2 = sb("cand2", [128, 256], F32)
            ts, b_ts, r_ts = sb("ts", [128, 8, 16], F32)
            ic, b_ic, r_ic = sb("ic", [128, 8, 16], U32)
            icw, b_icw, r_icw = sb("icw", [128, 2, 8, 16], U32)
            icf, b_icf, r_icf = sb("icf", [128, 2, 8, 16], F32)
            i1f, b_i1f, r_i1f = sb("i1f", [128, 16, 16], F32)
            iota16, b_iota16, r_iota16 = sb("iota16", [128, 16], F32)
            oh, b_oh = cand[:].rearrange("p h (a b) -> p h a b", a=16), b_cand
            ef, b_ef, r_ef = sb("ef", [128, 2, 8, 16], F32)
            gs2, b_gs2, r_gs2 = sb("gs2", [128, 16], F32)
            eidx, _, _ = sb("eidx", [128, 9, 128], I32, top=True)
            gsm, _, _ = sb("gsm", [128, 9, 128], F32, top=True)
            b_eidxs = [Buf("eidx%d" % i) for i in range(9)]
            b_gsms = [Buf("gsm%d" % i) for i in range(9)]
            sel_ops = [[] for _ in range(9)]
            S.op("pool", lambda e: e.iota(iota16[:], pattern=[[1, 16]], base=0, channel_multiplier=0,
                                          allow_small_or_imprecise_dtypes=True), writes=[b_iota16])
            S.op("pool", lambda e: e.memset(eidx[:], 0), writes=b_eidxs)
            for tt in range(9):
                rows = rows_of[tt]
                b_eidx, b_gsm = b_eidxs[tt], b_gsms[tt]
                DEF = lambda eng, fn, reads=(), writes=(), _l=sel_ops[tt]: _l.append((eng, fn, list(reads), list(writes)))
                T = T1[0:rows, tt].rearrange("p (h j) k -> p h j k", j=2)
                G = gsm[0:rows, tt, :].rearrange("p (h k) -> p h k", h=8)
                DEF("dve", lambda e, T=T, rows=rows: e.tensor_tensor(
                    out=cand[0:rows].rearrange("p h (a b) -> p h a b", a=16),
                    in0=T[:, :, 0, :].unsqueeze(3).broadcast_to([rows, 8, 16, 16]),
                    in1=T[:, :, 1, :].unsqueeze(2).broadcast_to([rows, 8, 16, 16]), op=ALU.add),
                    reads=[b_T1], writes=[b_cand])
                for h in range(8):
                    DEF("dve", lambda e, h=h, rows=rows: e.max(out=ts[0:rows, h, 0:8], in_=cand[0:rows, h, :]),
                         reads=[b_cand], writes=[b_ts])
                    DEF("dve", lambda e, h=h, rows=rows: e.max_index(out=ic[0:rows, h, 0:8], in_max=ts[0:rows, h, 0:8],
                                                                   in_values=cand[0:rows, h, :]),
                         reads=[b_cand, b_ts], writes=[b_ic])
                    DEF("dve", lambda e, h=h, rows=rows: e.match_replace(out=cand2[0:rows, :], in_to_replace=ts[0:rows, h, 0:8],
                                                                       in_values=cand[0:rows, h, :], imm_value=-3.0e38),
                         reads=[b_cand, b_ts], writes=[b_cand2])
                    DEF("dve", lambda e, h=h, rows=rows: e.max(out=ts[0:rows, h, 8:16], in_=cand2[0:rows, :]),
                         reads=[b_cand2], writes=[b_ts])
                    DEF("dve", lambda e, h=h, rows=rows: e.max_index(out=ic[0:rows, h, 8:16], in_max=ts[0:rows, h, 8:16],
                                                                   in_values=cand2[0:rows, :]),
                         reads=[b_cand2, b_ts], writes=[b_ic])
                DEF("dve", lambda e, rows=rows: e.tensor_single_scalar(out=icw[0:rows, 0], in_=ic[0:rows], scalar=c4[0:rows, 0:1],
                                                                       op=ALU.logical_shift_right), reads=[b_ic, b_c4], writes=[b_icw])
                DEF("dve", lambda e, rows=rows: e.tensor_single_scalar(out=icw[0:rows, 1], in_=ic[0:rows], scalar=c4[0:rows, 1:2],
                                                                       op=ALU.bitwise_and), reads=[b_ic, b_c4], writes=[b_icw])
                DEF("dve", lambda e, rows=rows: e.tensor_copy(out=icf[0:rows], in_=icw[0:rows]), reads=[b_icw], writes=[b_icf])
                DEF("dve", lambda e, rows=rows, tt=tt: e.tensor_copy(out=i1f[0:rows], in_=I1[0:rows, tt]), reads=[b_I1], writes=[b_i1f])
                I = i1f[0:rows].rearrange("p (h j) k -> p h j k", j=2)
                for side in range(2):
                    DEF("dve", lambda e, side=side, rows=rows: e.tensor_tensor(
                        out=oh[0:rows], in0=icf[0:rows, side].unsqueeze(3).broadcast_to([rows, 8, 16, 16]),
                        in1=iota16[0:rows].unsqueeze(1).unsqueeze(1).broadcast_to([rows, 8, 16, 16]), op=ALU.is_equal),
                        reads=[b_icf, b_iota16], writes=[b_oh])
                    DEF("dve", lambda e, side=side, rows=rows, I=I: e.tensor_tensor(
                        out=oh[0:rows], in0=oh[0:rows], in1=I[:, :, side, :].unsqueeze(2).broadcast_to([rows, 8, 16, 16]),
                        op=ALU.mult), reads=[b_oh, b_i1f], writes=[b_oh])
                    DEF("dve", lambda e, side=side, rows=rows: e.tensor_reduce(out=ef[0:rows, side], in_=oh[0:rows], axis=AX.X,
                                                                             op=ALU.add), reads=[b_oh], writes=[b_ef])
                DEF("dve", lambda e, rows=rows: e.scalar_tensor_tensor(
                    out=ef[0:rows, 0], in0=ef[0:rows, 0], scalar=128.0, in1=ef[0:rows, 1], op0=ALU.mult, op1=ALU.add),
                    reads=[b_ef], writes=[b_ef])
                DEF("dve", lambda e, rows=rows, tt=tt: e.tensor_copy(out=eidx[0:rows, tt, :].rearrange("p (h k) -> p h k", h=8),
                                                                   in_=ef[0:rows, 0]), reads=[b_ef], writes=[b_eidx])
                DEF("dve", lambda e, rows=rows, G=G: e.tensor_tensor(
                    out=G, in0=ts[0:rows], in1=ts[0:rows, :, 0:1].broadcast_to([rows, 8, 16]), op=ALU.subtract),
                    reads=[b_ts], writes=[b_gsm])
                DEF("act", lambda e, G=G: e.activation(out=G, in_=G, func=AF.Exp), reads=[b_gsm], writes=[b_gsm])
                DEF("dve", lambda e, rows=rows, G=G: e.tensor_reduce(out=gs2[0:rows, 0:8], in_=G, axis=AX.X, op=ALU.add),
                     reads=[b_gsm], writes=[b_gs2])
                DEF("dve", lambda e, rows=rows: e.reciprocal(out=gs2[0:rows, 8:16], in_=gs2[0:rows, 0:8]), reads=[b_gs2], writes=[b_gs2])
                DEF("dve", lambda e, rows=rows, G=G: e.tensor_tensor(
                    out=G, in0=G, in1=gs2[0:rows, 8:16].unsqueeze(2).broadcast_to([rows, 8, 16]), op=ALU.mult),
                    reads=[b_gsm, b_gs2], writes=[b_gsm])
            for o in sel_ops[0]:
                S.op(*o)
            sel_ops[0] = []

            gffn_b, b_gffn_b, r_gffn_b = sb("gffn_b", [128, D], F32)
            S.dma("sp", lambda e: e.dma_start(out=gffn_b[:], in_=gvec[1, :].partition_broadcast(128)), writes=[b_gffn_b])
            actp, _, r_actp = sb("actp", [128, 128], F32)
            coef, b_coef, r_coef = sb("coef", [128, 2, 128], F32)
            b_actps = [Buf("actp%d" % i) for i in range(4)]
            h2t, b_h2t, r_h2t = sb("h2t", [128, D], F32)
            NG = 8
            LOOK = NG - 2
            ug = [sb("ug%d" % i, [128, 2, D], BF16) for i in range(NG - 4)]
            for wi in range(2):
                wa, wb_ = wslot[wi][2]
                for hh in range(2):
                    h_ = nc.alloc_sbuf_tensor_at("ugw%d_%d" % (wi, hh), [128, 2, D], BF16, offset=wa + hh * 8192)
                    ug.append((h_, Buf("ugw%d_%d" % (wi, hh)), None))
            dgt = [sb("dgt%d" % i, [128, 128], BF16) for i in range(4)]
            pc_rows = pc16.rearrange("e j d -> e (j d)")
            jobs = [(tt, sidx) for tt in range(8) for sidx in range(128)] + [(8, k) for k in range(16)]
            gstate = {"issued": 0, "dg": 0}
            gbuf = {}
            eidx_s, b_eidx_s, r_eidx_s = sb("eidx_s", [128, 16], I32)
            gsm_s, b_gsm_s, r_gsm_s = sb("gsm_s", [128, 16], F32)
            h2s, b_h2s = h2t, b_h2t
            actp_s, b_actp_s, r_actp_s = sb("actp_s", [128, 16], F32)
            coef_s, b_coef_s, r_coef_s = sb("coef_s", [128, 2, 16], F32)
            SELH, b_SELH, r_SELH = sb("SELH", [128, NS], F32)
            lhs_s = [sb("lhs_s%d" % i, [128, NS], BF16) for i in range(2)]
            S.op("pool", lambda e: e.memset(SELH[:], 1.0), writes=[b_SELH])
            S.op("pool", lambda e: e.affine_select(out=SELH[:], in_=SELH[:], pattern=[[-8, NS]], compare_op=ALU.is_ge,
                                                   fill=0.0, base=0, channel_multiplier=1), reads=[b_SELH], writes=[b_SELH])
            S.op("pool", lambda e: e.affine_select(out=SELH[:], in_=SELH[:], pattern=[[8, NS]], compare_op=ALU.is_ge,
                                                   fill=0.0, base=7, channel_multiplier=-1), reads=[b_SELH], writes=[b_SELH])

            def prep_sample():
                h2tmp = cand[:].rearrange("p h c -> p (h c)")
                S.op("dve", lambda e: e.scalar_tensor_tensor(
                    out=h2tmp[0:NS, :], in0=X2[0:NS, 8, :], scalar=rstd2[0:NS, 8:9], in1=gffn_b[0:NS, :],
                    op0=ALU.mult, op1=ALU.mult), reads=[bX2[8], b_rstd2, b_gffn_b], writes=[b_cand])
                for h in range(8):
                    S.dma("sp", lambda e, h=h: e.dma_start(out=eidx_s[h:128:8, :], in_=eidx[0:NS, 8, h * 16:(h + 1) * 16]),
                          reads=[b_eidxs[8]], writes=[b_eidx_s])
                    S.dma("sp", lambda e, h=h: e.dma_start(out=gsm_s[h:128:8, :], in_=gsm[0:NS, 8, h * 16:(h + 1) * 16]),
                          reads=[b_gsms[8]], writes=[b_gsm_s])

            def g_issue_upto(n):
                while gstate["issued"] < min(n, len(jobs)):
                    ji = gstate["issued"]
                    tt, sidx = jobs[ji]
                    t, bf, _ = ug[ji % NG]
                    idx_ap = eidx[:, tt, sidx:sidx + 1] if tt < 8 else eidx_s[:, sidx:sidx + 1]
                    S.dma("pool", lambda e, t=t, idx_ap=idx_ap: e.indirect_dma_start(
                        out=t[:].rearrange("p j d -> p (j d)"), out_offset=None, in_=pc_rows,
                        in_offset=bass.IndirectOffsetOnAxis(ap=idx_ap, axis=0)),
                        reads=[b_eidxs[tt] if tt < 8 else b_eidx_s, b_tab], writes=[bf])
                    gbuf[ji] = (t, bf)
                    gstate["issued"] += 1

            for ji, (tt, sidx) in enumerate(jobs):
                rows = rows_of[tt]
                if tt == 8:
                    g_issue_upto(ji + 1 + LOOK)
                    t, bf = gbuf.pop(ji)
                    k = sidx
                    if k == 0:
                        h2tmp = cand[:].rearrange("p h c -> p (h c)")
                        for h in range(8):
                            S.dma("sp", lambda e, h=h: e.dma_start(out=h2s[h:128:8, :], in_=h2tmp[0:NS, :]),
                                  reads=[b_cand], writes=[b_h2s])
                    S.op("dve", lambda e, t=t, k=k: e.scalar_tensor_tensor(
                        out=t[:, 0, :], in0=t[:, 0, :], scalar=1.0, in1=h2s[:, :], op0=ALU.mult, op1=ALU.mult,
                        accum_out=actp_s[:, k:k + 1]), reads=[b_h2s], writes=[bf, b_actp_s])
                    S.op("act", lambda e, k=k: e.activation(out=coef_s[:, 0, k:k + 1], in_=actp_s[:, k:k + 1], func=AF.Gelu_apprx_tanh),
                         reads=[b_actp_s], writes=[b_coef_s])
                    S.op("act", lambda e, k=k: e.activation(out=coef_s[:, 1, k:k + 1], in_=coef_s[:, 0, k:k + 1], func=AF.Copy,
                                                            scale=gsm_s[:, k:k + 1]), reads=[b_coef_s, b_gsm_s], writes=[b_coef_s])
                    l_t, b_l, _ = lhs_s[k % 2]
                    S.op("act", lambda e, l_t=l_t, k=k: e.activation(out=l_t[:, :], in_=SELH[:, :], func=AF.Copy,
                                                                     scale=coef_s[:, 1, k:k + 1]),
                         reads=[b_SELH, b_coef_s], writes=[b_l])
                    for c in range(4):
                        pt, pbuf = PB[c]
                        S.op("pe", lambda e, pt=pt, c=c, t=t, l_t=l_t, k=k: e.matmul(
                            pt[0:NS, :], lhsT=l_t[:, :], rhs=t[:, 1, c * 512:(c + 1) * 512], start=(k == 0), stop=(k == 15)),
                            reads=[b_l, bf], writes=[pbuf], signal=(c == 3))
                    if k == 15:
                        for c in range(4):
                            pt, pbuf = PB[c]
                            S.op("dve", lambda e, pt=pt, c=c: e.tensor_tensor(
                                out=X2[0:NS, 8, c * 512:(c + 1) * 512], in0=X2[0:NS, 8, c * 512:(c + 1) * 512],
                                in1=pt[0:NS, :], op=ALU.add), reads=[pbuf, bX2[8]], writes=[bX2[8]])
                    continue
                if sidx == 0:
                    S.op("dve", lambda e, rows=rows, tt=tt: e.scalar_tensor_tensor(
                        out=h2t[0:rows, :], in0=X2[0:rows, tt, :], scalar=rstd2[0:rows, tt:tt + 1], in1=gffn_b[0:rows, :],
                        op0=ALU.mult, op1=ALU.mult), reads=[bX2[tt], b_rstd2, b_gffn_b], writes=[b_h2t])
                if tt + 1 < 9:
                    nxt = sel_ops[tt + 1]
                    take = len(nxt) if sidx >= 127 - LOOK - 1 else min(1, len(nxt))
                    for o in nxt[:take]:
                        S.op(*o)
                    del nxt[:take]
                    if tt == 7 and sidx == 127 - LOOK - 1:
                        prep_sample()
                g_issue_upto(ji + 1 + LOOK)
                t, bf = gbuf.pop(ji)
                b_ap = b_actps[sidx % 4]
                S.op("dve", lambda e, t=t, sidx=sidx, rows=rows: e.scalar_tensor_tensor(
                    out=t[0:rows, 0, :], in0=t[0:rows, 0, :], scalar=1.0, in1=h2t[0:rows, :], op0=ALU.mult, op1=ALU.mult,
                    accum_out=actp[0:rows, sidx:sidx + 1]), reads=[b_h2t], writes=[bf, b_ap])
                S.op("act", lambda e, rows=rows, sidx=sidx: e.activation(out=coef[0:rows, 0, sidx:sidx + 1], in_=actp[0:rows, sidx:sidx + 1],
                                                                    func=AF.Gelu_apprx_tanh), reads=[b_ap], writes=[b_coef])
                S.op("act", lambda e, rows=rows, sidx=sidx, tt=tt: e.activation(
                    out=coef[0:rows, 1, sidx:sidx + 1], in_=coef[0:rows, 0, sidx:sidx + 1], func=AF.Copy,
                    scale=gsm[0:rows, tt, sidx:sidx + 1]), reads=[b_coef, b_gsms[tt]], writes=[b_coef])
                dg_t, b_dg, _ = dgt[gstate["dg"] % 4]; gstate["dg"] += 1
                S.op("act", lambda e, dg_t=dg_t, rows=rows, sidx=sidx: e.activation(
                    out=dg_t[0:rows, 0:rows], in_=identB[0:rows, 0:rows], func=AF.Copy, scale=coef[0:rows, 1, sidx:sidx + 1]),
                    reads=[b_identB, b_coef], writes=[b_dg])
                first, last = (sidx == 0), (sidx == 127)
                for c in range(4):
                    pt, pbuf = PB[(tt % 2) * 4 + c]
                    S.op("pe", lambda e, pt=pt, c=c, t=t, dg_t=dg_t, rows=rows, first=first, last=last: e.matmul(
                        pt[0:rows, :], lhsT=dg_t[0:rows, 0:rows], rhs=t[0:rows, 1, c * 512:(c + 1) * 512], start=first, stop=last),
                        reads=[b_dg, bf], writes=[pbuf], signal=(c == 3))
                if last:
                    for c in range(4):
                        pt, pbuf = PB[(tt % 2) * 4 + c]
                        S.op("dve", lambda e, pt=pt, c=c, rows=rows, tt=tt: e.tensor_tensor(
                            out=X2[0:rows, tt, c * 512:(c + 1) * 512], in0=X2[0:rows, tt, c * 512:(c + 1) * 512],
                            in1=pt[0:rows, :], op=ALU.add), reads=[pbuf, bX2[tt]], writes=[bX2[tt]])

            if STOP < 8:
                return
            S.barrier()
            for r in ([r_gffn_b, r_h2t, r_actp, r_coef, r_cand, r_cand2, r_ts, r_ic, r_icw, r_icf, r_i1f, r_iota16, r_ef, r_gs2,
                       r_T1, r_I1, r_eidx_s, r_gsm_s, r_actp_s, r_coef_s, r_SELH, lhs_s[0][2], lhs_s[1][2]]
                      + [u[2] for u in ug if u[2] is not None] + [d_[2] for d_ in dgt]):
                AR.release(r)
            x3T, b_x3T, r_x3T = sb("x3T", [128, 16, TOK + NS], BF16)
            xnb = [sb("xnb%d" % i, [128, D], BF16) for i in range(2)]
            pT_, b_pT, _ = sb("pT", [128, 2, TOK + NS], BF16)
            pst, b_pst, _ = sb("pst", [128, 256], F32)
            psb, b_psb, _ = sb("psb", [128, 256], BF16)
            wpl, b_wpl, _ = sb("wpl", [128, 2, D], BF16)
            S.dma("pool", lambda e: e.dma_start(out=wpl[:], in_=w_ple.rearrange("(k p) c -> p k c", p=128)), writes=[b_wpl])
            for tt in range(9):
                rows = rows_of[tt]
                xn, bxn, _ = xnb[tt % 2]
                S.op("act", lambda e, xn=xn, tt=tt, rows=rows: e.copy(out=xn[0:rows, :], in_=X2[0:rows, tt, :]),
                     reads=[bX2[tt]], writes=[bxn])
                transpose_to(x3T, b_x3T, tt * 128, xn, bxn, rows, None)
                src = ploc[tt * 128:(tt + 1) * 128, :] if tt < 8 else psm
                S.dma("sp", lambda e, src=src, rows=rows: e.dma_start(out=pst[0:rows, 0:256], in_=src), writes=[b_pst])
                psbv = psb
                S.op("act", lambda e, rows=rows, psbv=psbv: e.copy(out=psbv[0:rows, 0:256], in_=pst[0:rows, 0:256]),
                     reads=[b_pst], writes=[b_psb])
                pt2, pbuf2 = bank("C")
                ptb = pt2[:].bitcast(BF16)
                for j in range(2):
                    S.op("pe", lambda e, j=j, ptb=ptb, rows=rows, psbv=psbv: e.transpose(
                        out=ptb[:, j * 128:j * 128 + rows], in_=psbv[0:rows, j * 128:(j + 1) * 128], identity=identB[0:rows, 0:rows]),
                        reads=[b_psb, b_identB], writes=[pbuf2], signal=(j == 1))
                S.op("dve", lambda e, ptb=ptb, tt=tt, rows=rows: e.tensor_copy(
                    out=pT_[:, :, tt * 128:tt * 128 + rows], in_=ptb[:, 0:256].rearrange("p (j t) -> p j t", j=2)[:, :, 0:rows]),
                    reads=[pbuf2], writes=[b_pT])
            sig, b_sig, _ = sb("sig", [128, 512], F32)
            for cb in range(4):
                wg, bwg = w_next()
                for tt in range(9):
                    rows = rows_of[tt]
                    pg, pbg = bank("A")
                    mm_group(pg[0:rows, :], pbg, [(x3T[:, kc, tt * 128:tt * 128 + rows], wg[:, kc, :]) for kc in range(16)],
                             [b_x3T, bwg])
                    pe_, pbe = bank("A")
                    mm_group(pe_[0:rows, :], pbe, [(pT_[:, kc, tt * 128:tt * 128 + rows], wpl[:, kc, cb * 512:(cb + 1) * 512])
                                                   for kc in range(2)], [b_pT, b_wpl])
                    S.op("act", lambda e, pg=pg, rows=rows: e.activation(out=sig[0:rows, 0:512], in_=pg[0:rows, :], func=AF.Sigmoid),
                         reads=[pbg], writes=[b_sig])
                    S.op("dve", lambda e, pe_=pe_, rows=rows: e.tensor_tensor(out=sig[0:rows, 0:512], in0=sig[0:rows, 0:512],
                                                                             in1=pe_[0:rows, :], op=ALU.mult),
                         reads=[pbe, b_sig], writes=[b_sig])
                    S.op("dve", lambda e, tt=tt, cb=cb, rows=rows: e.tensor_tensor(
                        out=X2[0:rows, tt, cb * 512:(cb + 1) * 512], in0=X2[0:rows, tt, cb * 512:(cb + 1) * 512],
                        in1=sig[0:rows, 0:512], op=ALU.add), reads=[b_sig, bX2[tt]], writes=[bX2[tt]])

            if STOP < 9:
                return
            S.barrier()
            AR.release(r_x3T)
            gfin_b, b_gfin_b, _ = sb("gfin_b", [128, D], F32)
            S.dma("sp", lambda e: e.dma_start(out=gfin_b[:], in_=gvec[2, :].partition_broadcast(128)), writes=[b_gfin_b])
            SQ["t"], SQ["b"], SQ["r"] = sb("sqjunk", [128, D], BF16)
            xst = [sb("yo%d" % i, [128, D], F32) for i in range(2)]
            for tt in range(9):
                rows = rows_of[tt]
                rs = rms_stats(X2[0:rows, tt, :], rows, bX2[tt], tt % 2)
                yo, b_yo, _ = xst[tt % 2]
                S.op("dve", lambda e, yo=yo, rs=rs, tt=tt, rows=rows: e.scalar_tensor_tensor(
                    out=yo[0:rows, :], in0=X2[0:rows, tt, :], scalar=rs, in1=gfin_b[0:rows, :], op0=ALU.mult, op1=ALU.mult),
                    reads=[bX2[tt], b_stat, b_gfin_b], writes=[b_yo])
                dst = y[tt * 128:(tt + 1) * 128, :] if tt < 8 else ys
                S.dma("sp", lambda e, yo=yo, dst=dst, rows=rows: e.dma_start(out=dst, in_=yo[0:rows, :]), reads=[b_yo], dbuf=outb)

        phases()
        S.barrier()
        build_nc.sbuf_base = (nc.sbuf_base, nc.sbuf_top)
        S.wait_all("sp", [outb])
        S._wait("sp", (outb.sem, outb.cnt, "d_outb"))
        build_nc.stats = dict(ninstr=dict(S.ninstr), nsem=5 + len(S.dbufs))
    return nc


def _rope_rows(pos):
    half = 16
    inv = (np.float32(500000.0) ** (-(np.arange(half, dtype=np.float32)) / np.float32(half))).astype(np.float32)
    ang = pos.astype(np.float32)[:, None] * inv[None, :]
    c, s = np.cos(ang).astype(np.float32), np.sin(ang).astype(np.float32)
    return np.concatenate([c, c, -s, s], axis=1).astype(np.float32)


def _consts(half):
    pos = np.maximum(np.arange(2 * TOK) - TOK + TOK * half, 0)
    tab = _rope_rows(pos)
    rope = np.zeros((128, 56, 64), np.float32)
    p = np.arange(128)
    for g, d in enumerate(DIL):
        tpr = (2 * TOK // d) // 128
        for n in range(16):
            r, i0 = n // tpr, (n % tpr) * 128
            rope[:, g * 16 + n, :] = tab[(i0 + p) * d + r]
    for j in range(8):
        r = 2 * j + (p >= 64)
        i = 64 + (p % 64)
        rope[:, 48 + j, :] = tab[i * 16 + r]
    ropes = np.repeat(_rope_rows(np.array([2048])), NS, axis=0)
    cb = NEG if half == 0 else 0.0
    pp, ff = np.meshgrid(np.arange(128), np.arange(128), indexing="ij")
    m = np.zeros((128, 5, 128), np.float32)
    m[:, 0, :] = np.where(ff <= pp, 0.0, NEG)
    m[:, 1, :] = np.where(pp <= ff, 0.0, NEG)
    m[:, 2, :] = m[:, 0, :] + cb
    m[:, 3, :] = cb
    m[:, 4, :] = np.where(pp < 64, cb, np.where(pp - 64 <= ff, 0.0, NEG))
    return rope, ropes, m


def _in_maps(x_prompt, x_sample, cache_kv_w128, cache_kv_w512, cache_kv_w2048, p_prompt, p_sample, g_mix, w_in,
             sgu_ln_g, sgu_ln_b, w_s, b_s, w_a_out, w_b_out, w_o, g_ffn, peer_w_q, peer_sub_k1, peer_sub_k2,
             peer_u, peer_v, w_ple, w_ple_gate, g_final, cores=None):
    f = lambda a: np.ascontiguousarray(np.asarray(a, dtype=np.float32))
    shared = {
        "w_in": f(w_in[0]), "w_a_out": f(w_a_out[0]), "w_b_out": f(w_b_out[0]), "w_o": f(w_o[0]), "w_q": f(peer_w_q[0]),
        "w_pg": f(w_ple_gate[0]), "w_ple": f(w_ple[0]), "peer_u": f(peer_u[0]), "peer_v": f(peer_v[0]),
        "w_s": f(w_s[0]), "b_s": f(b_s[0]), "subk": f(np.stack([peer_sub_k1[0], peer_sub_k2[0]])),
        "gvec": f(np.stack([g_mix[0], g_ffn[0], g_final])), "lnv": f(np.stack([sgu_ln_g[0], sgu_ln_b[0]])),
    }
    maps = []
    for c in (range(NCORES) if cores is None else cores):
        b, half = c // 2, c % 2
        own = x_prompt[b, half * TOK:(half + 1) * TOK]
        ctx = x_prompt[b, 0:TOK]
        sl = slice(c * NS, (c + 1) * NS)
        caches = np.stack([
            np.asarray(cache_kv_w128[0, sl]).reshape(NS, 128, 1024),
            np.asarray(cache_kv_w512[0, sl, 0::4]).reshape(NS, 128, 1024),
            np.asarray(cache_kv_w2048[0, sl, 0::16]).reshape(NS, 128, 1024)])
        rope, ropes, m = _consts(half)
        d = dict(shared)
        d.update({"xloc": f(np.concatenate([ctx, own], axis=0)), "xs": f(x_sample[sl, 0]),
                  "ploc": f(p_prompt[0, b, half * TOK:(half + 1) * TOK]), "psm": f(p_sample[0, sl, 0]),
                  "cache": f(caches), "rope": rope, "ropes": ropes, "masks": m})
        maps.append(d)
    return maps


def _assemble(res):
    y = np.zeros((4, 2 * TOK, D), np.float32)
    ysm = np.zeros((128, 1, D), np.float32)
    k0 = np.zeros((1, 4, 128, 2, 4, 128), np.float32)
    k1 = np.zeros((1, 4, 512, 2, 4, 128), np.float32)
    k2 = np.zeros((1, 4, 2 * TOK, 2, 4, 128), np.float32)
    ks = [np.zeros((1, 128, 1, 2, 4, 128), np.float32) for _ in range(3)]
    sg = np.zeros((1, 128, 1, A_W), np.float32)
    for c, r in enumerate(res):
        b, half = c // 2, c % 2
        y[b, half * TOK:(half + 1) * TOK] = r["y"]
        ysm[c * NS:(c + 1) * NS, 0] = r["ys"]
        k2[0, b, half * TOK:(half + 1) * TOK] = r["kv2"].reshape(TOK, 2, 4, 128)
        if half == 1:
            k0[0, b] = r["kv0"].reshape(128, 2, 4, 128)
            k1[0, b] = r["kv1"].reshape(512, 2, 4, 128)
        for g in range(3):
            ks[g][0, c * NS:(c + 1) * NS, 0] = r["kvs"][g].reshape(NS, 2, 4, 128)
        sg[0, c * NS:(c + 1) * NS, 0] = r["sguv"]
    return (y, ysm, k0, k1, k2, ks[0], ks[1], ks[2], sg)


def kernel(**inputs):
    maps = _in_maps(**inputs)
    nc = build_nc()
    res = run_bass_kernel_spmd(nc, maps, core_ids=list(range(NCORES)))
    return _assemble(res.results)
```
